# Optimizing a Trainium2 kernel written in Bass

```python
import math
import jax, jax.numpy as jnp
from jax import lax
import numpy as np

D_MODEL = 1024
BATCH = 8
SEQ = 4096
DEPTH = 1

HEAD_DIM = 64
N_ATTN_HEADS = 8
N_KV_HEADS = 2
KV_GROUP = N_ATTN_HEADS // N_KV_HEADS
N_RWKV_HEADS = 8
ATTN_WIDTH = N_ATTN_HEADS * HEAD_DIM
KV_WIDTH = N_KV_HEADS * HEAD_DIM
RWKV_WIDTH = N_RWKV_HEADS * HEAD_DIM
D_MIX = ATTN_WIDTH + RWKV_WIDTH
WINDOW = 128
ATTN_BLOCK = 128
ROT_DIM = HEAD_DIM // 4
ROPE_THETA = 500000.0
DECAY_LORA = 32
AAA_LORA = 32
GATE_LORA = 96
N_EXPERTS = 32
TOP_K = 4
D_FF = D_MODEL
SWIGLU_LIMIT = 7.0
SWIGLU_ALPHA = 1.702
MOE_BLOCK = 256
LN_EPS = 1e-5
RWKV_GN_EPS = 64e-5
NEG_INF = -1e30
DEEPNORM_ALPHA = (2 * DEPTH) ** 0.25
DEEPNORM_BETA = (8 * DEPTH) ** -0.25

IN_SIZES = (ATTN_WIDTH, KV_WIDTH, KV_WIDTH,
            RWKV_WIDTH, RWKV_WIDTH, RWKV_WIDTH,
            DECAY_LORA, AAA_LORA, GATE_LORA)
D_IN = sum(IN_SIZES)
ATTN_IN = ATTN_WIDTH + 2 * KV_WIDTH
RWKV_IN = D_IN - ATTN_IN

kernel_name = "hybrid_swa_rwkv7_moe_deepnorm_adaln"


def _layer_norm(x, g=None, b=None):
    xf = x.astype(jnp.float32)
    mu = jnp.mean(xf, axis=-1, keepdims=True)
    var = jnp.mean(jnp.square(xf - mu), axis=-1, keepdims=True)
    y = (xf - mu) * lax.rsqrt(var + LN_EPS)
    if g is not None:
        y = y * g.astype(jnp.float32) + b.astype(jnp.float32)
    return y.astype(x.dtype)


def _partial_rotary(t, positions):
    inv_freq = ROPE_THETA ** (-jnp.arange(0, ROT_DIM, 2, dtype=jnp.float32) / ROT_DIM)
    ang = positions.astype(jnp.float32)[..., None] * inv_freq
    cos, sin = jnp.cos(ang)[:, :, None, :], jnp.sin(ang)[:, :, None, :]
    tr = t[..., :ROT_DIM].astype(jnp.float32)
    t1, t2 = tr[..., :ROT_DIM // 2], tr[..., ROT_DIM // 2:]
    rot = jnp.concatenate([t1 * cos - t2 * sin, t2 * cos + t1 * sin], axis=-1).astype(t.dtype)
    return jnp.concatenate([rot, t[..., ROT_DIM:]], axis=-1)


def _sliding_window_attention(q, k, v, sinks):
    B, S = q.shape[0], q.shape[1]
    nb = S // ATTN_BLOCK
    qb = q.reshape(B, nb, ATTN_BLOCK, N_KV_HEADS, KV_GROUP, HEAD_DIM).astype(jnp.float32)

    def band(t):
        tp = jnp.pad(t, ((0, 0), (ATTN_BLOCK, 0), (0, 0), (0, 0)))
        tb = tp.reshape(B, nb + 1, ATTN_BLOCK, N_KV_HEADS, HEAD_DIM)
        return jnp.concatenate([tb[:, :-1], tb[:, 1:]], axis=2).astype(jnp.float32)

    kb, vb = band(k), band(v)
    s = jnp.einsum("bnqhgd,bnkhd->bhgnqk", qb, kb) / math.sqrt(HEAD_DIM)
    qi = jnp.arange(ATTN_BLOCK)[:, None]
    kj = jnp.arange(2 * ATTN_BLOCK)[None, :]
    rel = qi + ATTN_BLOCK - kj
    key_pos = jnp.arange(nb)[:, None, None] * ATTN_BLOCK - ATTN_BLOCK + kj[None]
    allowed = (rel >= 0) & (rel < WINDOW) & (key_pos >= 0)
    s = jnp.where(allowed, s, NEG_INF)
    sink = sinks.astype(jnp.float32).reshape(N_KV_HEADS, KV_GROUP)[None, :, :, None, None, None]
    m = jnp.maximum(jnp.max(s, axis=-1, keepdims=True), sink)
    p = jnp.exp(s - m)
    denom = jnp.sum(p, axis=-1, keepdims=True) + jnp.exp(sink - m)
    o = jnp.einsum("bhgnqk,bnkhd->bnqhgd", p / denom, vb)
    return o.reshape(B, S, ATTN_WIDTH).astype(q.dtype)


def _rwkv7_time_mix(r, k, v, wl, al, gl, w0, w2, a0, a2, g2, k_k, k_a, r_k, ln_w, ln_b):
    B, S, _ = r.shape
    heads = lambda t: t.reshape(B, S, N_RWKV_HEADS, HEAD_DIM)
    w = -jax.nn.softplus(-(w0 + jnp.tanh(wl) @ w2)) - 0.5
    decay = jnp.exp(-jnp.exp(w.astype(jnp.float32)))
    a = jax.nn.sigmoid(a0 + al @ a2)
    g = jax.nn.sigmoid(gl) @ g2
    kk = heads(k * k_k).astype(jnp.float32)
    kk = kk / jnp.maximum(jnp.sqrt(jnp.sum(kk * kk, axis=-1, keepdims=True)), 1e-12)
    k = k * (1.0 + (a - 1.0) * k_a)
    a_h = heads(a).astype(jnp.float32)

    to_time = lambda t: jnp.moveaxis(t.astype(jnp.float32), 1, 0)
    xs = (to_time(heads(r)), to_time(heads(decay)), to_time(heads(k)), to_time(heads(v)),
          to_time(-kk), to_time(kk * a_h))

    def step(state, inp):
        r_t, w_t, k_t, v_t, a_t, b_t = inp
        sa = jnp.einsum("bhvk,bhk->bhv", state, a_t)
        state = (state * w_t[:, :, None, :] + sa[..., None] * b_t[:, :, None, :]
                 + v_t[..., None] * k_t[:, :, None, :])
        return state, jnp.einsum("bhvk,bhk->bhv", state, r_t)

    s0 = jnp.zeros((B, N_RWKV_HEADS, HEAD_DIM, HEAD_DIM), jnp.float32)
    _, ys = lax.scan(step, s0, xs)
    y = jnp.moveaxis(ys, 0, 1)
    mu = jnp.mean(y, axis=-1, keepdims=True)
    var = jnp.mean(jnp.square(y - mu), axis=-1, keepdims=True)
    y = ((y - mu) * lax.rsqrt(var + RWKV_GN_EPS)).reshape(B, S, RWKV_WIDTH)
    y = y * ln_w.astype(jnp.float32) + ln_b.astype(jnp.float32)
    bonus = jnp.sum(heads(r * k).astype(jnp.float32) * r_k.astype(jnp.float32), axis=-1, keepdims=True)
    y = y + (bonus * heads(v).astype(jnp.float32)).reshape(B, S, RWKV_WIDTH)
    return (y * g.astype(jnp.float32)).astype(r.dtype)


def _moe(h, w_router, b_router, w_gate_up, b_gate_up, w_down, b_down):
    T = h.shape[0]
    n_assign = T * TOP_K
    logits = (h @ w_router + b_router).astype(jnp.float32)
    top_v, top_i = lax.top_k(logits, TOP_K)
    gates = jax.nn.softmax(top_v, axis=-1).astype(h.dtype)
    flat_e = top_i.reshape(n_assign).astype(jnp.int32)
    flat_g = gates.reshape(n_assign)
    flat_tok = jnp.arange(n_assign, dtype=jnp.int32) // TOP_K
    order = jnp.argsort(flat_e)
    se, stok, sg = flat_e[order], flat_tok[order], flat_g[order]
    counts = jax.ops.segment_sum(jnp.ones_like(flat_e), flat_e, num_segments=N_EXPERTS)
    padded = (counts + MOE_BLOCK - 1) // MOE_BLOCK * MOE_BLOCK
    start = jnp.cumsum(counts) - counts
    pend = jnp.cumsum(padded)
    pstart = pend - padded
    dest = pstart[se] + (jnp.arange(n_assign, dtype=jnp.int32) - start[se])
    n_blocks = n_assign // MOE_BLOCK + N_EXPERTS
    tok_buf = jnp.zeros((n_blocks * MOE_BLOCK,), jnp.int32).at[dest].set(stok)
    g_buf = jnp.zeros((n_blocks * MOE_BLOCK,), h.dtype).at[dest].set(sg)
    blk_e = jnp.clip(jnp.searchsorted(pend, jnp.arange(n_blocks, dtype=jnp.int32) * MOE_BLOCK,
                                      side="right"), 0, N_EXPERTS - 1).astype(jnp.int32)

    def body(out, blk):
        tok, gw, e = blk
        xb = h[tok]
        gu = xb @ w_gate_up[e] + b_gate_up[e]
        gate = jnp.minimum(gu[:, :D_FF], SWIGLU_LIMIT)
        up = jnp.clip(gu[:, D_FF:], -SWIGLU_LIMIT, SWIGLU_LIMIT)
        act = (up + 1.0) * (gate * jax.nn.sigmoid(SWIGLU_ALPHA * gate))
        yb = act @ w_down[e] + b_down[e]
        return out.at[tok].add(yb * gw[:, None]), None

    out, _ = lax.scan(body, jnp.zeros_like(h),
                      (tok_buf.reshape(n_blocks, MOE_BLOCK), g_buf.reshape(n_blocks, MOE_BLOCK), blk_e))
    return out


def _hybrid_layer(x, c, positions, w_ada, b_ada, w_in, shift_mu, rwkv_w0, rwkv_w2, rwkv_a0,
                  rwkv_a2, rwkv_g2, rwkv_k_k, rwkv_k_a, rwkv_r_k, rwkv_ln_w, rwkv_ln_b,
                  attn_sinks, w_out, ln1_g, ln1_b, w_router, b_router, w_gate_up, b_gate_up,
                  w_down, b_down, ln2_g, ln2_b):
    B, S, D = x.shape
    mod = (jax.nn.silu(c) @ w_ada + b_ada)[:, None, :]
    shift1, scale1, gate1, shift2, scale2, gate2 = jnp.split(mod, 6, axis=-1)

    h = _layer_norm(x) * (1.0 + scale1) + shift1
    proj = h @ w_in
    q, ka, va = jnp.split(proj[..., :ATTN_IN], [ATTN_WIDTH, ATTN_WIDTH + KV_WIDTH], axis=-1)
    rw = proj[..., ATTN_IN:]
    rw_prev = jnp.pad(rw, ((0, 0), (1, 0), (0, 0)))[:, :-1]
    rw = rw + (rw_prev - rw) * shift_mu
    r, kr, vr, wl, al, gl = jnp.split(
        rw, list(np.cumsum([RWKV_WIDTH, RWKV_WIDTH, RWKV_WIDTH, DECAY_LORA, AAA_LORA])), axis=-1)

    q = _partial_rotary(q.reshape(B, S, N_ATTN_HEADS, HEAD_DIM), positions)
    ka = _partial_rotary(ka.reshape(B, S, N_KV_HEADS, HEAD_DIM), positions)
    va = va.reshape(B, S, N_KV_HEADS, HEAD_DIM)
    attn_out = _sliding_window_attention(q, ka, va, attn_sinks)
    rwkv_out = _rwkv7_time_mix(r, kr, vr, wl, al, gl, rwkv_w0, rwkv_w2, rwkv_a0, rwkv_a2,
                               rwkv_g2, rwkv_k_k, rwkv_k_a, rwkv_r_k, rwkv_ln_w, rwkv_ln_b)
    y = jnp.concatenate([attn_out, rwkv_out], axis=-1) @ w_out
    x = _layer_norm(DEEPNORM_ALPHA * x + (1.0 + gate1) * y, ln1_g, ln1_b)

    h = _layer_norm(x) * (1.0 + scale2) + shift2
    y = _moe(h.reshape(B * S, D), w_router, b_router, w_gate_up, b_gate_up,
             w_down, b_down).reshape(B, S, D)
    return _layer_norm(DEEPNORM_ALPHA * x + (1.0 + gate2) * y, ln2_g, ln2_b)


def setup_inputs(seed: int = 0) -> dict:
    key = jax.random.key(seed)
    ks = jax.random.split(key, 32)
    L = DEPTH
    f32 = jnp.float32
    nrm = lambda k, shape, scale: jax.random.normal(k, shape, f32) * scale
    x = nrm(ks[0], (BATCH, SEQ, D_MODEL), 1.0)
    c = nrm(ks[1], (BATCH, D_MODEL), 1.0)
    positions = (jax.random.randint(ks[2], (BATCH, 1), 0, 4096, dtype=jnp.int32)
                 + jnp.arange(SEQ, dtype=jnp.int32)[None, :])
    w_ada = nrm(ks[3], (L, D_MODEL, 6 * D_MODEL), 0.1 * D_MODEL ** -0.5)
    b_ada = nrm(ks[4], (L, 6 * D_MODEL), 0.01)
    v_cols = (2, 5)
    col_scale = jnp.concatenate([jnp.full((n,), DEEPNORM_BETA if i in v_cols else 1.0, f32)
                                 for i, n in enumerate(IN_SIZES)])
    w_in = nrm(ks[5], (L, D_MODEL, D_IN), D_MODEL ** -0.5) * col_scale
    shift_mu = jax.random.uniform(ks[6], (L, RWKV_IN), f32)
    rwkv_w0 = jax.random.uniform(ks[7], (L, RWKV_WIDTH), f32, minval=-6.0, maxval=1.0)
    rwkv_w2 = nrm(ks[8], (L, DECAY_LORA, RWKV_WIDTH), 0.1 * DECAY_LORA ** -0.5)
    rwkv_a0 = nrm(ks[9], (L, RWKV_WIDTH), 0.5)
    rwkv_a2 = nrm(ks[10], (L, AAA_LORA, RWKV_WIDTH), AAA_LORA ** -0.5)
    rwkv_g2 = nrm(ks[11], (L, GATE_LORA, RWKV_WIDTH), GATE_LORA ** -0.5)
    rwkv_k_k = 0.85 + nrm(ks[12], (L, RWKV_WIDTH), 0.05)
    rwkv_k_a = 1.0 + nrm(ks[13], (L, RWKV_WIDTH), 0.05)
    rwkv_r_k = nrm(ks[14], (L, N_RWKV_HEADS, HEAD_DIM), 0.1)
    rwkv_ln_w = 1.0 + nrm(ks[15], (L, RWKV_WIDTH), 0.05)
    rwkv_ln_b = nrm(ks[16], (L, RWKV_WIDTH), 0.01)
    attn_sinks = nrm(ks[17], (L, N_ATTN_HEADS), 0.5)
    w_out = nrm(ks[18], (L, D_MIX, D_MODEL), D_MIX ** -0.5 * DEEPNORM_BETA)
    ln1_g = 1.0 + nrm(ks[19], (L, D_MODEL), 0.05)
    ln1_b = nrm(ks[20], (L, D_MODEL), 0.01)
    w_router = nrm(ks[21], (L, D_MODEL, N_EXPERTS), D_MODEL ** -0.5)
    b_router = nrm(ks[22], (L, N_EXPERTS), 0.01)
    w_gate_up = nrm(ks[23], (L, N_EXPERTS, D_MODEL, 2 * D_FF), D_MODEL ** -0.5 * DEEPNORM_BETA)
    b_gate_up = nrm(ks[24], (L, N_EXPERTS, 2 * D_FF), 0.01)
    w_down = nrm(ks[25], (L, N_EXPERTS, D_FF, D_MODEL), D_FF ** -0.5 * DEEPNORM_BETA)
    b_down = nrm(ks[26], (L, N_EXPERTS, D_MODEL), 0.01)
    ln2_g = 1.0 + nrm(ks[27], (L, D_MODEL), 0.05)
    ln2_b = nrm(ks[28], (L, D_MODEL), 0.01)
    return {"x": x, "c": c, "positions": positions, "w_ada": w_ada, "b_ada": b_ada,
            "w_in": w_in, "shift_mu": shift_mu, "rwkv_w0": rwkv_w0, "rwkv_w2": rwkv_w2,
            "rwkv_a0": rwkv_a0, "rwkv_a2": rwkv_a2, "rwkv_g2": rwkv_g2, "rwkv_k_k": rwkv_k_k,
            "rwkv_k_a": rwkv_k_a, "rwkv_r_k": rwkv_r_k, "rwkv_ln_w": rwkv_ln_w,
            "rwkv_ln_b": rwkv_ln_b, "attn_sinks": attn_sinks, "w_out": w_out,
            "ln1_g": ln1_g, "ln1_b": ln1_b, "w_router": w_router, "b_router": b_router,
            "w_gate_up": w_gate_up, "b_gate_up": b_gate_up, "w_down": w_down,
            "b_down": b_down, "ln2_g": ln2_g, "ln2_b": ln2_b}


def reference(x, c, positions, w_ada, b_ada, w_in, shift_mu, rwkv_w0, rwkv_w2, rwkv_a0,
              rwkv_a2, rwkv_g2, rwkv_k_k, rwkv_k_a, rwkv_r_k, rwkv_ln_w, rwkv_ln_b,
              attn_sinks, w_out, ln1_g, ln1_b, w_router, b_router, w_gate_up, b_gate_up,
              w_down, b_down, ln2_g, ln2_b):
    for l in range(DEPTH):
        x = _hybrid_layer(x, c, positions, w_ada[l], b_ada[l], w_in[l], shift_mu[l],
                          rwkv_w0[l], rwkv_w2[l], rwkv_a0[l], rwkv_a2[l], rwkv_g2[l],
                          rwkv_k_k[l], rwkv_k_a[l], rwkv_r_k[l], rwkv_ln_w[l], rwkv_ln_b[l],
                          attn_sinks[l], w_out[l], ln1_g[l], ln1_b[l], w_router[l],
                          b_router[l], w_gate_up[l], b_gate_up[l], w_down[l], b_down[l],
                          ln2_g[l], ln2_b[l])
    return x
```

```python
import contextlib
import os as _os
import numpy as np
import concourse.bass as bass
import concourse.mybir as mybir
from concourse.bass_utils import run_bass_kernel_spmd

F32 = mybir.dt.float32
BF16 = mybir.dt.bfloat16
I32 = mybir.dt.int32
U32 = mybir.dt.uint32
AF = mybir.ActivationFunctionType
ALU = mybir.AluOpType
AX = mybir.AxisListType

COMPUTE = ("pe", "act", "dve", "pool")
SEG = 8192
SAME_ENG_INORDER = ("pe",)
SAME_ENG_DRAIN = ()
SAME_ENG_HZ = ("act", "dve")
HZ_SMALL = 256
BUBBLE = False
NPOOL = {"pe": 24, "act": 24, "dve": 24, "pool": 4}
BUBBLE_DIST = 2
HZ_METHODS = ("tensor_reduce", "bn_stats", "bn_aggr", "max", "reciprocal")


class _Rec:
    def __init__(self):
        self.calls = []

    def __getattr__(self, name):
        def f(*a, **k):
            self.calls.append((name, a, k))
            return self
        return f


def _free_size(ap):
    try:
        sh = list(ap.shape)
        n = 1
        for x in sh[1:]:
            n *= int(x)
        return n
    except Exception:
        return 0


class _Cut(Exception):
    pass


class Sched:
    def __init__(self, nc, kdma=None):
        self.nc = nc
        self.ops = []
        self.last_w = {}
        self.rd_eng = {}
        self.rd_dma = {}
        self.kdma = kdma or {"sp": 16, "pool": 8, "act": 4}
        self.relay_of = {}
        self.relay_fn = None

    def add(self, eng, fn, reads=(), writes=(), dma=False, barrier=False):
        i = len(self.ops)
        reads = list(reads)
        writes = list(writes)
        if barrier:
            writes.append("PHASE")
        else:
            reads.append("PHASE")
        deps = set()
        for r in reads:
            if r in self.last_w:
                deps.add(self.last_w[r])
        for w in writes:
            if w in self.last_w:
                deps.add(self.last_w[w])
            for d in self.rd_eng.get(w, {}).values():
                deps.add(d)
            for d in self.rd_dma.get(w, ()):
                deps.add(d)
        if getattr(self, "relay_fn", None) is not None and not dma:
            nd = set()
            for d in deps:
                od = self.ops[d]
                if (not od["dma"]) and {eng, od["eng"]} in ({"pe", "dve"}, {"pe", "pool"}):
                    if d not in self.relay_of:
                        self.relay_of[d] = len(self.ops)
                        self.ops.append(dict(eng="act", fn=self.relay_fn, deps=[d], dma=False))
                    nd.add(self.relay_of[d])
                else:
                    nd.add(d)
            deps = nd
            i = len(self.ops)
        for w in writes:
            self.last_w[w] = i
            self.rd_eng[w] = {}
            self.rd_dma[w] = []
        ws = set(writes)
        for r in reads:
            if r in ws:
                continue
            if dma:
                self.rd_dma.setdefault(r, []).append(i)
            else:
                self.rd_eng.setdefault(r, {})[eng] = i
        self.ops.append(dict(eng=eng, fn=fn, deps=sorted(deps), dma=dma))
        return i

    def pe(self, fn, reads=(), writes=()):
        return self.add("pe", fn, reads, writes)

    def act(self, fn, reads=(), writes=()):
        return self.add("act", fn, reads, writes)

    def dve(self, fn, reads=(), writes=()):
        return self.add("dve", fn, reads, writes)

    def pool(self, fn, reads=(), writes=()):
        return self.add("pool", fn, reads, writes)

    def dma(self, q, fn, reads=(), writes=()):
        return self.add(q, fn, reads, writes, dma=True)

    def emit(self):
        nc = self.nc
        ops = self.ops
        n = len(ops)
        dcount = {q: [0] * k for q, k in self.kdma.items()}
        dnext = {q: 0 for q in self.kdma}
        tok = [None] * n
        prev_same = [None] * n
        last_on = {}
        order = [0] * n
        ecnt = {}
        for i, o in enumerate(ops):
            e = o["eng"]
            ecnt[e] = ecnt.get(e, 0) + 1
            order[i] = ecnt[e]
            if o["dma"]:
                q = e
                s_ = dnext[q] % self.kdma[q]
                dnext[q] += 1
                dcount[q][s_] += 1
                key = ("d", q, s_)
                tok[i] = (key, 16 * dcount[q][s_])
                prev_same[i] = last_on.get(key)
                last_on[key] = i
        per_eng = {}
        for i, o in enumerate(ops):
            per_eng.setdefault(o["eng"], []).append(i)

        def dep_list(i):
            o = ops[i]
            deps = list(o["deps"])
            if o["dma"] and prev_same[i] is not None:
                deps.append(prev_same[i])
            return deps

        hz = [True] * n
        for i, o in enumerate(ops):
            if o["dma"] or o["eng"] not in SAME_ENG_HZ:
                continue
            r = _Rec()
            try:
                o["fn"](r)
                name, a, k = r.calls[0]
                out = k.get("out", k.get("ap", a[0] if a else None))
                small = _free_size(out) < HZ_SMALL
                hz[i] = small or (name in HZ_METHODS) or ("accum_out" in k and k["accum_out"] is not None)
            except Exception:
                hz[i] = True
        self.n_hz = sum(1 for i, o in enumerate(ops) if (not o["dma"]) and o["eng"] in SAME_ENG_HZ and hz[i])
        needed = [False] * n
        needed_self = [False] * n
        bubble_before = [False] * n
        plan = {}
        drain_before = [False] * n
        for ename, idxs in per_eng.items():
            waited = {}
            drained_upto = 0
            for i in idxs:
                wl = []
                for d in dep_list(i):
                    od = ops[d]
                    if od["dma"]:
                        key, val = tok[d]
                        if waited.get(key, 0) >= val:
                            continue
                        waited[key] = val
                        wl.append(d)
                    else:
                        if od["eng"] == ename and ename in SAME_ENG_INORDER:
                            continue
                        if od["eng"] == ename and ename in SAME_ENG_HZ and not hz[d]:
                            continue
                        if od["eng"] == ename and ename in SAME_ENG_DRAIN:
                            if order[d] > drained_upto:
                                drain_before[i] = True
                                drained_upto = order[i] - 1
                            continue
                        if od["eng"] == ename and ename in SAME_ENG_HZ and BUBBLE:
                            if order[i] - order[d] <= BUBBLE_DIST:
                                bubble_before[i] = True
                            continue
                        if od["eng"] == ename:
                            key = ("s", od["eng"])
                            if waited.get(key, 0) >= order[d]:
                                continue
                            waited[key] = order[d]
                            needed_self[d] = True
                            wl.append((d, "s"))
                            continue
                        key = ("e", od["eng"])
                        if waited.get(key, 0) >= order[d]:
                            continue
                        waited[key] = order[d]
                        needed[d] = True
                        wl.append(d)
                plan[i] = wl
        ecount = {e: 0 for e in COMPUTE}
        scount = {e: 0 for e in COMPUTE}
        stok = [None] * n
        keys = set()
        for i, o in enumerate(ops):
            if o["dma"]:
                keys.add(tok[i][0])
            elif needed[i] or needed_self[i]:
                e = o["eng"]
                key = ("e", e, ecount[e] % NPOOL[e])
                tok[i] = (key, ecount[e] // NPOOL[e] + 1)
                stok[i] = tok[i]
                needed[i] = True
                ecount[e] += 1
                keys.add(key)
        self.n_incs = dict(ecount)
        with contextlib.ExitStack() as st:
            sems = {}
            for key in sorted(keys, key=str):
                sems[key] = st.enter_context(nc.semaphore("s_" + "_".join(str(x) for x in key)))
            block = st.enter_context(nc.Block())
            engmap = {"pe": "tensor", "act": "scalar", "dve": "vector", "pool": "gpsimd", "sp": "sync"}

            def make(ename, idxs):
                def body(eng):
                    for i in idxs:
                        o = ops[i]
                        if drain_before[i]:
                            eng.drain()
                        if bubble_before[i] and ename in getattr(self, "bubble", {}):
                            self.bubble[ename](eng)
                        for d in plan[i]:
                            if isinstance(d, tuple):
                                key, val = stok[d[0]]
                            else:
                                key, val = tok[d]
                            eng.wait_ge(sems[key], val)
                        inst = o["fn"](eng)
                        if o["dma"]:
                            inst.then_inc(sems[tok[i][0]], 16)
                        else:
                            if needed[i]:
                                inst.then_inc(sems[tok[i][0]], 1)
                            elif needed_self[i]:
                                inst.then_inc(sems[stok[i][0]], 1)
                            if (needed[i] or needed_self[i]) and ename in getattr(self, "spacer", {}):
                                self.spacer[ename](eng)
                    if ename in self.kdma:
                        for s_ in range(self.kdma[ename]):
                            if dcount[ename][s_] > 0:
                                eng.wait_ge(sems[("d", ename, s_)], 16 * dcount[ename][s_])
                return body

            for ename, idxs in per_eng.items():
                getattr(block, engmap[ename])(make(ename, idxs))
        return ecount


NT = 32
D = 1024
DIN = 2464
CAP = 768
NE = 32
NSLOT = NE * CAP
LN_EPS = 1e-5
GN_EPS = 64e-5
ALPHA = 2 ** 0.25
TWO_PI = 2.0 * np.pi


def build(stage="full", ntiles=NT, nexp=NE):
    nc = bass.Bass("TRN2", target_bir_lowering=False)
    dt = lambda name, shape, d, kind="ExternalInput": nc.dram_tensor(name, shape, d, kind=kind).ap()
    x = dt("x", [4096, D], F32)
    posT = dt("posT", [128, NT], I32)
    cB = dt("cB", [128, 8, 128], F32)
    w_ada = dt("w_ada", [D, 6 * D], F32)
    b_ada_b = dt("b_ada_b", [128, 6 * D], F32)
    w_in = dt("w_in", [D, DIN], F32)
    mu_b = dt("mu_b", [128, 1696], F32)
    rwc_b = dt("rwc_b", [128, 7, 512], F32)
    lora = dt("lora", [96, 3, 512], F32)
    sinks_b = dt("sinks_b", [128, 8], F32)
    invf_b = dt("invf_b", [128, 8], F32)
    w_out = dt("w_out", [D, D], F32)
    lnp = dt("lnp", [128, 4, D], F32)
    w_router = dt("w_router", [D, NE], F32)
    b_router_b = dt("b_router_b", [128, NE], F32)
    w_gu = dt("w_gu", [NE, D, 2 * D], F32)
    bguT = dt("bguT", [128, NE, 16], F32)
    w_dn = dt("w_dn", [NE, D, D], F32)
    b_dn = dt("b_dn", [1, NE, D], F32)
    consts = dt("consts", [128, 5, 128], F32)
    out = dt("out", [4096, D], F32, kind="ExternalOutput")
    dbg = dt("dbg", [4096, D], F32, kind="ExternalOutput") if stage != "full" else None
    x1s = dt("x1s", [4096, D], F32, kind="Internal")
    Xs = dt("Xs", [NSLOT + 128, D], BF16, kind="Internal")
    Ys = dt("Ys", [NSLOT + 128, D], F32, kind="Internal")
    lastrow = dt("lastrow", [2, 1696], F32, kind="Internal")

    S = Sched(nc)

    cut_tile = [0]

    def cut(name, ap, key, ncols=None):
        if stage == name and (name == "P" or cut_tile[0] == ntiles - 1):
            if ap.shape[-1] > 1024:
                ap = ap[:, 0:1024]
            ncols = ncols or ap.shape[-1]
            q = "sp" if ap.dtype == F32 else "pool"
            S.dma(q, lambda e: e.dma_start(out=dbg[0:ap.shape[0], 0:ncols], in_=ap), reads=[key])
            raise _Cut()

    try:
      with contextlib.ExitStack() as st0:
          sb0 = lambda name, shape, d: st0.enter_context(nc.sbuf_tensor(name, shape, d))
          ps = lambda name, shape, d: st0.enter_context(nc.psum_tensor(name, shape, d))
          pTb = ps("pTb", [128, 1024], BF16)
          pTb2 = ps("pTb2", [128, 1024], BF16)
          pA = ps("pA", [128, 512], F32)
          pB = ps("pB", [128, 512], F32)
          pC = ps("pC", [128, 512], F32)
          pD = ps("pD", [128, 512], F32)
          pE = ps("pE", [128, 512], F32)
          pF = ps("pF", [128, 512], F32)
          cst = sb0("cst", [128, 5, 128], F32)
          cstb = sb0("cstb", [128, 5, 128], BF16)
          modb = sb0("modb", [128, 6, D], F32)
          slot4 = sb0("slot4", [128, NT, 4], I32)
          gate4 = sb0("gate4", [128, NT, 4], F32)
          junk = sb0("junk", [128, 8], F32)
          S.relay_fn = lambda e: e.activation(out=junk[:, 6:7], in_=junk[:, 6:7], func=AF.Copy)
          if True:
              spc = sb0("spc", [128, 2, 512], F32)
              nsp = 512
              nsp = int(_os.environ.get("KSPACE", "0"))
              nbb = int(_os.environ.get("KBUB", "384"))
              S.spacer = {"dve": lambda e: e.memset(spc[:, 0, 0:nsp], 0.0),
                          "act": lambda e: e.activation(out=spc[:, 1, 0:nsp], in_=spc[:, 1, 0:nsp], func=AF.Copy)}
              if nsp == 0:
                  S.spacer = {}
              S.bubble = {"dve": lambda e: e.memset(spc[:, 0, 0:nbb], 0.0),
                          "act": lambda e: e.activation(out=spc[:, 1, 0:nbb], in_=spc[:, 1, 0:nbb], func=AF.Copy)}
          ident_f = cst[:, 0, :]
          ident_b = cstb[:, 0, :]

          S.dma("sp", lambda e: e.dma_start(out=cst[:], in_=consts[:, :, :]), writes=["cst"])
          S.dve(lambda e: e.tensor_copy(out=cstb[:], in_=cst[:]), reads=["cst"], writes=["cstb"])
          S.dma("sp", lambda e: e.dma_start(out=modb[:].rearrange("p a b -> p (a b)"), in_=b_ada_b[:, :]), writes=["modb"])

          with contextlib.ExitStack() as st:
              sb = lambda name, shape, d: st.enter_context(nc.sbuf_tensor(name, shape, d))
              w_in_bf = sb("w_in_bf", [128, 8, DIN], BF16)
              lnpb = sb("lnpbA", [128, 2, D], F32)
              S.dma("sp", lambda e: e.dma_start(out=lnpb[:], in_=lnp[:, 0:2, :]), writes=["lnpb"])
              w_out_bf = sb("w_out_bf", [128, 8, D], BF16)
              wr_f = sb("wr_f", [128, 8, NE], F32)
              brb = sb("brb", [128, NE], F32)
              mub = sb("mub", [128, 1696], F32)
              rwcb = sb("rwcb", [128, 7, 512], F32)
              lor = sb("lor", [96, 3, 512], F32)
              sinkb = sb("sinkb", [128, 8], F32)
              esink = sb("esink", [128, 8], F32)
              invf = sb("invf", [128, 8], F32)
              cosT = sb("cosT", [128, NT, 8], F32)
              sinT = sb("sinT", [128, NT, 8], F32)
              stp = contextlib.ExitStack()
              sbp = lambda name, shape, d: stp.enter_context(nc.sbuf_tensor(name, shape, d))
              posi = sbp("posi", [128, NT], I32)
              posf = sbp("posf", [128, NT], F32)
              ang = sbp("ang", [128, NT, 8], F32)
              scB = sbp("scB", [128, 8, 128], F32)
              wada = [sbp("wada%d" % j, [128, 8, 512], F32) for j in range(2)]
              w_in_v = w_in.rearrange("(k p) n -> p k n", p=128)
              for (c0, c1) in ((0, 1232), (1232, 2464)):
                  S.dma("pool", lambda e, c0=c0, c1=c1: e.dma_start(out=w_in_bf[:, :, c0:c1], in_=w_in_v[:, :, c0:c1]), writes=["w_in_bf"])
              S.dma("pool", lambda e: e.dma_start(out=w_out_bf[:], in_=w_out.rearrange("(k p) n -> p k n", p=128)), writes=["w_out_bf"])
              S.dma("sp", lambda e: e.dma_start(out=wr_f[:], in_=w_router.rearrange("(k p) n -> p k n", p=128)), writes=["wr_f"])
              S.dma("sp", lambda e: e.dma_start(out=brb[:], in_=b_router_b[:, :]), writes=["brb"])
              S.dma("sp", lambda e: e.dma_start(out=mub[:], in_=mu_b[:, :]), writes=["mub"])
              S.dma("sp", lambda e: e.dma_start(out=rwcb[:], in_=rwc_b[:, :, :]), writes=["rwcb"])
              S.dma("sp", lambda e: e.dma_start(out=lor[:], in_=lora[:, :, :]), writes=["lor"])
              S.dma("sp", lambda e: e.dma_start(out=sinkb[:], in_=sinks_b[:, :]), writes=["sinkb"])
              S.dma("sp", lambda e: e.dma_start(out=invf[:], in_=invf_b[:, :]), writes=["invf"])
              S.dma("sp", lambda e: e.dma_start(out=posi[:], in_=posT[:, :]), writes=["posi"])
              S.dma("sp", lambda e: e.dma_start(out=scB[:], in_=cB[:, :, :]), writes=["scB"])
              S.act(lambda e: e.activation(out=esink[:], in_=sinkb[:], func=AF.Exp), reads=["sinkb"], writes=["esink"])
              S.act(lambda e: e.activation(out=scB[:], in_=scB[:], func=AF.Silu), reads=["scB"], writes=["scB"])
              w_ada_v = w_ada.rearrange("(k p) n -> p k n", p=128)
              for j in range(12):
                  wb_ = wada[j % 2]
                  S.dma("sp", lambda e, j=j, wb_=wb_: e.dma_start(out=wb_[:], in_=w_ada_v[:, :, j * 512:(j + 1) * 512]), writes=[("wada", j % 2)])
                  pp = pA if j % 2 == 0 else pB
                  for k in range(8):
                      S.pe(lambda e, k=k, wb_=wb_, pp=pp: e.matmul(pp[:], lhsT=scB[:, k, :], rhs=wb_[:, k, :], start=(k == 0), stop=(k == 7)),
                           reads=["scB", ("wada", j % 2)], writes=[pp.name])
                  mflat = modb[:].rearrange("p a b -> p (a b)")
                  S.dve(lambda e, j=j, pp=pp, mflat=mflat: e.tensor_tensor(out=mflat[:, j * 512:(j + 1) * 512], in0=pp[:], in1=mflat[:, j * 512:(j + 1) * 512], op=ALU.add),
                        reads=[pp.name, "modb"], writes=["modb"])
              for a in (1, 2, 4, 5):
                  S.dve(lambda e, a=a: e.tensor_scalar(out=modb[:, a, :], in0=modb[:, a, :], scalar1=1.0, scalar2=None, op0=ALU.add), reads=["modb"], writes=["modb"])
              S.dve(lambda e: e.tensor_copy(out=posf[:], in_=posi[:]), reads=["posi"], writes=["posf"])
              S.dve(lambda e: e.tensor_tensor(out=ang[:], in0=posf[:].unsqueeze(2).broadcast_to([128, NT, 8]), in1=invf[:].unsqueeze(1).broadcast_to([128, NT, 8]), op=ALU.mult),
                    reads=["posf", "invf"], writes=["ang"])
              angi = sbp("angi", [128, NT, 8], I32)
              angf = sbp("angf", [128, NT, 8], F32)
              SC = float(TWO_PI * (1.0 - 1e-6))
              for (dst, key, off) in ((sinT, "sinT", 0.0), (cosT, "cosT", 0.25)):
                  S.dve(lambda e, dst=dst, off=off: e.tensor_scalar(out=dst[:], in0=ang[:], scalar1=float(1.0 / TWO_PI), scalar2=off, op0=ALU.mult, op1=ALU.add), reads=["ang"], writes=[key])
                  S.dve(lambda e, dst=dst: e.tensor_copy(out=angi[:], in_=dst[:]), reads=[key], writes=["angi"])
                  S.dve(lambda e: e.tensor_copy(out=angf[:], in_=angi[:]), reads=["angi"], writes=["angf"])
                  S.dve(lambda e, dst=dst: e.tensor_tensor(out=dst[:], in0=dst[:], in1=angf[:], op=ALU.subtract), reads=[key, "angf"], writes=[key])
                  S.dve(lambda e, dst=dst: e.tensor_scalar(out=angf[:], in0=dst[:], scalar1=0.5, scalar2=None, op0=ALU.is_gt), reads=[key], writes=["angf"])
                  S.dve(lambda e, dst=dst: e.tensor_tensor(out=dst[:], in0=dst[:], in1=angf[:], op=ALU.subtract), reads=[key, "angf"], writes=[key])
                  S.act(lambda e, dst=dst: e.activation(out=dst[:], in_=dst[:], func=AF.Sin, scale=SC), reads=[key], writes=[key])

              zrow = sbp("zrow", [1, 1696], F32)
              S.pool(lambda e: e.memset(zrow[:], 0.0), writes=["zrow"])
              S.dma("sp", lambda e: e.dma_start(out=lastrow[0:1, :], in_=zrow[:]), reads=["zrow"], writes=["lastrow0"])
              S.dve(lambda e: e.memset(junk[:, 4:5], 0.0), reads=["cosT", "sinT", "modb"], writes=["junk"])
              S.add("dve", lambda e: e.memset(junk[:, 5:6], 0.0), reads=[], writes=["junk"], barrier=True)
              stp.close()
              cut("P", modb[:, 1, :], "modb")
              xt = sb("xt", [128, D], F32)
              g4t = sb("g4t", [128, 416], F32)
              t1k = sb("t1k", [128, D], F32)
              hb = sb("hb", [128, D], BF16)
              hT = sb("hT", [128, 8, 128], BF16)
              st6 = sb("st6", [128, 2, 6], F32)
              mv = sb("mv", [128, 2], F32)
              rstd = sb("rstd", [128, 1], F32)
              qk = sb("qk", [128, 10, 64], F32)
              qkb = sb("qkb", [128, 10, 64], BF16)
              rt = sb("rt", [128, 4, 10, 8], F32)
              qT = sb("qT", [64, 8, 128], BF16)
              kT = [sb("kT%d" % j, [64, 2, 128], BF16) for j in range(2)]
              V1 = [sb("V1%d" % j, [128, 2, 66], BF16) for j in range(2)]
              rwc = sb("rwc", [128, 1696], F32)
              rwp = sb("rwp", [128, 1696], F32)
              Ee = sb("Ee", [128, 512], F32)
              PTp = sb("PTp", [128, 4, 128], BF16)
              PTc = sb("PTc", [128, 4, 128], BF16)
              den = sb("den", [128, 8], F32)
              mixb = sb("mixb", [128, D], BF16)
              lo_in = sb("lo_in", [128, 160], F32)
              loT = sb("loT", [96, 3, 128], F32)
              f1 = rwc[:, 0:512]
              f2 = rwc[:, 512:1024]
              f3 = rwc[:, 1024:1536]
              f4 = sb("f4", [128, 512], F32)
              eP = sb("eP", [128, 512], F32)
              eN = sb("eN", [128, 512], F32)
              lw = sb("lw", [128, 512], F32)
              av = sb("av", [128, 512], F32)
              kmod = sb("kmod", [128, 512], F32)
              s8 = sb("s8", [128, 4, 8], F32)
              At = sb("At", [128, 512], BF16)
              Bt = sb("Bt", [128, 512], BF16)
              Kt = sb("Kt", [128, 512], BF16)
              Rt = sb("Rt", [128, 512], BF16)
              Vb = sb("Vb", [128, 512], BF16)
              arT = sb("arT", [64, 8, 2, 128], BF16)
              bkT = sb("bkT", [64, 8, 2, 128], BF16)
              dC = sb("dC", [64, 8], F32)
              Tst = [sb("Tst%d" % j, [64, 8, 64], BF16) for j in range(2)]
              XA = sb("XA", [128, 2, 256], BF16)
              XN = [sb("XN%d" % j, [128, 256], BF16) for j in range(2)]
              Lp = [sb("Lp%d" % j, [128, 128], BF16) for j in range(2)]
              RHSs = sb("RHSs", [128, 64], BF16)
              Us = sb("Us", [128, 64], BF16)
              msk2 = sb("msk2", [128, 256], F32)
              ones_f = sb("ones_f", [128, 1], F32)
              h2T = xt[:].rearrange("p (a b) -> p a b", a=8)
              lg = sb("lg", [128, NE], F32)
              t8 = sb("t8", [128, 8], F32)
              eqk = sb("eqk", [128, NE], F32)
              posn = sb("posn", [128, NE], F32)
              tot = sb("tot", [128, NE], F32)
              mskb = sb("mskb", [128, NE], BF16)
              sl4 = sb("sl4", [128, 4], F32)
              ebase = sb("ebase", [128, NE], F32)
              trash = sb("trash", [128, 1], F32)
              ev4 = sb("ev4", [128, 4], F32)
              onesb = sb("onesb", [128, 128], BF16)

              MI = cst[:, 1, :]
              MS_ = cst[:, 2, :]
              MST = cst[:, 3, :]
              MP = cst[:, 4, :]
              for j in range(2):
                  S.pool(lambda e, j=j: e.memset(V1[j][:], 1.0), writes=[("V1", j)])
                  S.pool(lambda e, j=j: e.memset(Tst[j][:], 0.0), writes=[("Tst", j)])
              S.pool(lambda e: e.memset(ones_f[:], 1.0), writes=["ones_f"])
              S.pool(lambda e: e.memset(onesb[:], 1.0), writes=["onesb"])
              S.pool(lambda e: e.memset(tot[:], 0.0), writes=["tot"])
              S.pool(lambda e: e.iota(ebase[:], pattern=[[CAP, NE]], base=0, channel_multiplier=0, allow_small_or_imprecise_dtypes=True), writes=["ebase"])
              S.pool(lambda e: e.iota(trash[:], pattern=[[0, 1]], base=NSLOT, channel_multiplier=1, allow_small_or_imprecise_dtypes=True), writes=["trash"])
              S.dve(lambda e: e.tensor_copy(out=msk2[:, 0:128], in_=MS_), reads=["cst"], writes=["msk2"])
              S.dve(lambda e: e.tensor_copy(out=msk2[:, 128:256], in_=MI), reads=["cst"], writes=["msk2"])

              def layer_norm_stats(src, key):
                  for h in range(2):
                      S.dve(lambda e, h=h: e.bn_stats(out=st6[:, h, :], in_=src[:, h * 512:(h + 1) * 512]), reads=[key], writes=["st6"])
                  S.dve(lambda e: e.bn_aggr(out=mv[:], in_=st6[:].rearrange("p a b -> p (a b)")), reads=["st6"], writes=["mv"])
                  S.dve(lambda e: e.tensor_scalar(out=rstd[:], in0=mv[:, 1:2], scalar1=LN_EPS, scalar2=None, op0=ALU.add), reads=["mv"], writes=["rstd"])
                  S.act(lambda e: e.activation(out=rstd[:], in_=rstd[:], func=AF.Sqrt), reads=["rstd"], writes=["rstd"])
                  S.dve(lambda e: e.reciprocal(out=rstd[:], in_=rstd[:]), reads=["rstd"], writes=["rstd"])

              for i in range(ntiles):
                  cur, prv = i % 2, (i + 1) % 2
                  cut_tile[0] = i
                  rows = slice(i * 128, (i + 1) * 128)
                  S.dma("sp", lambda e, rows=rows: e.dma_start(out=xt[:], in_=x[rows, :]), writes=["xt"])
                  layer_norm_stats(xt, "xt")
                  S.dve(lambda e: e.tensor_scalar(out=t1k[:], in0=xt[:], scalar1=mv[:, 0:1], scalar2=rstd[:], op0=ALU.subtract, op1=ALU.mult), reads=["xt", "mv", "rstd"], writes=["t1k"])
                  S.dve(lambda e: e.tensor_tensor(out=t1k[:], in0=t1k[:], in1=modb[:, 1, :], op=ALU.mult), reads=["t1k", "modb"], writes=["t1k"])
                  S.dve(lambda e: e.tensor_tensor(out=hb[:], in0=t1k[:], in1=modb[:, 0, :], op=ALU.add), reads=["t1k", "modb"], writes=["hb"])
                  cut("A_h", t1k[:], "t1k")
                  for k in range(8):
                      S.pe(lambda e, k=k: e.transpose(out=pTb[:, k * 128:(k + 1) * 128], in_=hb[:, k * 128:(k + 1) * 128], identity=ident_b), reads=["hb", "cstb"], writes=["pTb"])
                  S.act(lambda e: e.copy(out=hT[:].rearrange("p a b -> p (a b)"), in_=pTb[:]), reads=["pTb"], writes=["hT"])
                  cut("A_hT", t1k[:], "hT")
                  groups = [(0, 512), (512, 1024), (1024, 1536), (1536, 2048), (2048, 2464)]
                  for g, (c0, c1) in enumerate(groups):
                      pp = pA if g % 2 == 0 else pB
                      n = c1 - c0
                      for k in range(8):
                          S.pe(lambda e, k=k, pp=pp, c0=c0, c1=c1, n=n: e.matmul(pp[:, 0:n], lhsT=hT[:, k, :], rhs=w_in_bf[:, k, c0:c1], start=(k == 0), stop=(k == 7)),
                               reads=["hT", "w_in_bf"], writes=[pp.name])
                      if g == 0:
                          S.act(lambda e, pp=pp: e.copy(out=qk[:, 0:8, :].rearrange("p a b -> p (a b)"), in_=pp[:]), reads=[pp.name], writes=["qk"])
                          cut("A_g0", qk[:, 0:8, :].rearrange("p a b -> p (a b)"), "qk")
                      elif g < 4:
                          S.act(lambda e, pp=pp, g=g: e.copy(out=rwc[:, (g - 1) * 512:g * 512], in_=pp[:]), reads=[pp.name], writes=["rwc"])
                      else:
                          S.act(lambda e, pp=pp: e.copy(out=g4t[:], in_=pp[:, 0:416]), reads=[pp.name], writes=["g4t"])
                          S.act(lambda e: e.copy(out=rwc[:, 1536:1696], in_=g4t[:, 0:160]), reads=["g4t"], writes=["rwc"])
                          S.act(lambda e: e.copy(out=qk[:, 8:10, :].rearrange("p a b -> p (a b)"), in_=g4t[:, 160:288]), reads=["g4t"], writes=["qk"])
                          S.act(lambda e, cur=cur: e.copy(out=V1[cur][:, 0, 0:64], in_=g4t[:, 288:352]), reads=["g4t"], writes=[("V1", cur)])
                          S.act(lambda e, cur=cur: e.copy(out=V1[cur][:, 1, 0:64], in_=g4t[:, 352:416]), reads=["g4t"], writes=[("V1", cur)])
                      if g >= 1:
                          cut("A_g%d" % g, rwc[:, 0:1024], "rwc")
                  cut("A_proj", rwc[:], "rwc")
                  S.dma("sp", lambda e: e.dma_start(out=rwp[1:128, :], in_=rwc[0:127, :]), reads=["rwc"], writes=["rwp"])
                  S.dma("sp", lambda e, cur=cur: e.dma_start(out=rwp[0:1, :], in_=lastrow[cur:cur + 1, :]), reads=["lastrow%d" % cur], writes=["rwp"])
                  S.dma("sp", lambda e, prv=prv: e.dma_start(out=lastrow[prv:prv + 1, :], in_=rwc[127:128, :]), reads=["rwc"], writes=["lastrow%d" % prv])
                  S.dve(lambda e: e.tensor_tensor(out=rwp[:], in0=rwp[:], in1=rwc[:], op=ALU.subtract), reads=["rwp", "rwc"], writes=["rwp"])
                  S.dve(lambda e: e.tensor_tensor(out=rwp[:], in0=rwp[:], in1=mub[:], op=ALU.mult), reads=["rwp", "mub"], writes=["rwp"])
                  S.dve(lambda e: e.tensor_tensor(out=rwp[:], in0=rwp[:], in1=rwc[:], op=ALU.add), reads=["rwp", "rwc"], writes=["rwp"])
                  cut("A_mix", rwp[:], "rwp")
                  cb = cosT[:, i, :].unsqueeze(1).broadcast_to([128, 10, 8])
                  sbn = sinT[:, i, :].unsqueeze(1).broadcast_to([128, 10, 8])
                  a1, a2 = qk[:, :, 0:8], qk[:, :, 8:16]
                  S.dve(lambda e, cb=cb: e.tensor_tensor(out=rt[:, 0], in0=a1, in1=cb, op=ALU.mult), reads=["qk", "cosT"], writes=["rt0"])
                  S.dve(lambda e, sbn=sbn: e.tensor_tensor(out=rt[:, 1], in0=a2, in1=sbn, op=ALU.mult), reads=["qk", "sinT"], writes=["rt1"])
                  S.dve(lambda e, cb=cb: e.tensor_tensor(out=rt[:, 2], in0=a2, in1=cb, op=ALU.mult), reads=["qk", "cosT"], writes=["rt2"])
                  S.dve(lambda e, sbn=sbn: e.tensor_tensor(out=rt[:, 3], in0=a1, in1=sbn, op=ALU.mult), reads=["qk", "sinT"], writes=["rt3"])
                  S.act(lambda e: e.copy(out=qkb[:, :, 16:64], in_=qk[:, :, 16:64]), reads=["qk"], writes=["qkb"])
                  S.dve(lambda e: e.tensor_tensor(out=qkb[:, :, 0:8], in0=rt[:, 0], in1=rt[:, 1], op=ALU.subtract), reads=["rt0", "rt1"], writes=["qkb"])
                  S.dve(lambda e: e.tensor_tensor(out=qkb[:, :, 8:16], in0=rt[:, 2], in1=rt[:, 3], op=ALU.add), reads=["rt2", "rt3"], writes=["qkb"])
                  for h in range(8):
                      S.pe(lambda e, h=h: e.transpose(out=pTb2[0:64, h * 128:(h + 1) * 128], in_=qkb[:, h, :], identity=ident_b), reads=["qkb", "cstb"], writes=["pTb2"])
                  S.act(lambda e: e.activation(out=qT[:].rearrange("p a b -> p (a b)"), in_=pTb2[0:64, :], func=AF.Copy, scale=0.125), reads=["pTb2"], writes=["qT"])
                  for h in range(2):
                      S.pe(lambda e, h=h: e.transpose(out=pTb[0:64, h * 128:(h + 1) * 128], in_=qkb[:, 8 + h, :], identity=ident_b), reads=["qkb", "cstb"], writes=["pTb"])
                  S.act(lambda e, cur=cur: e.copy(out=kT[cur][:].rearrange("p a b -> p (a b)"), in_=pTb[0:64, 0:256]), reads=["pTb"], writes=[("kT", cur)])
                  for g in range(2):
                      rq = qT[:, 4 * g:4 * g + 4, :]
                      if i > 0:
                          S.pe(lambda e, g=g, rq=rq, prv=prv: e.matmul(pC[:], lhsT=kT[prv][:, g, :], rhs=rq, start=True, stop=True), reads=[("kT", prv), "qT"], writes=["pC"])
                          S.act(lambda e: e.activation(out=Ee[:], in_=pC[:], func=AF.Exp), reads=["pC"], writes=["Ee"])
                          S.dve(lambda e: e.tensor_tensor(out=PTp[:], in0=Ee[:].rearrange("p (a b) -> p a b", a=4), in1=MP.unsqueeze(1).broadcast_to([128, 4, 128]), op=ALU.mult), reads=["Ee", "cst"], writes=["PTp"])
                      S.pe(lambda e, g=g, rq=rq, cur=cur: e.matmul(pD[:], lhsT=kT[cur][:, g, :], rhs=rq, start=True, stop=True), reads=[("kT", cur), "qT"], writes=["pD"])
                      S.act(lambda e: e.activation(out=Ee[:], in_=pD[:], func=AF.Exp), reads=["pD"], writes=["Ee"])
                      S.dve(lambda e: e.tensor_tensor(out=PTc[:], in0=Ee[:].rearrange("p (a b) -> p a b", a=4), in1=MI.unsqueeze(1).broadcast_to([128, 4, 128]), op=ALU.mult), reads=["Ee", "cst"], writes=["PTc"])
                      pO = pE[:, 0:264].rearrange("p (a b) -> p a b", a=4)
                      for h in range(4):
                          if i > 0:
                              S.pe(lambda e, h=h, g=g, prv=prv, pO=pO: e.matmul(pO[:, h, :], lhsT=PTp[:, h, :], rhs=V1[prv][:, g, :], start=True, stop=False), reads=["PTp", ("V1", prv)], writes=["pE"])
                          S.pe(lambda e, h=h, g=g, cur=cur, pO=pO, i=i: e.matmul(pO[:, h, :], lhsT=PTc[:, h, :], rhs=V1[cur][:, g, :], start=(i == 0), stop=True), reads=["PTc", ("V1", cur)], writes=["pE"])
                      S.dve(lambda e, g=g, pO=pO: e.tensor_tensor(out=den[:, 4 * g:4 * g + 4], in0=pO[:, :, 64], in1=esink[:, 4 * g:4 * g + 4], op=ALU.add), reads=["pE", "esink"], writes=["den"])
                      S.dve(lambda e, g=g: e.reciprocal(out=den[:, 4 * g:4 * g + 4], in_=den[:, 4 * g:4 * g + 4]), reads=["den"], writes=["den"])
                      S.dve(lambda e, g=g, pO=pO: e.tensor_tensor(out=mixb[:, 256 * g:256 * g + 256].rearrange("p (a b) -> p a b", a=4), in0=pO[:, :, 0:64],
                                                                   in1=den[:, 4 * g:4 * g + 4].unsqueeze(2).broadcast_to([128, 4, 64]), op=ALU.mult), reads=["pE", "den"], writes=["mixb"])
                  cut("A_attn", mixb[:, 0:512], "mixb")
                  r_ = rwp[:, 0:512]
                  k_ = rwp[:, 512:1024]
                  v_ = rwp[:, 1024:1536]
                  W0, A0, KK, KA, RK, LNW, LNB = [rwcb[:, j, :] for j in range(7)]
                  S.act(lambda e: e.activation(out=lo_in[:, 0:32], in_=rwp[:, 1536:1568], func=AF.Tanh), reads=["rwp"], writes=["lo_in"])
                  S.act(lambda e: e.activation(out=lo_in[:, 64:160], in_=rwp[:, 1600:1696], func=AF.Sigmoid), reads=["rwp"], writes=["lo_in"])
                  S.dve(lambda e: e.tensor_copy(out=lo_in[:, 32:64], in_=rwp[:, 1568:1600]), reads=["rwp"], writes=["lo_in"])
                  for j, (c0, n) in enumerate(((0, 32), (32, 32), (64, 96))):
                      S.pe(lambda e, j=j, c0=c0, n=n: e.transpose(out=pC[0:n, j * 128:(j + 1) * 128], in_=lo_in[:, c0:c0 + n], identity=ident_f), reads=["lo_in", "cst"], writes=["pC"])
                      S.act(lambda e, j=j, n=n: e.copy(out=loT[0:n, j, :], in_=pC[0:n, j * 128:(j + 1) * 128]), reads=["pC"], writes=["loT"])
                  for j, (pp, n) in enumerate(((pA, 32), (pB, 32))):
                      S.pe(lambda e, j=j, pp=pp, n=n: e.matmul(pp[:], lhsT=loT[0:n, j, :], rhs=lor[0:n, j, :], start=True, stop=True), reads=["loT", "lor"], writes=[pp.name])
                  S.dve(lambda e: e.tensor_tensor(out=f1[:], in0=pA[:], in1=W0, op=ALU.add), reads=["pA", "rwcb"], writes=["rwc"])
                  S.act(lambda e: e.activation(out=lw[:], in_=f1[:], func=AF.Sigmoid), reads=["rwc"], writes=["lw"])
                  S.dve(lambda e: e.tensor_scalar(out=lw[:], in0=lw[:], scalar1=-float(np.exp(-0.5)), scalar2=None, op0=ALU.mult), reads=["lw"], writes=["lw"])
                  S.dve(lambda e: e.tensor_tensor(out=f2[:], in0=pB[:], in1=A0, op=ALU.add), reads=["pB", "rwcb"], writes=["rwc"])
                  S.act(lambda e: e.activation(out=av[:], in_=f2[:], func=AF.Sigmoid), reads=["rwc"], writes=["av"])
                  S.pe(lambda e: e.matmul(pA[:], lhsT=MI, rhs=lw[:], start=True, stop=True), reads=["cst", "lw"], writes=["pA"])
                  for h in range(8):
                      S.pe(lambda e, h=h: e.matmul(pB[0:64, h:h + 1], lhsT=lw[:, h * 64:(h + 1) * 64], rhs=ones_f[:], start=True, stop=True), reads=["lw", "ones_f"], writes=["pB"])
                  S.act(lambda e: e.activation(out=dC[:], in_=pB[0:64, 0:8], func=AF.Exp), reads=["pB"], writes=["dC"])
                  S.act(lambda e: e.activation(out=eP[:], in_=pA[:], func=AF.Exp), reads=["pA"], writes=["eP"])
                  S.act(lambda e: e.activation(out=eN[:], in_=pA[:], func=AF.Exp, scale=-1.0), reads=["pA"], writes=["eN"])
                  S.dve(lambda e: e.tensor_tensor(out=f1[:], in0=pA[:], in1=lw[:], op=ALU.subtract), reads=["pA", "lw"], writes=["rwc"])
                  S.act(lambda e: e.activation(out=lw[:], in_=f1[:], func=AF.Exp), reads=["rwc"], writes=["lw"])
                  v3 = lambda ap: ap.rearrange("p (a b) -> p a b", a=8)
                  S.dve(lambda e: e.tensor_tensor(out=f2[:], in0=k_, in1=KK, op=ALU.mult), reads=["rwp", "rwcb"], writes=["rwc"])
                  S.dve(lambda e: e.tensor_tensor(out=f3[:], in0=f2[:], in1=f2[:], op=ALU.mult), reads=["rwc"], writes=["rwc"])
                  S.dve(lambda e: e.tensor_reduce(out=s8[:, 0, :], in_=v3(f3[:]), axis=AX.X, op=ALU.add), reads=["rwc"], writes=["s80"])
                  S.act(lambda e: e.activation(out=s8[:, 0, :], in_=s8[:, 0, :], func=AF.Sqrt), reads=["s80"], writes=["s80"])
                  S.dve(lambda e: e.tensor_scalar(out=s8[:, 0, :], in0=s8[:, 0, :], scalar1=1e-12, scalar2=None, op0=ALU.max), reads=["s80"], writes=["s80"])
                  S.dve(lambda e: e.reciprocal(out=s8[:, 0, :], in_=s8[:, 0, :]), reads=["s80"], writes=["s80"])
                  S.dve(lambda e: e.tensor_tensor(out=v3(f2[:]), in0=v3(f2[:]), in1=s8[:, 0, :].unsqueeze(2).broadcast_to([128, 8, 64]), op=ALU.mult), reads=["rwc", "s80"], writes=["rwc"])
                  S.dve(lambda e: e.scalar_tensor_tensor(out=f3[:], in0=av[:], scalar=-1.0, in1=KA, op0=ALU.add, op1=ALU.mult), reads=["av", "rwcb"], writes=["rwc"])
                  S.dve(lambda e: e.scalar_tensor_tensor(out=kmod[:], in0=f3[:], scalar=1.0, in1=k_, op0=ALU.add, op1=ALU.mult), reads=["rwc", "rwp"], writes=["kmod"])
                  S.dve(lambda e: e.scalar_tensor_tensor(out=At[:], in0=f2[:], scalar=-1.0, in1=lw[:], op0=ALU.mult, op1=ALU.mult), reads=["rwc", "lw"], writes=["At"])
                  S.dve(lambda e: e.tensor_tensor(out=f4[:], in0=f2[:], in1=av[:], op=ALU.mult), reads=["rwc", "av"], writes=["f4"])
                  S.dve(lambda e: e.tensor_tensor(out=Bt[:], in0=f4[:], in1=eN[:], op=ALU.mult), reads=["f4", "eN"], writes=["Bt"])
                  S.dve(lambda e: e.tensor_tensor(out=Kt[:], in0=kmod[:], in1=eN[:], op=ALU.mult), reads=["kmod", "eN"], writes=["Kt"])
                  S.dve(lambda e: e.tensor_tensor(out=Rt[:], in0=r_, in1=eP[:], op=ALU.mult), reads=["rwp", "eP"], writes=["Rt"])
                  S.act(lambda e: e.copy(out=Vb[:], in_=v_), reads=["rwp"], writes=["Vb"])
                  cut("A_prep", kmod[:], "Vb")
                  for (src, skey, dst, dkey, j, pp) in ((At, "At", arT, "arT", 0, pTb), (Rt, "Rt", arT, "arT", 1, pTb2), (Bt, "Bt", bkT, "bkT", 0, pTb), (Kt, "Kt", bkT, "bkT", 1, pTb2)):
                      for h in range(8):
                          S.pe(lambda e, h=h, src=src, pp=pp: e.transpose(out=pp[0:64, h * 128:(h + 1) * 128], in_=src[:, h * 64:(h + 1) * 64], identity=ident_b), reads=[skey, "cstb"], writes=[pp.name])
                      S.act(lambda e, dst=dst, j=j, pp=pp: e.copy(out=dst[:, :, j, :], in_=pp[0:64, :].rearrange("p (a b) -> p a b", a=8)), reads=[pp.name], writes=[dkey])
                  cut("A_tr", kmod[:], "bkT")
                  Told, Tnew = Tst[cur], Tst[prv]
                  for h in range(8):
                      arh = arT[:, h].rearrange("p a b -> p (a b)")
                      S.pe(lambda e, h=h, arh=arh: e.matmul(pC[:, 0:256], lhsT=bkT[:, h, 0, :], rhs=arh, start=True, stop=True), reads=["bkT", "arT"], writes=["pC"])
                      S.pe(lambda e, h=h, arh=arh: e.matmul(pC[:, 256:512], lhsT=bkT[:, h, 1, :], rhs=arh, start=True, stop=True), reads=["bkT", "arT"], writes=["pC"])
                      S.pe(lambda e, h=h: e.matmul(pD[:, 0:128], lhsT=arT[:, h, 0, :], rhs=bkT[:, h, 0, :], start=True, stop=True), reads=["bkT", "arT"], writes=["pD"])
                      S.dve(lambda e: e.tensor_tensor(out=XA[:], in0=pC[:].rearrange("p (a b) -> p a b", a=2), in1=msk2[:].unsqueeze(1).broadcast_to([128, 2, 256]), op=ALU.mult), reads=["pC", "msk2"], writes=["XA"])
                      S.dve(lambda e: e.tensor_tensor(out=Lp[0][:], in0=pD[:, 0:128], in1=MST, op=ALU.mult), reads=["pD", "cst"], writes=[("Lp", 0)])
                      S.add("dve", lambda e: e.tensor_copy(out=XN[0][:, 0:128], in_=XA[:, 0, 0:128]), reads=["XA"], writes=[("XN", 0)])
                      S.add("dve", lambda e: e.tensor_tensor(out=XN[0][:, 128:256], in0=XA[:, 0, 0:128], in1=ident_b, op=ALU.add), reads=["XA", "cstb"], writes=[("XN", 0)])
                      cut("H%d_a" % h, kmod[:], ("XN", 0))
                      S.pe(lambda e: e.matmul(pD[:, 0:128], lhsT=Lp[0][:], rhs=XN[0][:, 0:128], start=True, stop=True), reads=[("Lp", 0), ("XN", 0)], writes=["pD"])
                      S.pe(lambda e: e.matmul(pD[:, 256:384], lhsT=XN[0][:, 0:128], rhs=Lp[0][:], start=True, stop=True), reads=[("Lp", 0), ("XN", 0)], writes=["pD"])
                      S.add("act", lambda e: e.copy(out=XN[1][:, 0:128], in_=pD[:, 0:128]), reads=["pD"], writes=[("XN", 1)])
                      S.add("act", lambda e: e.copy(out=Lp[1][:], in_=pD[:, 256:384]), reads=["pD"], writes=[("Lp", 1)])
                      S.add("dve", lambda e: e.tensor_copy(out=XN[1][:, 128:256], in_=XN[0][:, 128:256]), reads=[("XN", 0)], writes=[("XN", 1)])
                      c = 1
                      for lvl in range(1, 7):
                          n = 1 - c
                          last = lvl == 6
                          if not last:
                              S.pe(lambda e, c=c: e.matmul(pD[:, 0:256], lhsT=Lp[c][:], rhs=XN[c][:], start=True, stop=True), reads=[("Lp", c), ("XN", c)], writes=["pD"])
                              S.pe(lambda e, c=c: e.matmul(pD[:, 256:384], lhsT=XN[c][:, 0:128], rhs=Lp[c][:], start=True, stop=True), reads=[("Lp", c), ("XN", c)], writes=["pD"])
                              S.add("act", lambda e, n=n: e.copy(out=XN[n][:, 0:128], in_=pD[:, 0:128]), reads=["pD"], writes=[("XN", n)])
                              S.add("act", lambda e, n=n: e.copy(out=Lp[n][:], in_=pD[:, 256:384]), reads=["pD"], writes=[("Lp", n)])
                          else:
                              S.pe(lambda e, c=c: e.matmul(pD[:, 128:256], lhsT=Lp[c][:], rhs=XN[c][:, 128:256], start=True, stop=True), reads=[("Lp", c), ("XN", c)], writes=["pD"])
                          S.dve(lambda e, c=c, n=n: e.tensor_tensor(out=XN[n][:, 128:256], in0=pD[:, 128:256], in1=XN[c][:, 128:256], op=ALU.add), reads=["pD", ("XN", c)], writes=[("XN", n)])
                          c = n
                      cut("H%d_b" % h, kmod[:], ("XN", c))
                      Nf = XN[c][:, 128:256]
                      S.pe(lambda e, h=h, Told=Told: e.matmul(pE[:, 300:364], lhsT=arT[:, h, 0, :], rhs=Told[:, h, :], start=True, stop=False), reads=["arT", ("Tst", cur)], writes=["pE"])
                      S.pe(lambda e, h=h: e.matmul(pE[:, 300:364], lhsT=XA[:, 1, 0:128], rhs=Vb[:, h * 64:(h + 1) * 64], start=False, stop=True), reads=["XA", "Vb"], writes=["pE"])
                      S.act(lambda e: e.copy(out=RHSs[:], in_=pE[:, 300:364]), reads=["pE"], writes=["RHSs"])
                      S.pe(lambda e, Nf=Nf: e.matmul(pE[:, 364:428], lhsT=Nf, rhs=RHSs[:], start=True, stop=True), reads=[("XN", c), "RHSs"], writes=["pE"])
                      S.act(lambda e: e.copy(out=Us[:], in_=pE[:, 364:428]), reads=["pE"], writes=["Us"])
                      cut("H%d_c" % h, kmod[:], "Us")
                      S.pe(lambda e, h=h, Told=Told: e.matmul(pF[:, h * 64:(h + 1) * 64], lhsT=arT[:, h, 1, :], rhs=Told[:, h, :], start=True, stop=False), reads=["arT", ("Tst", cur)], writes=["pF"])
                      S.pe(lambda e, h=h: e.matmul(pF[:, h * 64:(h + 1) * 64], lhsT=XA[:, 0, 128:256], rhs=Us[:], start=False, stop=False), reads=["XA", "Us"], writes=["pF"])
                      S.pe(lambda e, h=h: e.matmul(pF[:, h * 64:(h + 1) * 64], lhsT=XA[:, 1, 128:256], rhs=Vb[:, h * 64:(h + 1) * 64], start=False, stop=True), reads=["XA", "Vb"], writes=["pF"])
                      S.pe(lambda e, h=h, Told=Told: e.matmul(pE[0:64, 428:492], lhsT=ident_b[0:64, 0:64], rhs=Told[:, h, :], start=True, stop=False), reads=["cstb", ("Tst", cur)], writes=["pE"])
                      S.pe(lambda e, h=h: e.matmul(pE[0:64, 428:492], lhsT=Bt[:, h * 64:(h + 1) * 64], rhs=Us[:], start=False, stop=False), reads=["Bt", "Us"], writes=["pE"])
                      S.pe(lambda e, h=h: e.matmul(pE[0:64, 428:492], lhsT=Kt[:, h * 64:(h + 1) * 64], rhs=Vb[:, h * 64:(h + 1) * 64], start=False, stop=True), reads=["Kt", "Vb"], writes=["pE"])
                      S.act(lambda e, h=h, Tnew=Tnew: e.activation(out=Tnew[:, h, :], in_=pE[0:64, 428:492], func=AF.Copy, scale=dC[:, h:h + 1]), reads=["pE", "dC"], writes=[("Tst", prv)])
                      cut("A_hd%d" % h, kmod[:], ("Tst", prv))
                  cut("A_heads", kmod[:], "pF")
                  S.act(lambda e: e.copy(out=f4[:], in_=pF[:]), reads=["pF"], writes=["f4"])
                  S.dve(lambda e: e.tensor_reduce(out=s8[:, 1, :], in_=v3(f4[:]), axis=AX.X, op=ALU.add), reads=["f4"], writes=["s81"])
                  S.dve(lambda e: e.tensor_tensor(out=f3[:], in0=f4[:], in1=f4[:], op=ALU.mult), reads=["f4"], writes=["rwc"])
                  S.dve(lambda e: e.tensor_reduce(out=s8[:, 2, :], in_=v3(f3[:]), axis=AX.X, op=ALU.add), reads=["rwc"], writes=["s82"])
                  S.dve(lambda e: e.tensor_scalar(out=s8[:, 1, :], in0=s8[:, 1, :], scalar1=1.0 / 64, scalar2=None, op0=ALU.mult), reads=["s81"], writes=["s81"])
                  S.dve(lambda e: e.tensor_tensor(out=s8[:, 3, :], in0=s8[:, 1, :], in1=s8[:, 1, :], op=ALU.mult), reads=["s81"], writes=["s83"])
                  S.dve(lambda e: e.scalar_tensor_tensor(out=s8[:, 2, :], in0=s8[:, 2, :], scalar=1.0 / 64, in1=s8[:, 3, :], op0=ALU.mult, op1=ALU.subtract), reads=["s82", "s83"], writes=["s82"])
                  S.dve(lambda e: e.tensor_scalar(out=s8[:, 2, :], in0=s8[:, 2, :], scalar1=GN_EPS, scalar2=None, op0=ALU.add), reads=["s82"], writes=["s82"])
                  S.act(lambda e: e.activation(out=s8[:, 2, :], in_=s8[:, 2, :], func=AF.Sqrt), reads=["s82"], writes=["s82"])
                  S.dve(lambda e: e.reciprocal(out=s8[:, 2, :], in_=s8[:, 2, :]), reads=["s82"], writes=["s82"])
                  S.dve(lambda e: e.tensor_tensor(out=v3(f4[:]), in0=v3(f4[:]), in1=s8[:, 1, :].unsqueeze(2).broadcast_to([128, 8, 64]), op=ALU.subtract), reads=["f4", "s81"], writes=["f4"])
                  S.dve(lambda e: e.tensor_tensor(out=v3(f4[:]), in0=v3(f4[:]), in1=s8[:, 2, :].unsqueeze(2).broadcast_to([128, 8, 64]), op=ALU.mult), reads=["f4", "s82"], writes=["f4"])
                  S.dve(lambda e: e.tensor_tensor(out=f4[:], in0=f4[:], in1=LNW, op=ALU.mult), reads=["f4", "rwcb"], writes=["f4"])
                  S.dve(lambda e: e.tensor_tensor(out=f4[:], in0=f4[:], in1=LNB, op=ALU.add), reads=["f4", "rwcb"], writes=["f4"])
                  S.dve(lambda e: e.tensor_tensor(out=eP[:], in0=r_, in1=kmod[:], op=ALU.mult), reads=["rwp", "kmod"], writes=["eP"])
                  S.dve(lambda e: e.tensor_tensor(out=eP[:], in0=eP[:], in1=RK, op=ALU.mult), reads=["eP", "rwcb"], writes=["eP"])
                  S.dve(lambda e: e.tensor_reduce(out=s8[:, 3, :], in_=v3(eP[:]), axis=AX.X, op=ALU.add), reads=["eP"], writes=["s83"])
                  S.dve(lambda e: e.tensor_tensor(out=v3(eN[:]), in0=v3(v_), in1=s8[:, 3, :].unsqueeze(2).broadcast_to([128, 8, 64]), op=ALU.mult), reads=["rwp", "s83"], writes=["eN"])
                  S.dve(lambda e: e.tensor_tensor(out=f4[:], in0=f4[:], in1=eN[:], op=ALU.add), reads=["f4", "eN"], writes=["f4"])
                  S.pe(lambda e: e.matmul(pA[:], lhsT=loT[0:96, 2, :], rhs=lor[0:96, 2, :], start=True, stop=True), reads=["loT", "lor"], writes=["pA"])
                  S.dve(lambda e: e.tensor_tensor(out=mixb[:, 512:1024], in0=f4[:], in1=pA[:], op=ALU.mult), reads=["f4", "pA"], writes=["mixb"])
                  cut("A_rwkv", mixb[:, 512:1024], "mixb")
                  for k in range(8):
                      S.pe(lambda e, k=k: e.transpose(out=pTb[:, k * 128:(k + 1) * 128], in_=mixb[:, k * 128:(k + 1) * 128], identity=ident_b), reads=["mixb", "cstb"], writes=["pTb"])
                  S.act(lambda e: e.copy(out=hT[:].rearrange("p a b -> p (a b)"), in_=pTb[:]), reads=["pTb"], writes=["hT"])
                  for hf, pp in ((0, pA), (1, pB)):
                      for k in range(8):
                          S.pe(lambda e, k=k, hf=hf, pp=pp: e.matmul(pp[:], lhsT=hT[:, k, :], rhs=w_out_bf[:, k, hf * 512:(hf + 1) * 512], start=(k == 0), stop=(k == 7)), reads=["hT", "w_out_bf"], writes=[pp.name])
                      S.dve(lambda e, hf=hf, pp=pp: e.tensor_tensor(out=t1k[:, hf * 512:(hf + 1) * 512], in0=pp[:], in1=modb[:, 2, hf * 512:(hf + 1) * 512], op=ALU.mult), reads=[pp.name, "modb"], writes=["t1k"])
                  S.dve(lambda e: e.scalar_tensor_tensor(out=t1k[:], in0=xt[:], scalar=float(ALPHA), in1=t1k[:], op0=ALU.mult, op1=ALU.add), reads=["xt", "t1k"], writes=["t1k"])
                  layer_norm_stats(t1k, "t1k")
                  S.dve(lambda e: e.tensor_scalar(out=t1k[:], in0=t1k[:], scalar1=mv[:, 0:1], scalar2=rstd[:], op0=ALU.subtract, op1=ALU.mult), reads=["t1k", "mv", "rstd"], writes=["t1k"])
                  S.dve(lambda e: e.tensor_tensor(out=t1k[:], in0=t1k[:], in1=lnpb[:, 0, :], op=ALU.mult), reads=["t1k", "lnpb"], writes=["t1k"])
                  S.dve(lambda e: e.tensor_tensor(out=t1k[:], in0=t1k[:], in1=lnpb[:, 1, :], op=ALU.add), reads=["t1k", "lnpb"], writes=["t1k"])
                  S.dma("sp", lambda e, rows=rows: e.dma_start(out=x1s[rows, :], in_=t1k[:]), reads=["t1k"], writes=["x1s"])
                  if stage == "A1":
                      S.dma("sp", lambda e, rows=rows: e.dma_start(out=dbg[rows, :], in_=t1k[:]), reads=["t1k"])
                  if _os.environ.get("KSKIP") == "router":
                      continue
                  layer_norm_stats(t1k, "t1k")
                  S.dve(lambda e: e.tensor_scalar(out=t1k[:], in0=t1k[:], scalar1=mv[:, 0:1], scalar2=rstd[:], op0=ALU.subtract, op1=ALU.mult), reads=["t1k", "mv", "rstd"], writes=["t1k"])
                  S.dve(lambda e: e.tensor_tensor(out=t1k[:], in0=t1k[:], in1=modb[:, 4, :], op=ALU.mult), reads=["t1k", "modb"], writes=["t1k"])
                  S.dve(lambda e: e.tensor_tensor(out=t1k[:], in0=t1k[:], in1=modb[:, 3, :], op=ALU.add), reads=["t1k", "modb"], writes=["t1k"])
                  S.act(lambda e: e.copy(out=hb[:], in_=t1k[:]), reads=["t1k"], writes=["hb"])
                  for half in range(2):
                      for k in range(4):
                          kk_ = half * 4 + k
                          S.pe(lambda e, k=k, kk_=kk_: e.transpose(out=pC[:, k * 128:(k + 1) * 128], in_=t1k[:, kk_ * 128:(kk_ + 1) * 128], identity=ident_f), reads=["t1k", "cst"], writes=["pC"])
                      S.act(lambda e, half=half: e.copy(out=h2T[:, half * 4:half * 4 + 4, :].rearrange("p a b -> p (a b)"), in_=pC[:]), reads=["pC"], writes=["xt"])
                  for k in range(8):
                      S.pe(lambda e, k=k: e.matmul(pD[:, 0:NE], lhsT=h2T[:, k, :], rhs=wr_f[:, k, :], start=(k == 0), stop=(k == 7)), reads=["xt", "wr_f"], writes=["pD"])
                  S.dve(lambda e: e.tensor_tensor(out=lg[:], in0=pD[:, 0:NE], in1=brb[:], op=ALU.add), reads=["pD", "brb"], writes=["lg"])
                  S.dve(lambda e: e.max(out=t8[:], in_=lg[:]), reads=["lg"], writes=["t8"])
                  S.dve(lambda e: e.tensor_scalar(out=mskb[:], in0=lg[:], scalar1=t8[:, 3:4], scalar2=None, op0=ALU.is_ge), reads=["lg", "t8"], writes=["mskb"])
                  S.pe(lambda e: e.matmul(pD[:, 64:64 + NE], lhsT=cstb[:, 2, :], rhs=mskb[:], start=True, stop=True), reads=["cstb", "mskb"], writes=["pD"])
                  S.pe(lambda e: e.matmul(pD[:, 128:128 + NE], lhsT=onesb[:], rhs=mskb[:], start=True, stop=True), reads=["onesb", "mskb"], writes=["pD"])
                  S.dve(lambda e: e.tensor_tensor(out=posn[:], in0=pD[:, 64:64 + NE], in1=tot[:], op=ALU.add), reads=["pD", "tot"], writes=["posn"])
                  S.dve(lambda e: e.tensor_tensor(out=tot[:], in0=pD[:, 128:128 + NE], in1=tot[:], op=ALU.add), reads=["pD", "tot"], writes=["tot"])
                  S.dve(lambda e: e.tensor_scalar(out=eqk[:], in0=posn[:], scalar1=float(CAP), scalar2=None, op0=ALU.is_lt), reads=["posn"], writes=["eqk"])
                  S.dve(lambda e: e.tensor_tensor(out=posn[:], in0=posn[:], in1=ebase[:], op=ALU.add), reads=["posn", "ebase"], writes=["posn"])
                  S.dve(lambda e: e.tensor_scalar(out=posn[:], in0=posn[:], scalar1=trash[:, 0:1], scalar2=None, op0=ALU.subtract), reads=["posn", "trash"], writes=["posn"])
                  S.dve(lambda e: e.tensor_tensor(out=posn[:], in0=posn[:], in1=eqk[:], op=ALU.mult), reads=["posn", "eqk"], writes=["posn"])
                  S.dve(lambda e: e.tensor_scalar(out=posn[:], in0=posn[:], scalar1=trash[:, 0:1], scalar2=None, op0=ALU.add), reads=["posn", "trash"], writes=["posn"])
                  for k4 in range(4):
                      S.dve(lambda e, k4=k4: e.tensor_scalar(out=eqk[:], in0=lg[:], scalar1=t8[:, k4:k4 + 1], scalar2=None, op0=ALU.is_equal), reads=["lg", "t8"], writes=["eqk"])
                      S.dve(lambda e: e.tensor_tensor(out=eqk[:], in0=eqk[:], in1=posn[:], op=ALU.mult), reads=["eqk", "posn"], writes=["eqk"])
                      S.dve(lambda e, k4=k4: e.tensor_reduce(out=sl4[:, k4:k4 + 1], in_=eqk[:], axis=AX.X, op=ALU.add), reads=["eqk"], writes=["sl4"])
                  S.dve(lambda e, i=i: e.tensor_copy(out=slot4[:, i, :], in_=sl4[:]), reads=["sl4"], writes=["slot4"])
                  S.dve(lambda e: e.tensor_scalar(out=ev4[:], in0=t8[:, 0:4], scalar1=t8[:, 0:1], scalar2=None, op0=ALU.subtract), reads=["t8"], writes=["ev4"])
                  S.act(lambda e: e.activation(out=ev4[:], in_=ev4[:], func=AF.Exp), reads=["ev4"], writes=["ev4"])
                  S.dve(lambda e: e.tensor_reduce(out=t8[:, 7:8], in_=ev4[:], axis=AX.X, op=ALU.add), reads=["ev4"], writes=["t8"])
                  S.dve(lambda e: e.reciprocal(out=t8[:, 7:8], in_=t8[:, 7:8]), reads=["t8"], writes=["t8"])
                  S.dve(lambda e: e.tensor_scalar(out=ev4[:], in0=ev4[:], scalar1=t8[:, 7:8], scalar2=None, op0=ALU.mult), reads=["ev4", "t8"], writes=["ev4"])
                  S.dve(lambda e: e.tensor_scalar(out=sl4[:], in0=sl4[:], scalar1=float(NSLOT), scalar2=None, op0=ALU.is_lt), reads=["sl4"], writes=["sl4"])
                  S.dve(lambda e, i=i: e.tensor_tensor(out=gate4[:, i, :], in0=ev4[:], in1=sl4[:], op=ALU.mult), reads=["ev4", "sl4"], writes=["gate4"])
                  for k4 in range(4 if stage in ("full", "A_scat", "BC") else 0):
                      S.dma("pool", lambda e, i=i, k4=k4: e.indirect_dma_start(out=Xs[:, :], out_offset=bass.IndirectOffsetOnAxis(ap=slot4[:, i, k4:k4 + 1], axis=0), in_=hb[:], in_offset=None),
                            reads=["hb", "slot4"], writes=["Xs"])
              S.dve(lambda e: e.memset(junk[:, 0:1], 0.0), reads=["slot4", "gate4", "modb", "cst", "cstb"], writes=["junk"])
          S.add("dve", lambda e: e.memset(junk[:, 1:2], 0.0), reads=[], writes=["junk"], barrier=True)

          if stage in ("A1",):
              S.emit()
              return nc

          with contextlib.ExitStack() as st:
              sb = lambda name, shape, d: st.enter_context(nc.sbuf_tensor(name, shape, d))
              Wgu = [sb("Wgu%d" % j, [128, 8, 2 * D], BF16) for j in range(1)] * 2
              Wd = [sb("Wd%d" % j, [128, 8, D], BF16) for j in range(1)] * 2
              bdn = sb("bdn", [1, NE, D], BF16)
              bgu = sb("bgu", [128, NE, 16], F32)
              Xe = sb("Xe", [128, 6, D], BF16)
              XT = sb("XT", [128, 8, CAP], BF16)
              aT = sb("aT", [128, 8, CAP], BF16)
              G = sb("G", [128, 384], F32)
              Sg = sb("Sg", [128, 384], F32)
              Uc = sb("Uc", [128, 384], F32)
              Yo = [sb("Yo%d" % j, [128, D], F32) for j in range(2)]
              ones1 = sb("ones1", [1, 128], BF16)
              zt = sb("zt", [128, D], F32)
              S.dma("pool", lambda e: e.dma_start(out=bdn[:], in_=b_dn[:, :, :], max_dma_last_dim=4096), writes=["bdn"])
              S.dma("sp", lambda e: e.dma_start(out=bgu[:], in_=bguT[:, :, :]), writes=["bgu"])
              S.pool(lambda e: e.memset(ones1[:], 1.0), writes=["ones1"])
              S.pool(lambda e: e.memset(zt[:], 0.0), writes=["zt"])
              S.dma("sp", lambda e: e.dma_start(out=Ys[NSLOT:NSLOT + 128, :], in_=zt[:]), reads=["zt"], writes=["Ys"])

              def load_w(ex):
                  j = 0
                  gv_ = w_gu[ex].rearrange("(k p) n -> p k n", p=128)
                  dv_ = w_dn[ex].rearrange("(k p) n -> p k n", p=128)
                  for k in range(0, 8, 2):
                      S.dma("pool", lambda e, k=k, j=j, gv_=gv_: e.dma_start(out=Wgu[j][:, k:k + 2, :], in_=gv_[:, k:k + 2, :]), writes=[("Wgu", j)])
                  for k in range(0, 8, 4):
                      S.dma("pool", lambda e, k=k, j=j, dv_=dv_: e.dma_start(out=Wd[j][:, k:k + 4, :], in_=dv_[:, k:k + 4, :]), writes=[("Wd", j)])

              load_w(0)
              for ex in range(nexp):
                  j = 0
                  if ex > 0:
                      load_w(ex)
                  S.dma("sp", lambda e, ex=ex: e.dma_start(out=Xe[:], in_=Xs[ex * CAP:(ex + 1) * CAP, :].rearrange("(s p) d -> p s d", p=128)), reads=["Xs"], writes=["Xe"])
                  for s in range(6):
                      pp = pTb if s % 2 == 0 else pTb2
                      for k in range(8):
                          S.pe(lambda e, s=s, k=k, pp=pp: e.transpose(out=pp[:, k * 128:(k + 1) * 128], in_=Xe[:, s, k * 128:(k + 1) * 128], identity=ident_b), reads=["Xe", "cstb"], writes=[pp.name])
                      S.act(lambda e, s=s, pp=pp: e.copy(out=XT[:, :, s * 128:(s + 1) * 128], in_=pp[:].rearrange("p (a b) -> p a b", a=8)), reads=[pp.name], writes=["XT"])
                  for nh in range(2):
                      n0 = nh * 384
                      for fc in range(8):
                          for (pp, col) in ((pA, fc), (pB, 8 + fc)):
                              for k in range(8):
                                  S.pe(lambda e, k=k, pp=pp, col=col, j=j, n0=n0: e.matmul(pp[:, 0:384], lhsT=Wgu[j][:, k, col * 128:(col + 1) * 128], rhs=XT[:, k, n0:n0 + 384], start=(k == 0), stop=(k == 7)),
                                       reads=[("Wgu", j), "XT"], writes=[pp.name])
                          S.dve(lambda e, ex=ex, fc=fc: e.tensor_scalar(out=G[:], in0=pA[:, 0:384], scalar1=bgu[:, ex, fc:fc + 1], scalar2=7.0, op0=ALU.add, op1=ALU.min), reads=["pA", "bgu"], writes=["G"])
                          S.act(lambda e: e.activation(out=Sg[:], in_=G[:], func=AF.Sigmoid, scale=1.702), reads=["G"], writes=["Sg"])
                          S.dve(lambda e, ex=ex, fc=fc: e.tensor_scalar(out=Uc[:], in0=pB[:, 0:384], scalar1=bgu[:, ex, 8 + fc:9 + fc], scalar2=7.0, op0=ALU.add, op1=ALU.min), reads=["pB", "bgu"], writes=["Uc"])
                          S.dve(lambda e: e.tensor_scalar(out=Uc[:], in0=Uc[:], scalar1=-7.0, scalar2=1.0, op0=ALU.max, op1=ALU.add), reads=["Uc"], writes=["Uc"])
                          S.dve(lambda e: e.tensor_tensor(out=G[:], in0=G[:], in1=Sg[:], op=ALU.mult), reads=["G", "Sg"], writes=["G"])
                          S.dve(lambda e, fc=fc, n0=n0: e.tensor_tensor(out=aT[:, fc, n0:n0 + 384], in0=G[:], in1=Uc[:], op=ALU.mult), reads=["G", "Uc"], writes=["aT"])
                  for s in range(6):
                      yo = Yo[s % 2]
                      for hf, pp in ((0, pC), (1, pD)):
                          S.pe(lambda e, hf=hf, pp=pp, ex=ex: e.matmul(pp[:], lhsT=ones1[:], rhs=bdn[:, ex, hf * 512:(hf + 1) * 512], start=True, stop=False), reads=["ones1", "bdn"], writes=[pp.name])
                          for k in range(8):
                              S.pe(lambda e, k=k, hf=hf, pp=pp, s=s, j=j: e.matmul(pp[:], lhsT=aT[:, k, s * 128:(s + 1) * 128], rhs=Wd[j][:, k, hf * 512:(hf + 1) * 512], start=False, stop=(k == 7)),
                                   reads=["aT", ("Wd", j)], writes=[pp.name])
                          S.act(lambda e, hf=hf, pp=pp, yo=yo: e.copy(out=yo[:, hf * 512:(hf + 1) * 512], in_=pp[:]), reads=[pp.name], writes=[("Yo", s % 2)])
                      S.dma("sp", lambda e, ex=ex, s=s, yo=yo: e.dma_start(out=Ys[ex * CAP + s * 128:ex * CAP + (s + 1) * 128, :], in_=yo[:]), reads=[("Yo", s % 2)], writes=["Ys"])
              S.dve(lambda e: e.memset(junk[:, 2:3], 0.0), reads=["slot4", "gate4", "modb"], writes=["junk"])
          S.add("dve", lambda e: e.memset(junk[:, 3:4], 0.0), reads=[], writes=["junk"], barrier=True)

          with contextlib.ExitStack() as st:
              sb = lambda name, shape, d: st.enter_context(nc.sbuf_tensor(name, shape, d))
              Yg = [sb("Yg%d" % j, [128, 4, D], F32) for j in range(2)]
              x1t = [sb("x1t%d" % j, [128, D], F32) for j in range(2)]
              acc = sb("acc", [128, D], F32)
              lnpc = sb("lnpbC", [128, 2, D], F32)
              S.dma("sp", lambda e: e.dma_start(out=lnpc[:], in_=lnp[:, 2:4, :]), writes=["lnpb"])
              st6c = sb("st6c", [128, 2, 6], F32)
              mvc = sb("mvc", [128, 2], F32)
              rsc = sb("rsc", [128, 1], F32)
              for i in range(ntiles):
                  j = i % 2
                  rows = slice(i * 128, (i + 1) * 128)
                  S.dma("sp", lambda e, rows=rows, j=j: e.dma_start(out=x1t[j][:], in_=x1s[rows, :]), reads=["x1s"], writes=[("x1t", j)])
                  for k4 in range(4):
                      S.dma("pool", lambda e, i=i, k4=k4, j=j: e.indirect_dma_start(out=Yg[j][:, k4, :], out_offset=None, in_=Ys[:, :], in_offset=bass.IndirectOffsetOnAxis(ap=slot4[:, i, k4:k4 + 1], axis=0)),
                            reads=["Ys", "slot4"], writes=[("Yg", j, k4)])
                  S.dve(lambda e, i=i, j=j: e.tensor_scalar(out=acc[:], in0=Yg[j][:, 0, :], scalar1=gate4[:, i, 0:1], scalar2=None, op0=ALU.mult), reads=[("Yg", j, 0), "gate4"], writes=["acc"])
                  for k4 in range(1, 4):
                      S.dve(lambda e, i=i, j=j, k4=k4: e.scalar_tensor_tensor(out=acc[:], in0=Yg[j][:, k4, :], scalar=gate4[:, i, k4:k4 + 1], in1=acc[:], op0=ALU.mult, op1=ALU.add), reads=[("Yg", j, k4), "gate4", "acc"], writes=["acc"])
                  S.dve(lambda e: e.tensor_tensor(out=acc[:], in0=acc[:], in1=modb[:, 5, :], op=ALU.mult), reads=["acc", "modb"], writes=["acc"])
                  S.dve(lambda e, j=j: e.scalar_tensor_tensor(out=acc[:], in0=x1t[j][:], scalar=float(ALPHA), in1=acc[:], op0=ALU.mult, op1=ALU.add), reads=[("x1t", j), "acc"], writes=["acc"])
                  for h in range(2):
                      S.dve(lambda e, h=h: e.bn_stats(out=st6c[:, h, :], in_=acc[:, h * 512:(h + 1) * 512]), reads=["acc"], writes=["st6c"])
                  S.dve(lambda e: e.bn_aggr(out=mvc[:], in_=st6c[:].rearrange("p a b -> p (a b)")), reads=["st6c"], writes=["mvc"])
                  S.dve(lambda e: e.tensor_scalar(out=rsc[:], in0=mvc[:, 1:2], scalar1=LN_EPS, scalar2=None, op0=ALU.add), reads=["mvc"], writes=["rsc"])
                  S.act(lambda e: e.activation(out=rsc[:], in_=rsc[:], func=AF.Sqrt), reads=["rsc"], writes=["rsc"])
                  S.dve(lambda e: e.reciprocal(out=rsc[:], in_=rsc[:]), reads=["rsc"], writes=["rsc"])
                  S.dve(lambda e: e.tensor_scalar(out=acc[:], in0=acc[:], scalar1=mvc[:, 0:1], scalar2=rsc[:], op0=ALU.subtract, op1=ALU.mult), reads=["acc", "mvc", "rsc"], writes=["acc"])
                  S.dve(lambda e: e.tensor_tensor(out=acc[:], in0=acc[:], in1=lnpc[:, 0, :], op=ALU.mult), reads=["acc", "lnpb"], writes=["acc"])
                  S.dve(lambda e, j=j: e.tensor_tensor(out=x1t[j][:], in0=acc[:], in1=lnpc[:, 1, :], op=ALU.add), reads=["acc", "lnpb"], writes=[("x1t", j)])
                  S.dma("sp", lambda e, rows=rows, j=j: e.dma_start(out=out[rows, :], in_=x1t[j][:]), reads=[("x1t", j)], writes=["out"])

    except _Cut:
        pass
    S.emit()
    return nc


def _prep_shared(inp):
    f = lambda a: np.ascontiguousarray(np.asarray(a), dtype=np.float32)
    bc = lambda v, n=128: np.ascontiguousarray(np.broadcast_to(np.asarray(v, np.float32).reshape(1, -1), (n, np.asarray(v).size)))
    w_in = f(inp["w_in"][0])
    perm = np.concatenate([np.arange(0, 512), np.arange(768, 2464), np.arange(512, 640), np.arange(640, 768)])
    sh = {}
    sh["w_ada"] = f(inp["w_ada"][0])
    sh["b_ada_b"] = bc(inp["b_ada"][0])
    sh["w_in"] = np.ascontiguousarray(w_in[:, perm])
    sh["mu_b"] = bc(inp["shift_mu"][0])
    rows = [inp["rwkv_w0"][0], inp["rwkv_a0"][0], inp["rwkv_k_k"][0], inp["rwkv_k_a"][0], np.asarray(inp["rwkv_r_k"][0]).reshape(-1), inp["rwkv_ln_w"][0], inp["rwkv_ln_b"][0]]
    sh["rwc_b"] = np.ascontiguousarray(np.stack([bc(r) for r in rows], axis=1))
    lora = np.zeros((96, 3, 512), np.float32)
    lora[0:32, 0] = inp["rwkv_w2"][0]
    lora[0:32, 1] = inp["rwkv_a2"][0]
    lora[0:96, 2] = inp["rwkv_g2"][0]
    sh["lora"] = lora
    sh["sinks_b"] = bc(inp["attn_sinks"][0])
    invf = (500000.0 ** (-np.arange(0, 16, 2, dtype=np.float32) / 16)).astype(np.float32)
    sh["invf_b"] = bc(invf)
    sh["w_out"] = f(inp["w_out"][0])
    sh["lnp"] = np.ascontiguousarray(np.stack([bc(inp[k][0]) for k in ("ln1_g", "ln1_b", "ln2_g", "ln2_b")], axis=1))
    sh["w_router"] = f(inp["w_router"][0])
    sh["b_router_b"] = bc(inp["b_router"][0])
    sh["w_gu"] = f(inp["w_gate_up"][0])
    sh["bguT"] = np.ascontiguousarray(f(inp["b_gate_up"][0]).reshape(NE, 16, 128).transpose(2, 0, 1))
    sh["w_dn"] = f(inp["w_down"][0])
    sh["b_dn"] = f(inp["b_down"][0]).reshape(1, NE, D)
    jj = np.arange(128)[:, None]
    tt = np.arange(128)[None, :]
    sh["consts"] = np.ascontiguousarray(np.stack([np.eye(128), (jj <= tt), (jj < tt), (jj > tt), (jj > tt)], axis=1).astype(np.float32))
    return sh


def _prep_core(inp, b, sh):
    m = dict(sh)
    m["x"] = np.ascontiguousarray(np.asarray(inp["x"][b], np.float32))
    m["posT"] = np.ascontiguousarray(np.asarray(inp["positions"][b], np.int32).reshape(NT, 128).T)
    c = np.asarray(inp["c"][b], np.float32)
    m["cB"] = np.ascontiguousarray(np.broadcast_to(c.reshape(8, 128).T[:, :, None], (128, 8, 128)))
    return m


_NC_CACHE = {}


def kernel(**inputs):
    sh = _prep_shared(inputs)
    in_maps = [_prep_core(inputs, b, sh) for b in range(8)]
    if "full" not in _NC_CACHE:
        _NC_CACHE["full"] = build("full")
    nc = _NC_CACHE["full"]
    res = run_bass_kernel_spmd(nc, in_maps, core_ids=list(range(8)))
    return np.stack([np.asarray(r["out"], np.float32) for r in res.results], axis=0)
```

```python
import contextlib
import os as _os
import numpy as np
import concourse.bass as bass
import concourse.mybir as mybir
from concourse.bass_utils import run_bass_kernel_spmd

F32 = mybir.dt.float32
BF16 = mybir.dt.bfloat16
I32 = mybir.dt.int32
U32 = mybir.dt.uint32
AF = mybir.ActivationFunctionType
ALU = mybir.AluOpType
AX = mybir.AxisListType

COMPUTE = ("pe", "act", "dve", "pool")
SEG = 8192
SAME_ENG_INORDER = ("pe",)
SAME_ENG_DRAIN = ()
SAME_ENG_HZ = ("act", "dve")
HZ_SMALL = 256
BUBBLE = False
NPOOL = {"pe": 24, "act": 24, "dve": 24, "pool": 4}
BUBBLE_DIST = 2
HZ_METHODS = ("tensor_reduce", "bn_stats", "bn_aggr", "max", "reciprocal")


class _Rec:
    def __init__(self):
        self.calls = []

    def __getattr__(self, name):
        def f(*a, **k):
            self.calls.append((name, a, k))
            return self
        return f


def _free_size(ap):
    try:
        sh = list(ap.shape)
        n = 1
        for x in sh[1:]:
            n *= int(x)
        return n
    except Exception:
        return 0


class _Cut(Exception):
    pass


class Sched:
    def __init__(self, nc, kdma=None):
        self.nc = nc
        self.ops = []
        self.last_w = {}
        self.rd_eng = {}
        self.rd_dma = {}
        self.kdma = kdma or {"sp": 16, "pool": 8, "act": 4}
        self.relay_of = {}
        self.relay_fn = None

    def add(self, eng, fn, reads=(), writes=(), dma=False, barrier=False):
        i = len(self.ops)
        reads = list(reads)
        writes = list(writes)
        if barrier:
            writes.append("PHASE")
        else:
            reads.append("PHASE")
        deps = set()
        for r in reads:
            if r in self.last_w:
                deps.add(self.last_w[r])
        for w in writes:
            if w in self.last_w:
                deps.add(self.last_w[w])
            for d in self.rd_eng.get(w, {}).values():
                deps.add(d)
            for d in self.rd_dma.get(w, ()):
                deps.add(d)
        if getattr(self, "relay_fn", None) is not None and not dma:
            nd = set()
            for d in deps:
                od = self.ops[d]
                if (not od["dma"]) and {eng, od["eng"]} in ({"pe", "dve"}, {"pe", "pool"}):
                    if d not in self.relay_of:
                        self.relay_of[d] = len(self.ops)
                        self.ops.append(dict(eng="act", fn=self.relay_fn, deps=[d], dma=False))
                    nd.add(self.relay_of[d])
                else:
                    nd.add(d)
            deps = nd
            i = len(self.ops)
        for w in writes:
            self.last_w[w] = i
            self.rd_eng[w] = {}
            self.rd_dma[w] = []
        ws = set(writes)
        for r in reads:
            if r in ws:
                continue
            if dma:
                self.rd_dma.setdefault(r, []).append(i)
            else:
                self.rd_eng.setdefault(r, {})[eng] = i
        self.ops.append(dict(eng=eng, fn=fn, deps=sorted(deps), dma=dma))
        return i

    def pe(self, fn, reads=(), writes=()):
        return self.add("pe", fn, reads, writes)

    def act(self, fn, reads=(), writes=()):
        return self.add("act", fn, reads, writes)

    def dve(self, fn, reads=(), writes=()):
        return self.add("dve", fn, reads, writes)

    def pool(self, fn, reads=(), writes=()):
        return self.add("pool", fn, reads, writes)

    def dma(self, q, fn, reads=(), writes=()):
        return self.add(q, fn, reads, writes, dma=True)

    def emit(self):
        nc = self.nc
        ops = self.ops
        n = len(ops)
        dcount = {q: [0] * k for q, k in self.kdma.items()}
        dnext = {q: 0 for q in self.kdma}
        tok = [None] * n
        prev_same = [None] * n
        last_on = {}
        order = [0] * n
        ecnt = {}
        for i, o in enumerate(ops):
            e = o["eng"]
            ecnt[e] = ecnt.get(e, 0) + 1
            order[i] = ecnt[e]
            if o["dma"]:
                q = e
                s_ = dnext[q] % self.kdma[q]
                dnext[q] += 1
                dcount[q][s_] += 1
                key = ("d", q, s_)
                tok[i] = (key, 16 * dcount[q][s_])
                prev_same[i] = last_on.get(key)
                last_on[key] = i
        per_eng = {}
        for i, o in enumerate(ops):
            per_eng.setdefault(o["eng"], []).append(i)

        def dep_list(i):
            o = ops[i]
            deps = list(o["deps"])
            if o["dma"] and prev_same[i] is not None:
                deps.append(prev_same[i])
            return deps

        hz = [True] * n
        for i, o in enumerate(ops):
            if o["dma"] or o["eng"] not in SAME_ENG_HZ:
                continue
            r = _Rec()
            try:
                o["fn"](r)
                name, a, k = r.calls[0]
                out = k.get("out", k.get("ap", a[0] if a else None))
                small = _free_size(out) < HZ_SMALL
                hz[i] = small or (name in HZ_METHODS) or ("accum_out" in k and k["accum_out"] is not None)
            except Exception:
                hz[i] = True
        self.n_hz = sum(1 for i, o in enumerate(ops) if (not o["dma"]) and o["eng"] in SAME_ENG_HZ and hz[i])
        needed = [False] * n
        needed_self = [False] * n
        bubble_before = [False] * n
        plan = {}
        drain_before = [False] * n
        for ename, idxs in per_eng.items():
            waited = {}
            drained_upto = 0
            for i in idxs:
                wl = []
                for d in dep_list(i):
                    od = ops[d]
                    if od["dma"]:
                        key, val = tok[d]
                        if waited.get(key, 0) >= val:
                            continue
                        waited[key] = val
                        wl.append(d)
                    else:
                        if od["eng"] == ename and ename in SAME_ENG_INORDER:
                            continue
                        if od["eng"] == ename and ename in SAME_ENG_HZ and not hz[d]:
                            continue
                        if od["eng"] == ename and ename in SAME_ENG_DRAIN:
                            if order[d] > drained_upto:
                                drain_before[i] = True
                                drained_upto = order[i] - 1
                            continue
                        if od["eng"] == ename and ename in SAME_ENG_HZ and BUBBLE:
                            if order[i] - order[d] <= BUBBLE_DIST:
                                bubble_before[i] = True
                            continue
                        if od["eng"] == ename:
                            key = ("s", od["eng"])
                            if waited.get(key, 0) >= order[d]:
                                continue
                            waited[key] = order[d]
                            needed_self[d] = True
                            wl.append((d, "s"))
                            continue
                        key = ("e", od["eng"])
                        if waited.get(key, 0) >= order[d]:
                            continue
                        waited[key] = order[d]
                        needed[d] = True
                        wl.append(d)
                plan[i] = wl
        ecount = {e: 0 for e in COMPUTE}
        scount = {e: 0 for e in COMPUTE}
        stok = [None] * n
        keys = set()
        for i, o in enumerate(ops):
            if o["dma"]:
                keys.add(tok[i][0])
            elif needed[i] or needed_self[i]:
                e = o["eng"]
                key = ("e", e, ecount[e] % NPOOL[e])
                tok[i] = (key, ecount[e] // NPOOL[e] + 1)
                stok[i] = tok[i]
                needed[i] = True
                ecount[e] += 1
                keys.add(key)
        self.n_incs = dict(ecount)
        with contextlib.ExitStack() as st:
            sems = {}
            for key in sorted(keys, key=str):
                sems[key] = st.enter_context(nc.semaphore("s_" + "_".join(str(x) for x in key)))
            block = st.enter_context(nc.Block())
            engmap = {"pe": "tensor", "act": "scalar", "dve": "vector", "pool": "gpsimd", "sp": "sync"}

            def make(ename, idxs):
                def body(eng):
                    for i in idxs:
                        o = ops[i]
                        if drain_before[i]:
                            eng.drain()
                        if bubble_before[i] and ename in getattr(self, "bubble", {}):
                            self.bubble[ename](eng)
                        for d in plan[i]:
                            if isinstance(d, tuple):
                                key, val = stok[d[0]]
                            else:
                                key, val = tok[d]
                            eng.wait_ge(sems[key], val)
                        inst = o["fn"](eng)
                        if o["dma"]:
                            inst.then_inc(sems[tok[i][0]], 16)
                        else:
                            if needed[i]:
                                inst.then_inc(sems[tok[i][0]], 1)
                            elif needed_self[i]:
                                inst.then_inc(sems[stok[i][0]], 1)
                            if (needed[i] or needed_self[i]) and ename in getattr(self, "spacer", {}):
                                self.spacer[ename](eng)
                    if ename in self.kdma:
                        for s_ in range(self.kdma[ename]):
                            if dcount[ename][s_] > 0:
                                eng.wait_ge(sems[("d", ename, s_)], 16 * dcount[ename][s_])
                return body

            for ename, idxs in per_eng.items():
                getattr(block, engmap[ename])(make(ename, idxs))
        return ecount


NT = 32
D = 1024
DIN = 2464
CAP = 768
NE = 32
NSLOT = NE * CAP
LN_EPS = 1e-5
GN_EPS = 64e-5
ALPHA = 2 ** 0.25
TWO_PI = 2.0 * np.pi


def build(stage="full", ntiles=NT, nexp=NE):
    nc = bass.Bass("TRN2", target_bir_lowering=False)
    dt = lambda name, shape, d, kind="ExternalInput": nc.dram_tensor(name, shape, d, kind=kind).ap()
    x = dt("x", [4096, D], F32)
    posT = dt("posT", [128, NT], I32)
    cB = dt("cB", [128, 8, 128], F32)
    w_ada = dt("w_ada", [D, 6 * D], F32)
    b_ada_b = dt("b_ada_b", [128, 6 * D], F32)
    w_in = dt("w_in", [D, DIN], F32)
    mu_b = dt("mu_b", [128, 1696], F32)
    rwc_b = dt("rwc_b", [128, 7, 512], F32)
    lora = dt("lora", [96, 3, 512], F32)
    sinks_b = dt("sinks_b", [128, 8], F32)
    invf_b = dt("invf_b", [128, 8], F32)
    w_out = dt("w_out", [D, D], F32)
    lnp = dt("lnp", [128, 4, D], F32)
    w_router = dt("w_router", [D, NE], F32)
    b_router_b = dt("b_router_b", [128, NE], F32)
    w_gu = dt("w_gu", [NE, D, 2 * D], F32)
    bguT = dt("bguT", [128, NE, 16], F32)
    w_dn = dt("w_dn", [NE, D, D], F32)
    b_dn = dt("b_dn", [1, NE, D], F32)
    consts = dt("consts", [128, 5, 128], F32)
    out = dt("out", [4096, D], F32, kind="ExternalOutput")
    dbg = dt("dbg", [4096, D], F32, kind="ExternalOutput") if stage != "full" else None
    x1s = dt("x1s", [4096, D], F32, kind="Internal")
    Xs = dt("Xs", [NSLOT + 128, D], BF16, kind="Internal")
    Ys = dt("Ys", [NSLOT + 128, D], F32, kind="Internal")
    lastrow = dt("lastrow", [2, 1696], F32, kind="Internal")

    S = Sched(nc)

    cut_tile = [0]

    def cut(name, ap, key, ncols=None):
        if stage == name and (name == "P" or cut_tile[0] == ntiles - 1):
            if ap.shape[-1] > 1024:
                ap = ap[:, 0:1024]
            ncols = ncols or ap.shape[-1]
            q = "sp" if ap.dtype == F32 else "pool"
            S.dma(q, lambda e: e.dma_start(out=dbg[0:ap.shape[0], 0:ncols], in_=ap), reads=[key])
            raise _Cut()

    try:
      with contextlib.ExitStack() as st0:
          sb0 = lambda name, shape, d: st0.enter_context(nc.sbuf_tensor(name, shape, d))
          ps = lambda name, shape, d: st0.enter_context(nc.psum_tensor(name, shape, d))
          pTb = ps("pTb", [128, 1024], BF16)
          pTb2 = ps("pTb2", [128, 1024], BF16)
          pA = ps("pA", [128, 512], F32)
          pB = ps("pB", [128, 512], F32)
          pC = ps("pC", [128, 512], F32)
          pD = ps("pD", [128, 512], F32)
          pE = ps("pE", [128, 512], F32)
          pF = ps("pF", [128, 512], F32)
          cst = sb0("cst", [128, 5, 128], F32)
          cstb = sb0("cstb", [128, 5, 128], BF16)
          modb = sb0("modb", [128, 6, D], F32)
          slot4 = sb0("slot4", [128, NT, 4], I32)
          gate4 = sb0("gate4", [128, NT, 4], F32)
          junk = sb0("junk", [128, 8], F32)
          S.relay_fn = lambda e: e.activation(out=junk[:, 6:7], in_=junk[:, 6:7], func=AF.Copy)
          if True:
              spc = sb0("spc", [128, 2, 512], F32)
              nsp = 512
              nsp = int(_os.environ.get("KSPACE", "0"))
              nbb = int(_os.environ.get("KBUB", "384"))
              S.spacer = {"dve": lambda e: e.memset(spc[:, 0, 0:nsp], 0.0),
                          "act": lambda e: e.activation(out=spc[:, 1, 0:nsp], in_=spc[:, 1, 0:nsp], func=AF.Copy)}
              if nsp == 0:
                  S.spacer = {}
              S.bubble = {"dve": lambda e: e.memset(spc[:, 0, 0:nbb], 0.0),
                          "act": lambda e: e.activation(out=spc[:, 1, 0:nbb], in_=spc[:, 1, 0:nbb], func=AF.Copy)}
          ident_f = cst[:, 0, :]
          ident_b = cstb[:, 0, :]

          S.dma("sp", lambda e: e.dma_start(out=cst[:], in_=consts[:, :, :]), writes=["cst"])
          S.dve(lambda e: e.tensor_copy(out=cstb[:], in_=cst[:]), reads=["cst"], writes=["cstb"])
          S.dma("sp", lambda e: e.dma_start(out=modb[:].rearrange("p a b -> p (a b)"), in_=b_ada_b[:, :]), writes=["modb"])

          with contextlib.ExitStack() as st:
              sb = lambda name, shape, d: st.enter_context(nc.sbuf_tensor(name, shape, d))
              w_in_bf = sb("w_in_bf", [128, 8, DIN], BF16)
              lnpb = sb("lnpbA", [128, 2, D], F32)
              S.dma("sp", lambda e: e.dma_start(out=lnpb[:], in_=lnp[:, 0:2, :]), writes=["lnpb"])
              w_out_bf = sb("w_out_bf", [128, 8, D], BF16)
              wr_f = sb("wr_f", [128, 8, NE], F32)
              brb = sb("brb", [128, NE], F32)
              mub = sb("mub", [128, 1696], F32)
              rwcb = sb("rwcb", [128, 7, 512], F32)
              lor = sb("lor", [96, 3, 512], F32)
              sinkb = sb("sinkb", [128, 8], F32)
              esink = sb("esink", [128, 8], F32)
              invf = sb("invf", [128, 8], F32)
              cosT = sb("cosT", [128, NT, 8], F32)
              sinT = sb("sinT", [128, NT, 8], F32)
              stp = contextlib.ExitStack()
              sbp = lambda name, shape, d: stp.enter_context(nc.sbuf_tensor(name, shape, d))
              posi = sbp("posi", [128, NT], I32)
              posf = sbp("posf", [128, NT], F32)
              ang = sbp("ang", [128, NT, 8], F32)
              scB = sbp("scB", [128, 8, 128], F32)
              wada = [sbp("wada%d" % j, [128, 8, 512], F32) for j in range(2)]
              w_in_v = w_in.rearrange("(k p) n -> p k n", p=128)
              for (c0, c1) in ((0, 1232), (1232, 2464)):
                  S.dma("pool", lambda e, c0=c0, c1=c1: e.dma_start(out=w_in_bf[:, :, c0:c1], in_=w_in_v[:, :, c0:c1]), writes=["w_in_bf"])
              S.dma("pool", lambda e: e.dma_start(out=w_out_bf[:], in_=w_out.rearrange("(k p) n -> p k n", p=128)), writes=["w_out_bf"])
              S.dma("sp", lambda e: e.dma_start(out=wr_f[:], in_=w_router.rearrange("(k p) n -> p k n", p=128)), writes=["wr_f"])
              S.dma("sp", lambda e: e.dma_start(out=brb[:], in_=b_router_b[:, :]), writes=["brb"])
              S.dma("sp", lambda e: e.dma_start(out=mub[:], in_=mu_b[:, :]), writes=["mub"])
              S.dma("sp", lambda e: e.dma_start(out=rwcb[:], in_=rwc_b[:, :, :]), writes=["rwcb"])
              S.dma("sp", lambda e: e.dma_start(out=lor[:], in_=lora[:, :, :]), writes=["lor"])
              S.dma("sp", lambda e: e.dma_start(out=sinkb[:], in_=sinks_b[:, :]), writes=["sinkb"])
              S.dma("sp", lambda e: e.dma_start(out=invf[:], in_=invf_b[:, :]), writes=["invf"])
              S.dma("sp", lambda e: e.dma_start(out=posi[:], in_=posT[:, :]), writes=["posi"])
              S.dma("sp", lambda e: e.dma_start(out=scB[:], in_=cB[:, :, :]), writes=["scB"])
              S.act(lambda e: e.activation(out=esink[:], in_=sinkb[:], func=AF.Exp), reads=["sinkb"], writes=["esink"])
              S.act(lambda e: e.activation(out=scB[:], in_=scB[:], func=AF.Silu), reads=["scB"], writes=["scB"])
              w_ada_v = w_ada.rearrange("(k p) n -> p k n", p=128)
              for j in range(12):
                  wb_ = wada[j % 2]
                  S.dma("sp", lambda e, j=j, wb_=wb_: e.dma_start(out=wb_[:], in_=w_ada_v[:, :, j * 512:(j + 1) * 512]), writes=[("wada", j % 2)])
                  pp = pA if j % 2 == 0 else pB
                  for k in range(8):
                      S.pe(lambda e, k=k, wb_=wb_, pp=pp: e.matmul(pp[:], lhsT=scB[:, k, :], rhs=wb_[:, k, :], start=(k == 0), stop=(k == 7)),
                           reads=["scB", ("wada", j % 2)], writes=[pp.name])
                  mflat = modb[:].rearrange("p a b -> p (a b)")
                  S.dve(lambda e, j=j, pp=pp, mflat=mflat: e.tensor_tensor(out=mflat[:, j * 512:(j + 1) * 512], in0=pp[:], in1=mflat[:, j * 512:(j + 1) * 512], op=ALU.add),
                        reads=[pp.name, "modb"], writes=["modb"])
              for a in (1, 2, 4, 5):
                  S.dve(lambda e, a=a: e.tensor_scalar(out=modb[:, a, :], in0=modb[:, a, :], scalar1=1.0, scalar2=None, op0=ALU.add), reads=["modb"], writes=["modb"])
              S.dve(lambda e: e.tensor_copy(out=posf[:], in_=posi[:]), reads=["posi"], writes=["posf"])
              S.dve(lambda e: e.tensor_tensor(out=ang[:], in0=posf[:].unsqueeze(2).broadcast_to([128, NT, 8]), in1=invf[:].unsqueeze(1).broadcast_to([128, NT, 8]), op=ALU.mult),
                    reads=["posf", "invf"], writes=["ang"])
              angi = sbp("angi", [128, NT, 8], I32)
              angf = sbp("angf", [128, NT, 8], F32)
              SC = float(TWO_PI * (1.0 - 1e-6))
              for (dst, key, off) in ((sinT, "sinT", 0.0), (cosT, "cosT", 0.25)):
                  S.dve(lambda e, dst=dst, off=off: e.tensor_scalar(out=dst[:], in0=ang[:], scalar1=float(1.0 / TWO_PI), scalar2=off, op0=ALU.mult, op1=ALU.add), reads=["ang"], writes=[key])
                  S.dve(lambda e, dst=dst: e.tensor_copy(out=angi[:], in_=dst[:]), reads=[key], writes=["angi"])
                  S.dve(lambda e: e.tensor_copy(out=angf[:], in_=angi[:]), reads=["angi"], writes=["angf"])
                  S.dve(lambda e, dst=dst: e.tensor_tensor(out=dst[:], in0=dst[:], in1=angf[:], op=ALU.subtract), reads=[key, "angf"], writes=[key])
                  S.dve(lambda e, dst=dst: e.tensor_scalar(out=angf[:], in0=dst[:], scalar1=0.5, scalar2=None, op0=ALU.is_gt), reads=[key], writes=["angf"])
                  S.dve(lambda e, dst=dst: e.tensor_tensor(out=dst[:], in0=dst[:], in1=angf[:], op=ALU.subtract), reads=[key, "angf"], writes=[key])
                  S.act(lambda e, dst=dst: e.activation(out=dst[:], in_=dst[:], func=AF.Sin, scale=SC), reads=[key], writes=[key])

              zrow = sbp("zrow", [1, 1696], F32)
              S.pool(lambda e: e.memset(zrow[:], 0.0), writes=["zrow"])
              S.dma("sp", lambda e: e.dma_start(out=lastrow[0:1, :], in_=zrow[:]), reads=["zrow"], writes=["lastrow0"])
              S.dve(lambda e: e.memset(junk[:, 4:5], 0.0), reads=["cosT", "sinT", "modb"], writes=["junk"])
              S.add("dve", lambda e: e.memset(junk[:, 5:6], 0.0), reads=[], writes=["junk"], barrier=True)
              stp.close()
              cut("P", modb[:, 1, :], "modb")
              xt = sb("xt", [128, D], F32)
              g4t = sb("g4t", [128, 416], F32)
              t1k = sb("t1k", [128, D], F32)
              hb = sb("hb", [128, D], BF16)
              hT = sb("hT", [128, 8, 128], BF16)
              st6 = sb("st6", [128, 2, 6], F32)
              mv = sb("mv", [128, 2], F32)
              rstd = sb("rstd", [128, 1], F32)
              qk = sb("qk", [128, 10, 64], F32)
              qkb = sb("qkb", [128, 10, 64], BF16)
              rt = sb("rt", [128, 4, 10, 8], F32)
              qT = sb("qT", [64, 8, 128], BF16)
              kT = [sb("kT%d" % j, [64, 2, 128], BF16) for j in range(2)]
              V1 = [sb("V1%d" % j, [128, 2, 66], BF16) for j in range(2)]
              rwc = sb("rwc", [128, 1696], F32)
              rwp = sb("rwp", [128, 1696], F32)
              Ee = sb("Ee", [128, 512], F32)
              PTp = sb("PTp", [128, 4, 128], BF16)
              PTc = sb("PTc", [128, 4, 128], BF16)
              den = sb("den", [128, 8], F32)
              mixb = sb("mixb", [128, D], BF16)
              lo_in = sb("lo_in", [128, 160], F32)
              loT = sb("loT", [96, 3, 128], F32)
              f1 = rwc[:, 0:512]
              f2 = rwc[:, 512:1024]
              f3 = rwc[:, 1024:1536]
              f4 = sb("f4", [128, 512], F32)
              eP = sb("eP", [128, 512], F32)
              eN = sb("eN", [128, 512], F32)
              lw = sb("lw", [128, 512], F32)
              av = sb("av", [128, 512], F32)
              kmod = sb("kmod", [128, 512], F32)
              s8 = sb("s8", [128, 4, 8], F32)
              At = sb("At", [128, 512], BF16)
              Bt = sb("Bt", [128, 512], BF16)
              Kt = sb("Kt", [128, 512], BF16)
              Rt = sb("Rt", [128, 512], BF16)
              Vb = sb("Vb", [128, 512], BF16)
              arT = sb("arT", [64, 8, 2, 128], BF16)
              bkT = sb("bkT", [64, 8, 2, 128], BF16)
              dC = sb("dC", [64, 8], F32)
              Tst = [sb("Tst%d" % j, [64, 8, 64], BF16) for j in range(2)]
              XA = sb("XA", [128, 2, 256], BF16)
              XN = [sb("XN%d" % j, [128, 256], BF16) for j in range(2)]
              Lp = [sb("Lp%d" % j, [128, 128], BF16) for j in range(2)]
              RHSs = sb("RHSs", [128, 64], BF16)
              Us = sb("Us", [128, 64], BF16)
              msk2 = sb("msk2", [128, 256], F32)
              ones_f = sb("ones_f", [128, 1], F32)
              h2T = xt[:].rearrange("p (a b) -> p a b", a=8)
              lg = sb("lg", [128, NE], F32)
              t8 = sb("t8", [128, 8], F32)
              eqk = sb("eqk", [128, NE], F32)
              posn = sb("posn", [128, NE], F32)
              tot = sb("tot", [128, NE], F32)
              mskb = sb("mskb", [128, NE], BF16)
              sl4 = sb("sl4", [128, 4], F32)
              ebase = sb("ebase", [128, NE], F32)
              trash = sb("trash", [128, 1], F32)
              ev4 = sb("ev4", [128, 4], F32)
              onesb = sb("onesb", [128, 128], BF16)

              MI = cst[:, 1, :]
              MS_ = cst[:, 2, :]
              MST = cst[:, 3, :]
              MP = cst[:, 4, :]
              for j in range(2):
                  S.pool(lambda e, j=j: e.memset(V1[j][:], 1.0), writes=[("V1", j)])
                  S.pool(lambda e, j=j: e.memset(Tst[j][:], 0.0), writes=[("Tst", j)])
              S.pool(lambda e: e.memset(ones_f[:], 1.0), writes=["ones_f"])
              S.pool(lambda e: e.memset(onesb[:], 1.0), writes=["onesb"])
              S.pool(lambda e: e.memset(tot[:], 0.0), writes=["tot"])
              S.pool(lambda e: e.iota(ebase[:], pattern=[[CAP, NE]], base=0, channel_multiplier=0, allow_small_or_imprecise_dtypes=True), writes=["ebase"])
              S.pool(lambda e: e.iota(trash[:], pattern=[[0, 1]], base=NSLOT, channel_multiplier=1, allow_small_or_imprecise_dtypes=True), writes=["trash"])
              S.dve(lambda e: e.tensor_copy(out=msk2[:, 0:128], in_=MS_), reads=["cst"], writes=["msk2"])
              S.dve(lambda e: e.tensor_copy(out=msk2[:, 128:256], in_=MI), reads=["cst"], writes=["msk2"])

              def layer_norm_stats(src, key):
                  for h in range(2):
                      S.dve(lambda e, h=h: e.bn_stats(out=st6[:, h, :], in_=src[:, h * 512:(h + 1) * 512]), reads=[key], writes=["st6"])
                  S.dve(lambda e: e.bn_aggr(out=mv[:], in_=st6[:].rearrange("p a b -> p (a b)")), reads=["st6"], writes=["mv"])
                  S.dve(lambda e: e.tensor_scalar(out=rstd[:], in0=mv[:, 1:2], scalar1=LN_EPS, scalar2=None, op0=ALU.add), reads=["mv"], writes=["rstd"])
                  S.act(lambda e: e.activation(out=rstd[:], in_=rstd[:], func=AF.Sqrt), reads=["rstd"], writes=["rstd"])
                  S.dve(lambda e: e.reciprocal(out=rstd[:], in_=rstd[:]), reads=["rstd"], writes=["rstd"])

              for i in range(ntiles):
                  cur, prv = i % 2, (i + 1) % 2
                  cut_tile[0] = i
                  rows = slice(i * 128, (i + 1) * 128)
                  S.dma("sp", lambda e, rows=rows: e.dma_start(out=xt[:], in_=x[rows, :]), writes=["xt"])
                  layer_norm_stats(xt, "xt")
                  S.dve(lambda e: e.tensor_scalar(out=t1k[:], in0=xt[:], scalar1=mv[:, 0:1], scalar2=rstd[:], op0=ALU.subtract, op1=ALU.mult), reads=["xt", "mv", "rstd"], writes=["t1k"])
                  S.dve(lambda e: e.tensor_tensor(out=t1k[:], in0=t1k[:], in1=modb[:, 1, :], op=ALU.mult), reads=["t1k", "modb"], writes=["t1k"])
                  S.dve(lambda e: e.tensor_tensor(out=hb[:], in0=t1k[:], in1=modb[:, 0, :], op=ALU.add), reads=["t1k", "modb"], writes=["hb"])
                  cut("A_h", t1k[:], "t1k")
                  for k in range(8):
                      S.pe(lambda e, k=k: e.transpose(out=pTb[:, k * 128:(k + 1) * 128], in_=hb[:, k * 128:(k + 1) * 128], identity=ident_b), reads=["hb", "cstb"], writes=["pTb"])
                  S.act(lambda e: e.copy(out=hT[:].rearrange("p a b -> p (a b)"), in_=pTb[:]), reads=["pTb"], writes=["hT"])
                  cut("A_hT", t1k[:], "hT")
                  groups = [(0, 512), (512, 1024), (1024, 1536), (1536, 2048), (2048, 2464)]
                  for g, (c0, c1) in enumerate(groups):
                      pp = pA if g % 2 == 0 else pB
                      n = c1 - c0
                      for k in range(8):
                          S.pe(lambda e, k=k, pp=pp, c0=c0, c1=c1, n=n: e.matmul(pp[:, 0:n], lhsT=hT[:, k, :], rhs=w_in_bf[:, k, c0:c1], start=(k == 0), stop=(k == 7)),
                               reads=["hT", "w_in_bf"], writes=[pp.name])
                      if g == 0:
                          S.act(lambda e, pp=pp: e.copy(out=qk[:, 0:8, :].rearrange("p a b -> p (a b)"), in_=pp[:]), reads=[pp.name], writes=["qk"])
                          cut("A_g0", qk[:, 0:8, :].rearrange("p a b -> p (a b)"), "qk")
                      elif g < 4:
                          S.act(lambda e, pp=pp, g=g: e.copy(out=rwc[:, (g - 1) * 512:g * 512], in_=pp[:]), reads=[pp.name], writes=["rwc"])
                      else:
                          S.act(lambda e, pp=pp: e.copy(out=g4t[:], in_=pp[:, 0:416]), reads=[pp.name], writes=["g4t"])
                          S.act(lambda e: e.copy(out=rwc[:, 1536:1696], in_=g4t[:, 0:160]), reads=["g4t"], writes=["rwc"])
                          S.act(lambda e: e.copy(out=qk[:, 8:10, :].rearrange("p a b -> p (a b)"), in_=g4t[:, 160:288]), reads=["g4t"], writes=["qk"])
                          S.act(lambda e, cur=cur: e.copy(out=V1[cur][:, 0, 0:64], in_=g4t[:, 288:352]), reads=["g4t"], writes=[("V1", cur)])
                          S.act(lambda e, cur=cur: e.copy(out=V1[cur][:, 1, 0:64], in_=g4t[:, 352:416]), reads=["g4t"], writes=[("V1", cur)])
                      if g >= 1:
                          cut("A_g%d" % g, rwc[:, 0:1024], "rwc")
                  cut("A_proj", rwc[:], "rwc")
                  S.dma("sp", lambda e: e.dma_start(out=rwp[1:128, :], in_=rwc[0:127, :]), reads=["rwc"], writes=["rwp"])
                  S.dma("sp", lambda e, cur=cur: e.dma_start(out=rwp[0:1, :], in_=lastrow[cur:cur + 1, :]), reads=["lastrow%d" % cur], writes=["rwp"])
                  S.dma("sp", lambda e, prv=prv: e.dma_start(out=lastrow[prv:prv + 1, :], in_=rwc[127:128, :]), reads=["rwc"], writes=["lastrow%d" % prv])
                  S.dve(lambda e: e.tensor_tensor(out=rwp[:], in0=rwp[:], in1=rwc[:], op=ALU.subtract), reads=["rwp", "rwc"], writes=["rwp"])
                  S.dve(lambda e: e.tensor_tensor(out=rwp[:], in0=rwp[:], in1=mub[:], op=ALU.mult), reads=["rwp", "mub"], writes=["rwp"])
                  S.dve(lambda e: e.tensor_tensor(out=rwp[:], in0=rwp[:], in1=rwc[:], op=ALU.add), reads=["rwp", "rwc"], writes=["rwp"])
                  cut("A_mix", rwp[:], "rwp")
                  cb = cosT[:, i, :].unsqueeze(1).broadcast_to([128, 10, 8])
                  sbn = sinT[:, i, :].unsqueeze(1).broadcast_to([128, 10, 8])
                  a1, a2 = qk[:, :, 0:8], qk[:, :, 8:16]
                  S.dve(lambda e, cb=cb: e.tensor_tensor(out=rt[:, 0], in0=a1, in1=cb, op=ALU.mult), reads=["qk", "cosT"], writes=["rt0"])
                  S.dve(lambda e, sbn=sbn: e.tensor_tensor(out=rt[:, 1], in0=a2, in1=sbn, op=ALU.mult), reads=["qk", "sinT"], writes=["rt1"])
                  S.dve(lambda e, cb=cb: e.tensor_tensor(out=rt[:, 2], in0=a2, in1=cb, op=ALU.mult), reads=["qk", "cosT"], writes=["rt2"])
                  S.dve(lambda e, sbn=sbn: e.tensor_tensor(out=rt[:, 3], in0=a1, in1=sbn, op=ALU.mult), reads=["qk", "sinT"], writes=["rt3"])
                  S.act(lambda e: e.copy(out=qkb[:, :, 16:64], in_=qk[:, :, 16:64]), reads=["qk"], writes=["qkb"])
                  S.dve(lambda e: e.tensor_tensor(out=qkb[:, :, 0:8], in0=rt[:, 0], in1=rt[:, 1], op=ALU.subtract), reads=["rt0", "rt1"], writes=["qkb"])
                  S.dve(lambda e: e.tensor_tensor(out=qkb[:, :, 8:16], in0=rt[:, 2], in1=rt[:, 3], op=ALU.add), reads=["rt2", "rt3"], writes=["qkb"])
                  for h in range(8):
                      S.pe(lambda e, h=h: e.transpose(out=pTb2[0:64, h * 128:(h + 1) * 128], in_=qkb[:, h, :], identity=ident_b), reads=["qkb", "cstb"], writes=["pTb2"])
                  S.act(lambda e: e.activation(out=qT[:].rearrange("p a b -> p (a b)"), in_=pTb2[0:64, :], func=AF.Copy, scale=0.125), reads=["pTb2"], writes=["qT"])
                  for h in range(2):
                      S.pe(lambda e, h=h: e.transpose(out=pTb[0:64, h * 128:(h + 1) * 128], in_=qkb[:, 8 + h, :], identity=ident_b), reads=["qkb", "cstb"], writes=["pTb"])
                  S.act(lambda e, cur=cur: e.copy(out=kT[cur][:].rearrange("p a b -> p (a b)"), in_=pTb[0:64, 0:256]), reads=["pTb"], writes=[("kT", cur)])
                  for g in range(2):
                      rq = qT[:, 4 * g:4 * g + 4, :]
                      if i > 0:
                          S.pe(lambda e, g=g, rq=rq, prv=prv: e.matmul(pC[:], lhsT=kT[prv][:, g, :], rhs=rq, start=True, stop=True), reads=[("kT", prv), "qT"], writes=["pC"])
                          S.act(lambda e: e.activation(out=Ee[:], in_=pC[:], func=AF.Exp), reads=["pC"], writes=["Ee"])
                          S.dve(lambda e: e.tensor_tensor(out=PTp[:], in0=Ee[:].rearrange("p (a b) -> p a b", a=4), in1=MP.unsqueeze(1).broadcast_to([128, 4, 128]), op=ALU.mult), reads=["Ee", "cst"], writes=["PTp"])
                      S.pe(lambda e, g=g, rq=rq, cur=cur: e.matmul(pD[:], lhsT=kT[cur][:, g, :], rhs=rq, start=True, stop=True), reads=[("kT", cur), "qT"], writes=["pD"])
                      S.act(lambda e: e.activation(out=Ee[:], in_=pD[:], func=AF.Exp), reads=["pD"], writes=["Ee"])
                      S.dve(lambda e: e.tensor_tensor(out=PTc[:], in0=Ee[:].rearrange("p (a b) -> p a b", a=4), in1=MI.unsqueeze(1).broadcast_to([128, 4, 128]), op=ALU.mult), reads=["Ee", "cst"], writes=["PTc"])
                      pO = pE[:, 0:264].rearrange("p (a b) -> p a b", a=4)
                      for h in range(4):
                          if i > 0:
                              S.pe(lambda e, h=h, g=g, prv=prv, pO=pO: e.matmul(pO[:, h, :], lhsT=PTp[:, h, :], rhs=V1[prv][:, g, :], start=True, stop=False), reads=["PTp", ("V1", prv)], writes=["pE"])
                          S.pe(lambda e, h=h, g=g, cur=cur, pO=pO, i=i: e.matmul(pO[:, h, :], lhsT=PTc[:, h, :], rhs=V1[cur][:, g, :], start=(i == 0), stop=True), reads=["PTc", ("V1", cur)], writes=["pE"])
                      S.dve(lambda e, g=g, pO=pO: e.tensor_tensor(out=den[:, 4 * g:4 * g + 4], in0=pO[:, :, 64], in1=esink[:, 4 * g:4 * g + 4], op=ALU.add), reads=["pE", "esink"], writes=["den"])
                      S.dve(lambda e, g=g: e.reciprocal(out=den[:, 4 * g:4 * g + 4], in_=den[:, 4 * g:4 * g + 4]), reads=["den"], writes=["den"])
                      S.dve(lambda e, g=g, pO=pO: e.tensor_tensor(out=mixb[:, 256 * g:256 * g + 256].rearrange("p (a b) -> p a b", a=4), in0=pO[:, :, 0:64],
                                                                   in1=den[:, 4 * g:4 * g + 4].unsqueeze(2).broadcast_to([128, 4, 64]), op=ALU.mult), reads=["pE", "den"], writes=["mixb"])
                  cut("A_attn", mixb[:, 0:512], "mixb")
                  r_ = rwp[:, 0:512]
                  k_ = rwp[:, 512:1024]
                  v_ = rwp[:, 1024:1536]
                  W0, A0, KK, KA, RK, LNW, LNB = [rwcb[:, j, :] for j in range(7)]
                  S.act(lambda e: e.activation(out=lo_in[:, 0:32], in_=rwp[:, 1536:1568], func=AF.Tanh), reads=["rwp"], writes=["lo_in"])
                  S.act(lambda e: e.activation(out=lo_in[:, 64:160], in_=rwp[:, 1600:1696], func=AF.Sigmoid), reads=["rwp"], writes=["lo_in"])
                  S.dve(lambda e: e.tensor_copy(out=lo_in[:, 32:64], in_=rwp[:, 1568:1600]), reads=["rwp"], writes=["lo_in"])
                  for j, (c0, n) in enumerate(((0, 32), (32, 32), (64, 96))):
                      S.pe(lambda e, j=j, c0=c0, n=n: e.transpose(out=pC[0:n, j * 128:(j + 1) * 128], in_=lo_in[:, c0:c0 + n], identity=ident_f), reads=["lo_in", "cst"], writes=["pC"])
                      S.act(lambda e, j=j, n=n: e.copy(out=loT[0:n, j, :], in_=pC[0:n, j * 128:(j + 1) * 128]), reads=["pC"], writes=["loT"])
                  for j, (pp, n) in enumerate(((pA, 32), (pB, 32))):
                      S.pe(lambda e, j=j, pp=pp, n=n: e.matmul(pp[:], lhsT=loT[0:n, j, :], rhs=lor[0:n, j, :], start=True, stop=True), reads=["loT", "lor"], writes=[pp.name])
                  S.dve(lambda e: e.tensor_tensor(out=f1[:], in0=pA[:], in1=W0, op=ALU.add), reads=["pA", "rwcb"], writes=["rwc"])
                  S.act(lambda e: e.activation(out=lw[:], in_=f1[:], func=AF.Sigmoid), reads=["rwc"], writes=["lw"])
                  S.dve(lambda e: e.tensor_scalar(out=lw[:], in0=lw[:], scalar1=-float(np.exp(-0.5)), scalar2=None, op0=ALU.mult), reads=["lw"], writes=["lw"])
                  S.dve(lambda e: e.tensor_tensor(out=f2[:], in0=pB[:], in1=A0, op=ALU.add), reads=["pB", "rwcb"], writes=["rwc"])
                  S.act(lambda e: e.activation(out=av[:], in_=f2[:], func=AF.Sigmoid), reads=["rwc"], writes=["av"])
                  S.pe(lambda e: e.matmul(pA[:], lhsT=MI, rhs=lw[:], start=True, stop=True), reads=["cst", "lw"], writes=["pA"])
                  for h in range(8):
                      S.pe(lambda e, h=h: e.matmul(pB[0:64, h:h + 1], lhsT=lw[:, h * 64:(h + 1) * 64], rhs=ones_f[:], start=True, stop=True), reads=["lw", "ones_f"], writes=["pB"])
                  S.act(lambda e: e.activation(out=dC[:], in_=pB[0:64, 0:8], func=AF.Exp), reads=["pB"], writes=["dC"])
                  S.act(lambda e: e.activation(out=eP[:], in_=pA[:], func=AF.Exp), reads=["pA"], writes=["eP"])
                  S.act(lambda e: e.activation(out=eN[:], in_=pA[:], func=AF.Exp, scale=-1.0), reads=["pA"], writes=["eN"])
                  S.dve(lambda e: e.tensor_tensor(out=f1[:], in0=pA[:], in1=lw[:], op=ALU.subtract), reads=["pA", "lw"], writes=["rwc"])
                  S.act(lambda e: e.activation(out=lw[:], in_=f1[:], func=AF.Exp), reads=["rwc"], writes=["lw"])
                  v3 = lambda ap: ap.rearrange("p (a b) -> p a b", a=8)
                  S.dve(lambda e: e.tensor_tensor(out=f2[:], in0=k_, in1=KK, op=ALU.mult), reads=["rwp", "rwcb"], writes=["rwc"])
                  S.dve(lambda e: e.tensor_tensor(out=f3[:], in0=f2[:], in1=f2[:], op=ALU.mult), reads=["rwc"], writes=["rwc"])
                  S.dve(lambda e: e.tensor_reduce(out=s8[:, 0, :], in_=v3(f3[:]), axis=AX.X, op=ALU.add), reads=["rwc"], writes=["s80"])
                  S.act(lambda e: e.activation(out=s8[:, 0, :], in_=s8[:, 0, :], func=AF.Sqrt), reads=["s80"], writes=["s80"])
                  S.dve(lambda e: e.tensor_scalar(out=s8[:, 0, :], in0=s8[:, 0, :], scalar1=1e-12, scalar2=None, op0=ALU.max), reads=["s80"], writes=["s80"])
                  S.dve(lambda e: e.reciprocal(out=s8[:, 0, :], in_=s8[:, 0, :]), reads=["s80"], writes=["s80"])
                  S.dve(lambda e: e.tensor_tensor(out=v3(f2[:]), in0=v3(f2[:]), in1=s8[:, 0, :].unsqueeze(2).broadcast_to([128, 8, 64]), op=ALU.mult), reads=["rwc", "s80"], writes=["rwc"])
                  S.dve(lambda e: e.scalar_tensor_tensor(out=f3[:], in0=av[:], scalar=-1.0, in1=KA, op0=ALU.add, op1=ALU.mult), reads=["av", "rwcb"], writes=["rwc"])
                  S.dve(lambda e: e.scalar_tensor_tensor(out=kmod[:], in0=f3[:], scalar=1.0, in1=k_, op0=ALU.add, op1=ALU.mult), reads=["rwc", "rwp"], writes=["kmod"])
                  S.dve(lambda e: e.scalar_tensor_tensor(out=At[:], in0=f2[:], scalar=-1.0, in1=lw[:], op0=ALU.mult, op1=ALU.mult), reads=["rwc", "lw"], writes=["At"])
                  S.dve(lambda e: e.tensor_tensor(out=f4[:], in0=f2[:], in1=av[:], op=ALU.mult), reads=["rwc", "av"], writes=["f4"])
                  S.dve(lambda e: e.tensor_tensor(out=Bt[:], in0=f4[:], in1=eN[:], op=ALU.mult), reads=["f4", "eN"], writes=["Bt"])
                  S.dve(lambda e: e.tensor_tensor(out=Kt[:], in0=kmod[:], in1=eN[:], op=ALU.mult), reads=["kmod", "eN"], writes=["Kt"])
                  S.dve(lambda e: e.tensor_tensor(out=Rt[:], in0=r_, in1=eP[:], op=ALU.mult), reads=["rwp", "eP"], writes=["Rt"])
                  S.act(lambda e: e.copy(out=Vb[:], in_=v_), reads=["rwp"], writes=["Vb"])
                  cut("A_prep", kmod[:], "Vb")
                  for (src, skey, dst, dkey, j, pp) in ((At, "At", arT, "arT", 0, pTb), (Rt, "Rt", arT, "arT", 1, pTb2), (Bt, "Bt", bkT, "bkT", 0, pTb), (Kt, "Kt", bkT, "bkT", 1, pTb2)):
                      for h in range(8):
                          S.pe(lambda e, h=h, src=src, pp=pp: e.transpose(out=pp[0:64, h * 128:(h + 1) * 128], in_=src[:, h * 64:(h + 1) * 64], identity=ident_b), reads=[skey, "cstb"], writes=[pp.name])
                      S.act(lambda e, dst=dst, j=j, pp=pp: e.copy(out=dst[:, :, j, :], in_=pp[0:64, :].rearrange("p (a b) -> p a b", a=8)), reads=[pp.name], writes=[dkey])
                  cut("A_tr", kmod[:], "bkT")
                  Told, Tnew = Tst[cur], Tst[prv]
                  for h in range(8):
                      arh = arT[:, h].rearrange("p a b -> p (a b)")
                      S.pe(lambda e, h=h, arh=arh: e.matmul(pC[:, 0:256], lhsT=bkT[:, h, 0, :], rhs=arh, start=True, stop=True), reads=["bkT", "arT"], writes=["pC"])
                      S.pe(lambda e, h=h, arh=arh: e.matmul(pC[:, 256:512], lhsT=bkT[:, h, 1, :], rhs=arh, start=True, stop=True), reads=["bkT", "arT"], writes=["pC"])
                      S.pe(lambda e, h=h: e.matmul(pD[:, 0:128], lhsT=arT[:, h, 0, :], rhs=bkT[:, h, 0, :], start=True, stop=True), reads=["bkT", "arT"], writes=["pD"])
                      S.dve(lambda e: e.tensor_tensor(out=XA[:], in0=pC[:].rearrange("p (a b) -> p a b", a=2), in1=msk2[:].unsqueeze(1).broadcast_to([128, 2, 256]), op=ALU.mult), reads=["pC", "msk2"], writes=["XA"])
                      S.dve(lambda e: e.tensor_tensor(out=Lp[0][:], in0=pD[:, 0:128], in1=MST, op=ALU.mult), reads=["pD", "cst"], writes=[("Lp", 0)])
                      S.add("dve", lambda e: e.tensor_copy(out=XN[0][:, 0:128], in_=XA[:, 0, 0:128]), reads=["XA"], writes=[("XN", 0)])
                      S.add("dve", lambda e: e.tensor_tensor(out=XN[0][:, 128:256], in0=XA[:, 0, 0:128], in1=ident_b, op=ALU.add), reads=["XA", "cstb"], writes=[("XN", 0)])
                      cut("H%d_a" % h, kmod[:], ("XN", 0))
                      S.pe(lambda e: e.matmul(pD[:, 0:128], lhsT=Lp[0][:], rhs=XN[0][:, 0:128], start=True, stop=True), reads=[("Lp", 0), ("XN", 0)], writes=["pD"])
                      S.pe(lambda e: e.matmul(pD[:, 256:384], lhsT=XN[0][:, 0:128], rhs=Lp[0][:], start=True, stop=True), reads=[("Lp", 0), ("XN", 0)], writes=["pD"])
                      S.add("act", lambda e: e.copy(out=XN[1][:, 0:128], in_=pD[:, 0:128]), reads=["pD"], writes=[("XN", 1)])
                      S.add("act", lambda e: e.copy(out=Lp[1][:], in_=pD[:, 256:384]), reads=["pD"], writes=[("Lp", 1)])
                      S.add("dve", lambda e: e.tensor_copy(out=XN[1][:, 128:256], in_=XN[0][:, 128:256]), reads=[("XN", 0)], writes=[("XN", 1)])
                      c = 1
                      for lvl in range(1, 7):
                          n = 1 - c
                          last = lvl == 6
                          if not last:
                              S.pe(lambda e, c=c: e.matmul(pD[:, 0:256], lhsT=Lp[c][:], rhs=XN[c][:], start=True, stop=True), reads=[("Lp", c), ("XN", c)], writes=["pD"])
                              S.pe(lambda e, c=c: e.matmul(pD[:, 256:384], lhsT=XN[c][:, 0:128], rhs=Lp[c][:], start=True, stop=True), reads=[("Lp", c), ("XN", c)], writes=["pD"])
                              S.add("act", lambda e, n=n: e.copy(out=XN[n][:, 0:128], in_=pD[:, 0:128]), reads=["pD"], writes=[("XN", n)])
                              S.add("act", lambda e, n=n: e.copy(out=Lp[n][:], in_=pD[:, 256:384]), reads=["pD"], writes=[("Lp", n)])
                          else:
                              S.pe(lambda e, c=c: e.matmul(pD[:, 128:256], lhsT=Lp[c][:], rhs=XN[c][:, 128:256], start=True, stop=True), reads=[("Lp", c), ("XN", c)], writes=["pD"])
                          S.dve(lambda e, c=c, n=n: e.tensor_tensor(out=XN[n][:, 128:256], in0=pD[:, 128:256], in1=XN[c][:, 128:256], op=ALU.add), reads=["pD", ("XN", c)], writes=[("XN", n)])
                          c = n
                      cut("H%d_b" % h, kmod[:], ("XN", c))
                      Nf = XN[c][:, 128:256]
                      S.pe(lambda e, h=h, Told=Told: e.matmul(pE[:, 300:364], lhsT=arT[:, h, 0, :], rhs=Told[:, h, :], start=True, stop=False), reads=["arT", ("Tst", cur)], writes=["pE"])
                      S.pe(lambda e, h=h: e.matmul(pE[:, 300:364], lhsT=XA[:, 1, 0:128], rhs=Vb[:, h * 64:(h + 1) * 64], start=False, stop=True), reads=["XA", "Vb"], writes=["pE"])
                      S.act(lambda e: e.copy(out=RHSs[:], in_=pE[:, 300:364]), reads=["pE"], writes=["RHSs"])
                      S.pe(lambda e, Nf=Nf: e.matmul(pE[:, 364:428], lhsT=Nf, rhs=RHSs[:], start=True, stop=True), reads=[("XN", c), "RHSs"], writes=["pE"])
                      S.act(lambda e: e.copy(out=Us[:], in_=pE[:, 364:428]), reads=["pE"], writes=["Us"])
                      cut("H%d_c" % h, kmod[:], "Us")
                      S.pe(lambda e, h=h, Told=Told: e.matmul(pF[:, h * 64:(h + 1) * 64], lhsT=arT[:, h, 1, :], rhs=Told[:, h, :], start=True, stop=False), reads=["arT", ("Tst", cur)], writes=["pF"])
                      S.pe(lambda e, h=h: e.matmul(pF[:, h * 64:(h + 1) * 64], lhsT=XA[:, 0, 128:256], rhs=Us[:], start=False, stop=False), reads=["XA", "Us"], writes=["pF"])
                      S.pe(lambda e, h=h: e.matmul(pF[:, h * 64:(h + 1) * 64], lhsT=XA[:, 1, 128:256], rhs=Vb[:, h * 64:(h + 1) * 64], start=False, stop=True), reads=["XA", "Vb"], writes=["pF"])
                      S.pe(lambda e, h=h, Told=Told: e.matmul(pE[0:64, 428:492], lhsT=ident_b[0:64, 0:64], rhs=Told[:, h, :], start=True, stop=False), reads=["cstb", ("Tst", cur)], writes=["pE"])
                      S.pe(lambda e, h=h: e.matmul(pE[0:64, 428:492], lhsT=Bt[:, h * 64:(h + 1) * 64], rhs=Us[:], start=False, stop=False), reads=["Bt", "Us"], writes=["pE"])
                      S.pe(lambda e, h=h: e.matmul(pE[0:64, 428:492], lhsT=Kt[:, h * 64:(h + 1) * 64], rhs=Vb[:, h * 64:(h + 1) * 64], start=False, stop=True), reads=["Kt", "Vb"], writes=["pE"])
                      S.act(lambda e, h=h, Tnew=Tnew: e.activation(out=Tnew[:, h, :], in_=pE[0:64, 428:492], func=AF.Copy, scale=dC[:, h:h + 1]), reads=["pE", "dC"], writes=[("Tst", prv)])
                      cut("A_hd%d" % h, kmod[:], ("Tst", prv))
                  cut("A_heads", kmod[:], "pF")
                  S.act(lambda e: e.copy(out=f4[:], in_=pF[:]), reads=["pF"], writes=["f4"])
                  S.dve(lambda e: e.tensor_reduce(out=s8[:, 1, :], in_=v3(f4[:]), axis=AX.X, op=ALU.add), reads=["f4"], writes=["s81"])
                  S.dve(lambda e: e.tensor_tensor(out=f3[:], in0=f4[:], in1=f4[:], op=ALU.mult), reads=["f4"], writes=["rwc"])
                  S.dve(lambda e: e.tensor_reduce(out=s8[:, 2, :], in_=v3(f3[:]), axis=AX.X, op=ALU.add), reads=["rwc"], writes=["s82"])
                  S.dve(lambda e: e.tensor_scalar(out=s8[:, 1, :], in0=s8[:, 1, :], scalar1=1.0 / 64, scalar2=None, op0=ALU.mult), reads=["s81"], writes=["s81"])
                  S.dve(lambda e: e.tensor_tensor(out=s8[:, 3, :], in0=s8[:, 1, :], in1=s8[:, 1, :], op=ALU.mult), reads=["s81"], writes=["s83"])
                  S.dve(lambda e: e.scalar_tensor_tensor(out=s8[:, 2, :], in0=s8[:, 2, :], scalar=1.0 / 64, in1=s8[:, 3, :], op0=ALU.mult, op1=ALU.subtract), reads=["s82", "s83"], writes=["s82"])
                  S.dve(lambda e: e.tensor_scalar(out=s8[:, 2, :], in0=s8[:, 2, :], scalar1=GN_EPS, scalar2=None, op0=ALU.add), reads=["s82"], writes=["s82"])
                  S.act(lambda e: e.activation(out=s8[:, 2, :], in_=s8[:, 2, :], func=AF.Sqrt), reads=["s82"], writes=["s82"])
                  S.dve(lambda e: e.reciprocal(out=s8[:, 2, :], in_=s8[:, 2, :]), reads=["s82"], writes=["s82"])
                  S.dve(lambda e: e.tensor_tensor(out=v3(f4[:]), in0=v3(f4[:]), in1=s8[:, 1, :].unsqueeze(2).broadcast_to([128, 8, 64]), op=ALU.subtract), reads=["f4", "s81"], writes=["f4"])
                  S.dve(lambda e: e.tensor_tensor(out=v3(f4[:]), in0=v3(f4[:]), in1=s8[:, 2, :].unsqueeze(2).broadcast_to([128, 8, 64]), op=ALU.mult), reads=["f4", "s82"], writes=["f4"])
                  S.dve(lambda e: e.tensor_tensor(out=f4[:], in0=f4[:], in1=LNW, op=ALU.mult), reads=["f4", "rwcb"], writes=["f4"])
                  S.dve(lambda e: e.tensor_tensor(out=f4[:], in0=f4[:], in1=LNB, op=ALU.add), reads=["f4", "rwcb"], writes=["f4"])
                  S.dve(lambda e: e.tensor_tensor(out=eP[:], in0=r_, in1=kmod[:], op=ALU.mult), reads=["rwp", "kmod"], writes=["eP"])
                  S.dve(lambda e: e.tensor_tensor(out=eP[:], in0=eP[:], in1=RK, op=ALU.mult), reads=["eP", "rwcb"], writes=["eP"])
                  S.dve(lambda e: e.tensor_reduce(out=s8[:, 3, :], in_=v3(eP[:]), axis=AX.X, op=ALU.add), reads=["eP"], writes=["s83"])
                  S.dve(lambda e: e.tensor_tensor(out=v3(eN[:]), in0=v3(v_), in1=s8[:, 3, :].unsqueeze(2).broadcast_to([128, 8, 64]), op=ALU.mult), reads=["rwp", "s83"], writes=["eN"])
                  S.dve(lambda e: e.tensor_tensor(out=f4[:], in0=f4[:], in1=eN[:], op=ALU.add), reads=["f4", "eN"], writes=["f4"])
                  S.pe(lambda e: e.matmul(pA[:], lhsT=loT[0:96, 2, :], rhs=lor[0:96, 2, :], start=True, stop=True), reads=["loT", "lor"], writes=["pA"])
                  S.dve(lambda e: e.tensor_tensor(out=mixb[:, 512:1024], in0=f4[:], in1=pA[:], op=ALU.mult), reads=["f4", "pA"], writes=["mixb"])
                  cut("A_rwkv", mixb[:, 512:1024], "mixb")
                  for k in range(8):
                      S.pe(lambda e, k=k: e.transpose(out=pTb[:, k * 128:(k + 1) * 128], in_=mixb[:, k * 128:(k + 1) * 128], identity=ident_b), reads=["mixb", "cstb"], writes=["pTb"])
                  S.act(lambda e: e.copy(out=hT[:].rearrange("p a b -> p (a b)"), in_=pTb[:]), reads=["pTb"], writes=["hT"])
                  for hf, pp in ((0, pA), (1, pB)):
                      for k in range(8):
                          S.pe(lambda e, k=k, hf=hf, pp=pp: e.matmul(pp[:], lhsT=hT[:, k, :], rhs=w_out_bf[:, k, hf * 512:(hf + 1) * 512], start=(k == 0), stop=(k == 7)), reads=["hT", "w_out_bf"], writes=[pp.name])
                      S.dve(lambda e, hf=hf, pp=pp: e.tensor_tensor(out=t1k[:, hf * 512:(hf + 1) * 512], in0=pp[:], in1=modb[:, 2, hf * 512:(hf + 1) * 512], op=ALU.mult), reads=[pp.name, "modb"], writes=["t1k"])
                  S.dve(lambda e: e.scalar_tensor_tensor(out=t1k[:], in0=xt[:], scalar=float(ALPHA), in1=t1k[:], op0=ALU.mult, op1=ALU.add), reads=["xt", "t1k"], writes=["t1k"])
                  layer_norm_stats(t1k, "t1k")
                  S.dve(lambda e: e.tensor_scalar(out=t1k[:], in0=t1k[:], scalar1=mv[:, 0:1], scalar2=rstd[:], op0=ALU.subtract, op1=ALU.mult), reads=["t1k", "mv", "rstd"], writes=["t1k"])
                  S.dve(lambda e: e.tensor_tensor(out=t1k[:], in0=t1k[:], in1=lnpb[:, 0, :], op=ALU.mult), reads=["t1k", "lnpb"], writes=["t1k"])
                  S.dve(lambda e: e.tensor_tensor(out=t1k[:], in0=t1k[:], in1=lnpb[:, 1, :], op=ALU.add), reads=["t1k", "lnpb"], writes=["t1k"])
                  S.dma("sp", lambda e, rows=rows: e.dma_start(out=x1s[rows, :], in_=t1k[:]), reads=["t1k"], writes=["x1s"])
                  if stage == "A1":
                      S.dma("sp", lambda e, rows=rows: e.dma_start(out=dbg[rows, :], in_=t1k[:]), reads=["t1k"])
                  if _os.environ.get("KSKIP") == "router":
                      continue
                  layer_norm_stats(t1k, "t1k")
                  S.dve(lambda e: e.tensor_scalar(out=t1k[:], in0=t1k[:], scalar1=mv[:, 0:1], scalar2=rstd[:], op0=ALU.subtract, op1=ALU.mult), reads=["t1k", "mv", "rstd"], writes=["t1k"])
                  S.dve(lambda e: e.tensor_tensor(out=t1k[:], in0=t1k[:], in1=modb[:, 4, :], op=ALU.mult), reads=["t1k", "modb"], writes=["t1k"])
                  S.dve(lambda e: e.tensor_tensor(out=t1k[:], in0=t1k[:], in1=modb[:, 3, :], op=ALU.add), reads=["t1k", "modb"], writes=["t1k"])
                  S.act(lambda e: e.copy(out=hb[:], in_=t1k[:]), reads=["t1k"], writes=["hb"])
                  for half in range(2):
                      for k in range(4):
                          kk_ = half * 4 + k
                          S.pe(lambda e, k=k, kk_=kk_: e.transpose(out=pC[:, k * 128:(k + 1) * 128], in_=t1k[:, kk_ * 128:(kk_ + 1) * 128], identity=ident_f), reads=["t1k", "cst"], writes=["pC"])
                      S.act(lambda e, half=half: e.copy(out=h2T[:, half * 4:half * 4 + 4, :].rearrange("p a b -> p (a b)"), in_=pC[:]), reads=["pC"], writes=["xt"])
                  for k in range(8):
                      S.pe(lambda e, k=k: e.matmul(pD[:, 0:NE], lhsT=h2T[:, k, :], rhs=wr_f[:, k, :], start=(k == 0), stop=(k == 7)), reads=["xt", "wr_f"], writes=["pD"])
                  S.dve(lambda e: e.tensor_tensor(out=lg[:], in0=pD[:, 0:NE], in1=brb[:], op=ALU.add), reads=["pD", "brb"], writes=["lg"])
                  S.dve(lambda e: e.max(out=t8[:], in_=lg[:]), reads=["lg"], writes=["t8"])
                  S.dve(lambda e: e.tensor_scalar(out=mskb[:], in0=lg[:], scalar1=t8[:, 3:4], scalar2=None, op0=ALU.is_ge), reads=["lg", "t8"], writes=["mskb"])
                  S.pe(lambda e: e.matmul(pD[:, 64:64 + NE], lhsT=cstb[:, 2, :], rhs=mskb[:], start=True, stop=True), reads=["cstb", "mskb"], writes=["pD"])
                  S.pe(lambda e: e.matmul(pD[:, 128:128 + NE], lhsT=onesb[:], rhs=mskb[:], start=True, stop=True), reads=["onesb", "mskb"], writes=["pD"])
                  S.dve(lambda e: e.tensor_tensor(out=posn[:], in0=pD[:, 64:64 + NE], in1=tot[:], op=ALU.add), reads=["pD", "tot"], writes=["posn"])
                  S.dve(lambda e: e.tensor_tensor(out=tot[:], in0=pD[:, 128:128 + NE], in1=tot[:], op=ALU.add), reads=["pD", "tot"], writes=["tot"])
                  S.dve(lambda e: e.tensor_scalar(out=eqk[:], in0=posn[:], scalar1=float(CAP), scalar2=None, op0=ALU.is_lt), reads=["posn"], writes=["eqk"])
                  S.dve(lambda e: e.tensor_tensor(out=posn[:], in0=posn[:], in1=ebase[:], op=ALU.add), reads=["posn", "ebase"], writes=["posn"])
                  S.dve(lambda e: e.tensor_scalar(out=posn[:], in0=posn[:], scalar1=trash[:, 0:1], scalar2=None, op0=ALU.subtract), reads=["posn", "trash"], writes=["posn"])
                  S.dve(lambda e: e.tensor_tensor(out=posn[:], in0=posn[:], in1=eqk[:], op=ALU.mult), reads=["posn", "eqk"], writes=["posn"])
                  S.dve(lambda e: e.tensor_scalar(out=posn[:], in0=posn[:], scalar1=trash[:, 0:1], scalar2=None, op0=ALU.add), reads=["posn", "trash"], writes=["posn"])
                  for k4 in range(4):
                      S.dve(lambda e, k4=k4: e.tensor_scalar(out=eqk[:], in0=lg[:], scalar1=t8[:, k4:k4 + 1], scalar2=None, op0=ALU.is_equal), reads=["lg", "t8"], writes=["eqk"])
                      S.dve(lambda e: e.tensor_tensor(out=eqk[:], in0=eqk[:], in1=posn[:], op=ALU.mult), reads=["eqk", "posn"], writes=["eqk"])
                      S.dve(lambda e, k4=k4: e.tensor_reduce(out=sl4[:, k4:k4 + 1], in_=eqk[:], axis=AX.X, op=ALU.add), reads=["eqk"], writes=["sl4"])
                  S.dve(lambda e, i=i: e.tensor_copy(out=slot4[:, i, :], in_=sl4[:]), reads=["sl4"], writes=["slot4"])
                  S.dve(lambda e: e.tensor_scalar(out=ev4[:], in0=t8[:, 0:4], scalar1=t8[:, 0:1], scalar2=None, op0=ALU.subtract), reads=["t8"], writes=["ev4"])
                  S.act(lambda e: e.activation(out=ev4[:], in_=ev4[:], func=AF.Exp), reads=["ev4"], writes=["ev4"])
                  S.dve(lambda e: e.tensor_reduce(out=t8[:, 7:8], in_=ev4[:], axis=AX.X, op=ALU.add), reads=["ev4"], writes=["t8"])
                  S.dve(lambda e: e.reciprocal(out=t8[:, 7:8], in_=t8[:, 7:8]), reads=["t8"], writes=["t8"])
                  S.dve(lambda e: e.tensor_scalar(out=ev4[:], in0=ev4[:], scalar1=t8[:, 7:8], scalar2=None, op0=ALU.mult), reads=["ev4", "t8"], writes=["ev4"])
                  S.dve(lambda e: e.tensor_scalar(out=sl4[:], in0=sl4[:], scalar1=float(NSLOT), scalar2=None, op0=ALU.is_lt), reads=["sl4"], writes=["sl4"])
                  S.dve(lambda e, i=i: e.tensor_tensor(out=gate4[:, i, :], in0=ev4[:], in1=sl4[:], op=ALU.mult), reads=["ev4", "sl4"], writes=["gate4"])
                  for k4 in range(4 if stage in ("full", "A_scat", "BC") else 0):
                      S.dma("pool", lambda e, i=i, k4=k4: e.indirect_dma_start(out=Xs[:, :], out_offset=bass.IndirectOffsetOnAxis(ap=slot4[:, i, k4:k4 + 1], axis=0), in_=hb[:], in_offset=None),
                            reads=["hb", "slot4"], writes=["Xs"])
              S.dve(lambda e: e.memset(junk[:, 0:1], 0.0), reads=["slot4", "gate4", "modb", "cst", "cstb"], writes=["junk"])
          S.add("dve", lambda e: e.memset(junk[:, 1:2], 0.0), reads=[], writes=["junk"], barrier=True)

          if stage in ("A1",):
              S.emit()
              return nc

          with contextlib.ExitStack() as st:
              sb = lambda name, shape, d: st.enter_context(nc.sbuf_tensor(name, shape, d))
              Wgu = [sb("Wgu%d" % j, [128, 8, 2 * D], BF16) for j in range(2)]
              Wd = [sb("Wd%d" % j, [128, 8, D], BF16) for j in range(2)]
              bdn = [sb("bdn%d" % j, [1, D], BF16) for j in range(2)]
              bgu = sb("bgu", [128, NE, 16], F32)
              Xe = sb("Xe", [128, 6, D], BF16)
              XT = sb("XT", [128, 8, CAP], BF16)
              aT = sb("aT", [128, 8, CAP], BF16)
              G = sb("G", [128, 384], F32)
              Sg = sb("Sg", [128, 384], F32)
              Uc = sb("Uc", [128, 384], F32)
              Yo = [sb("Yo%d" % j, [128, D], F32) for j in range(2)]
              ones1 = sb("ones1", [1, 128], BF16)
              zt = sb("zt", [128, D], F32)
              S.dma("sp", lambda e: e.dma_start(out=bgu[:], in_=bguT[:, :, :]), writes=["bgu"])
              S.pool(lambda e: e.memset(ones1[:], 1.0), writes=["ones1"])
              S.pool(lambda e: e.memset(zt[:], 0.0), writes=["zt"])
              S.dma("sp", lambda e: e.dma_start(out=Ys[NSLOT:NSLOT + 128, :], in_=zt[:]), reads=["zt"], writes=["Ys"])

              def load_w(ex):
                  j = ex % 2
                  S.dma("pool", lambda e, j=j, ex=ex: e.dma_start(out=bdn[j][:], in_=b_dn[:, ex, :]), writes=[("bdn", j)])
                  gv_ = w_gu[ex].rearrange("(k p) n -> p k n", p=128)
                  dv_ = w_dn[ex].rearrange("(k p) n -> p k n", p=128)
                  for k in range(0, 8, 2):
                      S.dma("pool", lambda e, k=k, j=j, gv_=gv_: e.dma_start(out=Wgu[j][:, k:k + 2, :], in_=gv_[:, k:k + 2, :]), writes=[("Wgu", j)])
                  for k in range(0, 8, 4):
                      S.dma("pool", lambda e, k=k, j=j, dv_=dv_: e.dma_start(out=Wd[j][:, k:k + 4, :], in_=dv_[:, k:k + 4, :]), writes=[("Wd", j)])

              load_w(0)
              for ex in range(nexp):
                  j = ex % 2
                  if ex + 1 < nexp:
                      load_w(ex + 1)
                  S.dma("sp", lambda e, ex=ex: e.dma_start(out=Xe[:], in_=Xs[ex * CAP:(ex + 1) * CAP, :].rearrange("(s p) d -> p s d", p=128)), reads=["Xs"], writes=["Xe"])
                  for s in range(6):
                      pp = pTb if s % 2 == 0 else pTb2
                      for k in range(8):
                          S.pe(lambda e, s=s, k=k, pp=pp: e.transpose(out=pp[:, k * 128:(k + 1) * 128], in_=Xe[:, s, k * 128:(k + 1) * 128], identity=ident_b), reads=["Xe", "cstb"], writes=[pp.name])
                      S.act(lambda e, s=s, pp=pp: e.copy(out=XT[:, :, s * 128:(s + 1) * 128], in_=pp[:].rearrange("p (a b) -> p a b", a=8)), reads=[pp.name], writes=["XT"])
                  for nh in range(2):
                      n0 = nh * 384
                      for fc in range(8):
                          for (pp, col) in ((pA, fc), (pB, 8 + fc)):
                              for k in range(8):
                                  S.pe(lambda e, k=k, pp=pp, col=col, j=j, n0=n0: e.matmul(pp[:, 0:384], lhsT=Wgu[j][:, k, col * 128:(col + 1) * 128], rhs=XT[:, k, n0:n0 + 384], start=(k == 0), stop=(k == 7)),
                                       reads=[("Wgu", j), "XT"], writes=[pp.name])
                          S.dve(lambda e, ex=ex, fc=fc: e.tensor_scalar(out=G[:], in0=pA[:, 0:384], scalar1=bgu[:, ex, fc:fc + 1], scalar2=7.0, op0=ALU.add, op1=ALU.min), reads=["pA", "bgu"], writes=["G"])
                          S.act(lambda e: e.activation(out=Sg[:], in_=G[:], func=AF.Sigmoid, scale=1.702), reads=["G"], writes=["Sg"])
                          S.dve(lambda e, ex=ex, fc=fc: e.tensor_scalar(out=Uc[:], in0=pB[:, 0:384], scalar1=bgu[:, ex, 8 + fc:9 + fc], scalar2=7.0, op0=ALU.add, op1=ALU.min), reads=["pB", "bgu"], writes=["Uc"])
                          S.dve(lambda e: e.tensor_scalar(out=Uc[:], in0=Uc[:], scalar1=-7.0, scalar2=1.0, op0=ALU.max, op1=ALU.add), reads=["Uc"], writes=["Uc"])
                          S.dve(lambda e: e.tensor_tensor(out=G[:], in0=G[:], in1=Sg[:], op=ALU.mult), reads=["G", "Sg"], writes=["G"])
                          S.dve(lambda e, fc=fc, n0=n0: e.tensor_tensor(out=aT[:, fc, n0:n0 + 384], in0=G[:], in1=Uc[:], op=ALU.mult), reads=["G", "Uc"], writes=["aT"])
                  for s in range(6):
                      yo = Yo[s % 2]
                      for hf, pp in ((0, pC), (1, pD)):
                          S.pe(lambda e, hf=hf, pp=pp, j=j: e.matmul(pp[:], lhsT=ones1[:], rhs=bdn[j][:, hf * 512:(hf + 1) * 512], start=True, stop=False), reads=["ones1", ("bdn", j)], writes=[pp.name])
                          for k in range(8):
                              S.pe(lambda e, k=k, hf=hf, pp=pp, s=s, j=j: e.matmul(pp[:], lhsT=aT[:, k, s * 128:(s + 1) * 128], rhs=Wd[j][:, k, hf * 512:(hf + 1) * 512], start=False, stop=(k == 7)),
                                   reads=["aT", ("Wd", j)], writes=[pp.name])
                          S.act(lambda e, hf=hf, pp=pp, yo=yo: e.copy(out=yo[:, hf * 512:(hf + 1) * 512], in_=pp[:]), reads=[pp.name], writes=[("Yo", s % 2)])
                      S.dma("sp", lambda e, ex=ex, s=s, yo=yo: e.dma_start(out=Ys[ex * CAP + s * 128:ex * CAP + (s + 1) * 128, :], in_=yo[:]), reads=[("Yo", s % 2)], writes=["Ys"])
              S.dve(lambda e: e.memset(junk[:, 2:3], 0.0), reads=["slot4", "gate4", "modb"], writes=["junk"])
          S.add("dve", lambda e: e.memset(junk[:, 3:4], 0.0), reads=[], writes=["junk"], barrier=True)

          with contextlib.ExitStack() as st:
              sb = lambda name, shape, d: st.enter_context(nc.sbuf_tensor(name, shape, d))
              Yg = [sb("Yg%d" % j, [128, 4, D], F32) for j in range(2)]
              x1t = [sb("x1t%d" % j, [128, D], F32) for j in range(2)]
              acc = sb("acc", [128, D], F32)
              lnpc = sb("lnpbC", [128, 2, D], F32)
              S.dma("sp", lambda e: e.dma_start(out=lnpc[:], in_=lnp[:, 2:4, :]), writes=["lnpb"])
              st6c = sb("st6c", [128, 2, 6], F32)
              mvc = sb("mvc", [128, 2], F32)
              rsc = sb("rsc", [128, 1], F32)
              for i in range(ntiles):
                  j = i % 2
                  rows = slice(i * 128, (i + 1) * 128)
                  S.dma("sp", lambda e, rows=rows, j=j: e.dma_start(out=x1t[j][:], in_=x1s[rows, :]), reads=["x1s"], writes=[("x1t", j)])
                  for k4 in range(4):
                      S.dma("pool", lambda e, i=i, k4=k4, j=j: e.indirect_dma_start(out=Yg[j][:, k4, :], out_offset=None, in_=Ys[:, :], in_offset=bass.IndirectOffsetOnAxis(ap=slot4[:, i, k4:k4 + 1], axis=0)),
                            reads=["Ys", "slot4"], writes=[("Yg", j, k4)])
                  S.dve(lambda e, i=i, j=j: e.tensor_scalar(out=acc[:], in0=Yg[j][:, 0, :], scalar1=gate4[:, i, 0:1], scalar2=None, op0=ALU.mult), reads=[("Yg", j, 0), "gate4"], writes=["acc"])
                  for k4 in range(1, 4):
                      S.dve(lambda e, i=i, j=j, k4=k4: e.scalar_tensor_tensor(out=acc[:], in0=Yg[j][:, k4, :], scalar=gate4[:, i, k4:k4 + 1], in1=acc[:], op0=ALU.mult, op1=ALU.add), reads=[("Yg", j, k4), "gate4", "acc"], writes=["acc"])
                  S.dve(lambda e: e.tensor_tensor(out=acc[:], in0=acc[:], in1=modb[:, 5, :], op=ALU.mult), reads=["acc", "modb"], writes=["acc"])
                  S.dve(lambda e, j=j: e.scalar_tensor_tensor(out=acc[:], in0=x1t[j][:], scalar=float(ALPHA), in1=acc[:], op0=ALU.mult, op1=ALU.add), reads=[("x1t", j), "acc"], writes=["acc"])
                  for h in range(2):
                      S.dve(lambda e, h=h: e.bn_stats(out=st6c[:, h, :], in_=acc[:, h * 512:(h + 1) * 512]), reads=["acc"], writes=["st6c"])
                  S.dve(lambda e: e.bn_aggr(out=mvc[:], in_=st6c[:].rearrange("p a b -> p (a b)")), reads=["st6c"], writes=["mvc"])
                  S.dve(lambda e: e.tensor_scalar(out=rsc[:], in0=mvc[:, 1:2], scalar1=LN_EPS, scalar2=None, op0=ALU.add), reads=["mvc"], writes=["rsc"])
                  S.act(lambda e: e.activation(out=rsc[:], in_=rsc[:], func=AF.Sqrt), reads=["rsc"], writes=["rsc"])
                  S.dve(lambda e: e.reciprocal(out=rsc[:], in_=rsc[:]), reads=["rsc"], writes=["rsc"])
                  S.dve(lambda e: e.tensor_scalar(out=acc[:], in0=acc[:], scalar1=mvc[:, 0:1], scalar2=rsc[:], op0=ALU.subtract, op1=ALU.mult), reads=["acc", "mvc", "rsc"], writes=["acc"])
                  S.dve(lambda e: e.tensor_tensor(out=acc[:], in0=acc[:], in1=lnpc[:, 0, :], op=ALU.mult), reads=["acc", "lnpb"], writes=["acc"])
                  S.dve(lambda e, j=j: e.tensor_tensor(out=x1t[j][:], in0=acc[:], in1=lnpc[:, 1, :], op=ALU.add), reads=["acc", "lnpb"], writes=[("x1t", j)])
                  S.dma("sp", lambda e, rows=rows, j=j: e.dma_start(out=out[rows, :], in_=x1t[j][:]), reads=[("x1t", j)], writes=["out"])

    except _Cut:
        pass
    S.emit()
    return nc


def _prep_shared(inp):
    f = lambda a: np.ascontiguousarray(np.asarray(a), dtype=np.float32)
    bc = lambda v, n=128: np.ascontiguousarray(np.broadcast_to(np.asarray(v, np.float32).reshape(1, -1), (n, np.asarray(v).size)))
    w_in = f(inp["w_in"][0])
    perm = np.concatenate([np.arange(0, 512), np.arange(768, 2464), np.arange(512, 640), np.arange(640, 768)])
    sh = {}
    sh["w_ada"] = f(inp["w_ada"][0])
    sh["b_ada_b"] = bc(inp["b_ada"][0])
    sh["w_in"] = np.ascontiguousarray(w_in[:, perm])
    sh["mu_b"] = bc(inp["shift_mu"][0])
    rows = [inp["rwkv_w0"][0], inp["rwkv_a0"][0], inp["rwkv_k_k"][0], inp["rwkv_k_a"][0], np.asarray(inp["rwkv_r_k"][0]).reshape(-1), inp["rwkv_ln_w"][0], inp["rwkv_ln_b"][0]]
    sh["rwc_b"] = np.ascontiguousarray(np.stack([bc(r) for r in rows], axis=1))
    lora = np.zeros((96, 3, 512), np.float32)
    lora[0:32, 0] = inp["rwkv_w2"][0]
    lora[0:32, 1] = inp["rwkv_a2"][0]
    lora[0:96, 2] = inp["rwkv_g2"][0]
    sh["lora"] = lora
    sh["sinks_b"] = bc(inp["attn_sinks"][0])
    invf = (500000.0 ** (-np.arange(0, 16, 2, dtype=np.float32) / 16)).astype(np.float32)
    sh["invf_b"] = bc(invf)
    sh["w_out"] = f(inp["w_out"][0])
    sh["lnp"] = np.ascontiguousarray(np.stack([bc(inp[k][0]) for k in ("ln1_g", "ln1_b", "ln2_g", "ln2_b")], axis=1))
    sh["w_router"] = f(inp["w_router"][0])
    sh["b_router_b"] = bc(inp["b_router"][0])
    sh["w_gu"] = f(inp["w_gate_up"][0])
    sh["bguT"] = np.ascontiguousarray(f(inp["b_gate_up"][0]).reshape(NE, 16, 128).transpose(2, 0, 1))
    sh["w_dn"] = f(inp["w_down"][0])
    sh["b_dn"] = f(inp["b_down"][0]).reshape(1, NE, D)
    jj = np.arange(128)[:, None]
    tt = np.arange(128)[None, :]
    sh["consts"] = np.ascontiguousarray(np.stack([np.eye(128), (jj <= tt), (jj < tt), (jj > tt), (jj > tt)], axis=1).astype(np.float32))
    return sh


def _prep_core(inp, b, sh):
    m = dict(sh)
    m["x"] = np.ascontiguousarray(np.asarray(inp["x"][b], np.float32))
    m["posT"] = np.ascontiguousarray(np.asarray(inp["positions"][b], np.int32).reshape(NT, 128).T)
    c = np.asarray(inp["c"][b], np.float32)
    m["cB"] = np.ascontiguousarray(np.broadcast_to(c.reshape(8, 128).T[:, :, None], (128, 8, 128)))
    return m


_NC_CACHE = {}


def kernel(**inputs):
    sh = _prep_shared(inputs)
    in_maps = [_prep_core(inputs, b, sh) for b in range(8)]
    if "full" not in _NC_CACHE:
        _NC_CACHE["full"] = build("full")
    nc = _NC_CACHE["full"]
    res = run_bass_kernel_spmd(nc, in_maps, core_ids=list(range(8)))
    return np.stack([np.asarray(r["out"], np.float32) for r in res.results], axis=0)
```

```python
import contextlib
import os as _os
import numpy as np
import concourse.bass as bass
import concourse.mybir as mybir
from concourse.bass_utils import run_bass_kernel_spmd

F32 = mybir.dt.float32
BF16 = mybir.dt.bfloat16
I32 = mybir.dt.int32
U32 = mybir.dt.uint32
AF = mybir.ActivationFunctionType
ALU = mybir.AluOpType
AX = mybir.AxisListType

COMPUTE = ("pe", "act", "dve", "pool")
SEG = 8192
SAME_ENG_INORDER = ("pe",)
SAME_ENG_DRAIN = ()
SAME_ENG_HZ = ("act", "dve")
HZ_SMALL = 256
BUBBLE = False
NPOOL = {"pe": 24, "act": 24, "dve": 24, "pool": 4}
BUBBLE_DIST = 2
HZ_METHODS = ("tensor_reduce", "bn_stats", "bn_aggr", "max", "reciprocal")


class _Rec:
    def __init__(self):
        self.calls = []

    def __getattr__(self, name):
        def f(*a, **k):
            self.calls.append((name, a, k))
            return self
        return f


def _free_size(ap):
    try:
        sh = list(ap.shape)
        n = 1
        for x in sh[1:]:
            n *= int(x)
        return n
    except Exception:
        return 0


class _Cut(Exception):
    pass


class Sched:
    def __init__(self, nc, kdma=None):
        self.nc = nc
        self.ops = []
        self.last_w = {}
        self.rd_eng = {}
        self.rd_dma = {}
        self.kdma = kdma or {"sp": 16, "pool": 8, "act": 4}
        self.relay_of = {}
        self.relay_fn = None

    def add(self, eng, fn, reads=(), writes=(), dma=False, barrier=False):
        i = len(self.ops)
        reads = list(reads)
        writes = list(writes)
        if barrier:
            writes.append("PHASE")
        else:
            reads.append("PHASE")
        deps = set()
        for r in reads:
            if r in self.last_w:
                deps.add(self.last_w[r])
        for w in writes:
            if w in self.last_w:
                deps.add(self.last_w[w])
            for d in self.rd_eng.get(w, {}).values():
                deps.add(d)
            for d in self.rd_dma.get(w, ()):
                deps.add(d)
        if getattr(self, "relay_fn", None) is not None and not dma:
            nd = set()
            for d in deps:
                od = self.ops[d]
                if (not od["dma"]) and {eng, od["eng"]} in ({"pe", "dve"}, {"pe", "pool"}):
                    if d not in self.relay_of:
                        self.relay_of[d] = len(self.ops)
                        self.ops.append(dict(eng="act", fn=self.relay_fn, deps=[d], dma=False))
                    nd.add(self.relay_of[d])
                else:
                    nd.add(d)
            deps = nd
            i = len(self.ops)
        for w in writes:
            self.last_w[w] = i
            self.rd_eng[w] = {}
            self.rd_dma[w] = []
        ws = set(writes)
        for r in reads:
            if r in ws:
                continue
            if dma:
                self.rd_dma.setdefault(r, []).append(i)
            else:
                self.rd_eng.setdefault(r, {})[eng] = i
        self.ops.append(dict(eng=eng, fn=fn, deps=sorted(deps), dma=dma))
        return i

    def pe(self, fn, reads=(), writes=()):
        return self.add("pe", fn, reads, writes)

    def act(self, fn, reads=(), writes=()):
        return self.add("act", fn, reads, writes)

    def dve(self, fn, reads=(), writes=()):
        return self.add("dve", fn, reads, writes)

    def pool(self, fn, reads=(), writes=()):
        return self.add("pool", fn, reads, writes)

    def dma(self, q, fn, reads=(), writes=()):
        return self.add(q, fn, reads, writes, dma=True)

    def emit(self):
        nc = self.nc
        ops = self.ops
        n = len(ops)
        dcount = {q: [0] * k for q, k in self.kdma.items()}
        dnext = {q: 0 for q in self.kdma}
        tok = [None] * n
        prev_same = [None] * n
        last_on = {}
        order = [0] * n
        ecnt = {}
        for i, o in enumerate(ops):
            e = o["eng"]
            ecnt[e] = ecnt.get(e, 0) + 1
            order[i] = ecnt[e]
            if o["dma"]:
                q = e
                s_ = dnext[q] % self.kdma[q]
                dnext[q] += 1
                dcount[q][s_] += 1
                key = ("d", q, s_)
                tok[i] = (key, 16 * dcount[q][s_])
                prev_same[i] = last_on.get(key)
                last_on[key] = i
        per_eng = {}
        for i, o in enumerate(ops):
            per_eng.setdefault(o["eng"], []).append(i)

        def dep_list(i):
            o = ops[i]
            deps = list(o["deps"])
            if o["dma"] and prev_same[i] is not None:
                deps.append(prev_same[i])
            return deps

        hz = [True] * n
        for i, o in enumerate(ops):
            if o["dma"] or o["eng"] not in SAME_ENG_HZ:
                continue
            r = _Rec()
            try:
                o["fn"](r)
                name, a, k = r.calls[0]
                out = k.get("out", k.get("ap", a[0] if a else None))
                small = _free_size(out) < HZ_SMALL
                hz[i] = small or (name in HZ_METHODS) or ("accum_out" in k and k["accum_out"] is not None)
            except Exception:
                hz[i] = True
        self.n_hz = sum(1 for i, o in enumerate(ops) if (not o["dma"]) and o["eng"] in SAME_ENG_HZ and hz[i])
        needed = [False] * n
        needed_self = [False] * n
        bubble_before = [False] * n
        plan = {}
        drain_before = [False] * n
        for ename, idxs in per_eng.items():
            waited = {}
            drained_upto = 0
            for i in idxs:
                wl = []
                for d in dep_list(i):
                    od = ops[d]
                    if od["dma"]:
                        key, val = tok[d]
                        if waited.get(key, 0) >= val:
                            continue
                        waited[key] = val
                        wl.append(d)
                    else:
                        if od["eng"] == ename and ename in SAME_ENG_INORDER:
                            continue
                        if od["eng"] == ename and ename in SAME_ENG_HZ and not hz[d]:
                            continue
                        if od["eng"] == ename and ename in SAME_ENG_DRAIN:
                            if order[d] > drained_upto:
                                drain_before[i] = True
                                drained_upto = order[i] - 1
                            continue
                        if od["eng"] == ename and ename in SAME_ENG_HZ and BUBBLE:
                            if order[i] - order[d] <= BUBBLE_DIST:
                                bubble_before[i] = True
                            continue
                        if od["eng"] == ename:
                            key = ("s", od["eng"])
                            if waited.get(key, 0) >= order[d]:
                                continue
                            waited[key] = order[d]
                            needed_self[d] = True
                            wl.append((d, "s"))
                            continue
                        key = ("e", od["eng"])
                        if waited.get(key, 0) >= order[d]:
                            continue
                        waited[key] = order[d]
                        needed[d] = True
                        wl.append(d)
                plan[i] = wl
        ecount = {e: 0 for e in COMPUTE}
        scount = {e: 0 for e in COMPUTE}
        stok = [None] * n
        keys = set()
        for i, o in enumerate(ops):
            if o["dma"]:
                keys.add(tok[i][0])
            elif needed[i] or needed_self[i]:
                e = o["eng"]
                key = ("e", e, ecount[e] % NPOOL[e])
                tok[i] = (key, ecount[e] // NPOOL[e] + 1)
                stok[i] = tok[i]
                needed[i] = True
                ecount[e] += 1
                keys.add(key)
        self.n_incs = dict(ecount)
        with contextlib.ExitStack() as st:
            sems = {}
            for key in sorted(keys, key=str):
                sems[key] = st.enter_context(nc.semaphore("s_" + "_".join(str(x) for x in key)))
            block = st.enter_context(nc.Block())
            engmap = {"pe": "tensor", "act": "scalar", "dve": "vector", "pool": "gpsimd", "sp": "sync"}

            def make(ename, idxs):
                def body(eng):
                    for i in idxs:
                        o = ops[i]
                        if drain_before[i]:
                            eng.drain()
                        if bubble_before[i] and ename in getattr(self, "bubble", {}):
                            self.bubble[ename](eng)
                        for d in plan[i]:
                            if isinstance(d, tuple):
                                key, val = stok[d[0]]
                            else:
                                key, val = tok[d]
                            eng.wait_ge(sems[key], val)
                        inst = o["fn"](eng)
                        if o["dma"]:
                            inst.then_inc(sems[tok[i][0]], 16)
                        else:
                            if needed[i]:
                                inst.then_inc(sems[tok[i][0]], 1)
                            elif needed_self[i]:
                                inst.then_inc(sems[stok[i][0]], 1)
                            if (needed[i] or needed_self[i]) and ename in getattr(self, "spacer", {}):
                                self.spacer[ename](eng)
                    if ename in self.kdma:
                        for s_ in range(self.kdma[ename]):
                            if dcount[ename][s_] > 0:
                                eng.wait_ge(sems[("d", ename, s_)], 16 * dcount[ename][s_])
                return body

            for ename, idxs in per_eng.items():
                getattr(block, engmap[ename])(make(ename, idxs))
        return ecount


NT = 32
D = 1024
DIN = 2464
CAP = 768
NE = 32
NSLOT = NE * CAP
LN_EPS = 1e-5
GN_EPS = 64e-5
ALPHA = 2 ** 0.25
TWO_PI = 2.0 * np.pi


def build(stage="full", ntiles=NT, nexp=NE):
    nc = bass.Bass("TRN2", target_bir_lowering=False)
    dt = lambda name, shape, d, kind="ExternalInput": nc.dram_tensor(name, shape, d, kind=kind).ap()
    x = dt("x", [4096, D], F32)
    posT = dt("posT", [128, NT], I32)
    cB = dt("cB", [128, 8, 128], F32)
    w_ada = dt("w_ada", [D, 6 * D], F32)
    b_ada_b = dt("b_ada_b", [128, 6 * D], F32)
    w_in = dt("w_in", [D, DIN], F32)
    mu_b = dt("mu_b", [128, 1696], F32)
    rwc_b = dt("rwc_b", [128, 7, 512], F32)
    lora = dt("lora", [96, 3, 512], F32)
    sinks_b = dt("sinks_b", [128, 8], F32)
    invf_b = dt("invf_b", [128, 8], F32)
    w_out = dt("w_out", [D, D], F32)
    lnp = dt("lnp", [128, 4, D], F32)
    w_router = dt("w_router", [D, NE], F32)
    b_router_b = dt("b_router_b", [128, NE], F32)
    w_gu = dt("w_gu", [NE, D, 2 * D], F32)
    bguT = dt("bguT", [128, NE, 16], F32)
    w_dn = dt("w_dn", [NE, D, D], F32)
    b_dn = dt("b_dn", [1, NE, D], F32)
    consts = dt("consts", [128, 5, 128], F32)
    out = dt("out", [4096, D], F32, kind="ExternalOutput")
    dbg = dt("dbg", [4096, D], F32, kind="ExternalOutput") if stage != "full" else None
    x1s = dt("x1s", [4096, D], F32, kind="Internal")
    Xs = dt("Xs", [NSLOT + 128, D], BF16, kind="Internal")
    Ys = dt("Ys", [NSLOT + 128, D], F32, kind="Internal")
    lastrow = dt("lastrow", [2, 1696], F32, kind="Internal")

    S = Sched(nc)

    cut_tile = [0]

    def cut(name, ap, key, ncols=None):
        if stage == name and (name == "P" or cut_tile[0] == ntiles - 1):
            if ap.shape[-1] > 1024:
                ap = ap[:, 0:1024]
            ncols = ncols or ap.shape[-1]
            q = "sp" if ap.dtype == F32 else "pool"
            S.dma(q, lambda e: e.dma_start(out=dbg[0:ap.shape[0], 0:ncols], in_=ap), reads=[key])
            raise _Cut()

    try:
      with contextlib.ExitStack() as st0:
          sb0 = lambda name, shape, d: st0.enter_context(nc.sbuf_tensor(name, shape, d))
          ps = lambda name, shape, d: st0.enter_context(nc.psum_tensor(name, shape, d))
          pTb = ps("pTb", [128, 1024], BF16)
          pTb2 = ps("pTb2", [128, 1024], BF16)
          pA = ps("pA", [128, 512], F32)
          pB = ps("pB", [128, 512], F32)
          pC = ps("pC", [128, 512], F32)
          pD = ps("pD", [128, 512], F32)
          pE = ps("pE", [128, 512], F32)
          pF = ps("pF", [128, 512], F32)
          cst = sb0("cst", [128, 5, 128], F32)
          cstb = sb0("cstb", [128, 5, 128], BF16)
          modb = sb0("modb", [128, 6, D], F32)
          slot4 = sb0("slot4", [128, NT, 4], I32)
          gate4 = sb0("gate4", [128, NT, 4], F32)
          junk = sb0("junk", [128, 8], F32)
          S.relay_fn = lambda e: e.activation(out=junk[:, 6:7], in_=junk[:, 6:7], func=AF.Copy)
          if True:
              spc = sb0("spc", [128, 2, 512], F32)
              nsp = 512
              nsp = int(_os.environ.get("KSPACE", "0"))
              nbb = int(_os.environ.get("KBUB", "384"))
              S.spacer = {"dve": lambda e: e.memset(spc[:, 0, 0:nsp], 0.0),
                          "act": lambda e: e.activation(out=spc[:, 1, 0:nsp], in_=spc[:, 1, 0:nsp], func=AF.Copy)}
              if nsp == 0:
                  S.spacer = {}
              S.bubble = {"dve": lambda e: e.memset(spc[:, 0, 0:nbb], 0.0),
                          "act": lambda e: e.activation(out=spc[:, 1, 0:nbb], in_=spc[:, 1, 0:nbb], func=AF.Copy)}
          ident_f = cst[:, 0, :]
          ident_b = cstb[:, 0, :]

          S.dma("sp", lambda e: e.dma_start(out=cst[:], in_=consts[:, :, :]), writes=["cst"])
          S.dve(lambda e: e.tensor_copy(out=cstb[:], in_=cst[:]), reads=["cst"], writes=["cstb"])
          S.dma("sp", lambda e: e.dma_start(out=modb[:].rearrange("p a b -> p (a b)"), in_=b_ada_b[:, :]), writes=["modb"])

          with contextlib.ExitStack() as st:
              sb = lambda name, shape, d: st.enter_context(nc.sbuf_tensor(name, shape, d))
              w_in_bf = sb("w_in_bf", [128, 8, DIN], BF16)
              lnpb = sb("lnpbA", [128, 2, D], F32)
              S.dma("sp", lambda e: e.dma_start(out=lnpb[:], in_=lnp[:, 0:2, :]), writes=["lnpb"])
              w_out_bf = sb("w_out_bf", [128, 8, D], BF16)
              wr_f = sb("wr_f", [128, 8, NE], F32)
              brb = sb("brb", [128, NE], F32)
              mub = sb("mub", [128, 1696], F32)
              rwcb = sb("rwcb", [128, 7, 512], F32)
              lor = sb("lor", [96, 3, 512], F32)
              sinkb = sb("sinkb", [128, 8], F32)
              esink = sb("esink", [128, 8], F32)
              invf = sb("invf", [128, 8], F32)
              cosT = sb("cosT", [128, NT, 8], F32)
              sinT = sb("sinT", [128, NT, 8], F32)
              stp = contextlib.ExitStack()
              sbp = lambda name, shape, d: stp.enter_context(nc.sbuf_tensor(name, shape, d))
              posi = sbp("posi", [128, NT], I32)
              posf = sbp("posf", [128, NT], F32)
              ang = sbp("ang", [128, NT, 8], F32)
              scB = sbp("scB", [128, 8, 128], F32)
              wada = [sbp("wada%d" % j, [128, 8, 512], F32) for j in range(2)]
              w_in_v = w_in.rearrange("(k p) n -> p k n", p=128)
              for (c0, c1) in ((0, 1232), (1232, 2464)):
                  S.dma("pool", lambda e, c0=c0, c1=c1: e.dma_start(out=w_in_bf[:, :, c0:c1], in_=w_in_v[:, :, c0:c1]), writes=["w_in_bf"])
              S.dma("pool", lambda e: e.dma_start(out=w_out_bf[:], in_=w_out.rearrange("(k p) n -> p k n", p=128)), writes=["w_out_bf"])
              S.dma("sp", lambda e: e.dma_start(out=wr_f[:], in_=w_router.rearrange("(k p) n -> p k n", p=128)), writes=["wr_f"])
              S.dma("sp", lambda e: e.dma_start(out=brb[:], in_=b_router_b[:, :]), writes=["brb"])
              S.dma("sp", lambda e: e.dma_start(out=mub[:], in_=mu_b[:, :]), writes=["mub"])
              S.dma("sp", lambda e: e.dma_start(out=rwcb[:], in_=rwc_b[:, :, :]), writes=["rwcb"])
              S.dma("sp", lambda e: e.dma_start(out=lor[:], in_=lora[:, :, :]), writes=["lor"])
              S.dma("sp", lambda e: e.dma_start(out=sinkb[:], in_=sinks_b[:, :]), writes=["sinkb"])
              S.dma("sp", lambda e: e.dma_start(out=invf[:], in_=invf_b[:, :]), writes=["invf"])
              S.dma("sp", lambda e: e.dma_start(out=posi[:], in_=posT[:, :]), writes=["posi"])
              S.dma("sp", lambda e: e.dma_start(out=scB[:], in_=cB[:, :, :]), writes=["scB"])
              S.act(lambda e: e.activation(out=esink[:], in_=sinkb[:], func=AF.Exp), reads=["sinkb"], writes=["esink"])
              S.act(lambda e: e.activation(out=scB[:], in_=scB[:], func=AF.Silu), reads=["scB"], writes=["scB"])
              w_ada_v = w_ada.rearrange("(k p) n -> p k n", p=128)
              for j in range(12):
                  wb_ = wada[j % 2]
                  S.dma("sp", lambda e, j=j, wb_=wb_: e.dma_start(out=wb_[:], in_=w_ada_v[:, :, j * 512:(j + 1) * 512]), writes=[("wada", j % 2)])
                  pp = pA if j % 2 == 0 else pB
                  for k in range(8):
                      S.pe(lambda e, k=k, wb_=wb_, pp=pp: e.matmul(pp[:], lhsT=scB[:, k, :], rhs=wb_[:, k, :], start=(k == 0), stop=(k == 7)),
                           reads=["scB", ("wada", j % 2)], writes=[pp.name])
                  mflat = modb[:].rearrange("p a b -> p (a b)")
                  S.dve(lambda e, j=j, pp=pp, mflat=mflat: e.tensor_tensor(out=mflat[:, j * 512:(j + 1) * 512], in0=pp[:], in1=mflat[:, j * 512:(j + 1) * 512], op=ALU.add),
                        reads=[pp.name, "modb"], writes=["modb"])
              for a in (1, 2, 4, 5):
                  S.dve(lambda e, a=a: e.tensor_scalar(out=modb[:, a, :], in0=modb[:, a, :], scalar1=1.0, scalar2=None, op0=ALU.add), reads=["modb"], writes=["modb"])
              S.dve(lambda e: e.tensor_copy(out=posf[:], in_=posi[:]), reads=["posi"], writes=["posf"])
              S.dve(lambda e: e.tensor_tensor(out=ang[:], in0=posf[:].unsqueeze(2).broadcast_to([128, NT, 8]), in1=invf[:].unsqueeze(1).broadcast_to([128, NT, 8]), op=ALU.mult),
                    reads=["posf", "invf"], writes=["ang"])
              angi = sbp("angi", [128, NT, 8], I32)
              angf = sbp("angf", [128, NT, 8], F32)
              SC = float(TWO_PI * (1.0 - 1e-6))
              for (dst, key, off) in ((sinT, "sinT", 0.0), (cosT, "cosT", 0.25)):
                  S.dve(lambda e, dst=dst, off=off: e.tensor_scalar(out=dst[:], in0=ang[:], scalar1=float(1.0 / TWO_PI), scalar2=off, op0=ALU.mult, op1=ALU.add), reads=["ang"], writes=[key])
                  S.dve(lambda e, dst=dst: e.tensor_copy(out=angi[:], in_=dst[:]), reads=[key], writes=["angi"])
                  S.dve(lambda e: e.tensor_copy(out=angf[:], in_=angi[:]), reads=["angi"], writes=["angf"])
                  S.dve(lambda e, dst=dst: e.tensor_tensor(out=dst[:], in0=dst[:], in1=angf[:], op=ALU.subtract), reads=[key, "angf"], writes=[key])
                  S.dve(lambda e, dst=dst: e.tensor_scalar(out=angf[:], in0=dst[:], scalar1=0.5, scalar2=None, op0=ALU.is_gt), reads=[key], writes=["angf"])
                  S.dve(lambda e, dst=dst: e.tensor_tensor(out=dst[:], in0=dst[:], in1=angf[:], op=ALU.subtract), reads=[key, "angf"], writes=[key])
                  S.act(lambda e, dst=dst: e.activation(out=dst[:], in_=dst[:], func=AF.Sin, scale=SC), reads=[key], writes=[key])

              zrow = sbp("zrow", [1, 1696], F32)
              S.pool(lambda e: e.memset(zrow[:], 0.0), writes=["zrow"])
              S.dma("sp", lambda e: e.dma_start(out=lastrow[0:1, :], in_=zrow[:]), reads=["zrow"], writes=["lastrow0"])
              S.dve(lambda e: e.memset(junk[:, 4:5], 0.0), reads=["cosT", "sinT", "modb"], writes=["junk"])
              S.add("dve", lambda e: e.memset(junk[:, 5:6], 0.0), reads=[], writes=["junk"], barrier=True)
              stp.close()
              cut("P", modb[:, 1, :], "modb")
              xt = sb("xt", [128, D], F32)
              g4t = sb("g4t", [128, 416], F32)
              t1k = sb("t1k", [128, D], F32)
              hb = sb("hb", [128, D], BF16)
              hT = sb("hT", [128, 8, 128], BF16)
              st6 = sb("st6", [128, 2, 6], F32)
              mv = sb("mv", [128, 2], F32)
              rstd = sb("rstd", [128, 1], F32)
              qk = sb("qk", [128, 10, 64], F32)
              qkb = sb("qkb", [128, 10, 64], BF16)
              rt = sb("rt", [128, 4, 10, 8], F32)
              qT = sb("qT", [64, 8, 128], BF16)
              kT = [sb("kT%d" % j, [64, 2, 128], BF16) for j in range(2)]
              V1 = [sb("V1%d" % j, [128, 2, 66], BF16) for j in range(2)]
              rwc = sb("rwc", [128, 1696], F32)
              rwp = sb("rwp", [128, 1696], F32)
              Ee = sb("Ee", [128, 512], F32)
              PTp = sb("PTp", [128, 4, 128], BF16)
              PTc = sb("PTc", [128, 4, 128], BF16)
              den = sb("den", [128, 8], F32)
              mixb = sb("mixb", [128, D], BF16)
              lo_in = sb("lo_in", [128, 160], F32)
              loT = sb("loT", [96, 3, 128], F32)
              f1 = rwc[:, 0:512]
              f2 = rwc[:, 512:1024]
              f3 = rwc[:, 1024:1536]
              f4 = sb("f4", [128, 512], F32)
              eP = sb("eP", [128, 512], F32)
              eN = sb("eN", [128, 512], F32)
              lw = sb("lw", [128, 512], F32)
              av = sb("av", [128, 512], F32)
              kmod = sb("kmod", [128, 512], F32)
              s8 = sb("s8", [128, 4, 8], F32)
              At = sb("At", [128, 512], BF16)
              Bt = sb("Bt", [128, 512], BF16)
              Kt = sb("Kt", [128, 512], BF16)
              Rt = sb("Rt", [128, 512], BF16)
              Vb = sb("Vb", [128, 512], BF16)
              arT = sb("arT", [64, 8, 2, 128], BF16)
              bkT = sb("bkT", [64, 8, 2, 128], BF16)
              dC = sb("dC", [64, 8], F32)
              Tst = [sb("Tst%d" % j, [64, 8, 64], BF16) for j in range(2)]
              XAs = [sb("XA%d" % p, [128, 2, 256], BF16) for p in range(2)]
              XNs = [[sb("XN%d_%d" % (p, j), [128, 256], BF16) for j in range(2)] for p in range(2)]
              Lps = [[sb("Lp%d_%d" % (p, j), [128, 128], BF16) for j in range(2)] for p in range(2)]
              RHSss = [sb("RHSs%d" % p, [128, 64], BF16) for p in range(2)]
              Uss = [sb("Us%d" % p, [128, 64], BF16) for p in range(2)]
              msk2 = sb("msk2", [128, 256], F32)
              ones_f = sb("ones_f", [128, 1], F32)
              h2T = xt[:].rearrange("p (a b) -> p a b", a=8)
              lg = sb("lg", [128, NE], F32)
              t8 = sb("t8", [128, 8], F32)
              eqk = sb("eqk", [128, NE], F32)
              posn = sb("posn", [128, NE], F32)
              tot = sb("tot", [128, NE], F32)
              mskb = sb("mskb", [128, NE], BF16)
              sl4 = sb("sl4", [128, 4], F32)
              ebase = sb("ebase", [128, NE], F32)
              trash = sb("trash", [128, 1], F32)
              ev4 = sb("ev4", [128, 4], F32)
              onesb = sb("onesb", [128, 128], BF16)

              MI = cst[:, 1, :]
              MS_ = cst[:, 2, :]
              MST = cst[:, 3, :]
              MP = cst[:, 4, :]
              for j in range(2):
                  S.pool(lambda e, j=j: e.memset(V1[j][:], 1.0), writes=[("V1", j)])
                  S.pool(lambda e, j=j: e.memset(Tst[j][:], 0.0), writes=[("Tst", j, hh) for hh in range(8)])
              S.pool(lambda e: e.memset(ones_f[:], 1.0), writes=["ones_f"])
              S.pool(lambda e: e.memset(onesb[:], 1.0), writes=["onesb"])
              S.pool(lambda e: e.memset(tot[:], 0.0), writes=["tot"])
              S.pool(lambda e: e.iota(ebase[:], pattern=[[CAP, NE]], base=0, channel_multiplier=0, allow_small_or_imprecise_dtypes=True), writes=["ebase"])
              S.pool(lambda e: e.iota(trash[:], pattern=[[0, 1]], base=NSLOT, channel_multiplier=1, allow_small_or_imprecise_dtypes=True), writes=["trash"])
              S.dve(lambda e: e.tensor_copy(out=msk2[:, 0:128], in_=MS_), reads=["cst"], writes=["msk2"])
              S.dve(lambda e: e.tensor_copy(out=msk2[:, 128:256], in_=MI), reads=["cst"], writes=["msk2"])

              def layer_norm_stats(src, key):
                  for h in range(2):
                      S.dve(lambda e, h=h: e.bn_stats(out=st6[:, h, :], in_=src[:, h * 512:(h + 1) * 512]), reads=[key], writes=["st6"])
                  S.dve(lambda e: e.bn_aggr(out=mv[:], in_=st6[:].rearrange("p a b -> p (a b)")), reads=["st6"], writes=["mv"])
                  S.dve(lambda e: e.tensor_scalar(out=rstd[:], in0=mv[:, 1:2], scalar1=LN_EPS, scalar2=None, op0=ALU.add), reads=["mv"], writes=["rstd"])
                  S.act(lambda e: e.activation(out=rstd[:], in_=rstd[:], func=AF.Sqrt), reads=["rstd"], writes=["rstd"])
                  S.dve(lambda e: e.reciprocal(out=rstd[:], in_=rstd[:]), reads=["rstd"], writes=["rstd"])

              for i in range(ntiles):
                  cur, prv = i % 2, (i + 1) % 2
                  cut_tile[0] = i
                  rows = slice(i * 128, (i + 1) * 128)
                  S.dma("sp", lambda e, rows=rows: e.dma_start(out=xt[:], in_=x[rows, :]), writes=["xt"])
                  layer_norm_stats(xt, "xt")
                  S.dve(lambda e: e.tensor_scalar(out=t1k[:], in0=xt[:], scalar1=mv[:, 0:1], scalar2=rstd[:], op0=ALU.subtract, op1=ALU.mult), reads=["xt", "mv", "rstd"], writes=["t1k"])
                  S.dve(lambda e: e.tensor_tensor(out=t1k[:], in0=t1k[:], in1=modb[:, 1, :], op=ALU.mult), reads=["t1k", "modb"], writes=["t1k"])
                  S.dve(lambda e: e.tensor_tensor(out=hb[:], in0=t1k[:], in1=modb[:, 0, :], op=ALU.add), reads=["t1k", "modb"], writes=["hb"])
                  cut("A_h", t1k[:], "t1k")
                  for k in range(8):
                      S.pe(lambda e, k=k: e.transpose(out=pTb[:, k * 128:(k + 1) * 128], in_=hb[:, k * 128:(k + 1) * 128], identity=ident_b), reads=["hb", "cstb"], writes=["pTb"])
                  S.act(lambda e: e.copy(out=hT[:].rearrange("p a b -> p (a b)"), in_=pTb[:]), reads=["pTb"], writes=["hT"])
                  cut("A_hT", t1k[:], "hT")
                  groups = [(0, 512), (512, 1024), (1024, 1536), (1536, 2048), (2048, 2464)]
                  for g, (c0, c1) in enumerate(groups):
                      pp = pA if g % 2 == 0 else pB
                      n = c1 - c0
                      for k in range(8):
                          S.pe(lambda e, k=k, pp=pp, c0=c0, c1=c1, n=n: e.matmul(pp[:, 0:n], lhsT=hT[:, k, :], rhs=w_in_bf[:, k, c0:c1], start=(k == 0), stop=(k == 7)),
                               reads=["hT", "w_in_bf"], writes=[pp.name])
                      if g == 0:
                          S.act(lambda e, pp=pp: e.copy(out=qk[:, 0:8, :].rearrange("p a b -> p (a b)"), in_=pp[:]), reads=[pp.name], writes=["qk"])
                          cut("A_g0", qk[:, 0:8, :].rearrange("p a b -> p (a b)"), "qk")
                      elif g < 4:
                          S.act(lambda e, pp=pp, g=g: e.copy(out=rwc[:, (g - 1) * 512:g * 512], in_=pp[:]), reads=[pp.name], writes=["rwc"])
                      else:
                          S.act(lambda e, pp=pp: e.copy(out=g4t[:], in_=pp[:, 0:416]), reads=[pp.name], writes=["g4t"])
                          S.act(lambda e: e.copy(out=rwc[:, 1536:1696], in_=g4t[:, 0:160]), reads=["g4t"], writes=["rwc"])
                          S.act(lambda e: e.copy(out=qk[:, 8:10, :].rearrange("p a b -> p (a b)"), in_=g4t[:, 160:288]), reads=["g4t"], writes=["qk"])
                          S.act(lambda e, cur=cur: e.copy(out=V1[cur][:, 0, 0:64], in_=g4t[:, 288:352]), reads=["g4t"], writes=[("V1", cur)])
                          S.act(lambda e, cur=cur: e.copy(out=V1[cur][:, 1, 0:64], in_=g4t[:, 352:416]), reads=["g4t"], writes=[("V1", cur)])
                      if g >= 1:
                          cut("A_g%d" % g, rwc[:, 0:1024], "rwc")
                  cut("A_proj", rwc[:], "rwc")
                  S.dma("sp", lambda e: e.dma_start(out=rwp[1:128, :], in_=rwc[0:127, :]), reads=["rwc"], writes=["rwp"])
                  S.dma("sp", lambda e, cur=cur: e.dma_start(out=rwp[0:1, :], in_=lastrow[cur:cur + 1, :]), reads=["lastrow%d" % cur], writes=["rwp"])
                  S.dma("sp", lambda e, prv=prv: e.dma_start(out=lastrow[prv:prv + 1, :], in_=rwc[127:128, :]), reads=["rwc"], writes=["lastrow%d" % prv])
                  S.dve(lambda e: e.tensor_tensor(out=rwp[:], in0=rwp[:], in1=rwc[:], op=ALU.subtract), reads=["rwp", "rwc"], writes=["rwp"])
                  S.dve(lambda e: e.tensor_tensor(out=rwp[:], in0=rwp[:], in1=mub[:], op=ALU.mult), reads=["rwp", "mub"], writes=["rwp"])
                  S.dve(lambda e: e.tensor_tensor(out=rwp[:], in0=rwp[:], in1=rwc[:], op=ALU.add), reads=["rwp", "rwc"], writes=["rwp"])
                  cut("A_mix", rwp[:], "rwp")
                  cb = cosT[:, i, :].unsqueeze(1).broadcast_to([128, 10, 8])
                  sbn = sinT[:, i, :].unsqueeze(1).broadcast_to([128, 10, 8])
                  a1, a2 = qk[:, :, 0:8], qk[:, :, 8:16]
                  S.dve(lambda e, cb=cb: e.tensor_tensor(out=rt[:, 0], in0=a1, in1=cb, op=ALU.mult), reads=["qk", "cosT"], writes=["rt0"])
                  S.dve(lambda e, sbn=sbn: e.tensor_tensor(out=rt[:, 1], in0=a2, in1=sbn, op=ALU.mult), reads=["qk", "sinT"], writes=["rt1"])
                  S.dve(lambda e, cb=cb: e.tensor_tensor(out=rt[:, 2], in0=a2, in1=cb, op=ALU.mult), reads=["qk", "cosT"], writes=["rt2"])
                  S.dve(lambda e, sbn=sbn: e.tensor_tensor(out=rt[:, 3], in0=a1, in1=sbn, op=ALU.mult), reads=["qk", "sinT"], writes=["rt3"])
                  S.act(lambda e: e.copy(out=qkb[:, :, 16:64], in_=qk[:, :, 16:64]), reads=["qk"], writes=["qkb"])
                  S.dve(lambda e: e.tensor_tensor(out=qkb[:, :, 0:8], in0=rt[:, 0], in1=rt[:, 1], op=ALU.subtract), reads=["rt0", "rt1"], writes=["qkb"])
                  S.dve(lambda e: e.tensor_tensor(out=qkb[:, :, 8:16], in0=rt[:, 2], in1=rt[:, 3], op=ALU.add), reads=["rt2", "rt3"], writes=["qkb"])
                  for h in range(8):
                      S.pe(lambda e, h=h: e.transpose(out=pTb2[0:64, h * 128:(h + 1) * 128], in_=qkb[:, h, :], identity=ident_b), reads=["qkb", "cstb"], writes=["pTb2"])
                  S.act(lambda e: e.activation(out=qT[:].rearrange("p a b -> p (a b)"), in_=pTb2[0:64, :], func=AF.Copy, scale=0.125), reads=["pTb2"], writes=["qT"])
                  for h in range(2):
                      S.pe(lambda e, h=h: e.transpose(out=pTb[0:64, h * 128:(h + 1) * 128], in_=qkb[:, 8 + h, :], identity=ident_b), reads=["qkb", "cstb"], writes=["pTb"])
                  S.act(lambda e, cur=cur: e.copy(out=kT[cur][:].rearrange("p a b -> p (a b)"), in_=pTb[0:64, 0:256]), reads=["pTb"], writes=[("kT", cur)])
                  for g in range(2):
                      rq = qT[:, 4 * g:4 * g + 4, :]
                      if i > 0:
                          S.pe(lambda e, g=g, rq=rq, prv=prv: e.matmul(pC[:], lhsT=kT[prv][:, g, :], rhs=rq, start=True, stop=True), reads=[("kT", prv), "qT"], writes=["pC"])
                          S.act(lambda e: e.activation(out=Ee[:], in_=pC[:], func=AF.Exp), reads=["pC"], writes=["Ee"])
                          S.dve(lambda e: e.tensor_tensor(out=PTp[:], in0=Ee[:].rearrange("p (a b) -> p a b", a=4), in1=MP.unsqueeze(1).broadcast_to([128, 4, 128]), op=ALU.mult), reads=["Ee", "cst"], writes=["PTp"])
                      S.pe(lambda e, g=g, rq=rq, cur=cur: e.matmul(pD[:], lhsT=kT[cur][:, g, :], rhs=rq, start=True, stop=True), reads=[("kT", cur), "qT"], writes=["pD"])
                      S.act(lambda e: e.activation(out=Ee[:], in_=pD[:], func=AF.Exp), reads=["pD"], writes=["Ee"])
                      S.dve(lambda e: e.tensor_tensor(out=PTc[:], in0=Ee[:].rearrange("p (a b) -> p a b", a=4), in1=MI.unsqueeze(1).broadcast_to([128, 4, 128]), op=ALU.mult), reads=["Ee", "cst"], writes=["PTc"])
                      pO = pE[:, 0:264].rearrange("p (a b) -> p a b", a=4)
                      for h in range(4):
                          if i > 0:
                              S.pe(lambda e, h=h, g=g, prv=prv, pO=pO: e.matmul(pO[:, h, :], lhsT=PTp[:, h, :], rhs=V1[prv][:, g, :], start=True, stop=False), reads=["PTp", ("V1", prv)], writes=["pE"])
                          S.pe(lambda e, h=h, g=g, cur=cur, pO=pO, i=i: e.matmul(pO[:, h, :], lhsT=PTc[:, h, :], rhs=V1[cur][:, g, :], start=(i == 0), stop=True), reads=["PTc", ("V1", cur)], writes=["pE"])
                      S.dve(lambda e, g=g, pO=pO: e.tensor_tensor(out=den[:, 4 * g:4 * g + 4], in0=pO[:, :, 64], in1=esink[:, 4 * g:4 * g + 4], op=ALU.add), reads=["pE", "esink"], writes=["den"])
                      S.dve(lambda e, g=g: e.reciprocal(out=den[:, 4 * g:4 * g + 4], in_=den[:, 4 * g:4 * g + 4]), reads=["den"], writes=["den"])
                      S.dve(lambda e, g=g, pO=pO: e.tensor_tensor(out=mixb[:, 256 * g:256 * g + 256].rearrange("p (a b) -> p a b", a=4), in0=pO[:, :, 0:64],
                                                                   in1=den[:, 4 * g:4 * g + 4].unsqueeze(2).broadcast_to([128, 4, 64]), op=ALU.mult), reads=["pE", "den"], writes=["mixb"])
                  cut("A_attn", mixb[:, 0:512], "mixb")
                  r_ = rwp[:, 0:512]
                  k_ = rwp[:, 512:1024]
                  v_ = rwp[:, 1024:1536]
                  W0, A0, KK, KA, RK, LNW, LNB = [rwcb[:, j, :] for j in range(7)]
                  S.act(lambda e: e.activation(out=lo_in[:, 0:32], in_=rwp[:, 1536:1568], func=AF.Tanh), reads=["rwp"], writes=["lo_in"])
                  S.act(lambda e: e.activation(out=lo_in[:, 64:160], in_=rwp[:, 1600:1696], func=AF.Sigmoid), reads=["rwp"], writes=["lo_in"])
                  S.dve(lambda e: e.tensor_copy(out=lo_in[:, 32:64], in_=rwp[:, 1568:1600]), reads=["rwp"], writes=["lo_in"])
                  for j, (c0, n) in enumerate(((0, 32), (32, 32), (64, 96))):
                      S.pe(lambda e, j=j, c0=c0, n=n: e.transpose(out=pC[0:n, j * 128:(j + 1) * 128], in_=lo_in[:, c0:c0 + n], identity=ident_f), reads=["lo_in", "cst"], writes=["pC"])
                      S.act(lambda e, j=j, n=n: e.copy(out=loT[0:n, j, :], in_=pC[0:n, j * 128:(j + 1) * 128]), reads=["pC"], writes=["loT"])
                  for j, (pp, n) in enumerate(((pA, 32), (pB, 32))):
                      S.pe(lambda e, j=j, pp=pp, n=n: e.matmul(pp[:], lhsT=loT[0:n, j, :], rhs=lor[0:n, j, :], start=True, stop=True), reads=["loT", "lor"], writes=[pp.name])
                  S.dve(lambda e: e.tensor_tensor(out=f1[:], in0=pA[:], in1=W0, op=ALU.add), reads=["pA", "rwcb"], writes=["rwc"])
                  S.act(lambda e: e.activation(out=lw[:], in_=f1[:], func=AF.Sigmoid), reads=["rwc"], writes=["lw"])
                  S.dve(lambda e: e.tensor_scalar(out=lw[:], in0=lw[:], scalar1=-float(np.exp(-0.5)), scalar2=None, op0=ALU.mult), reads=["lw"], writes=["lw"])
                  S.dve(lambda e: e.tensor_tensor(out=f2[:], in0=pB[:], in1=A0, op=ALU.add), reads=["pB", "rwcb"], writes=["rwc"])
                  S.act(lambda e: e.activation(out=av[:], in_=f2[:], func=AF.Sigmoid), reads=["rwc"], writes=["av"])
                  S.pe(lambda e: e.matmul(pA[:], lhsT=MI, rhs=lw[:], start=True, stop=True), reads=["cst", "lw"], writes=["pA"])
                  for h in range(8):
                      S.pe(lambda e, h=h: e.matmul(pB[0:64, h:h + 1], lhsT=lw[:, h * 64:(h + 1) * 64], rhs=ones_f[:], start=True, stop=True), reads=["lw", "ones_f"], writes=["pB"])
                  S.act(lambda e: e.activation(out=dC[:], in_=pB[0:64, 0:8], func=AF.Exp), reads=["pB"], writes=["dC"])
                  S.act(lambda e: e.activation(out=eP[:], in_=pA[:], func=AF.Exp), reads=["pA"], writes=["eP"])
                  S.act(lambda e: e.activation(out=eN[:], in_=pA[:], func=AF.Exp, scale=-1.0), reads=["pA"], writes=["eN"])
                  S.dve(lambda e: e.tensor_tensor(out=f1[:], in0=pA[:], in1=lw[:], op=ALU.subtract), reads=["pA", "lw"], writes=["rwc"])
                  S.act(lambda e: e.activation(out=lw[:], in_=f1[:], func=AF.Exp), reads=["rwc"], writes=["lw"])
                  v3 = lambda ap: ap.rearrange("p (a b) -> p a b", a=8)
                  S.dve(lambda e: e.tensor_tensor(out=f2[:], in0=k_, in1=KK, op=ALU.mult), reads=["rwp", "rwcb"], writes=["rwc"])
                  S.dve(lambda e: e.tensor_tensor(out=f3[:], in0=f2[:], in1=f2[:], op=ALU.mult), reads=["rwc"], writes=["rwc"])
                  S.dve(lambda e: e.tensor_reduce(out=s8[:, 0, :], in_=v3(f3[:]), axis=AX.X, op=ALU.add), reads=["rwc"], writes=["s80"])
                  S.act(lambda e: e.activation(out=s8[:, 0, :], in_=s8[:, 0, :], func=AF.Sqrt), reads=["s80"], writes=["s80"])
                  S.dve(lambda e: e.tensor_scalar(out=s8[:, 0, :], in0=s8[:, 0, :], scalar1=1e-12, scalar2=None, op0=ALU.max), reads=["s80"], writes=["s80"])
                  S.dve(lambda e: e.reciprocal(out=s8[:, 0, :], in_=s8[:, 0, :]), reads=["s80"], writes=["s80"])
                  S.dve(lambda e: e.tensor_tensor(out=v3(f2[:]), in0=v3(f2[:]), in1=s8[:, 0, :].unsqueeze(2).broadcast_to([128, 8, 64]), op=ALU.mult), reads=["rwc", "s80"], writes=["rwc"])
                  S.dve(lambda e: e.scalar_tensor_tensor(out=f3[:], in0=av[:], scalar=-1.0, in1=KA, op0=ALU.add, op1=ALU.mult), reads=["av", "rwcb"], writes=["rwc"])
                  S.dve(lambda e: e.scalar_tensor_tensor(out=kmod[:], in0=f3[:], scalar=1.0, in1=k_, op0=ALU.add, op1=ALU.mult), reads=["rwc", "rwp"], writes=["kmod"])
                  S.dve(lambda e: e.scalar_tensor_tensor(out=At[:], in0=f2[:], scalar=-1.0, in1=lw[:], op0=ALU.mult, op1=ALU.mult), reads=["rwc", "lw"], writes=["At"])
                  S.dve(lambda e: e.tensor_tensor(out=f4[:], in0=f2[:], in1=av[:], op=ALU.mult), reads=["rwc", "av"], writes=["f4"])
                  S.dve(lambda e: e.tensor_tensor(out=Bt[:], in0=f4[:], in1=eN[:], op=ALU.mult), reads=["f4", "eN"], writes=["Bt"])
                  S.dve(lambda e: e.tensor_tensor(out=Kt[:], in0=kmod[:], in1=eN[:], op=ALU.mult), reads=["kmod", "eN"], writes=["Kt"])
                  S.dve(lambda e: e.tensor_tensor(out=Rt[:], in0=r_, in1=eP[:], op=ALU.mult), reads=["rwp", "eP"], writes=["Rt"])
                  S.act(lambda e: e.copy(out=Vb[:], in_=v_), reads=["rwp"], writes=["Vb"])
                  cut("A_prep", kmod[:], "Vb")
                  for (src, skey, dst, dkey, j, pp) in ((At, "At", arT, "arT", 0, pTb), (Rt, "Rt", arT, "arT", 1, pTb2), (Bt, "Bt", bkT, "bkT", 0, pTb), (Kt, "Kt", bkT, "bkT", 1, pTb2)):
                      for h in range(8):
                          S.pe(lambda e, h=h, src=src, pp=pp: e.transpose(out=pp[0:64, h * 128:(h + 1) * 128], in_=src[:, h * 64:(h + 1) * 64], identity=ident_b), reads=[skey, "cstb"], writes=[pp.name])
                      S.act(lambda e, dst=dst, j=j, pp=pp: e.copy(out=dst[:, :, j, :], in_=pp[0:64, :].rearrange("p (a b) -> p a b", a=8)), reads=[pp.name], writes=[dkey])
                  cut("A_tr", kmod[:], "bkT")
                  Told, Tnew = Tst[cur], Tst[prv]

                  def head_gen(h, Told=Told, Tnew=Tnew, cur=cur, prv=prv):
                      p = h % 2
                      bX = pC if p == 0 else pA
                      bN = pD if p == 0 else pB
                      XA, XN, Lp, RHSs, Us = XAs[p], XNs[p], Lps[p], RHSss[p], Uss[p]
                      kXA, kRH, kUs = ("XA", p), ("RHSs", p), ("Us", p)
                      kXN = lambda j: ("XN", p, j)
                      kLp = lambda j: ("Lp", p, j)
                      RU = bN[:, 384:448]
                      TT = bN[0:64, 448:512]
                      arh = arT[:, h].rearrange("p a b -> p (a b)")
                      S.pe(lambda e: e.matmul(bX[:, 0:256], lhsT=bkT[:, h, 0, :], rhs=arh, start=True, stop=True), reads=["bkT", "arT"], writes=[bX.name])
                      S.pe(lambda e: e.matmul(bX[:, 256:512], lhsT=bkT[:, h, 1, :], rhs=arh, start=True, stop=True), reads=["bkT", "arT"], writes=[bX.name])
                      S.pe(lambda e: e.matmul(bN[:, 0:128], lhsT=arT[:, h, 0, :], rhs=bkT[:, h, 0, :], start=True, stop=True), reads=["bkT", "arT"], writes=[bN.name])
                      S.dve(lambda e: e.tensor_tensor(out=XA[:], in0=bX[:].rearrange("p (a b) -> p a b", a=2), in1=msk2[:].unsqueeze(1).broadcast_to([128, 2, 256]), op=ALU.mult), reads=[bX.name, "msk2"], writes=[kXA])
                      S.dve(lambda e: e.tensor_tensor(out=Lp[0][:], in0=bN[:, 0:128], in1=MST, op=ALU.mult), reads=[bN.name, "cst"], writes=[kLp(0)])
                      S.dve(lambda e: e.tensor_copy(out=XN[0][:, 0:128], in_=XA[:, 0, 0:128]), reads=[kXA], writes=[kXN(0)])
                      S.dve(lambda e: e.tensor_tensor(out=XN[0][:, 128:256], in0=XA[:, 0, 0:128], in1=ident_b, op=ALU.add), reads=[kXA, "cstb"], writes=[kXN(0)])
                      yield
                      S.pe(lambda e: e.matmul(bN[:, 0:128], lhsT=Lp[0][:], rhs=XN[0][:, 0:128], start=True, stop=True), reads=[kLp(0), kXN(0)], writes=[bN.name])
                      S.pe(lambda e: e.matmul(bN[:, 256:384], lhsT=XN[0][:, 0:128], rhs=Lp[0][:], start=True, stop=True), reads=[kLp(0), kXN(0)], writes=[bN.name])
                      S.act(lambda e: e.copy(out=XN[1][:, 0:128], in_=bN[:, 0:128]), reads=[bN.name], writes=[kXN(1)])
                      S.act(lambda e: e.copy(out=Lp[1][:], in_=bN[:, 256:384]), reads=[bN.name], writes=[kLp(1)])
                      S.dve(lambda e: e.tensor_copy(out=XN[1][:, 128:256], in_=XN[0][:, 128:256]), reads=[kXN(0)], writes=[kXN(1)])
                      yield
                      c = 1
                      for lvl in range(1, 7):
                          n = 1 - c
                          if lvl < 6:
                              S.pe(lambda e, c=c: e.matmul(bN[:, 0:256], lhsT=Lp[c][:], rhs=XN[c][:], start=True, stop=True), reads=[kLp(c), kXN(c)], writes=[bN.name])
                              S.pe(lambda e, c=c: e.matmul(bN[:, 256:384], lhsT=XN[c][:, 0:128], rhs=Lp[c][:], start=True, stop=True), reads=[kLp(c), kXN(c)], writes=[bN.name])
                              S.act(lambda e, n=n: e.copy(out=XN[n][:, 0:128], in_=bN[:, 0:128]), reads=[bN.name], writes=[kXN(n)])
                              S.act(lambda e, n=n: e.copy(out=Lp[n][:], in_=bN[:, 256:384]), reads=[bN.name], writes=[kLp(n)])
                          else:
                              S.pe(lambda e, c=c: e.matmul(bN[:, 128:256], lhsT=Lp[c][:], rhs=XN[c][:, 128:256], start=True, stop=True), reads=[kLp(c), kXN(c)], writes=[bN.name])
                          S.dve(lambda e, c=c, n=n: e.tensor_tensor(out=XN[n][:, 128:256], in0=bN[:, 128:256], in1=XN[c][:, 128:256], op=ALU.add), reads=[bN.name, kXN(c)], writes=[kXN(n)])
                          c = n
                          yield
                      Nf = XN[c][:, 128:256]
                      S.pe(lambda e: e.matmul(RU, lhsT=arT[:, h, 0, :], rhs=Told[:, h, :], start=True, stop=False), reads=["arT", ("Tst", cur, h)], writes=[bN.name])
                      S.pe(lambda e: e.matmul(RU, lhsT=XA[:, 1, 0:128], rhs=Vb[:, h * 64:(h + 1) * 64], start=False, stop=True), reads=[kXA, "Vb"], writes=[bN.name])
                      S.act(lambda e: e.copy(out=RHSs[:], in_=RU), reads=[bN.name], writes=[kRH])
                      yield
                      S.pe(lambda e: e.matmul(RU, lhsT=Nf, rhs=RHSs[:], start=True, stop=True), reads=[kXN(c), kRH], writes=[bN.name])
                      S.act(lambda e: e.copy(out=Us[:], in_=RU), reads=[bN.name], writes=[kUs])
                      yield
                      S.pe(lambda e: e.matmul(pF[:, h * 64:(h + 1) * 64], lhsT=arT[:, h, 1, :], rhs=Told[:, h, :], start=True, stop=False), reads=["arT", ("Tst", cur, h)], writes=["pF"])
                      S.pe(lambda e: e.matmul(pF[:, h * 64:(h + 1) * 64], lhsT=XA[:, 0, 128:256], rhs=Us[:], start=False, stop=False), reads=[kXA, kUs], writes=["pF"])
                      S.pe(lambda e: e.matmul(pF[:, h * 64:(h + 1) * 64], lhsT=XA[:, 1, 128:256], rhs=Vb[:, h * 64:(h + 1) * 64], start=False, stop=True), reads=[kXA, "Vb"], writes=["pF"])
                      S.pe(lambda e: e.matmul(TT, lhsT=ident_b[0:64, 0:64], rhs=Told[:, h, :], start=True, stop=False), reads=["cstb", ("Tst", cur, h)], writes=[bN.name])
                      S.pe(lambda e: e.matmul(TT, lhsT=Bt[:, h * 64:(h + 1) * 64], rhs=Us[:], start=False, stop=False), reads=["Bt", kUs], writes=[bN.name])
                      S.pe(lambda e: e.matmul(TT, lhsT=Kt[:, h * 64:(h + 1) * 64], rhs=Vb[:, h * 64:(h + 1) * 64], start=False, stop=True), reads=["Kt", "Vb"], writes=[bN.name])
                      S.act(lambda e: e.activation(out=Tnew[:, h, :], in_=TT, func=AF.Copy, scale=dC[:, h:h + 1]), reads=[bN.name, "dC"], writes=[("Tst", prv, h)])
                      yield

                  for hp in range(0, 8, 2):
                      gens = [head_gen(hp), head_gen(hp + 1)]
                      alive = [True, True]
                      while any(alive):
                          for gi, g in enumerate(gens):
                              if alive[gi]:
                                  try:
                                      next(g)
                                  except StopIteration:
                                      alive[gi] = False
                  cut("A_heads", kmod[:], "pF")
                  S.act(lambda e: e.copy(out=f4[:], in_=pF[:]), reads=["pF"], writes=["f4"])
                  S.dve(lambda e: e.tensor_reduce(out=s8[:, 1, :], in_=v3(f4[:]), axis=AX.X, op=ALU.add), reads=["f4"], writes=["s81"])
                  S.dve(lambda e: e.tensor_tensor(out=f3[:], in0=f4[:], in1=f4[:], op=ALU.mult), reads=["f4"], writes=["rwc"])
                  S.dve(lambda e: e.tensor_reduce(out=s8[:, 2, :], in_=v3(f3[:]), axis=AX.X, op=ALU.add), reads=["rwc"], writes=["s82"])
                  S.dve(lambda e: e.tensor_scalar(out=s8[:, 1, :], in0=s8[:, 1, :], scalar1=1.0 / 64, scalar2=None, op0=ALU.mult), reads=["s81"], writes=["s81"])
                  S.dve(lambda e: e.tensor_tensor(out=s8[:, 3, :], in0=s8[:, 1, :], in1=s8[:, 1, :], op=ALU.mult), reads=["s81"], writes=["s83"])
                  S.dve(lambda e: e.scalar_tensor_tensor(out=s8[:, 2, :], in0=s8[:, 2, :], scalar=1.0 / 64, in1=s8[:, 3, :], op0=ALU.mult, op1=ALU.subtract), reads=["s82", "s83"], writes=["s82"])
                  S.dve(lambda e: e.tensor_scalar(out=s8[:, 2, :], in0=s8[:, 2, :], scalar1=GN_EPS, scalar2=None, op0=ALU.add), reads=["s82"], writes=["s82"])
                  S.act(lambda e: e.activation(out=s8[:, 2, :], in_=s8[:, 2, :], func=AF.Sqrt), reads=["s82"], writes=["s82"])
                  S.dve(lambda e: e.reciprocal(out=s8[:, 2, :], in_=s8[:, 2, :]), reads=["s82"], writes=["s82"])
                  S.dve(lambda e: e.tensor_tensor(out=v3(f4[:]), in0=v3(f4[:]), in1=s8[:, 1, :].unsqueeze(2).broadcast_to([128, 8, 64]), op=ALU.subtract), reads=["f4", "s81"], writes=["f4"])
                  S.dve(lambda e: e.tensor_tensor(out=v3(f4[:]), in0=v3(f4[:]), in1=s8[:, 2, :].unsqueeze(2).broadcast_to([128, 8, 64]), op=ALU.mult), reads=["f4", "s82"], writes=["f4"])
                  S.dve(lambda e: e.tensor_tensor(out=f4[:], in0=f4[:], in1=LNW, op=ALU.mult), reads=["f4", "rwcb"], writes=["f4"])
                  S.dve(lambda e: e.tensor_tensor(out=f4[:], in0=f4[:], in1=LNB, op=ALU.add), reads=["f4", "rwcb"], writes=["f4"])
                  S.dve(lambda e: e.tensor_tensor(out=eP[:], in0=r_, in1=kmod[:], op=ALU.mult), reads=["rwp", "kmod"], writes=["eP"])
                  S.dve(lambda e: e.tensor_tensor(out=eP[:], in0=eP[:], in1=RK, op=ALU.mult), reads=["eP", "rwcb"], writes=["eP"])
                  S.dve(lambda e: e.tensor_reduce(out=s8[:, 3, :], in_=v3(eP[:]), axis=AX.X, op=ALU.add), reads=["eP"], writes=["s83"])
                  S.dve(lambda e: e.tensor_tensor(out=v3(eN[:]), in0=v3(v_), in1=s8[:, 3, :].unsqueeze(2).broadcast_to([128, 8, 64]), op=ALU.mult), reads=["rwp", "s83"], writes=["eN"])
                  S.dve(lambda e: e.tensor_tensor(out=f4[:], in0=f4[:], in1=eN[:], op=ALU.add), reads=["f4", "eN"], writes=["f4"])
                  S.pe(lambda e: e.matmul(pA[:], lhsT=loT[0:96, 2, :], rhs=lor[0:96, 2, :], start=True, stop=True), reads=["loT", "lor"], writes=["pA"])
                  S.dve(lambda e: e.tensor_tensor(out=mixb[:, 512:1024], in0=f4[:], in1=pA[:], op=ALU.mult), reads=["f4", "pA"], writes=["mixb"])
                  cut("A_rwkv", mixb[:, 512:1024], "mixb")
                  for k in range(8):
                      S.pe(lambda e, k=k: e.transpose(out=pTb[:, k * 128:(k + 1) * 128], in_=mixb[:, k * 128:(k + 1) * 128], identity=ident_b), reads=["mixb", "cstb"], writes=["pTb"])
                  S.act(lambda e: e.copy(out=hT[:].rearrange("p a b -> p (a b)"), in_=pTb[:]), reads=["pTb"], writes=["hT"])
                  for hf, pp in ((0, pA), (1, pB)):
                      for k in range(8):
                          S.pe(lambda e, k=k, hf=hf, pp=pp: e.matmul(pp[:], lhsT=hT[:, k, :], rhs=w_out_bf[:, k, hf * 512:(hf + 1) * 512], start=(k == 0), stop=(k == 7)), reads=["hT", "w_out_bf"], writes=[pp.name])
                      S.dve(lambda e, hf=hf, pp=pp: e.tensor_tensor(out=t1k[:, hf * 512:(hf + 1) * 512], in0=pp[:], in1=modb[:, 2, hf * 512:(hf + 1) * 512], op=ALU.mult), reads=[pp.name, "modb"], writes=["t1k"])
                  S.dve(lambda e: e.scalar_tensor_tensor(out=t1k[:], in0=xt[:], scalar=float(ALPHA), in1=t1k[:], op0=ALU.mult, op1=ALU.add), reads=["xt", "t1k"], writes=["t1k"])
                  layer_norm_stats(t1k, "t1k")
                  S.dve(lambda e: e.tensor_scalar(out=t1k[:], in0=t1k[:], scalar1=mv[:, 0:1], scalar2=rstd[:], op0=ALU.subtract, op1=ALU.mult), reads=["t1k", "mv", "rstd"], writes=["t1k"])
                  S.dve(lambda e: e.tensor_tensor(out=t1k[:], in0=t1k[:], in1=lnpb[:, 0, :], op=ALU.mult), reads=["t1k", "lnpb"], writes=["t1k"])
                  S.dve(lambda e: e.tensor_tensor(out=t1k[:], in0=t1k[:], in1=lnpb[:, 1, :], op=ALU.add), reads=["t1k", "lnpb"], writes=["t1k"])
                  S.dma("sp", lambda e, rows=rows: e.dma_start(out=x1s[rows, :], in_=t1k[:]), reads=["t1k"], writes=["x1s"])
                  if stage == "A1":
                      S.dma("sp", lambda e, rows=rows: e.dma_start(out=dbg[rows, :], in_=t1k[:]), reads=["t1k"])
                  if _os.environ.get("KSKIP") == "router":
                      continue
                  layer_norm_stats(t1k, "t1k")
                  S.dve(lambda e: e.tensor_scalar(out=t1k[:], in0=t1k[:], scalar1=mv[:, 0:1], scalar2=rstd[:], op0=ALU.subtract, op1=ALU.mult), reads=["t1k", "mv", "rstd"], writes=["t1k"])
                  S.dve(lambda e: e.tensor_tensor(out=t1k[:], in0=t1k[:], in1=modb[:, 4, :], op=ALU.mult), reads=["t1k", "modb"], writes=["t1k"])
                  S.dve(lambda e: e.tensor_tensor(out=t1k[:], in0=t1k[:], in1=modb[:, 3, :], op=ALU.add), reads=["t1k", "modb"], writes=["t1k"])
                  S.act(lambda e: e.copy(out=hb[:], in_=t1k[:]), reads=["t1k"], writes=["hb"])
                  for half in range(2):
                      for k in range(4):
                          kk_ = half * 4 + k
                          S.pe(lambda e, k=k, kk_=kk_: e.transpose(out=pC[:, k * 128:(k + 1) * 128], in_=t1k[:, kk_ * 128:(kk_ + 1) * 128], identity=ident_f), reads=["t1k", "cst"], writes=["pC"])
                      S.act(lambda e, half=half: e.copy(out=h2T[:, half * 4:half * 4 + 4, :].rearrange("p a b -> p (a b)"), in_=pC[:]), reads=["pC"], writes=["xt"])
                  for k in range(8):
                      S.pe(lambda e, k=k: e.matmul(pD[:, 0:NE], lhsT=h2T[:, k, :], rhs=wr_f[:, k, :], start=(k == 0), stop=(k == 7)), reads=["xt", "wr_f"], writes=["pD"])
                  S.dve(lambda e: e.tensor_tensor(out=lg[:], in0=pD[:, 0:NE], in1=brb[:], op=ALU.add), reads=["pD", "brb"], writes=["lg"])
                  S.dve(lambda e: e.max(out=t8[:], in_=lg[:]), reads=["lg"], writes=["t8"])
                  S.dve(lambda e: e.tensor_scalar(out=mskb[:], in0=lg[:], scalar1=t8[:, 3:4], scalar2=None, op0=ALU.is_ge), reads=["lg", "t8"], writes=["mskb"])
                  S.pe(lambda e: e.matmul(pD[:, 64:64 + NE], lhsT=cstb[:, 2, :], rhs=mskb[:], start=True, stop=True), reads=["cstb", "mskb"], writes=["pD"])
                  S.pe(lambda e: e.matmul(pD[:, 128:128 + NE], lhsT=onesb[:], rhs=mskb[:], start=True, stop=True), reads=["onesb", "mskb"], writes=["pD"])
                  S.dve(lambda e: e.tensor_tensor(out=posn[:], in0=pD[:, 64:64 + NE], in1=tot[:], op=ALU.add), reads=["pD", "tot"], writes=["posn"])
                  S.dve(lambda e: e.tensor_tensor(out=tot[:], in0=pD[:, 128:128 + NE], in1=tot[:], op=ALU.add), reads=["pD", "tot"], writes=["tot"])
                  S.dve(lambda e: e.tensor_scalar(out=eqk[:], in0=posn[:], scalar1=float(CAP), scalar2=None, op0=ALU.is_lt), reads=["posn"], writes=["eqk"])
                  S.dve(lambda e: e.tensor_tensor(out=posn[:], in0=posn[:], in1=ebase[:], op=ALU.add), reads=["posn", "ebase"], writes=["posn"])
                  S.dve(lambda e: e.tensor_scalar(out=posn[:], in0=posn[:], scalar1=trash[:, 0:1], scalar2=None, op0=ALU.subtract), reads=["posn", "trash"], writes=["posn"])
                  S.dve(lambda e: e.tensor_tensor(out=posn[:], in0=posn[:], in1=eqk[:], op=ALU.mult), reads=["posn", "eqk"], writes=["posn"])
                  S.dve(lambda e: e.tensor_scalar(out=posn[:], in0=posn[:], scalar1=trash[:, 0:1], scalar2=None, op0=ALU.add), reads=["posn", "trash"], writes=["posn"])
                  for k4 in range(4):
                      S.dve(lambda e, k4=k4: e.tensor_scalar(out=eqk[:], in0=lg[:], scalar1=t8[:, k4:k4 + 1], scalar2=None, op0=ALU.is_equal), reads=["lg", "t8"], writes=["eqk"])
                      S.dve(lambda e: e.tensor_tensor(out=eqk[:], in0=eqk[:], in1=posn[:], op=ALU.mult), reads=["eqk", "posn"], writes=["eqk"])
                      S.dve(lambda e, k4=k4: e.tensor_reduce(out=sl4[:, k4:k4 + 1], in_=eqk[:], axis=AX.X, op=ALU.add), reads=["eqk"], writes=["sl4"])
                  S.dve(lambda e, i=i: e.tensor_copy(out=slot4[:, i, :], in_=sl4[:]), reads=["sl4"], writes=["slot4"])
                  S.dve(lambda e: e.tensor_scalar(out=ev4[:], in0=t8[:, 0:4], scalar1=t8[:, 0:1], scalar2=None, op0=ALU.subtract), reads=["t8"], writes=["ev4"])
                  S.act(lambda e: e.activation(out=ev4[:], in_=ev4[:], func=AF.Exp), reads=["ev4"], writes=["ev4"])
                  S.dve(lambda e: e.tensor_reduce(out=t8[:, 7:8], in_=ev4[:], axis=AX.X, op=ALU.add), reads=["ev4"], writes=["t8"])
                  S.dve(lambda e: e.reciprocal(out=t8[:, 7:8], in_=t8[:, 7:8]), reads=["t8"], writes=["t8"])
                  S.dve(lambda e: e.tensor_scalar(out=ev4[:], in0=ev4[:], scalar1=t8[:, 7:8], scalar2=None, op0=ALU.mult), reads=["ev4", "t8"], writes=["ev4"])
                  S.dve(lambda e: e.tensor_scalar(out=sl4[:], in0=sl4[:], scalar1=float(NSLOT), scalar2=None, op0=ALU.is_lt), reads=["sl4"], writes=["sl4"])
                  S.dve(lambda e, i=i: e.tensor_tensor(out=gate4[:, i, :], in0=ev4[:], in1=sl4[:], op=ALU.mult), reads=["ev4", "sl4"], writes=["gate4"])
                  for k4 in range(4 if stage in ("full", "A_scat", "BC") else 0):
                      S.dma("pool", lambda e, i=i, k4=k4: e.indirect_dma_start(out=Xs[:, :], out_offset=bass.IndirectOffsetOnAxis(ap=slot4[:, i, k4:k4 + 1], axis=0), in_=hb[:], in_offset=None),
                            reads=["hb", "slot4"], writes=["Xs"])
              S.dve(lambda e: e.memset(junk[:, 0:1], 0.0), reads=["slot4", "gate4", "modb", "cst", "cstb"], writes=["junk"])
          S.add("dve", lambda e: e.memset(junk[:, 1:2], 0.0), reads=[], writes=["junk"], barrier=True)

          if stage in ("A1",):
              S.emit()
              return nc

          with contextlib.ExitStack() as st:
              sb = lambda name, shape, d: st.enter_context(nc.sbuf_tensor(name, shape, d))
              Wgu = [sb("Wgu%d" % j, [128, 8, 2 * D], BF16) for j in range(2)]
              Wd = [sb("Wd%d" % j, [128, 8, D], BF16) for j in range(2)]
              bdn = [sb("bdn%d" % j, [1, D], BF16) for j in range(2)]
              bgu = sb("bgu", [128, NE, 16], F32)
              Xe = sb("Xe", [128, 6, D], BF16)
              XT = sb("XT", [128, 8, CAP], BF16)
              aT = sb("aT", [128, 8, CAP], BF16)
              G = sb("G", [128, 384], F32)
              Sg = sb("Sg", [128, 384], F32)
              Uc = sb("Uc", [128, 384], F32)
              Yo = [sb("Yo%d" % j, [128, D], F32) for j in range(2)]
              ones1 = sb("ones1", [1, 128], BF16)
              zt = sb("zt", [128, D], F32)
              S.dma("sp", lambda e: e.dma_start(out=bgu[:], in_=bguT[:, :, :]), writes=["bgu"])
              S.pool(lambda e: e.memset(ones1[:], 1.0), writes=["ones1"])
              S.pool(lambda e: e.memset(zt[:], 0.0), writes=["zt"])
              S.dma("sp", lambda e: e.dma_start(out=Ys[NSLOT:NSLOT + 128, :], in_=zt[:]), reads=["zt"], writes=["Ys"])

              def load_w(ex):
                  j = ex % 2
                  S.dma("pool", lambda e, j=j, ex=ex: e.dma_start(out=bdn[j][:], in_=b_dn[:, ex, :]), writes=[("bdn", j)])
                  gv_ = w_gu[ex].rearrange("(k p) n -> p k n", p=128)
                  dv_ = w_dn[ex].rearrange("(k p) n -> p k n", p=128)
                  for k in range(0, 8, 2):
                      S.dma("pool", lambda e, k=k, j=j, gv_=gv_: e.dma_start(out=Wgu[j][:, k:k + 2, :], in_=gv_[:, k:k + 2, :]), writes=[("Wgu", j)])
                  for k in range(0, 8, 4):
                      S.dma("pool", lambda e, k=k, j=j, dv_=dv_: e.dma_start(out=Wd[j][:, k:k + 4, :], in_=dv_[:, k:k + 4, :]), writes=[("Wd", j)])

              load_w(0)
              for ex in range(nexp):
                  j = ex % 2
                  if ex + 1 < nexp:
                      load_w(ex + 1)
                  S.dma("sp", lambda e, ex=ex: e.dma_start(out=Xe[:], in_=Xs[ex * CAP:(ex + 1) * CAP, :].rearrange("(s p) d -> p s d", p=128)), reads=["Xs"], writes=["Xe"])
                  for s in range(6):
                      pp = pTb if s % 2 == 0 else pTb2
                      for k in range(8):
                          S.pe(lambda e, s=s, k=k, pp=pp: e.transpose(out=pp[:, k * 128:(k + 1) * 128], in_=Xe[:, s, k * 128:(k + 1) * 128], identity=ident_b), reads=["Xe", "cstb"], writes=[pp.name])
                      S.act(lambda e, s=s, pp=pp: e.copy(out=XT[:, :, s * 128:(s + 1) * 128], in_=pp[:].rearrange("p (a b) -> p a b", a=8)), reads=[pp.name], writes=["XT"])
                  for nh in range(2):
                      n0 = nh * 384
                      for fc in range(8):
                          for (pp, col) in ((pA, fc), (pB, 8 + fc)):
                              for k in range(8):
                                  S.pe(lambda e, k=k, pp=pp, col=col, j=j, n0=n0: e.matmul(pp[:, 0:384], lhsT=Wgu[j][:, k, col * 128:(col + 1) * 128], rhs=XT[:, k, n0:n0 + 384], start=(k == 0), stop=(k == 7)),
                                       reads=[("Wgu", j), "XT"], writes=[pp.name])
                          S.dve(lambda e, ex=ex, fc=fc: e.tensor_scalar(out=G[:], in0=pA[:, 0:384], scalar1=bgu[:, ex, fc:fc + 1], scalar2=7.0, op0=ALU.add, op1=ALU.min), reads=["pA", "bgu"], writes=["G"])
                          S.act(lambda e: e.activation(out=Sg[:], in_=G[:], func=AF.Sigmoid, scale=1.702), reads=["G"], writes=["Sg"])
                          S.dve(lambda e, ex=ex, fc=fc: e.tensor_scalar(out=Uc[:], in0=pB[:, 0:384], scalar1=bgu[:, ex, 8 + fc:9 + fc], scalar2=7.0, op0=ALU.add, op1=ALU.min), reads=["pB", "bgu"], writes=["Uc"])
                          S.dve(lambda e: e.tensor_scalar(out=Uc[:], in0=Uc[:], scalar1=-7.0, scalar2=1.0, op0=ALU.max, op1=ALU.add), reads=["Uc"], writes=["Uc"])
                          S.dve(lambda e: e.tensor_tensor(out=G[:], in0=G[:], in1=Sg[:], op=ALU.mult), reads=["G", "Sg"], writes=["G"])
                          S.dve(lambda e, fc=fc, n0=n0: e.tensor_tensor(out=aT[:, fc, n0:n0 + 384], in0=G[:], in1=Uc[:], op=ALU.mult), reads=["G", "Uc"], writes=["aT"])
                  for s in range(6):
                      yo = Yo[s % 2]
                      for hf, pp in ((0, pC), (1, pD)):
                          S.pe(lambda e, hf=hf, pp=pp, j=j: e.matmul(pp[:], lhsT=ones1[:], rhs=bdn[j][:, hf * 512:(hf + 1) * 512], start=True, stop=False), reads=["ones1", ("bdn", j)], writes=[pp.name])
                          for k in range(8):
                              S.pe(lambda e, k=k, hf=hf, pp=pp, s=s, j=j: e.matmul(pp[:], lhsT=aT[:, k, s * 128:(s + 1) * 128], rhs=Wd[j][:, k, hf * 512:(hf + 1) * 512], start=False, stop=(k == 7)),
                                   reads=["aT", ("Wd", j)], writes=[pp.name])
                          S.act(lambda e, hf=hf, pp=pp, yo=yo: e.copy(out=yo[:, hf * 512:(hf + 1) * 512], in_=pp[:]), reads=[pp.name], writes=[("Yo", s % 2)])
                      S.dma("sp", lambda e, ex=ex, s=s, yo=yo: e.dma_start(out=Ys[ex * CAP + s * 128:ex * CAP + (s + 1) * 128, :], in_=yo[:]), reads=[("Yo", s % 2)], writes=["Ys"])
              S.dve(lambda e: e.memset(junk[:, 2:3], 0.0), reads=["slot4", "gate4", "modb"], writes=["junk"])
          S.add("dve", lambda e: e.memset(junk[:, 3:4], 0.0), reads=[], writes=["junk"], barrier=True)

          with contextlib.ExitStack() as st:
              sb = lambda name, shape, d: st.enter_context(nc.sbuf_tensor(name, shape, d))
              Yg = [sb("Yg%d" % j, [128, 4, D], F32) for j in range(2)]
              x1t = [sb("x1t%d" % j, [128, D], F32) for j in range(2)]
              acc = sb("acc", [128, D], F32)
              lnpc = sb("lnpbC", [128, 2, D], F32)
              S.dma("sp", lambda e: e.dma_start(out=lnpc[:], in_=lnp[:, 2:4, :]), writes=["lnpb"])
              st6c = sb("st6c", [128, 2, 6], F32)
              mvc = sb("mvc", [128, 2], F32)
              rsc = sb("rsc", [128, 1], F32)
              for i in range(ntiles):
                  j = i % 2
                  rows = slice(i * 128, (i + 1) * 128)
                  S.dma("sp", lambda e, rows=rows, j=j: e.dma_start(out=x1t[j][:], in_=x1s[rows, :]), reads=["x1s"], writes=[("x1t", j)])
                  for k4 in range(4):
                      S.dma("pool", lambda e, i=i, k4=k4, j=j: e.indirect_dma_start(out=Yg[j][:, k4, :], out_offset=None, in_=Ys[:, :], in_offset=bass.IndirectOffsetOnAxis(ap=slot4[:, i, k4:k4 + 1], axis=0)),
                            reads=["Ys", "slot4"], writes=[("Yg", j, k4)])
                  S.dve(lambda e, i=i, j=j: e.tensor_scalar(out=acc[:], in0=Yg[j][:, 0, :], scalar1=gate4[:, i, 0:1], scalar2=None, op0=ALU.mult), reads=[("Yg", j, 0), "gate4"], writes=["acc"])
                  for k4 in range(1, 4):
                      S.dve(lambda e, i=i, j=j, k4=k4: e.scalar_tensor_tensor(out=acc[:], in0=Yg[j][:, k4, :], scalar=gate4[:, i, k4:k4 + 1], in1=acc[:], op0=ALU.mult, op1=ALU.add), reads=[("Yg", j, k4), "gate4", "acc"], writes=["acc"])
                  S.dve(lambda e: e.tensor_tensor(out=acc[:], in0=acc[:], in1=modb[:, 5, :], op=ALU.mult), reads=["acc", "modb"], writes=["acc"])
                  S.dve(lambda e, j=j: e.scalar_tensor_tensor(out=acc[:], in0=x1t[j][:], scalar=float(ALPHA), in1=acc[:], op0=ALU.mult, op1=ALU.add), reads=[("x1t", j), "acc"], writes=["acc"])
                  for h in range(2):
                      S.dve(lambda e, h=h: e.bn_stats(out=st6c[:, h, :], in_=acc[:, h * 512:(h + 1) * 512]), reads=["acc"], writes=["st6c"])
                  S.dve(lambda e: e.bn_aggr(out=mvc[:], in_=st6c[:].rearrange("p a b -> p (a b)")), reads=["st6c"], writes=["mvc"])
                  S.dve(lambda e: e.tensor_scalar(out=rsc[:], in0=mvc[:, 1:2], scalar1=LN_EPS, scalar2=None, op0=ALU.add), reads=["mvc"], writes=["rsc"])
                  S.act(lambda e: e.activation(out=rsc[:], in_=rsc[:], func=AF.Sqrt), reads=["rsc"], writes=["rsc"])
                  S.dve(lambda e: e.reciprocal(out=rsc[:], in_=rsc[:]), reads=["rsc"], writes=["rsc"])
                  S.dve(lambda e: e.tensor_scalar(out=acc[:], in0=acc[:], scalar1=mvc[:, 0:1], scalar2=rsc[:], op0=ALU.subtract, op1=ALU.mult), reads=["acc", "mvc", "rsc"], writes=["acc"])
                  S.dve(lambda e: e.tensor_tensor(out=acc[:], in0=acc[:], in1=lnpc[:, 0, :], op=ALU.mult), reads=["acc", "lnpb"], writes=["acc"])
                  S.dve(lambda e, j=j: e.tensor_tensor(out=x1t[j][:], in0=acc[:], in1=lnpc[:, 1, :], op=ALU.add), reads=["acc", "lnpb"], writes=[("x1t", j)])
                  S.dma("sp", lambda e, rows=rows, j=j: e.dma_start(out=out[rows, :], in_=x1t[j][:]), reads=[("x1t", j)], writes=["out"])

    except _Cut:
        pass
    S.emit()
    return nc


def _prep_shared(inp):
    f = lambda a: np.ascontiguousarray(np.asarray(a), dtype=np.float32)
    bc = lambda v, n=128: np.ascontiguousarray(np.broadcast_to(np.asarray(v, np.float32).reshape(1, -1), (n, np.asarray(v).size)))
    w_in = f(inp["w_in"][0])
    perm = np.concatenate([np.arange(0, 512), np.arange(768, 2464), np.arange(512, 640), np.arange(640, 768)])
    sh = {}
    sh["w_ada"] = f(inp["w_ada"][0])
    sh["b_ada_b"] = bc(inp["b_ada"][0])
    sh["w_in"] = np.ascontiguousarray(w_in[:, perm])
    sh["mu_b"] = bc(inp["shift_mu"][0])
    rows = [inp["rwkv_w0"][0], inp["rwkv_a0"][0], inp["rwkv_k_k"][0], inp["rwkv_k_a"][0], np.asarray(inp["rwkv_r_k"][0]).reshape(-1), inp["rwkv_ln_w"][0], inp["rwkv_ln_b"][0]]
    sh["rwc_b"] = np.ascontiguousarray(np.stack([bc(r) for r in rows], axis=1))
    lora = np.zeros((96, 3, 512), np.float32)
    lora[0:32, 0] = inp["rwkv_w2"][0]
    lora[0:32, 1] = inp["rwkv_a2"][0]
    lora[0:96, 2] = inp["rwkv_g2"][0]
    sh["lora"] = lora
    sh["sinks_b"] = bc(inp["attn_sinks"][0])
    invf = (500000.0 ** (-np.arange(0, 16, 2, dtype=np.float32) / 16)).astype(np.float32)
    sh["invf_b"] = bc(invf)
    sh["w_out"] = f(inp["w_out"][0])
    sh["lnp"] = np.ascontiguousarray(np.stack([bc(inp[k][0]) for k in ("ln1_g", "ln1_b", "ln2_g", "ln2_b")], axis=1))
    sh["w_router"] = f(inp["w_router"][0])
    sh["b_router_b"] = bc(inp["b_router"][0])
    sh["w_gu"] = f(inp["w_gate_up"][0])
    sh["bguT"] = np.ascontiguousarray(f(inp["b_gate_up"][0]).reshape(NE, 16, 128).transpose(2, 0, 1))
    sh["w_dn"] = f(inp["w_down"][0])
    sh["b_dn"] = f(inp["b_down"][0]).reshape(1, NE, D)
    jj = np.arange(128)[:, None]
    tt = np.arange(128)[None, :]
    sh["consts"] = np.ascontiguousarray(np.stack([np.eye(128), (jj <= tt), (jj < tt), (jj > tt), (jj > tt)], axis=1).astype(np.float32))
    return sh


def _prep_core(inp, b, sh):
    m = dict(sh)
    m["x"] = np.ascontiguousarray(np.asarray(inp["x"][b], np.float32))
    m["posT"] = np.ascontiguousarray(np.asarray(inp["positions"][b], np.int32).reshape(NT, 128).T)
    c = np.asarray(inp["c"][b], np.float32)
    m["cB"] = np.ascontiguousarray(np.broadcast_to(c.reshape(8, 128).T[:, :, None], (128, 8, 128)))
    return m


_NC_CACHE = {}


def kernel(**inputs):
    sh = _prep_shared(inputs)
    in_maps = [_prep_core(inputs, b, sh) for b in range(8)]
    if "full" not in _NC_CACHE:
        _NC_CACHE["full"] = build("full")
    nc = _NC_CACHE["full"]
    res = run_bass_kernel_spmd(nc, in_maps, core_ids=list(range(8)))
    return np.stack([np.asarray(r["out"], np.float32) for r in res.results], axis=0)
```

```python
import contextlib
import os as _os
import numpy as np
import concourse.bass as bass
import concourse.mybir as mybir
from concourse.bass_utils import run_bass_kernel_spmd

F32 = mybir.dt.float32
BF16 = mybir.dt.bfloat16
I32 = mybir.dt.int32
U32 = mybir.dt.uint32
AF = mybir.ActivationFunctionType
ALU = mybir.AluOpType
AX = mybir.AxisListType

COMPUTE = ("pe", "act", "dve", "pool")
SEG = 8192
SAME_ENG_INORDER = ("pe",)
SAME_ENG_DRAIN = ()
SAME_ENG_HZ = ("act", "dve")
HZ_SMALL = 256
BUBBLE = False
NPOOL = {"pe": 24, "act": 24, "dve": 24, "pool": 4}
BUBBLE_DIST = 2
HZ_METHODS = ("tensor_reduce", "bn_stats", "bn_aggr", "max", "reciprocal")


class _Rec:
    def __init__(self):
        self.calls = []

    def __getattr__(self, name):
        def f(*a, **k):
            self.calls.append((name, a, k))
            return self
        return f


def _free_size(ap):
    try:
        sh = list(ap.shape)
        n = 1
        for x in sh[1:]:
            n *= int(x)
        return n
    except Exception:
        return 0


class _Cut(Exception):
    pass


class Sched:
    def __init__(self, nc, kdma=None):
        self.nc = nc
        self.ops = []
        self.last_w = {}
        self.rd_eng = {}
        self.rd_dma = {}
        self.kdma = kdma or {"sp": 16, "pool": 8, "act": 4}
        self.relay_of = {}
        self.relay_fn = None

    def add(self, eng, fn, reads=(), writes=(), dma=False, barrier=False):
        i = len(self.ops)
        reads = list(reads)
        writes = list(writes)
        if barrier:
            writes.append("PHASE")
        else:
            reads.append("PHASE")
        deps = set()
        for r in reads:
            if r in self.last_w:
                deps.add(self.last_w[r])
        for w in writes:
            if w in self.last_w:
                deps.add(self.last_w[w])
            for d in self.rd_eng.get(w, {}).values():
                deps.add(d)
            for d in self.rd_dma.get(w, ()):
                deps.add(d)
        if getattr(self, "relay_fn", None) is not None and not dma:
            nd = set()
            for d in deps:
                od = self.ops[d]
                if (not od["dma"]) and {eng, od["eng"]} in ({"pe", "dve"}, {"pe", "pool"}):
                    if d not in self.relay_of:
                        self.relay_of[d] = len(self.ops)
                        self.ops.append(dict(eng="act", fn=self.relay_fn, deps=[d], dma=False))
                    nd.add(self.relay_of[d])
                else:
                    nd.add(d)
            deps = nd
            i = len(self.ops)
        for w in writes:
            self.last_w[w] = i
            self.rd_eng[w] = {}
            self.rd_dma[w] = []
        ws = set(writes)
        for r in reads:
            if r in ws:
                continue
            if dma:
                self.rd_dma.setdefault(r, []).append(i)
            else:
                self.rd_eng.setdefault(r, {})[eng] = i
        self.ops.append(dict(eng=eng, fn=fn, deps=sorted(deps), dma=dma))
        return i

    def pe(self, fn, reads=(), writes=()):
        return self.add("pe", fn, reads, writes)

    def act(self, fn, reads=(), writes=()):
        return self.add("act", fn, reads, writes)

    def dve(self, fn, reads=(), writes=()):
        return self.add("dve", fn, reads, writes)

    def pool(self, fn, reads=(), writes=()):
        return self.add("pool", fn, reads, writes)

    def dma(self, q, fn, reads=(), writes=()):
        return self.add(q, fn, reads, writes, dma=True)

    def emit(self):
        nc = self.nc
        ops = self.ops
        n = len(ops)
        dcount = {q: [0] * k for q, k in self.kdma.items()}
        dnext = {q: 0 for q in self.kdma}
        tok = [None] * n
        prev_same = [None] * n
        last_on = {}
        order = [0] * n
        ecnt = {}
        for i, o in enumerate(ops):
            e = o["eng"]
            ecnt[e] = ecnt.get(e, 0) + 1
            order[i] = ecnt[e]
            if o["dma"]:
                q = e
                s_ = dnext[q] % self.kdma[q]
                dnext[q] += 1
                dcount[q][s_] += 1
                key = ("d", q, s_)
                tok[i] = (key, 16 * dcount[q][s_])
                prev_same[i] = last_on.get(key)
                last_on[key] = i
        per_eng = {}
        for i, o in enumerate(ops):
            per_eng.setdefault(o["eng"], []).append(i)

        def dep_list(i):
            o = ops[i]
            deps = list(o["deps"])
            if o["dma"] and prev_same[i] is not None:
                deps.append(prev_same[i])
            return deps

        hz = [True] * n
        for i, o in enumerate(ops):
            if o["dma"] or o["eng"] not in SAME_ENG_HZ:
                continue
            r = _Rec()
            try:
                o["fn"](r)
                name, a, k = r.calls[0]
                out = k.get("out", k.get("ap", a[0] if a else None))
                small = _free_size(out) < HZ_SMALL
                hz[i] = small or (name in HZ_METHODS) or ("accum_out" in k and k["accum_out"] is not None)
            except Exception:
                hz[i] = True
        self.n_hz = sum(1 for i, o in enumerate(ops) if (not o["dma"]) and o["eng"] in SAME_ENG_HZ and hz[i])
        needed = [False] * n
        needed_self = [False] * n
        bubble_before = [False] * n
        plan = {}
        drain_before = [False] * n
        for ename, idxs in per_eng.items():
            waited = {}
            drained_upto = 0
            for i in idxs:
                wl = []
                for d in dep_list(i):
                    od = ops[d]
                    if od["dma"]:
                        key, val = tok[d]
                        if waited.get(key, 0) >= val:
                            continue
                        waited[key] = val
                        wl.append(d)
                    else:
                        if od["eng"] == ename and ename in SAME_ENG_INORDER:
                            continue
                        if od["eng"] == ename and ename in SAME_ENG_HZ and not hz[d]:
                            continue
                        if od["eng"] == ename and ename in SAME_ENG_DRAIN:
                            if order[d] > drained_upto:
                                drain_before[i] = True
                                drained_upto = order[i] - 1
                            continue
                        if od["eng"] == ename and ename in SAME_ENG_HZ and BUBBLE:
                            if order[i] - order[d] <= BUBBLE_DIST:
                                bubble_before[i] = True
                            continue
                        if od["eng"] == ename:
                            key = ("s", od["eng"])
                            if waited.get(key, 0) >= order[d]:
                                continue
                            waited[key] = order[d]
                            needed_self[d] = True
                            wl.append((d, "s"))
                            continue
                        key = ("e", od["eng"])
                        if waited.get(key, 0) >= order[d]:
                            continue
                        waited[key] = order[d]
                        needed[d] = True
                        wl.append(d)
                plan[i] = wl
        ecount = {e: 0 for e in COMPUTE}
        scount = {e: 0 for e in COMPUTE}
        stok = [None] * n
        keys = set()
        for i, o in enumerate(ops):
            if o["dma"]:
                keys.add(tok[i][0])
            elif needed[i] or needed_self[i]:
                e = o["eng"]
                key = ("e", e, ecount[e] % NPOOL[e])
                tok[i] = (key, ecount[e] // NPOOL[e] + 1)
                stok[i] = tok[i]
                needed[i] = True
                ecount[e] += 1
                keys.add(key)
        self.n_incs = dict(ecount)
        with contextlib.ExitStack() as st:
            sems = {}
            for key in sorted(keys, key=str):
                sems[key] = st.enter_context(nc.semaphore("s_" + "_".join(str(x) for x in key)))
            block = st.enter_context(nc.Block())
            engmap = {"pe": "tensor", "act": "scalar", "dve": "vector", "pool": "gpsimd", "sp": "sync"}

            def make(ename, idxs):
                def body(eng):
                    for i in idxs:
                        o = ops[i]
                        if drain_before[i]:
                            eng.drain()
                        if bubble_before[i] and ename in getattr(self, "bubble", {}):
                            self.bubble[ename](eng)
                        for d in plan[i]:
                            if isinstance(d, tuple):
                                key, val = stok[d[0]]
                            else:
                                key, val = tok[d]
                            eng.wait_ge(sems[key], val)
                        inst = o["fn"](eng)
                        if o["dma"]:
                            inst.then_inc(sems[tok[i][0]], 16)
                        else:
                            if needed[i]:
                                inst.then_inc(sems[tok[i][0]], 1)
                            elif needed_self[i]:
                                inst.then_inc(sems[stok[i][0]], 1)
                            if (needed[i] or needed_self[i]) and ename in getattr(self, "spacer", {}):
                                self.spacer[ename](eng)
                    if ename in self.kdma:
                        for s_ in range(self.kdma[ename]):
                            if dcount[ename][s_] > 0:
                                eng.wait_ge(sems[("d", ename, s_)], 16 * dcount[ename][s_])
                return body

            for ename, idxs in per_eng.items():
                getattr(block, engmap[ename])(make(ename, idxs))
        return ecount


NT = 32
D = 1024
DIN = 2464
CAP = 768
NE = 32
NSLOT = NE * CAP
LN_EPS = 1e-5
GN_EPS = 64e-5
ALPHA = 2 ** 0.25
TWO_PI = 2.0 * np.pi


def build(stage="full", ntiles=NT, nexp=NE):
    nc = bass.Bass("TRN2", target_bir_lowering=False)
    dt = lambda name, shape, d, kind="ExternalInput": nc.dram_tensor(name, shape, d, kind=kind).ap()
    x = dt("x", [4096, D], F32)
    posT = dt("posT", [128, NT], I32)
    cB = dt("cB", [128, 8, 128], F32)
    w_ada = dt("w_ada", [D, 6 * D], F32)
    b_ada_b = dt("b_ada_b", [128, 6 * D], F32)
    w_in = dt("w_in", [D, DIN], F32)
    mu_b = dt("mu_b", [128, 1696], F32)
    rwc_b = dt("rwc_b", [128, 7, 512], F32)
    lora = dt("lora", [96, 3, 512], F32)
    sinks_b = dt("sinks_b", [128, 8], F32)
    invf_b = dt("invf_b", [128, 8], F32)
    w_out = dt("w_out", [D, D], F32)
    lnp = dt("lnp", [128, 4, D], F32)
    w_router = dt("w_router", [D, NE], F32)
    b_router_b = dt("b_router_b", [128, NE], F32)
    w_gu = dt("w_gu", [NE, D, 2 * D], F32)
    bguT = dt("bguT", [128, NE, 16], F32)
    w_dn = dt("w_dn", [NE, D, D], F32)
    b_dn = dt("b_dn", [1, NE, D], F32)
    consts = dt("consts", [128, 5, 128], F32)
    out = dt("out", [4096, D], F32, kind="ExternalOutput")
    dbg = dt("dbg", [4096, D], F32, kind="ExternalOutput") if stage != "full" else None
    x1s = dt("x1s", [4096, D], F32, kind="Internal")
    Xs = dt("Xs", [NSLOT + 128, D], BF16, kind="Internal")
    Ys = dt("Ys", [NSLOT + 128, D], F32, kind="Internal")
    lastrow = dt("lastrow", [2, 1696], F32, kind="Internal")

    S = Sched(nc)

    cut_tile = [0]

    def cut(name, ap, key, ncols=None):
        if stage == name and (name == "P" or cut_tile[0] == ntiles - 1):
            if ap.shape[-1] > 1024:
                ap = ap[:, 0:1024]
            ncols = ncols or ap.shape[-1]
            q = "sp" if ap.dtype == F32 else "pool"
            S.dma(q, lambda e: e.dma_start(out=dbg[0:ap.shape[0], 0:ncols], in_=ap), reads=[key])
            raise _Cut()

    try:
      with contextlib.ExitStack() as st0:
          sb0 = lambda name, shape, d: st0.enter_context(nc.sbuf_tensor(name, shape, d))
          ps = lambda name, shape, d: st0.enter_context(nc.psum_tensor(name, shape, d))
          pTb = ps("pTb", [128, 1024], BF16)
          pTb2 = ps("pTb2", [128, 1024], BF16)
          pA = ps("pA", [128, 512], F32)
          pB = ps("pB", [128, 512], F32)
          pC = ps("pC", [128, 512], F32)
          pD = ps("pD", [128, 512], F32)
          pE = ps("pE", [128, 512], F32)
          pF = ps("pF", [128, 512], F32)
          cst = sb0("cst", [128, 5, 128], F32)
          cstb = sb0("cstb", [128, 5, 128], BF16)
          modb = sb0("modb", [128, 6, D], F32)
          slot4 = sb0("slot4", [128, NT, 4], I32)
          gate4 = sb0("gate4", [128, NT, 4], F32)
          junk = sb0("junk", [128, 8], F32)
          S.relay_fn = lambda e: e.activation(out=junk[:, 6:7], in_=junk[:, 6:7], func=AF.Copy)
          if True:
              spc = sb0("spc", [128, 2, 512], F32)
              nsp = 512
              nsp = int(_os.environ.get("KSPACE", "0"))
              nbb = int(_os.environ.get("KBUB", "384"))
              S.spacer = {"dve": lambda e: e.memset(spc[:, 0, 0:nsp], 0.0),
                          "act": lambda e: e.activation(out=spc[:, 1, 0:nsp], in_=spc[:, 1, 0:nsp], func=AF.Copy)}
              if nsp == 0:
                  S.spacer = {}
              S.bubble = {"dve": lambda e: e.memset(spc[:, 0, 0:nbb], 0.0),
                          "act": lambda e: e.activation(out=spc[:, 1, 0:nbb], in_=spc[:, 1, 0:nbb], func=AF.Copy)}
          ident_f = cst[:, 0, :]
          ident_b = cstb[:, 0, :]

          S.dma("sp", lambda e: e.dma_start(out=cst[:], in_=consts[:, :, :]), writes=["cst"])
          S.dve(lambda e: e.tensor_copy(out=cstb[:], in_=cst[:]), reads=["cst"], writes=["cstb"])
          S.dma("sp", lambda e: e.dma_start(out=modb[:].rearrange("p a b -> p (a b)"), in_=b_ada_b[:, :]), writes=["modb"])

          with contextlib.ExitStack() as st:
              sb = lambda name, shape, d: st.enter_context(nc.sbuf_tensor(name, shape, d))
              w_in_bf = sb("w_in_bf", [128, 8, DIN], BF16)
              lnpb = sb("lnpbA", [128, 2, D], F32)
              S.dma("sp", lambda e: e.dma_start(out=lnpb[:], in_=lnp[:, 0:2, :]), writes=["lnpb"])
              w_out_bf = sb("w_out_bf", [128, 8, D], BF16)
              wr_f = sb("wr_f", [128, 8, NE], F32)
              brb = sb("brb", [128, NE], F32)
              mub = sb("mub", [128, 1696], F32)
              rwcb = sb("rwcb", [128, 7, 512], F32)
              lor = sb("lor", [96, 3, 512], F32)
              sinkb = sb("sinkb", [128, 8], F32)
              esink = sb("esink", [128, 8], F32)
              invf = sb("invf", [128, 8], F32)
              cosT = sb("cosT", [128, NT, 8], F32)
              sinT = sb("sinT", [128, NT, 8], F32)
              stp = contextlib.ExitStack()
              sbp = lambda name, shape, d: stp.enter_context(nc.sbuf_tensor(name, shape, d))
              posi = sbp("posi", [128, NT], I32)
              posf = sbp("posf", [128, NT], F32)
              ang = sbp("ang", [128, NT, 8], F32)
              scB = sbp("scB", [128, 8, 128], F32)
              wada = [sbp("wada%d" % j, [128, 8, 512], F32) for j in range(2)]
              w_in_v = w_in.rearrange("(k p) n -> p k n", p=128)
              for (c0, c1) in ((0, 1232), (1232, 2464)):
                  S.dma("pool", lambda e, c0=c0, c1=c1: e.dma_start(out=w_in_bf[:, :, c0:c1], in_=w_in_v[:, :, c0:c1]), writes=["w_in_bf"])
              S.dma("pool", lambda e: e.dma_start(out=w_out_bf[:], in_=w_out.rearrange("(k p) n -> p k n", p=128)), writes=["w_out_bf"])
              S.dma("sp", lambda e: e.dma_start(out=wr_f[:], in_=w_router.rearrange("(k p) n -> p k n", p=128)), writes=["wr_f"])
              S.dma("sp", lambda e: e.dma_start(out=brb[:], in_=b_router_b[:, :]), writes=["brb"])
              S.dma("sp", lambda e: e.dma_start(out=mub[:], in_=mu_b[:, :]), writes=["mub"])
              S.dma("sp", lambda e: e.dma_start(out=rwcb[:], in_=rwc_b[:, :, :]), writes=["rwcb"])
              S.dma("sp", lambda e: e.dma_start(out=lor[:], in_=lora[:, :, :]), writes=["lor"])
              S.dma("sp", lambda e: e.dma_start(out=sinkb[:], in_=sinks_b[:, :]), writes=["sinkb"])
              S.dma("sp", lambda e: e.dma_start(out=invf[:], in_=invf_b[:, :]), writes=["invf"])
              S.dma("sp", lambda e: e.dma_start(out=posi[:], in_=posT[:, :]), writes=["posi"])
              S.dma("sp", lambda e: e.dma_start(out=scB[:], in_=cB[:, :, :]), writes=["scB"])
              S.act(lambda e: e.activation(out=esink[:], in_=sinkb[:], func=AF.Exp), reads=["sinkb"], writes=["esink"])
              S.act(lambda e: e.activation(out=scB[:], in_=scB[:], func=AF.Silu), reads=["scB"], writes=["scB"])
              w_ada_v = w_ada.rearrange("(k p) n -> p k n", p=128)
              for j in range(12):
                  wb_ = wada[j % 2]
                  S.dma("sp", lambda e, j=j, wb_=wb_: e.dma_start(out=wb_[:], in_=w_ada_v[:, :, j * 512:(j + 1) * 512]), writes=[("wada", j % 2)])
                  pp = pA if j % 2 == 0 else pB
                  for k in range(8):
                      S.pe(lambda e, k=k, wb_=wb_, pp=pp: e.matmul(pp[:], lhsT=scB[:, k, :], rhs=wb_[:, k, :], start=(k == 0), stop=(k == 7)),
                           reads=["scB", ("wada", j % 2)], writes=[pp.name])
                  mflat = modb[:].rearrange("p a b -> p (a b)")
                  S.dve(lambda e, j=j, pp=pp, mflat=mflat: e.tensor_tensor(out=mflat[:, j * 512:(j + 1) * 512], in0=pp[:], in1=mflat[:, j * 512:(j + 1) * 512], op=ALU.add),
                        reads=[pp.name, "modb"], writes=["modb"])
              for a in (1, 2, 4, 5):
                  S.dve(lambda e, a=a: e.tensor_scalar(out=modb[:, a, :], in0=modb[:, a, :], scalar1=1.0, scalar2=None, op0=ALU.add), reads=["modb"], writes=["modb"])
              S.dve(lambda e: e.tensor_copy(out=posf[:], in_=posi[:]), reads=["posi"], writes=["posf"])
              S.dve(lambda e: e.tensor_tensor(out=ang[:], in0=posf[:].unsqueeze(2).broadcast_to([128, NT, 8]), in1=invf[:].unsqueeze(1).broadcast_to([128, NT, 8]), op=ALU.mult),
                    reads=["posf", "invf"], writes=["ang"])
              angi = sbp("angi", [128, NT, 8], I32)
              angf = sbp("angf", [128, NT, 8], F32)
              SC = float(TWO_PI * (1.0 - 1e-6))
              for (dst, key, off) in ((sinT, "sinT", 0.0), (cosT, "cosT", 0.25)):
                  S.dve(lambda e, dst=dst, off=off: e.tensor_scalar(out=dst[:], in0=ang[:], scalar1=float(1.0 / TWO_PI), scalar2=off, op0=ALU.mult, op1=ALU.add), reads=["ang"], writes=[key])
                  S.dve(lambda e, dst=dst: e.tensor_copy(out=angi[:], in_=dst[:]), reads=[key], writes=["angi"])
                  S.dve(lambda e: e.tensor_copy(out=angf[:], in_=angi[:]), reads=["angi"], writes=["angf"])
                  S.dve(lambda e, dst=dst: e.tensor_tensor(out=dst[:], in0=dst[:], in1=angf[:], op=ALU.subtract), reads=[key, "angf"], writes=[key])
                  S.dve(lambda e, dst=dst: e.tensor_scalar(out=angf[:], in0=dst[:], scalar1=0.5, scalar2=None, op0=ALU.is_gt), reads=[key], writes=["angf"])
                  S.dve(lambda e, dst=dst: e.tensor_tensor(out=dst[:], in0=dst[:], in1=angf[:], op=ALU.subtract), reads=[key, "angf"], writes=[key])
                  S.act(lambda e, dst=dst: e.activation(out=dst[:], in_=dst[:], func=AF.Sin, scale=SC), reads=[key], writes=[key])

              zrow = sbp("zrow", [1, 1696], F32)
              S.pool(lambda e: e.memset(zrow[:], 0.0), writes=["zrow"])
              S.dma("sp", lambda e: e.dma_start(out=lastrow[0:1, :], in_=zrow[:]), reads=["zrow"], writes=["lastrow0"])
              S.dve(lambda e: e.memset(junk[:, 4:5], 0.0), reads=["cosT", "sinT", "modb"], writes=["junk"])
              S.add("dve", lambda e: e.memset(junk[:, 5:6], 0.0), reads=[], writes=["junk"], barrier=True)
              stp.close()
              cut("P", modb[:, 1, :], "modb")
              xt = sb("xt", [128, D], F32)
              g4t = sb("g4t", [128, 416], F32)
              t1k = sb("t1k", [128, D], F32)
              hb = sb("hb", [128, D], BF16)
              hT = sb("hT", [128, 8, 128], BF16)
              st6 = sb("st6", [128, 2, 6], F32)
              mv = sb("mv", [128, 2], F32)
              rstd = sb("rstd", [128, 1], F32)
              qk = sb("qk", [128, 10, 64], F32)
              qkb = sb("qkb", [128, 10, 64], BF16)
              rt = sb("rt", [128, 4, 10, 8], F32)
              qT = sb("qT", [64, 8, 128], BF16)
              kT = [sb("kT%d" % j, [64, 2, 128], BF16) for j in range(2)]
              V1 = [sb("V1%d" % j, [128, 2, 66], BF16) for j in range(2)]
              rwc = sb("rwc", [128, 1696], F32)
              rwp = sb("rwp", [128, 1696], F32)
              Ee = sb("Ee", [128, 512], F32)
              PTp = sb("PTp", [128, 4, 128], BF16)
              PTc = sb("PTc", [128, 4, 128], BF16)
              den = sb("den", [128, 8], F32)
              mixb = sb("mixb", [128, D], BF16)
              lo_in = sb("lo_in", [128, 160], F32)
              loT = sb("loT", [96, 3, 128], F32)
              f1 = rwc[:, 0:512]
              f2 = rwc[:, 512:1024]
              f3 = rwc[:, 1024:1536]
              f4 = sb("f4", [128, 512], F32)
              eP = sb("eP", [128, 512], F32)
              eN = sb("eN", [128, 512], F32)
              lw = sb("lw", [128, 512], F32)
              av = sb("av", [128, 512], F32)
              kmod = sb("kmod", [128, 512], F32)
              s8 = sb("s8", [128, 4, 8], F32)
              At = sb("At", [128, 512], BF16)
              Bt = sb("Bt", [128, 512], BF16)
              Kt = sb("Kt", [128, 512], BF16)
              Rt = sb("Rt", [128, 512], BF16)
              Vb = sb("Vb", [128, 512], BF16)
              arT = sb("arT", [64, 8, 2, 128], BF16)
              bkT = sb("bkT", [64, 8, 2, 128], BF16)
              dC = sb("dC", [64, 8], F32)
              Tst = [sb("Tst%d" % j, [64, 8, 64], BF16) for j in range(2)]
              XAs = [sb("XA%d" % p, [128, 2, 256], BF16) for p in range(2)]
              XNs = [[sb("XN%d_%d" % (p, j), [128, 256], BF16) for j in range(2)] for p in range(2)]
              Lps = [[sb("Lp%d_%d" % (p, j), [128, 128], BF16) for j in range(2)] for p in range(2)]
              RHSss = [sb("RHSs%d" % p, [128, 64], BF16) for p in range(2)]
              Uss = [sb("Us%d" % p, [128, 64], BF16) for p in range(2)]
              msk2 = sb("msk2", [128, 256], F32)
              ones_f = sb("ones_f", [128, 1], F32)
              h2T = xt[:].rearrange("p (a b) -> p a b", a=8)
              lg = sb("lg", [128, NE], F32)
              t8 = sb("t8", [128, 8], F32)
              eqk = sb("eqk", [128, NE], F32)
              posn = sb("posn", [128, NE], F32)
              tot = sb("tot", [128, NE], F32)
              mskb = sb("mskb", [128, NE], BF16)
              sl4 = sb("sl4", [128, 4], F32)
              ebase = sb("ebase", [128, NE], F32)
              trash = sb("trash", [128, 1], F32)
              ev4 = sb("ev4", [128, 4], F32)
              onesb = sb("onesb", [128, 128], BF16)

              MI = cst[:, 1, :]
              MS_ = cst[:, 2, :]
              MST = cst[:, 3, :]
              MP = cst[:, 4, :]
              for j in range(2):
                  S.pool(lambda e, j=j: e.memset(V1[j][:], 1.0), writes=[("V1", j)])
                  S.pool(lambda e, j=j: e.memset(Tst[j][:], 0.0), writes=[("Tst", j, hh) for hh in range(8)])
              S.pool(lambda e: e.memset(ones_f[:], 1.0), writes=["ones_f"])
              S.pool(lambda e: e.memset(onesb[:], 1.0), writes=["onesb"])
              S.pool(lambda e: e.memset(tot[:], 0.0), writes=["tot"])
              S.pool(lambda e: e.iota(ebase[:], pattern=[[CAP, NE]], base=0, channel_multiplier=0, allow_small_or_imprecise_dtypes=True), writes=["ebase"])
              S.pool(lambda e: e.iota(trash[:], pattern=[[0, 1]], base=NSLOT, channel_multiplier=1, allow_small_or_imprecise_dtypes=True), writes=["trash"])
              S.dve(lambda e: e.tensor_copy(out=msk2[:, 0:128], in_=MS_), reads=["cst"], writes=["msk2"])
              S.dve(lambda e: e.tensor_copy(out=msk2[:, 128:256], in_=MI), reads=["cst"], writes=["msk2"])

              def layer_norm_stats(src, key):
                  for h in range(2):
                      S.dve(lambda e, h=h: e.bn_stats(out=st6[:, h, :], in_=src[:, h * 512:(h + 1) * 512]), reads=[key], writes=["st6"])
                  S.dve(lambda e: e.bn_aggr(out=mv[:], in_=st6[:].rearrange("p a b -> p (a b)")), reads=["st6"], writes=["mv"])
                  S.dve(lambda e: e.tensor_scalar(out=rstd[:], in0=mv[:, 1:2], scalar1=LN_EPS, scalar2=None, op0=ALU.add), reads=["mv"], writes=["rstd"])
                  S.act(lambda e: e.activation(out=rstd[:], in_=rstd[:], func=AF.Sqrt), reads=["rstd"], writes=["rstd"])
                  S.dve(lambda e: e.reciprocal(out=rstd[:], in_=rstd[:]), reads=["rstd"], writes=["rstd"])

              for i in range(ntiles):
                  cur, prv = i % 2, (i + 1) % 2
                  cut_tile[0] = i
                  rows = slice(i * 128, (i + 1) * 128)
                  S.dma("sp", lambda e, rows=rows: e.dma_start(out=xt[:], in_=x[rows, :]), writes=["xt"])
                  layer_norm_stats(xt, "xt")
                  S.dve(lambda e: e.tensor_scalar(out=t1k[:], in0=xt[:], scalar1=mv[:, 0:1], scalar2=rstd[:], op0=ALU.subtract, op1=ALU.mult), reads=["xt", "mv", "rstd"], writes=["t1k"])
                  S.dve(lambda e: e.tensor_tensor(out=t1k[:], in0=t1k[:], in1=modb[:, 1, :], op=ALU.mult), reads=["t1k", "modb"], writes=["t1k"])
                  S.dve(lambda e: e.tensor_tensor(out=hb[:], in0=t1k[:], in1=modb[:, 0, :], op=ALU.add), reads=["t1k", "modb"], writes=["hb"])
                  cut("A_h", t1k[:], "t1k")
                  for k in range(8):
                      S.pe(lambda e, k=k: e.transpose(out=pTb[:, k * 128:(k + 1) * 128], in_=hb[:, k * 128:(k + 1) * 128], identity=ident_b), reads=["hb", "cstb"], writes=["pTb"])
                  S.act(lambda e: e.copy(out=hT[:].rearrange("p a b -> p (a b)"), in_=pTb[:]), reads=["pTb"], writes=["hT"])
                  cut("A_hT", t1k[:], "hT")
                  groups = [(0, 512), (512, 1024), (1024, 1536), (1536, 2048), (2048, 2464)]
                  for g, (c0, c1) in enumerate(groups):
                      pp = pA if g % 2 == 0 else pB
                      n = c1 - c0
                      for k in range(8):
                          S.pe(lambda e, k=k, pp=pp, c0=c0, c1=c1, n=n: e.matmul(pp[:, 0:n], lhsT=hT[:, k, :], rhs=w_in_bf[:, k, c0:c1], start=(k == 0), stop=(k == 7)),
                               reads=["hT", "w_in_bf"], writes=[pp.name])
                      if g == 0:
                          S.act(lambda e, pp=pp: e.copy(out=qk[:, 0:8, :].rearrange("p a b -> p (a b)"), in_=pp[:]), reads=[pp.name], writes=["qk"])
                          cut("A_g0", qk[:, 0:8, :].rearrange("p a b -> p (a b)"), "qk")
                      elif g < 4:
                          S.act(lambda e, pp=pp, g=g: e.copy(out=rwc[:, (g - 1) * 512:g * 512], in_=pp[:]), reads=[pp.name], writes=["rwc"])
                      else:
                          S.act(lambda e, pp=pp: e.copy(out=g4t[:], in_=pp[:, 0:416]), reads=[pp.name], writes=["g4t"])
                          S.act(lambda e: e.copy(out=rwc[:, 1536:1696], in_=g4t[:, 0:160]), reads=["g4t"], writes=["rwc"])
                          S.act(lambda e: e.copy(out=qk[:, 8:10, :].rearrange("p a b -> p (a b)"), in_=g4t[:, 160:288]), reads=["g4t"], writes=["qk"])
                          S.act(lambda e, cur=cur: e.copy(out=V1[cur][:, 0, 0:64], in_=g4t[:, 288:352]), reads=["g4t"], writes=[("V1", cur)])
                          S.act(lambda e, cur=cur: e.copy(out=V1[cur][:, 1, 0:64], in_=g4t[:, 352:416]), reads=["g4t"], writes=[("V1", cur)])
                      if g >= 1:
                          cut("A_g%d" % g, rwc[:, 0:1024], "rwc")
                  cut("A_proj", rwc[:], "rwc")
                  S.dma("sp", lambda e: e.dma_start(out=rwp[1:128, :], in_=rwc[0:127, :]), reads=["rwc"], writes=["rwp"])
                  S.dma("sp", lambda e, cur=cur: e.dma_start(out=rwp[0:1, :], in_=lastrow[cur:cur + 1, :]), reads=["lastrow%d" % cur], writes=["rwp"])
                  S.dma("sp", lambda e, prv=prv: e.dma_start(out=lastrow[prv:prv + 1, :], in_=rwc[127:128, :]), reads=["rwc"], writes=["lastrow%d" % prv])
                  S.dve(lambda e: e.tensor_tensor(out=rwp[:], in0=rwp[:], in1=rwc[:], op=ALU.subtract), reads=["rwp", "rwc"], writes=["rwp"])
                  S.dve(lambda e: e.tensor_tensor(out=rwp[:], in0=rwp[:], in1=mub[:], op=ALU.mult), reads=["rwp", "mub"], writes=["rwp"])
                  S.dve(lambda e: e.tensor_tensor(out=rwp[:], in0=rwp[:], in1=rwc[:], op=ALU.add), reads=["rwp", "rwc"], writes=["rwp"])
                  cut("A_mix", rwp[:], "rwp")
                  cb = cosT[:, i, :].unsqueeze(1).broadcast_to([128, 10, 8])
                  sbn = sinT[:, i, :].unsqueeze(1).broadcast_to([128, 10, 8])
                  a1, a2 = qk[:, :, 0:8], qk[:, :, 8:16]
                  S.dve(lambda e, cb=cb: e.tensor_tensor(out=rt[:, 0], in0=a1, in1=cb, op=ALU.mult), reads=["qk", "cosT"], writes=["rt0"])
                  S.dve(lambda e, sbn=sbn: e.tensor_tensor(out=rt[:, 1], in0=a2, in1=sbn, op=ALU.mult), reads=["qk", "sinT"], writes=["rt1"])
                  S.dve(lambda e, cb=cb: e.tensor_tensor(out=rt[:, 2], in0=a2, in1=cb, op=ALU.mult), reads=["qk", "cosT"], writes=["rt2"])
                  S.dve(lambda e, sbn=sbn: e.tensor_tensor(out=rt[:, 3], in0=a1, in1=sbn, op=ALU.mult), reads=["qk", "sinT"], writes=["rt3"])
                  S.act(lambda e: e.copy(out=qkb[:, :, 16:64], in_=qk[:, :, 16:64]), reads=["qk"], writes=["qkb"])
                  S.dve(lambda e: e.tensor_tensor(out=qkb[:, :, 0:8], in0=rt[:, 0], in1=rt[:, 1], op=ALU.subtract), reads=["rt0", "rt1"], writes=["qkb"])
                  S.dve(lambda e: e.tensor_tensor(out=qkb[:, :, 8:16], in0=rt[:, 2], in1=rt[:, 3], op=ALU.add), reads=["rt2", "rt3"], writes=["qkb"])
                  for h in range(8):
                      S.pe(lambda e, h=h: e.transpose(out=pTb2[0:64, h * 128:(h + 1) * 128], in_=qkb[:, h, :], identity=ident_b), reads=["qkb", "cstb"], writes=["pTb2"])
                  S.act(lambda e: e.activation(out=qT[:].rearrange("p a b -> p (a b)"), in_=pTb2[0:64, :], func=AF.Copy, scale=0.125), reads=["pTb2"], writes=["qT"])
                  for h in range(2):
                      S.pe(lambda e, h=h: e.transpose(out=pTb[0:64, h * 128:(h + 1) * 128], in_=qkb[:, 8 + h, :], identity=ident_b), reads=["qkb", "cstb"], writes=["pTb"])
                  S.act(lambda e, cur=cur: e.copy(out=kT[cur][:].rearrange("p a b -> p (a b)"), in_=pTb[0:64, 0:256]), reads=["pTb"], writes=[("kT", cur)])
                  for g in range(2):
                      rq = qT[:, 4 * g:4 * g + 4, :]
                      if i > 0:
                          S.pe(lambda e, g=g, rq=rq, prv=prv: e.matmul(pC[:], lhsT=kT[prv][:, g, :], rhs=rq, start=True, stop=True), reads=[("kT", prv), "qT"], writes=["pC"])
                          S.act(lambda e: e.activation(out=Ee[:], in_=pC[:], func=AF.Exp), reads=["pC"], writes=["Ee"])
                          S.dve(lambda e: e.tensor_tensor(out=PTp[:], in0=Ee[:].rearrange("p (a b) -> p a b", a=4), in1=MP.unsqueeze(1).broadcast_to([128, 4, 128]), op=ALU.mult), reads=["Ee", "cst"], writes=["PTp"])
                      S.pe(lambda e, g=g, rq=rq, cur=cur: e.matmul(pD[:], lhsT=kT[cur][:, g, :], rhs=rq, start=True, stop=True), reads=[("kT", cur), "qT"], writes=["pD"])
                      S.act(lambda e: e.activation(out=Ee[:], in_=pD[:], func=AF.Exp), reads=["pD"], writes=["Ee"])
                      S.dve(lambda e: e.tensor_tensor(out=PTc[:], in0=Ee[:].rearrange("p (a b) -> p a b", a=4), in1=MI.unsqueeze(1).broadcast_to([128, 4, 128]), op=ALU.mult), reads=["Ee", "cst"], writes=["PTc"])
                      pO = pE[:, 0:264].rearrange("p (a b) -> p a b", a=4)
                      for h in range(4):
                          if i > 0:
                              S.pe(lambda e, h=h, g=g, prv=prv, pO=pO: e.matmul(pO[:, h, :], lhsT=PTp[:, h, :], rhs=V1[prv][:, g, :], start=True, stop=False), reads=["PTp", ("V1", prv)], writes=["pE"])
                          S.pe(lambda e, h=h, g=g, cur=cur, pO=pO, i=i: e.matmul(pO[:, h, :], lhsT=PTc[:, h, :], rhs=V1[cur][:, g, :], start=(i == 0), stop=True), reads=["PTc", ("V1", cur)], writes=["pE"])
                      S.dve(lambda e, g=g, pO=pO: e.tensor_tensor(out=den[:, 4 * g:4 * g + 4], in0=pO[:, :, 64], in1=esink[:, 4 * g:4 * g + 4], op=ALU.add), reads=["pE", "esink"], writes=["den"])
                      S.dve(lambda e, g=g: e.reciprocal(out=den[:, 4 * g:4 * g + 4], in_=den[:, 4 * g:4 * g + 4]), reads=["den"], writes=["den"])
                      S.dve(lambda e, g=g, pO=pO: e.tensor_tensor(out=mixb[:, 256 * g:256 * g + 256].rearrange("p (a b) -> p a b", a=4), in0=pO[:, :, 0:64],
                                                                   in1=den[:, 4 * g:4 * g + 4].unsqueeze(2).broadcast_to([128, 4, 64]), op=ALU.mult), reads=["pE", "den"], writes=["mixb"])
                  cut("A_attn", mixb[:, 0:512], "mixb")
                  r_ = rwp[:, 0:512]
                  k_ = rwp[:, 512:1024]
                  v_ = rwp[:, 1024:1536]
                  W0, A0, KK, KA, RK, LNW, LNB = [rwcb[:, j, :] for j in range(7)]
                  S.act(lambda e: e.activation(out=lo_in[:, 0:32], in_=rwp[:, 1536:1568], func=AF.Tanh), reads=["rwp"], writes=["lo_in"])
                  S.act(lambda e: e.activation(out=lo_in[:, 64:160], in_=rwp[:, 1600:1696], func=AF.Sigmoid), reads=["rwp"], writes=["lo_in"])
                  S.dve(lambda e: e.tensor_copy(out=lo_in[:, 32:64], in_=rwp[:, 1568:1600]), reads=["rwp"], writes=["lo_in"])
                  for j, (c0, n) in enumerate(((0, 32), (32, 32), (64, 96))):
                      S.pe(lambda e, j=j, c0=c0, n=n: e.transpose(out=pC[0:n, j * 128:(j + 1) * 128], in_=lo_in[:, c0:c0 + n], identity=ident_f), reads=["lo_in", "cst"], writes=["pC"])
                      S.act(lambda e, j=j, n=n: e.copy(out=loT[0:n, j, :], in_=pC[0:n, j * 128:(j + 1) * 128]), reads=["pC"], writes=["loT"])
                  for j, (pp, n) in enumerate(((pA, 32), (pB, 32))):
                      S.pe(lambda e, j=j, pp=pp, n=n: e.matmul(pp[:], lhsT=loT[0:n, j, :], rhs=lor[0:n, j, :], start=True, stop=True), reads=["loT", "lor"], writes=[pp.name])
                  S.dve(lambda e: e.tensor_tensor(out=f1[:], in0=pA[:], in1=W0, op=ALU.add), reads=["pA", "rwcb"], writes=["rwc"])
                  S.act(lambda e: e.activation(out=lw[:], in_=f1[:], func=AF.Sigmoid), reads=["rwc"], writes=["lw"])
                  S.dve(lambda e: e.tensor_scalar(out=lw[:], in0=lw[:], scalar1=-float(np.exp(-0.5)), scalar2=None, op0=ALU.mult), reads=["lw"], writes=["lw"])
                  S.dve(lambda e: e.tensor_tensor(out=f2[:], in0=pB[:], in1=A0, op=ALU.add), reads=["pB", "rwcb"], writes=["rwc"])
                  S.act(lambda e: e.activation(out=av[:], in_=f2[:], func=AF.Sigmoid), reads=["rwc"], writes=["av"])
                  S.pe(lambda e: e.matmul(pA[:], lhsT=MI, rhs=lw[:], start=True, stop=True), reads=["cst", "lw"], writes=["pA"])
                  for h in range(8):
                      S.pe(lambda e, h=h: e.matmul(pB[0:64, h:h + 1], lhsT=lw[:, h * 64:(h + 1) * 64], rhs=ones_f[:], start=True, stop=True), reads=["lw", "ones_f"], writes=["pB"])
                  S.act(lambda e: e.activation(out=dC[:], in_=pB[0:64, 0:8], func=AF.Exp), reads=["pB"], writes=["dC"])
                  S.act(lambda e: e.activation(out=eP[:], in_=pA[:], func=AF.Exp), reads=["pA"], writes=["eP"])
                  S.act(lambda e: e.activation(out=eN[:], in_=pA[:], func=AF.Exp, scale=-1.0), reads=["pA"], writes=["eN"])
                  S.dve(lambda e: e.tensor_tensor(out=f1[:], in0=pA[:], in1=lw[:], op=ALU.subtract), reads=["pA", "lw"], writes=["rwc"])
                  S.act(lambda e: e.activation(out=lw[:], in_=f1[:], func=AF.Exp), reads=["rwc"], writes=["lw"])
                  v3 = lambda ap: ap.rearrange("p (a b) -> p a b", a=8)
                  S.dve(lambda e: e.tensor_tensor(out=f2[:], in0=k_, in1=KK, op=ALU.mult), reads=["rwp", "rwcb"], writes=["rwc"])
                  S.dve(lambda e: e.tensor_tensor(out=f3[:], in0=f2[:], in1=f2[:], op=ALU.mult), reads=["rwc"], writes=["rwc"])
                  S.dve(lambda e: e.tensor_reduce(out=s8[:, 0, :], in_=v3(f3[:]), axis=AX.X, op=ALU.add), reads=["rwc"], writes=["s80"])
                  S.act(lambda e: e.activation(out=s8[:, 0, :], in_=s8[:, 0, :], func=AF.Sqrt), reads=["s80"], writes=["s80"])
                  S.dve(lambda e: e.tensor_scalar(out=s8[:, 0, :], in0=s8[:, 0, :], scalar1=1e-12, scalar2=None, op0=ALU.max), reads=["s80"], writes=["s80"])
                  S.dve(lambda e: e.reciprocal(out=s8[:, 0, :], in_=s8[:, 0, :]), reads=["s80"], writes=["s80"])
                  S.dve(lambda e: e.tensor_tensor(out=v3(f2[:]), in0=v3(f2[:]), in1=s8[:, 0, :].unsqueeze(2).broadcast_to([128, 8, 64]), op=ALU.mult), reads=["rwc", "s80"], writes=["rwc"])
                  S.dve(lambda e: e.scalar_tensor_tensor(out=f3[:], in0=av[:], scalar=-1.0, in1=KA, op0=ALU.add, op1=ALU.mult), reads=["av", "rwcb"], writes=["rwc"])
                  S.dve(lambda e: e.scalar_tensor_tensor(out=kmod[:], in0=f3[:], scalar=1.0, in1=k_, op0=ALU.add, op1=ALU.mult), reads=["rwc", "rwp"], writes=["kmod"])
                  S.dve(lambda e: e.scalar_tensor_tensor(out=At[:], in0=f2[:], scalar=-1.0, in1=lw[:], op0=ALU.mult, op1=ALU.mult), reads=["rwc", "lw"], writes=["At"])
                  S.dve(lambda e: e.tensor_tensor(out=f4[:], in0=f2[:], in1=av[:], op=ALU.mult), reads=["rwc", "av"], writes=["f4"])
                  S.dve(lambda e: e.tensor_tensor(out=Bt[:], in0=f4[:], in1=eN[:], op=ALU.mult), reads=["f4", "eN"], writes=["Bt"])
                  S.dve(lambda e: e.tensor_tensor(out=Kt[:], in0=kmod[:], in1=eN[:], op=ALU.mult), reads=["kmod", "eN"], writes=["Kt"])
                  S.dve(lambda e: e.tensor_tensor(out=Rt[:], in0=r_, in1=eP[:], op=ALU.mult), reads=["rwp", "eP"], writes=["Rt"])
                  S.act(lambda e: e.copy(out=Vb[:], in_=v_), reads=["rwp"], writes=["Vb"])
                  cut("A_prep", kmod[:], "Vb")
                  for (src, skey, dst, dkey, j, pp) in ((At, "At", arT, "arT", 0, pTb), (Rt, "Rt", arT, "arT", 1, pTb2), (Bt, "Bt", bkT, "bkT", 0, pTb), (Kt, "Kt", bkT, "bkT", 1, pTb2)):
                      for h in range(8):
                          S.pe(lambda e, h=h, src=src, pp=pp: e.transpose(out=pp[0:64, h * 128:(h + 1) * 128], in_=src[:, h * 64:(h + 1) * 64], identity=ident_b), reads=[skey, "cstb"], writes=[pp.name])
                      S.act(lambda e, dst=dst, j=j, pp=pp: e.copy(out=dst[:, :, j, :], in_=pp[0:64, :].rearrange("p (a b) -> p a b", a=8)), reads=[pp.name], writes=[dkey])
                  cut("A_tr", kmod[:], "bkT")
                  Told, Tnew = Tst[cur], Tst[prv]

                  def head_gen(h, Told=Told, Tnew=Tnew, cur=cur, prv=prv):
                      p = h % 2
                      bX = pC if p == 0 else pA
                      bN = pD if p == 0 else pB
                      XA, XN, Lp, RHSs, Us = XAs[p], XNs[p], Lps[p], RHSss[p], Uss[p]
                      kXA, kRH, kUs = ("XA", p), ("RHSs", p), ("Us", p)
                      kXN = lambda j: ("XN", p, j)
                      kLp = lambda j: ("Lp", p, j)
                      RU = bN[:, 384:448]
                      TT = bN[0:64, 448:512]
                      arh = arT[:, h].rearrange("p a b -> p (a b)")
                      S.pe(lambda e: e.matmul(bX[:, 0:256], lhsT=bkT[:, h, 0, :], rhs=arh, start=True, stop=True), reads=["bkT", "arT"], writes=[bX.name])
                      S.pe(lambda e: e.matmul(bX[:, 256:512], lhsT=bkT[:, h, 1, :], rhs=arh, start=True, stop=True), reads=["bkT", "arT"], writes=[bX.name])
                      S.pe(lambda e: e.matmul(bN[:, 0:128], lhsT=arT[:, h, 0, :], rhs=bkT[:, h, 0, :], start=True, stop=True), reads=["bkT", "arT"], writes=[bN.name])
                      S.dve(lambda e: e.tensor_tensor(out=XA[:], in0=bX[:].rearrange("p (a b) -> p a b", a=2), in1=msk2[:].unsqueeze(1).broadcast_to([128, 2, 256]), op=ALU.mult), reads=[bX.name, "msk2"], writes=[kXA])
                      S.dve(lambda e: e.tensor_tensor(out=Lp[0][:], in0=bN[:, 0:128], in1=MST, op=ALU.mult), reads=[bN.name, "cst"], writes=[kLp(0)])
                      S.dve(lambda e: e.tensor_copy(out=XN[0][:, 0:128], in_=XA[:, 0, 0:128]), reads=[kXA], writes=[kXN(0)])
                      S.dve(lambda e: e.tensor_tensor(out=XN[0][:, 128:256], in0=XA[:, 0, 0:128], in1=ident_b, op=ALU.add), reads=[kXA, "cstb"], writes=[kXN(0)])
                      yield
                      S.pe(lambda e: e.matmul(bN[:, 0:128], lhsT=Lp[0][:], rhs=XN[0][:, 0:128], start=True, stop=True), reads=[kLp(0), kXN(0)], writes=[bN.name])
                      S.pe(lambda e: e.matmul(bN[:, 128:256], lhsT=ident_b, rhs=XN[0][:, 128:256], start=True, stop=True), reads=["cstb", kXN(0)], writes=[bN.name])
                      S.pe(lambda e: e.matmul(bN[:, 256:384], lhsT=XN[0][:, 0:128], rhs=Lp[0][:], start=True, stop=True), reads=[kLp(0), kXN(0)], writes=[bN.name])
                      S.act(lambda e: e.copy(out=XN[1][:], in_=bN[:, 0:256]), reads=[bN.name], writes=[kXN(1)])
                      S.act(lambda e: e.copy(out=Lp[1][:], in_=bN[:, 256:384]), reads=[bN.name], writes=[kLp(1)])
                      yield
                      c = 1
                      for lvl in range(1, 7):
                          n = 1 - c
                          if lvl < 6:
                              S.pe(lambda e, c=c: e.matmul(bN[:, 0:256], lhsT=Lp[c][:], rhs=XN[c][:], start=True, stop=False), reads=[kLp(c), kXN(c)], writes=[bN.name])
                              S.pe(lambda e, c=c: e.matmul(bN[:, 128:256], lhsT=ident_b, rhs=XN[c][:, 128:256], start=False, stop=True), reads=["cstb", kXN(c)], writes=[bN.name])
                              S.pe(lambda e, c=c: e.matmul(bN[:, 256:384], lhsT=XN[c][:, 0:128], rhs=Lp[c][:], start=True, stop=True), reads=[kLp(c), kXN(c)], writes=[bN.name])
                              S.act(lambda e, n=n: e.copy(out=XN[n][:], in_=bN[:, 0:256]), reads=[bN.name], writes=[kXN(n)])
                              S.act(lambda e, n=n: e.copy(out=Lp[n][:], in_=bN[:, 256:384]), reads=[bN.name], writes=[kLp(n)])
                          else:
                              S.pe(lambda e, c=c: e.matmul(bN[:, 128:256], lhsT=Lp[c][:], rhs=XN[c][:, 128:256], start=True, stop=False), reads=[kLp(c), kXN(c)], writes=[bN.name])
                              S.pe(lambda e, c=c: e.matmul(bN[:, 128:256], lhsT=ident_b, rhs=XN[c][:, 128:256], start=False, stop=True), reads=["cstb", kXN(c)], writes=[bN.name])
                              S.act(lambda e, n=n: e.copy(out=XN[n][:, 128:256], in_=bN[:, 128:256]), reads=[bN.name], writes=[kXN(n)])
                          c = n
                          yield
                      Nf = XN[c][:, 128:256]
                      S.pe(lambda e: e.matmul(RU, lhsT=arT[:, h, 0, :], rhs=Told[:, h, :], start=True, stop=False), reads=["arT", ("Tst", cur, h)], writes=[bN.name])
                      S.pe(lambda e: e.matmul(RU, lhsT=XA[:, 1, 0:128], rhs=Vb[:, h * 64:(h + 1) * 64], start=False, stop=True), reads=[kXA, "Vb"], writes=[bN.name])
                      S.act(lambda e: e.copy(out=RHSs[:], in_=RU), reads=[bN.name], writes=[kRH])
                      yield
                      S.pe(lambda e: e.matmul(RU, lhsT=Nf, rhs=RHSs[:], start=True, stop=True), reads=[kXN(c), kRH], writes=[bN.name])
                      S.act(lambda e: e.copy(out=Us[:], in_=RU), reads=[bN.name], writes=[kUs])
                      yield
                      S.pe(lambda e: e.matmul(pF[:, h * 64:(h + 1) * 64], lhsT=arT[:, h, 1, :], rhs=Told[:, h, :], start=True, stop=False), reads=["arT", ("Tst", cur, h)], writes=["pF"])
                      S.pe(lambda e: e.matmul(pF[:, h * 64:(h + 1) * 64], lhsT=XA[:, 0, 128:256], rhs=Us[:], start=False, stop=False), reads=[kXA, kUs], writes=["pF"])
                      S.pe(lambda e: e.matmul(pF[:, h * 64:(h + 1) * 64], lhsT=XA[:, 1, 128:256], rhs=Vb[:, h * 64:(h + 1) * 64], start=False, stop=True), reads=[kXA, "Vb"], writes=["pF"])
                      S.pe(lambda e: e.matmul(TT, lhsT=ident_b[0:64, 0:64], rhs=Told[:, h, :], start=True, stop=False), reads=["cstb", ("Tst", cur, h)], writes=[bN.name])
                      S.pe(lambda e: e.matmul(TT, lhsT=Bt[:, h * 64:(h + 1) * 64], rhs=Us[:], start=False, stop=False), reads=["Bt", kUs], writes=[bN.name])
                      S.pe(lambda e: e.matmul(TT, lhsT=Kt[:, h * 64:(h + 1) * 64], rhs=Vb[:, h * 64:(h + 1) * 64], start=False, stop=True), reads=["Kt", "Vb"], writes=[bN.name])
                      S.act(lambda e: e.activation(out=Tnew[:, h, :], in_=TT, func=AF.Copy, scale=dC[:, h:h + 1]), reads=[bN.name, "dC"], writes=[("Tst", prv, h)])
                      yield

                  for hp in range(0, 8, 2):
                      gens = [head_gen(hp), head_gen(hp + 1)]
                      alive = [True, True]
                      while any(alive):
                          for gi, g in enumerate(gens):
                              if alive[gi]:
                                  try:
                                      next(g)
                                  except StopIteration:
                                      alive[gi] = False
                  cut("A_heads", kmod[:], "pF")
                  S.act(lambda e: e.copy(out=f4[:], in_=pF[:]), reads=["pF"], writes=["f4"])
                  S.dve(lambda e: e.tensor_reduce(out=s8[:, 1, :], in_=v3(f4[:]), axis=AX.X, op=ALU.add), reads=["f4"], writes=["s81"])
                  S.dve(lambda e: e.tensor_tensor(out=f3[:], in0=f4[:], in1=f4[:], op=ALU.mult), reads=["f4"], writes=["rwc"])
                  S.dve(lambda e: e.tensor_reduce(out=s8[:, 2, :], in_=v3(f3[:]), axis=AX.X, op=ALU.add), reads=["rwc"], writes=["s82"])
                  S.dve(lambda e: e.tensor_scalar(out=s8[:, 1, :], in0=s8[:, 1, :], scalar1=1.0 / 64, scalar2=None, op0=ALU.mult), reads=["s81"], writes=["s81"])
                  S.dve(lambda e: e.tensor_tensor(out=s8[:, 3, :], in0=s8[:, 1, :], in1=s8[:, 1, :], op=ALU.mult), reads=["s81"], writes=["s83"])
                  S.dve(lambda e: e.scalar_tensor_tensor(out=s8[:, 2, :], in0=s8[:, 2, :], scalar=1.0 / 64, in1=s8[:, 3, :], op0=ALU.mult, op1=ALU.subtract), reads=["s82", "s83"], writes=["s82"])
                  S.dve(lambda e: e.tensor_scalar(out=s8[:, 2, :], in0=s8[:, 2, :], scalar1=GN_EPS, scalar2=None, op0=ALU.add), reads=["s82"], writes=["s82"])
                  S.act(lambda e: e.activation(out=s8[:, 2, :], in_=s8[:, 2, :], func=AF.Sqrt), reads=["s82"], writes=["s82"])
                  S.dve(lambda e: e.reciprocal(out=s8[:, 2, :], in_=s8[:, 2, :]), reads=["s82"], writes=["s82"])
                  S.dve(lambda e: e.tensor_tensor(out=v3(f4[:]), in0=v3(f4[:]), in1=s8[:, 1, :].unsqueeze(2).broadcast_to([128, 8, 64]), op=ALU.subtract), reads=["f4", "s81"], writes=["f4"])
                  S.dve(lambda e: e.tensor_tensor(out=v3(f4[:]), in0=v3(f4[:]), in1=s8[:, 2, :].unsqueeze(2).broadcast_to([128, 8, 64]), op=ALU.mult), reads=["f4", "s82"], writes=["f4"])
                  S.dve(lambda e: e.tensor_tensor(out=f4[:], in0=f4[:], in1=LNW, op=ALU.mult), reads=["f4", "rwcb"], writes=["f4"])
                  S.dve(lambda e: e.tensor_tensor(out=f4[:], in0=f4[:], in1=LNB, op=ALU.add), reads=["f4", "rwcb"], writes=["f4"])
                  S.dve(lambda e: e.tensor_tensor(out=eP[:], in0=r_, in1=kmod[:], op=ALU.mult), reads=["rwp", "kmod"], writes=["eP"])
                  S.dve(lambda e: e.tensor_tensor(out=eP[:], in0=eP[:], in1=RK, op=ALU.mult), reads=["eP", "rwcb"], writes=["eP"])
                  S.dve(lambda e: e.tensor_reduce(out=s8[:, 3, :], in_=v3(eP[:]), axis=AX.X, op=ALU.add), reads=["eP"], writes=["s83"])
                  S.dve(lambda e: e.tensor_tensor(out=v3(eN[:]), in0=v3(v_), in1=s8[:, 3, :].unsqueeze(2).broadcast_to([128, 8, 64]), op=ALU.mult), reads=["rwp", "s83"], writes=["eN"])
                  S.dve(lambda e: e.tensor_tensor(out=f4[:], in0=f4[:], in1=eN[:], op=ALU.add), reads=["f4", "eN"], writes=["f4"])
                  S.pe(lambda e: e.matmul(pA[:], lhsT=loT[0:96, 2, :], rhs=lor[0:96, 2, :], start=True, stop=True), reads=["loT", "lor"], writes=["pA"])
                  S.dve(lambda e: e.tensor_tensor(out=mixb[:, 512:1024], in0=f4[:], in1=pA[:], op=ALU.mult), reads=["f4", "pA"], writes=["mixb"])
                  cut("A_rwkv", mixb[:, 512:1024], "mixb")
                  for k in range(8):
                      S.pe(lambda e, k=k: e.transpose(out=pTb[:, k * 128:(k + 1) * 128], in_=mixb[:, k * 128:(k + 1) * 128], identity=ident_b), reads=["mixb", "cstb"], writes=["pTb"])
                  S.act(lambda e: e.copy(out=hT[:].rearrange("p a b -> p (a b)"), in_=pTb[:]), reads=["pTb"], writes=["hT"])
                  for hf, pp in ((0, pA), (1, pB)):
                      for k in range(8):
                          S.pe(lambda e, k=k, hf=hf, pp=pp: e.matmul(pp[:], lhsT=hT[:, k, :], rhs=w_out_bf[:, k, hf * 512:(hf + 1) * 512], start=(k == 0), stop=(k == 7)), reads=["hT", "w_out_bf"], writes=[pp.name])
                      S.dve(lambda e, hf=hf, pp=pp: e.tensor_tensor(out=t1k[:, hf * 512:(hf + 1) * 512], in0=pp[:], in1=modb[:, 2, hf * 512:(hf + 1) * 512], op=ALU.mult), reads=[pp.name, "modb"], writes=["t1k"])
                  S.dve(lambda e: e.scalar_tensor_tensor(out=t1k[:], in0=xt[:], scalar=float(ALPHA), in1=t1k[:], op0=ALU.mult, op1=ALU.add), reads=["xt", "t1k"], writes=["t1k"])
                  layer_norm_stats(t1k, "t1k")
                  S.dve(lambda e: e.tensor_scalar(out=t1k[:], in0=t1k[:], scalar1=mv[:, 0:1], scalar2=rstd[:], op0=ALU.subtract, op1=ALU.mult), reads=["t1k", "mv", "rstd"], writes=["t1k"])
                  S.dve(lambda e: e.tensor_tensor(out=t1k[:], in0=t1k[:], in1=lnpb[:, 0, :], op=ALU.mult), reads=["t1k", "lnpb"], writes=["t1k"])
                  S.dve(lambda e: e.tensor_tensor(out=t1k[:], in0=t1k[:], in1=lnpb[:, 1, :], op=ALU.add), reads=["t1k", "lnpb"], writes=["t1k"])
                  S.dma("sp", lambda e, rows=rows: e.dma_start(out=x1s[rows, :], in_=t1k[:]), reads=["t1k"], writes=["x1s"])
                  if stage == "A1":
                      S.dma("sp", lambda e, rows=rows: e.dma_start(out=dbg[rows, :], in_=t1k[:]), reads=["t1k"])
                  if _os.environ.get("KSKIP") == "router":
                      continue
                  layer_norm_stats(t1k, "t1k")
                  S.dve(lambda e: e.tensor_scalar(out=t1k[:], in0=t1k[:], scalar1=mv[:, 0:1], scalar2=rstd[:], op0=ALU.subtract, op1=ALU.mult), reads=["t1k", "mv", "rstd"], writes=["t1k"])
                  S.dve(lambda e: e.tensor_tensor(out=t1k[:], in0=t1k[:], in1=modb[:, 4, :], op=ALU.mult), reads=["t1k", "modb"], writes=["t1k"])
                  S.dve(lambda e: e.tensor_tensor(out=t1k[:], in0=t1k[:], in1=modb[:, 3, :], op=ALU.add), reads=["t1k", "modb"], writes=["t1k"])
                  S.act(lambda e: e.copy(out=hb[:], in_=t1k[:]), reads=["t1k"], writes=["hb"])
                  for half in range(2):
                      for k in range(4):
                          kk_ = half * 4 + k
                          S.pe(lambda e, k=k, kk_=kk_: e.transpose(out=pC[:, k * 128:(k + 1) * 128], in_=t1k[:, kk_ * 128:(kk_ + 1) * 128], identity=ident_f), reads=["t1k", "cst"], writes=["pC"])
                      S.act(lambda e, half=half: e.copy(out=h2T[:, half * 4:half * 4 + 4, :].rearrange("p a b -> p (a b)"), in_=pC[:]), reads=["pC"], writes=["xt"])
                  for k in range(8):
                      S.pe(lambda e, k=k: e.matmul(pD[:, 0:NE], lhsT=h2T[:, k, :], rhs=wr_f[:, k, :], start=(k == 0), stop=(k == 7)), reads=["xt", "wr_f"], writes=["pD"])
                  S.dve(lambda e: e.tensor_tensor(out=lg[:], in0=pD[:, 0:NE], in1=brb[:], op=ALU.add), reads=["pD", "brb"], writes=["lg"])
                  S.dve(lambda e: e.max(out=t8[:], in_=lg[:]), reads=["lg"], writes=["t8"])
                  S.dve(lambda e: e.tensor_scalar(out=mskb[:], in0=lg[:], scalar1=t8[:, 3:4], scalar2=None, op0=ALU.is_ge), reads=["lg", "t8"], writes=["mskb"])
                  S.pe(lambda e: e.matmul(pD[:, 64:64 + NE], lhsT=cstb[:, 2, :], rhs=mskb[:], start=True, stop=True), reads=["cstb", "mskb"], writes=["pD"])
                  S.pe(lambda e: e.matmul(pD[:, 128:128 + NE], lhsT=onesb[:], rhs=mskb[:], start=True, stop=True), reads=["onesb", "mskb"], writes=["pD"])
                  S.dve(lambda e: e.tensor_tensor(out=posn[:], in0=pD[:, 64:64 + NE], in1=tot[:], op=ALU.add), reads=["pD", "tot"], writes=["posn"])
                  S.dve(lambda e: e.tensor_tensor(out=tot[:], in0=pD[:, 128:128 + NE], in1=tot[:], op=ALU.add), reads=["pD", "tot"], writes=["tot"])
                  S.dve(lambda e: e.tensor_scalar(out=eqk[:], in0=posn[:], scalar1=float(CAP), scalar2=None, op0=ALU.is_lt), reads=["posn"], writes=["eqk"])
                  S.dve(lambda e: e.tensor_tensor(out=posn[:], in0=posn[:], in1=ebase[:], op=ALU.add), reads=["posn", "ebase"], writes=["posn"])
                  S.dve(lambda e: e.tensor_scalar(out=posn[:], in0=posn[:], scalar1=trash[:, 0:1], scalar2=None, op0=ALU.subtract), reads=["posn", "trash"], writes=["posn"])
                  S.dve(lambda e: e.tensor_tensor(out=posn[:], in0=posn[:], in1=eqk[:], op=ALU.mult), reads=["posn", "eqk"], writes=["posn"])
                  S.dve(lambda e: e.tensor_scalar(out=posn[:], in0=posn[:], scalar1=trash[:, 0:1], scalar2=None, op0=ALU.add), reads=["posn", "trash"], writes=["posn"])
                  for k4 in range(4):
                      S.dve(lambda e, k4=k4: e.tensor_scalar(out=eqk[:], in0=lg[:], scalar1=t8[:, k4:k4 + 1], scalar2=None, op0=ALU.is_equal), reads=["lg", "t8"], writes=["eqk"])
                      S.dve(lambda e: e.tensor_tensor(out=eqk[:], in0=eqk[:], in1=posn[:], op=ALU.mult), reads=["eqk", "posn"], writes=["eqk"])
                      S.dve(lambda e, k4=k4: e.tensor_reduce(out=sl4[:, k4:k4 + 1], in_=eqk[:], axis=AX.X, op=ALU.add), reads=["eqk"], writes=["sl4"])
                  S.dve(lambda e, i=i: e.tensor_copy(out=slot4[:, i, :], in_=sl4[:]), reads=["sl4"], writes=["slot4"])
                  S.dve(lambda e: e.tensor_scalar(out=ev4[:], in0=t8[:, 0:4], scalar1=t8[:, 0:1], scalar2=None, op0=ALU.subtract), reads=["t8"], writes=["ev4"])
                  S.act(lambda e: e.activation(out=ev4[:], in_=ev4[:], func=AF.Exp), reads=["ev4"], writes=["ev4"])
                  S.dve(lambda e: e.tensor_reduce(out=t8[:, 7:8], in_=ev4[:], axis=AX.X, op=ALU.add), reads=["ev4"], writes=["t8"])
                  S.dve(lambda e: e.reciprocal(out=t8[:, 7:8], in_=t8[:, 7:8]), reads=["t8"], writes=["t8"])
                  S.dve(lambda e: e.tensor_scalar(out=ev4[:], in0=ev4[:], scalar1=t8[:, 7:8], scalar2=None, op0=ALU.mult), reads=["ev4", "t8"], writes=["ev4"])
                  S.dve(lambda e: e.tensor_scalar(out=sl4[:], in0=sl4[:], scalar1=float(NSLOT), scalar2=None, op0=ALU.is_lt), reads=["sl4"], writes=["sl4"])
                  S.dve(lambda e, i=i: e.tensor_tensor(out=gate4[:, i, :], in0=ev4[:], in1=sl4[:], op=ALU.mult), reads=["ev4", "sl4"], writes=["gate4"])
                  for k4 in range(4 if stage in ("full", "A_scat", "BC") else 0):
                      S.dma("pool", lambda e, i=i, k4=k4: e.indirect_dma_start(out=Xs[:, :], out_offset=bass.IndirectOffsetOnAxis(ap=slot4[:, i, k4:k4 + 1], axis=0), in_=hb[:], in_offset=None),
                            reads=["hb", "slot4"], writes=["Xs"])
              S.dve(lambda e: e.memset(junk[:, 0:1], 0.0), reads=["slot4", "gate4", "modb", "cst", "cstb"], writes=["junk"])
          S.add("dve", lambda e: e.memset(junk[:, 1:2], 0.0), reads=[], writes=["junk"], barrier=True)

          if stage in ("A1",):
              S.emit()
              return nc

          with contextlib.ExitStack() as st:
              sb = lambda name, shape, d: st.enter_context(nc.sbuf_tensor(name, shape, d))
              Wgu = [sb("Wgu%d" % j, [128, 8, 2 * D], BF16) for j in range(2)]
              Wd = [sb("Wd%d" % j, [128, 8, D], BF16) for j in range(2)]
              bdn = [sb("bdn%d" % j, [1, D], BF16) for j in range(2)]
              bgu = sb("bgu", [128, NE, 16], F32)
              Xe = sb("Xe", [128, 6, D], BF16)
              XT = sb("XT", [128, 8, CAP], BF16)
              aT = sb("aT", [128, 8, CAP], BF16)
              G = sb("G", [128, 384], F32)
              Sg = sb("Sg", [128, 384], F32)
              Uc = sb("Uc", [128, 384], F32)
              Yo = [sb("Yo%d" % j, [128, D], F32) for j in range(2)]
              ones1 = sb("ones1", [1, 128], BF16)
              zt = sb("zt", [128, D], F32)
              S.dma("sp", lambda e: e.dma_start(out=bgu[:], in_=bguT[:, :, :]), writes=["bgu"])
              S.pool(lambda e: e.memset(ones1[:], 1.0), writes=["ones1"])
              S.pool(lambda e: e.memset(zt[:], 0.0), writes=["zt"])
              S.dma("sp", lambda e: e.dma_start(out=Ys[NSLOT:NSLOT + 128, :], in_=zt[:]), reads=["zt"], writes=["Ys"])

              def load_w(ex):
                  j = ex % 2
                  S.dma("pool", lambda e, j=j, ex=ex: e.dma_start(out=bdn[j][:], in_=b_dn[:, ex, :]), writes=[("bdn", j)])
                  gv_ = w_gu[ex].rearrange("(k p) n -> p k n", p=128)
                  dv_ = w_dn[ex].rearrange("(k p) n -> p k n", p=128)
                  for k in range(0, 8, 2):
                      S.dma("pool", lambda e, k=k, j=j, gv_=gv_: e.dma_start(out=Wgu[j][:, k:k + 2, :], in_=gv_[:, k:k + 2, :]), writes=[("Wgu", j)])
                  for k in range(0, 8, 4):
                      S.dma("pool", lambda e, k=k, j=j, dv_=dv_: e.dma_start(out=Wd[j][:, k:k + 4, :], in_=dv_[:, k:k + 4, :]), writes=[("Wd", j)])

              load_w(0)
              for ex in range(nexp):
                  j = ex % 2
                  if ex + 1 < nexp:
                      load_w(ex + 1)
                  S.dma("sp", lambda e, ex=ex: e.dma_start(out=Xe[:], in_=Xs[ex * CAP:(ex + 1) * CAP, :].rearrange("(s p) d -> p s d", p=128)), reads=["Xs"], writes=["Xe"])
                  for s in range(6):
                      pp = pTb if s % 2 == 0 else pTb2
                      for k in range(8):
                          S.pe(lambda e, s=s, k=k, pp=pp: e.transpose(out=pp[:, k * 128:(k + 1) * 128], in_=Xe[:, s, k * 128:(k + 1) * 128], identity=ident_b), reads=["Xe", "cstb"], writes=[pp.name])
                      S.act(lambda e, s=s, pp=pp: e.copy(out=XT[:, :, s * 128:(s + 1) * 128], in_=pp[:].rearrange("p (a b) -> p a b", a=8)), reads=[pp.name], writes=["XT"])
                  for nh in range(2):
                      n0 = nh * 384
                      for fc in range(8):
                          for (pp, col) in ((pA, fc), (pB, 8 + fc)):
                              for k in range(8):
                                  S.pe(lambda e, k=k, pp=pp, col=col, j=j, n0=n0: e.matmul(pp[:, 0:384], lhsT=Wgu[j][:, k, col * 128:(col + 1) * 128], rhs=XT[:, k, n0:n0 + 384], start=(k == 0), stop=(k == 7)),
                                       reads=[("Wgu", j), "XT"], writes=[pp.name])
                          S.dve(lambda e, ex=ex, fc=fc: e.tensor_scalar(out=G[:], in0=pA[:, 0:384], scalar1=bgu[:, ex, fc:fc + 1], scalar2=7.0, op0=ALU.add, op1=ALU.min), reads=["pA", "bgu"], writes=["G"])
                          S.act(lambda e: e.activation(out=Sg[:], in_=G[:], func=AF.Sigmoid, scale=1.702), reads=["G"], writes=["Sg"])
                          S.dve(lambda e, ex=ex, fc=fc: e.tensor_scalar(out=Uc[:], in0=pB[:, 0:384], scalar1=bgu[:, ex, 8 + fc:9 + fc], scalar2=7.0, op0=ALU.add, op1=ALU.min), reads=["pB", "bgu"], writes=["Uc"])
                          S.dve(lambda e: e.tensor_scalar(out=Uc[:], in0=Uc[:], scalar1=-7.0, scalar2=1.0, op0=ALU.max, op1=ALU.add), reads=["Uc"], writes=["Uc"])
                          S.dve(lambda e: e.tensor_tensor(out=G[:], in0=G[:], in1=Sg[:], op=ALU.mult), reads=["G", "Sg"], writes=["G"])
                          S.dve(lambda e, fc=fc, n0=n0: e.tensor_tensor(out=aT[:, fc, n0:n0 + 384], in0=G[:], in1=Uc[:], op=ALU.mult), reads=["G", "Uc"], writes=["aT"])
                  for s in range(6):
                      yo = Yo[s % 2]
                      for hf, pp in ((0, pC), (1, pD)):
                          S.pe(lambda e, hf=hf, pp=pp, j=j: e.matmul(pp[:], lhsT=ones1[:], rhs=bdn[j][:, hf * 512:(hf + 1) * 512], start=True, stop=False), reads=["ones1", ("bdn", j)], writes=[pp.name])
                          for k in range(8):
                              S.pe(lambda e, k=k, hf=hf, pp=pp, s=s, j=j: e.matmul(pp[:], lhsT=aT[:, k, s * 128:(s + 1) * 128], rhs=Wd[j][:, k, hf * 512:(hf + 1) * 512], start=False, stop=(k == 7)),
                                   reads=["aT", ("Wd", j)], writes=[pp.name])
                          S.act(lambda e, hf=hf, pp=pp, yo=yo: e.copy(out=yo[:, hf * 512:(hf + 1) * 512], in_=pp[:]), reads=[pp.name], writes=[("Yo", s % 2)])
                      S.dma("sp", lambda e, ex=ex, s=s, yo=yo: e.dma_start(out=Ys[ex * CAP + s * 128:ex * CAP + (s + 1) * 128, :], in_=yo[:]), reads=[("Yo", s % 2)], writes=["Ys"])
              S.dve(lambda e: e.memset(junk[:, 2:3], 0.0), reads=["slot4", "gate4", "modb"], writes=["junk"])
          S.add("dve", lambda e: e.memset(junk[:, 3:4], 0.0), reads=[], writes=["junk"], barrier=True)

          with contextlib.ExitStack() as st:
              sb = lambda name, shape, d: st.enter_context(nc.sbuf_tensor(name, shape, d))
              Yg = [sb("Yg%d" % j, [128, 4, D], F32) for j in range(2)]
              x1t = [sb("x1t%d" % j, [128, D], F32) for j in range(2)]
              acc = sb("acc", [128, D], F32)
              lnpc = sb("lnpbC", [128, 2, D], F32)
              S.dma("sp", lambda e: e.dma_start(out=lnpc[:], in_=lnp[:, 2:4, :]), writes=["lnpb"])
              st6c = sb("st6c", [128, 2, 6], F32)
              mvc = sb("mvc", [128, 2], F32)
              rsc = sb("rsc", [128, 1], F32)
              for i in range(ntiles):
                  j = i % 2
                  rows = slice(i * 128, (i + 1) * 128)
                  S.dma("sp", lambda e, rows=rows, j=j: e.dma_start(out=x1t[j][:], in_=x1s[rows, :]), reads=["x1s"], writes=[("x1t", j)])
                  for k4 in range(4):
                      S.dma("pool", lambda e, i=i, k4=k4, j=j: e.indirect_dma_start(out=Yg[j][:, k4, :], out_offset=None, in_=Ys[:, :], in_offset=bass.IndirectOffsetOnAxis(ap=slot4[:, i, k4:k4 + 1], axis=0)),
                            reads=["Ys", "slot4"], writes=[("Yg", j, k4)])
                  S.dve(lambda e, i=i, j=j: e.tensor_scalar(out=acc[:], in0=Yg[j][:, 0, :], scalar1=gate4[:, i, 0:1], scalar2=None, op0=ALU.mult), reads=[("Yg", j, 0), "gate4"], writes=["acc"])
                  for k4 in range(1, 4):
                      S.dve(lambda e, i=i, j=j, k4=k4: e.scalar_tensor_tensor(out=acc[:], in0=Yg[j][:, k4, :], scalar=gate4[:, i, k4:k4 + 1], in1=acc[:], op0=ALU.mult, op1=ALU.add), reads=[("Yg", j, k4), "gate4", "acc"], writes=["acc"])
                  S.dve(lambda e: e.tensor_tensor(out=acc[:], in0=acc[:], in1=modb[:, 5, :], op=ALU.mult), reads=["acc", "modb"], writes=["acc"])
                  S.dve(lambda e, j=j: e.scalar_tensor_tensor(out=acc[:], in0=x1t[j][:], scalar=float(ALPHA), in1=acc[:], op0=ALU.mult, op1=ALU.add), reads=[("x1t", j), "acc"], writes=["acc"])
                  for h in range(2):
                      S.dve(lambda e, h=h: e.bn_stats(out=st6c[:, h, :], in_=acc[:, h * 512:(h + 1) * 512]), reads=["acc"], writes=["st6c"])
                  S.dve(lambda e: e.bn_aggr(out=mvc[:], in_=st6c[:].rearrange("p a b -> p (a b)")), reads=["st6c"], writes=["mvc"])
                  S.dve(lambda e: e.tensor_scalar(out=rsc[:], in0=mvc[:, 1:2], scalar1=LN_EPS, scalar2=None, op0=ALU.add), reads=["mvc"], writes=["rsc"])
                  S.act(lambda e: e.activation(out=rsc[:], in_=rsc[:], func=AF.Sqrt), reads=["rsc"], writes=["rsc"])
                  S.dve(lambda e: e.reciprocal(out=rsc[:], in_=rsc[:]), reads=["rsc"], writes=["rsc"])
                  S.dve(lambda e: e.tensor_scalar(out=acc[:], in0=acc[:], scalar1=mvc[:, 0:1], scalar2=rsc[:], op0=ALU.subtract, op1=ALU.mult), reads=["acc", "mvc", "rsc"], writes=["acc"])
                  S.dve(lambda e: e.tensor_tensor(out=acc[:], in0=acc[:], in1=lnpc[:, 0, :], op=ALU.mult), reads=["acc", "lnpb"], writes=["acc"])
                  S.dve(lambda e, j=j: e.tensor_tensor(out=x1t[j][:], in0=acc[:], in1=lnpc[:, 1, :], op=ALU.add), reads=["acc", "lnpb"], writes=[("x1t", j)])
                  S.dma("sp", lambda e, rows=rows, j=j: e.dma_start(out=out[rows, :], in_=x1t[j][:]), reads=[("x1t", j)], writes=["out"])

    except _Cut:
        pass
    S.emit()
    return nc


def _prep_shared(inp):
    f = lambda a: np.ascontiguousarray(np.asarray(a), dtype=np.float32)
    bc = lambda v, n=128: np.ascontiguousarray(np.broadcast_to(np.asarray(v, np.float32).reshape(1, -1), (n, np.asarray(v).size)))
    w_in = f(inp["w_in"][0])
    perm = np.concatenate([np.arange(0, 512), np.arange(768, 2464), np.arange(512, 640), np.arange(640, 768)])
    sh = {}
    sh["w_ada"] = f(inp["w_ada"][0])
    sh["b_ada_b"] = bc(inp["b_ada"][0])
    sh["w_in"] = np.ascontiguousarray(w_in[:, perm])
    sh["mu_b"] = bc(inp["shift_mu"][0])
    rows = [inp["rwkv_w0"][0], inp["rwkv_a0"][0], inp["rwkv_k_k"][0], inp["rwkv_k_a"][0], np.asarray(inp["rwkv_r_k"][0]).reshape(-1), inp["rwkv_ln_w"][0], inp["rwkv_ln_b"][0]]
    sh["rwc_b"] = np.ascontiguousarray(np.stack([bc(r) for r in rows], axis=1))
    lora = np.zeros((96, 3, 512), np.float32)
    lora[0:32, 0] = inp["rwkv_w2"][0]
    lora[0:32, 1] = inp["rwkv_a2"][0]
    lora[0:96, 2] = inp["rwkv_g2"][0]
    sh["lora"] = lora
    sh["sinks_b"] = bc(inp["attn_sinks"][0])
    invf = (500000.0 ** (-np.arange(0, 16, 2, dtype=np.float32) / 16)).astype(np.float32)
    sh["invf_b"] = bc(invf)
    sh["w_out"] = f(inp["w_out"][0])
    sh["lnp"] = np.ascontiguousarray(np.stack([bc(inp[k][0]) for k in ("ln1_g", "ln1_b", "ln2_g", "ln2_b")], axis=1))
    sh["w_router"] = f(inp["w_router"][0])
    sh["b_router_b"] = bc(inp["b_router"][0])
    sh["w_gu"] = f(inp["w_gate_up"][0])
    sh["bguT"] = np.ascontiguousarray(f(inp["b_gate_up"][0]).reshape(NE, 16, 128).transpose(2, 0, 1))
    sh["w_dn"] = f(inp["w_down"][0])
    sh["b_dn"] = f(inp["b_down"][0]).reshape(1, NE, D)
    jj = np.arange(128)[:, None]
    tt = np.arange(128)[None, :]
    sh["consts"] = np.ascontiguousarray(np.stack([np.eye(128), (jj <= tt), (jj < tt), (jj > tt), (jj > tt)], axis=1).astype(np.float32))
    return sh


def _prep_core(inp, b, sh):
    m = dict(sh)
    m["x"] = np.ascontiguousarray(np.asarray(inp["x"][b], np.float32))
    m["posT"] = np.ascontiguousarray(np.asarray(inp["positions"][b], np.int32).reshape(NT, 128).T)
    c = np.asarray(inp["c"][b], np.float32)
    m["cB"] = np.ascontiguousarray(np.broadcast_to(c.reshape(8, 128).T[:, :, None], (128, 8, 128)))
    return m


_NC_CACHE = {}


def kernel(**inputs):
    sh = _prep_shared(inputs)
    in_maps = [_prep_core(inputs, b, sh) for b in range(8)]
    if "full" not in _NC_CACHE:
        _NC_CACHE["full"] = build("full")
    nc = _NC_CACHE["full"]
    res = run_bass_kernel_spmd(nc, in_maps, core_ids=list(range(8)))
    return np.stack([np.asarray(r["out"], np.float32) for r in res.results], axis=0)
```

```python
import contextlib
import os as _os
import numpy as np
import concourse.bass as bass
import concourse.mybir as mybir
from concourse.bass_utils import run_bass_kernel_spmd

F32 = mybir.dt.float32
BF16 = mybir.dt.bfloat16
I32 = mybir.dt.int32
U32 = mybir.dt.uint32
AF = mybir.ActivationFunctionType
ALU = mybir.AluOpType
AX = mybir.AxisListType

COMPUTE = ("pe", "act", "dve", "pool")
SEG = 8192
SAME_ENG_INORDER = ("pe",)
SAME_ENG_DRAIN = ()
SAME_ENG_HZ = ("act", "dve")
HZ_SMALL = 256
BUBBLE = False
NPOOL = {"pe": 24, "act": 24, "dve": 24, "pool": 4}
BUBBLE_DIST = 2
HZ_METHODS = ("tensor_reduce", "bn_stats", "bn_aggr", "max", "reciprocal")


class _Rec:
    def __init__(self):
        self.calls = []

    def __getattr__(self, name):
        def f(*a, **k):
            self.calls.append((name, a, k))
            return self
        return f


def _free_size(ap):
    try:
        sh = list(ap.shape)
        n = 1
        for x in sh[1:]:
            n *= int(x)
        return n
    except Exception:
        return 0


class _Cut(Exception):
    pass


class Sched:
    def __init__(self, nc, kdma=None):
        self.nc = nc
        self.ops = []
        self.last_w = {}
        self.rd_eng = {}
        self.rd_dma = {}
        self.kdma = kdma or {"sp": 16, "pool": 8, "act": 4}
        self.relay_of = {}
        self.relay_fn = None

    def add(self, eng, fn, reads=(), writes=(), dma=False, barrier=False):
        i = len(self.ops)
        reads = list(reads)
        writes = list(writes)
        if barrier:
            writes.append("PHASE")
        else:
            reads.append("PHASE")
        deps = set()
        for r in reads:
            if r in self.last_w:
                deps.add(self.last_w[r])
        for w in writes:
            if w in self.last_w:
                deps.add(self.last_w[w])
            for d in self.rd_eng.get(w, {}).values():
                deps.add(d)
            for d in self.rd_dma.get(w, ()):
                deps.add(d)
        if getattr(self, "relay_fn", None) is not None and not dma:
            nd = set()
            for d in deps:
                od = self.ops[d]
                if (not od["dma"]) and {eng, od["eng"]} in ({"pe", "dve"}, {"pe", "pool"}):
                    if d not in self.relay_of:
                        self.relay_of[d] = len(self.ops)
                        self.ops.append(dict(eng="act", fn=self.relay_fn, deps=[d], dma=False))
                    nd.add(self.relay_of[d])
                else:
                    nd.add(d)
            deps = nd
            i = len(self.ops)
        for w in writes:
            self.last_w[w] = i
            self.rd_eng[w] = {}
            self.rd_dma[w] = []
        ws = set(writes)
        for r in reads:
            if r in ws:
                continue
            if dma:
                self.rd_dma.setdefault(r, []).append(i)
            else:
                self.rd_eng.setdefault(r, {})[eng] = i
        self.ops.append(dict(eng=eng, fn=fn, deps=sorted(deps), dma=dma))
        return i

    def pe(self, fn, reads=(), writes=()):
        return self.add("pe", fn, reads, writes)

    def act(self, fn, reads=(), writes=()):
        return self.add("act", fn, reads, writes)

    def dve(self, fn, reads=(), writes=()):
        return self.add("dve", fn, reads, writes)

    def pool(self, fn, reads=(), writes=()):
        return self.add("pool", fn, reads, writes)

    def dma(self, q, fn, reads=(), writes=()):
        return self.add(q, fn, reads, writes, dma=True)

    def emit(self):
        nc = self.nc
        ops = self.ops
        n = len(ops)
        dcount = {q: [0] * k for q, k in self.kdma.items()}
        dnext = {q: 0 for q in self.kdma}
        tok = [None] * n
        prev_same = [None] * n
        last_on = {}
        order = [0] * n
        ecnt = {}
        for i, o in enumerate(ops):
            e = o["eng"]
            ecnt[e] = ecnt.get(e, 0) + 1
            order[i] = ecnt[e]
            if o["dma"]:
                q = e
                s_ = dnext[q] % self.kdma[q]
                dnext[q] += 1
                dcount[q][s_] += 1
                key = ("d", q, s_)
                tok[i] = (key, 16 * dcount[q][s_])
                prev_same[i] = last_on.get(key)
                last_on[key] = i
        per_eng = {}
        for i, o in enumerate(ops):
            per_eng.setdefault(o["eng"], []).append(i)

        def dep_list(i):
            o = ops[i]
            deps = list(o["deps"])
            if o["dma"] and prev_same[i] is not None:
                deps.append(prev_same[i])
            return deps

        hz = [True] * n
        for i, o in enumerate(ops):
            if o["dma"] or o["eng"] not in SAME_ENG_HZ:
                continue
            r = _Rec()
            try:
                o["fn"](r)
                name, a, k = r.calls[0]
                out = k.get("out", k.get("ap", a[0] if a else None))
                small = _free_size(out) < HZ_SMALL
                hz[i] = small or (name in HZ_METHODS) or ("accum_out" in k and k["accum_out"] is not None)
            except Exception:
                hz[i] = True
        self.n_hz = sum(1 for i, o in enumerate(ops) if (not o["dma"]) and o["eng"] in SAME_ENG_HZ and hz[i])
        needed = [False] * n
        needed_self = [False] * n
        bubble_before = [False] * n
        plan = {}
        drain_before = [False] * n
        for ename, idxs in per_eng.items():
            waited = {}
            drained_upto = 0
            for i in idxs:
                wl = []
                for d in dep_list(i):
                    od = ops[d]
                    if od["dma"]:
                        key, val = tok[d]
                        if waited.get(key, 0) >= val:
                            continue
                        waited[key] = val
                        wl.append(d)
                    else:
                        if od["eng"] == ename and ename in SAME_ENG_INORDER:
                            continue
                        if od["eng"] == ename and ename in SAME_ENG_HZ and not hz[d]:
                            continue
                        if od["eng"] == ename and ename in SAME_ENG_DRAIN:
                            if order[d] > drained_upto:
                                drain_before[i] = True
                                drained_upto = order[i] - 1
                            continue
                        if od["eng"] == ename and ename in SAME_ENG_HZ and BUBBLE:
                            if order[i] - order[d] <= BUBBLE_DIST:
                                bubble_before[i] = True
                            continue
                        if od["eng"] == ename:
                            key = ("s", od["eng"])
                            if waited.get(key, 0) >= order[d]:
                                continue
                            waited[key] = order[d]
                            needed_self[d] = True
                            wl.append((d, "s"))
                            continue
                        key = ("e", od["eng"])
                        if waited.get(key, 0) >= order[d]:
                            continue
                        waited[key] = order[d]
                        needed[d] = True
                        wl.append(d)
                plan[i] = wl
        ecount = {e: 0 for e in COMPUTE}
        scount = {e: 0 for e in COMPUTE}
        stok = [None] * n
        keys = set()
        for i, o in enumerate(ops):
            if o["dma"]:
                keys.add(tok[i][0])
            elif needed[i] or needed_self[i]:
                e = o["eng"]
                key = ("e", e, ecount[e] % NPOOL[e])
                tok[i] = (key, ecount[e] // NPOOL[e] + 1)
                stok[i] = tok[i]
                needed[i] = True
                ecount[e] += 1
                keys.add(key)
        self.n_incs = dict(ecount)
        with contextlib.ExitStack() as st:
            sems = {}
            for key in sorted(keys, key=str):
                sems[key] = st.enter_context(nc.semaphore("s_" + "_".join(str(x) for x in key)))
            block = st.enter_context(nc.Block())
            engmap = {"pe": "tensor", "act": "scalar", "dve": "vector", "pool": "gpsimd", "sp": "sync"}

            def make(ename, idxs):
                def body(eng):
                    for i in idxs:
                        o = ops[i]
                        if drain_before[i]:
                            eng.drain()
                        if bubble_before[i] and ename in getattr(self, "bubble", {}):
                            self.bubble[ename](eng)
                        for d in plan[i]:
                            if isinstance(d, tuple):
                                key, val = stok[d[0]]
                            else:
                                key, val = tok[d]
                            eng.wait_ge(sems[key], val)
                        inst = o["fn"](eng)
                        if o["dma"]:
                            inst.then_inc(sems[tok[i][0]], 16)
                        else:
                            if needed[i]:
                                inst.then_inc(sems[tok[i][0]], 1)
                            elif needed_self[i]:
                                inst.then_inc(sems[stok[i][0]], 1)
                            if (needed[i] or needed_self[i]) and ename in getattr(self, "spacer", {}):
                                self.spacer[ename](eng)
                    if ename in self.kdma:
                        for s_ in range(self.kdma[ename]):
                            if dcount[ename][s_] > 0:
                                eng.wait_ge(sems[("d", ename, s_)], 16 * dcount[ename][s_])
                return body

            for ename, idxs in per_eng.items():
                getattr(block, engmap[ename])(make(ename, idxs))
        return ecount


NT = 32
D = 1024
DIN = 2464
CAP = 768
NE = 32
NSLOT = NE * CAP
LN_EPS = 1e-5
GN_EPS = 64e-5
ALPHA = 2 ** 0.25
TWO_PI = 2.0 * np.pi


def build(stage="full", ntiles=NT, nexp=NE):
    nc = bass.Bass("TRN2", target_bir_lowering=False)
    dt = lambda name, shape, d, kind="ExternalInput": nc.dram_tensor(name, shape, d, kind=kind).ap()
    x = dt("x", [4096, D], F32)
    posT = dt("posT", [128, NT], I32)
    cB = dt("cB", [128, 8, 128], F32)
    w_ada = dt("w_ada", [D, 6 * D], F32)
    b_ada_b = dt("b_ada_b", [128, 6 * D], F32)
    w_in = dt("w_in", [D, DIN], F32)
    mu_b = dt("mu_b", [128, 1696], F32)
    rwc_b = dt("rwc_b", [128, 7, 512], F32)
    lora = dt("lora", [96, 3, 512], F32)
    sinks_b = dt("sinks_b", [128, 8], F32)
    invf_b = dt("invf_b", [128, 8], F32)
    w_out = dt("w_out", [D, D], F32)
    lnp = dt("lnp", [128, 4, D], F32)
    w_router = dt("w_router", [D, NE], F32)
    b_router_b = dt("b_router_b", [128, NE], F32)
    w_gu = dt("w_gu", [NE, D, 2 * D], F32)
    bguT = dt("bguT", [128, NE, 16], F32)
    w_dn = dt("w_dn", [NE, D, D], F32)
    b_dn = dt("b_dn", [1, NE, D], F32)
    consts = dt("consts", [128, 5, 128], F32)
    out = dt("out", [4096, D], F32, kind="ExternalOutput")
    dbg = dt("dbg", [4096, D], F32, kind="ExternalOutput") if stage != "full" else None
    x1s = dt("x1s", [4096, D], F32, kind="Internal")
    Xs = dt("Xs", [NSLOT + 128, D], BF16, kind="Internal")
    Ys = dt("Ys", [NSLOT + 128, D], F32, kind="Internal")
    lastrow = dt("lastrow", [2, 1696], F32, kind="Internal")

    S = Sched(nc)

    cut_tile = [0]

    def cut(name, ap, key, ncols=None):
        if stage == name and (name == "P" or cut_tile[0] == ntiles - 1):
            if ap.shape[-1] > 1024:
                ap = ap[:, 0:1024]
            ncols = ncols or ap.shape[-1]
            q = "sp" if ap.dtype == F32 else "pool"
            S.dma(q, lambda e: e.dma_start(out=dbg[0:ap.shape[0], 0:ncols], in_=ap), reads=[key])
            raise _Cut()

    try:
      with contextlib.ExitStack() as st0:
          sb0 = lambda name, shape, d: st0.enter_context(nc.sbuf_tensor(name, shape, d))
          ps = lambda name, shape, d: st0.enter_context(nc.psum_tensor(name, shape, d))
          pTb = ps("pTb", [128, 1024], BF16)
          pTb2 = ps("pTb2", [128, 1024], BF16)
          pA = ps("pA", [128, 512], F32)
          pB = ps("pB", [128, 512], F32)
          pC = ps("pC", [128, 512], F32)
          pD = ps("pD", [128, 512], F32)
          pE = ps("pE", [128, 512], F32)
          pF = ps("pF", [128, 512], F32)
          cst = sb0("cst", [128, 5, 128], F32)
          cstb = sb0("cstb", [128, 5, 128], BF16)
          modb = sb0("modb", [128, 6, D], F32)
          slot4 = sb0("slot4", [128, NT, 4], I32)
          gate4 = sb0("gate4", [128, NT, 4], F32)
          junk = sb0("junk", [128, 8], F32)
          S.relay_fn = lambda e: e.activation(out=junk[:, 6:7], in_=junk[:, 6:7], func=AF.Copy)
          if True:
              spc = sb0("spc", [128, 2, 512], F32)
              nsp = 512
              nsp = int(_os.environ.get("KSPACE", "0"))
              nbb = int(_os.environ.get("KBUB", "384"))
              S.spacer = {"dve": lambda e: e.memset(spc[:, 0, 0:nsp], 0.0),
                          "act": lambda e: e.activation(out=spc[:, 1, 0:nsp], in_=spc[:, 1, 0:nsp], func=AF.Copy)}
              if nsp == 0:
                  S.spacer = {}
              S.bubble = {"dve": lambda e: e.memset(spc[:, 0, 0:nbb], 0.0),
                          "act": lambda e: e.activation(out=spc[:, 1, 0:nbb], in_=spc[:, 1, 0:nbb], func=AF.Copy)}
          ident_f = cst[:, 0, :]
          ident_b = cstb[:, 0, :]

          S.dma("sp", lambda e: e.dma_start(out=cst[:], in_=consts[:, :, :]), writes=["cst"])
          S.dve(lambda e: e.tensor_copy(out=cstb[:], in_=cst[:]), reads=["cst"], writes=["cstb"])
          S.dma("sp", lambda e: e.dma_start(out=modb[:].rearrange("p a b -> p (a b)"), in_=b_ada_b[:, :]), writes=["modb"])

          with contextlib.ExitStack() as st:
              sb = lambda name, shape, d: st.enter_context(nc.sbuf_tensor(name, shape, d))
              w_in_bf = sb("w_in_bf", [128, 8, DIN], BF16)
              lnpb = sb("lnpbA", [128, 2, D], F32)
              S.dma("sp", lambda e: e.dma_start(out=lnpb[:], in_=lnp[:, 0:2, :]), writes=["lnpb"])
              w_out_bf = sb("w_out_bf", [128, 8, D], BF16)
              wr_f = sb("wr_f", [128, 8, NE], F32)
              brb = sb("brb", [128, NE], F32)
              mub = sb("mub", [128, 1696], F32)
              rwcb = sb("rwcb", [128, 7, 512], F32)
              lor = sb("lor", [96, 3, 512], F32)
              sinkb = sb("sinkb", [128, 8], F32)
              esink = sb("esink", [128, 8], F32)
              invf = sb("invf", [128, 8], F32)
              cosT = sb("cosT", [128, NT, 8], F32)
              sinT = sb("sinT", [128, NT, 8], F32)
              stp = contextlib.ExitStack()
              sbp = lambda name, shape, d: stp.enter_context(nc.sbuf_tensor(name, shape, d))
              posi = sbp("posi", [128, NT], I32)
              posf = sbp("posf", [128, NT], F32)
              ang = sbp("ang", [128, NT, 8], F32)
              scB = sbp("scB", [128, 8, 128], F32)
              wada = [sbp("wada%d" % j, [128, 8, 512], F32) for j in range(2)]
              w_in_v = w_in.rearrange("(k p) n -> p k n", p=128)
              for (c0, c1) in ((0, 1232), (1232, 2464)):
                  S.dma("pool", lambda e, c0=c0, c1=c1: e.dma_start(out=w_in_bf[:, :, c0:c1], in_=w_in_v[:, :, c0:c1]), writes=["w_in_bf"])
              S.dma("pool", lambda e: e.dma_start(out=w_out_bf[:], in_=w_out.rearrange("(k p) n -> p k n", p=128)), writes=["w_out_bf"])
              S.dma("sp", lambda e: e.dma_start(out=wr_f[:], in_=w_router.rearrange("(k p) n -> p k n", p=128)), writes=["wr_f"])
              S.dma("sp", lambda e: e.dma_start(out=brb[:], in_=b_router_b[:, :]), writes=["brb"])
              S.dma("sp", lambda e: e.dma_start(out=mub[:], in_=mu_b[:, :]), writes=["mub"])
              S.dma("sp", lambda e: e.dma_start(out=rwcb[:], in_=rwc_b[:, :, :]), writes=["rwcb"])
              S.dma("sp", lambda e: e.dma_start(out=lor[:], in_=lora[:, :, :]), writes=["lor"])
              S.dma("sp", lambda e: e.dma_start(out=sinkb[:], in_=sinks_b[:, :]), writes=["sinkb"])
              S.dma("sp", lambda e: e.dma_start(out=invf[:], in_=invf_b[:, :]), writes=["invf"])
              S.dma("sp", lambda e: e.dma_start(out=posi[:], in_=posT[:, :]), writes=["posi"])
              S.dma("sp", lambda e: e.dma_start(out=scB[:], in_=cB[:, :, :]), writes=["scB"])
              S.act(lambda e: e.activation(out=esink[:], in_=sinkb[:], func=AF.Exp), reads=["sinkb"], writes=["esink"])
              S.act(lambda e: e.activation(out=scB[:], in_=scB[:], func=AF.Silu), reads=["scB"], writes=["scB"])
              w_ada_v = w_ada.rearrange("(k p) n -> p k n", p=128)
              for j in range(12):
                  wb_ = wada[j % 2]
                  S.dma("sp", lambda e, j=j, wb_=wb_: e.dma_start(out=wb_[:], in_=w_ada_v[:, :, j * 512:(j + 1) * 512]), writes=[("wada", j % 2)])
                  pp = pA if j % 2 == 0 else pB
                  for k in range(8):
                      S.pe(lambda e, k=k, wb_=wb_, pp=pp: e.matmul(pp[:], lhsT=scB[:, k, :], rhs=wb_[:, k, :], start=(k == 0), stop=(k == 7)),
                           reads=["scB", ("wada", j % 2)], writes=[pp.name])
                  mflat = modb[:].rearrange("p a b -> p (a b)")
                  S.dve(lambda e, j=j, pp=pp, mflat=mflat: e.tensor_tensor(out=mflat[:, j * 512:(j + 1) * 512], in0=pp[:], in1=mflat[:, j * 512:(j + 1) * 512], op=ALU.add),
                        reads=[pp.name, "modb"], writes=["modb"])
              for a in (1, 2, 4, 5):
                  S.dve(lambda e, a=a: e.tensor_scalar(out=modb[:, a, :], in0=modb[:, a, :], scalar1=1.0, scalar2=None, op0=ALU.add), reads=["modb"], writes=["modb"])
              S.dve(lambda e: e.tensor_copy(out=posf[:], in_=posi[:]), reads=["posi"], writes=["posf"])
              S.dve(lambda e: e.tensor_tensor(out=ang[:], in0=posf[:].unsqueeze(2).broadcast_to([128, NT, 8]), in1=invf[:].unsqueeze(1).broadcast_to([128, NT, 8]), op=ALU.mult),
                    reads=["posf", "invf"], writes=["ang"])
              angi = sbp("angi", [128, NT, 8], I32)
              angf = sbp("angf", [128, NT, 8], F32)
              SC = float(TWO_PI * (1.0 - 1e-6))
              for (dst, key, off) in ((sinT, "sinT", 0.0), (cosT, "cosT", 0.25)):
                  S.dve(lambda e, dst=dst, off=off: e.tensor_scalar(out=dst[:], in0=ang[:], scalar1=float(1.0 / TWO_PI), scalar2=off, op0=ALU.mult, op1=ALU.add), reads=["ang"], writes=[key])
                  S.dve(lambda e, dst=dst: e.tensor_copy(out=angi[:], in_=dst[:]), reads=[key], writes=["angi"])
                  S.dve(lambda e: e.tensor_copy(out=angf[:], in_=angi[:]), reads=["angi"], writes=["angf"])
                  S.dve(lambda e, dst=dst: e.tensor_tensor(out=dst[:], in0=dst[:], in1=angf[:], op=ALU.subtract), reads=[key, "angf"], writes=[key])
                  S.dve(lambda e, dst=dst: e.tensor_scalar(out=angf[:], in0=dst[:], scalar1=0.5, scalar2=None, op0=ALU.is_gt), reads=[key], writes=["angf"])
                  S.dve(lambda e, dst=dst: e.tensor_tensor(out=dst[:], in0=dst[:], in1=angf[:], op=ALU.subtract), reads=[key, "angf"], writes=[key])
                  S.act(lambda e, dst=dst: e.activation(out=dst[:], in_=dst[:], func=AF.Sin, scale=SC), reads=[key], writes=[key])

              zrow = sbp("zrow", [1, 1696], F32)
              S.pool(lambda e: e.memset(zrow[:], 0.0), writes=["zrow"])
              S.dma("sp", lambda e: e.dma_start(out=lastrow[0:1, :], in_=zrow[:]), reads=["zrow"], writes=["lastrow0"])
              S.dve(lambda e: e.memset(junk[:, 4:5], 0.0), reads=["cosT", "sinT", "modb"], writes=["junk"])
              S.add("dve", lambda e: e.memset(junk[:, 5:6], 0.0), reads=[], writes=["junk"], barrier=True)
              stp.close()
              cut("P", modb[:, 1, :], "modb")
              xt = sb("xt", [128, D], F32)
              g4t = sb("g4t", [128, 416], F32)
              t1k = sb("t1k", [128, D], F32)
              hb = sb("hb", [128, D], BF16)
              hT = sb("hT", [128, 8, 128], BF16)
              st6 = sb("st6", [128, 2, 6], F32)
              mv = sb("mv", [128, 2], F32)
              rstd = sb("rstd", [128, 1], F32)
              qk = sb("qk", [128, 10, 64], F32)
              qkb = sb("qkb", [128, 10, 64], BF16)
              rt = sb("rt", [128, 4, 10, 8], F32)
              qT = sb("qT", [64, 8, 128], BF16)
              kT = [sb("kT%d" % j, [64, 2, 128], BF16) for j in range(2)]
              V1 = [sb("V1%d" % j, [128, 2, 66], BF16) for j in range(2)]
              rwc = sb("rwc", [128, 1696], F32)
              rwp = sb("rwp", [128, 1696], F32)
              Ee = sb("Ee", [128, 512], F32)
              PTp = sb("PTp", [128, 4, 128], BF16)
              PTc = sb("PTc", [128, 4, 128], BF16)
              den = sb("den", [128, 8], F32)
              mixb = sb("mixb", [128, D], BF16)
              lo_in = sb("lo_in", [128, 160], F32)
              loT = sb("loT", [96, 3, 128], F32)
              f1 = rwc[:, 0:512]
              f2 = rwc[:, 512:1024]
              f3 = rwc[:, 1024:1536]
              f4 = sb("f4", [128, 512], F32)
              eP = sb("eP", [128, 512], F32)
              eN = sb("eN", [128, 512], F32)
              lw = sb("lw", [128, 512], F32)
              av = sb("av", [128, 512], F32)
              kmod = sb("kmod", [128, 512], F32)
              s8 = sb("s8", [128, 4, 8], F32)
              At = sb("At", [128, 512], BF16)
              Bt = sb("Bt", [128, 512], BF16)
              Kt = sb("Kt", [128, 512], BF16)
              Rt = sb("Rt", [128, 512], BF16)
              Vb = sb("Vb", [128, 512], BF16)
              arT = sb("arT", [64, 8, 2, 128], BF16)
              bkT = sb("bkT", [64, 8, 2, 128], BF16)
              dC = sb("dC", [64, 8], F32)
              Tst = [sb("Tst%d" % j, [64, 8, 64], BF16) for j in range(2)]
              XAs = [sb("XA%d" % p, [128, 2, 256], BF16) for p in range(2)]
              XNs = [[sb("XN%d_%d" % (p, j), [128, 256], BF16) for j in range(2)] for p in range(2)]
              Lps = [[sb("Lp%d_%d" % (p, j), [128, 128], BF16) for j in range(2)] for p in range(2)]
              RHSss = [sb("RHSs%d" % p, [128, 64], BF16) for p in range(2)]
              Uss = [sb("Us%d" % p, [128, 64], BF16) for p in range(2)]
              msk2 = sb("msk2", [128, 256], F32)
              ones_f = sb("ones_f", [128, 1], F32)
              h2T = xt[:].rearrange("p (a b) -> p a b", a=8)
              lg = sb("lg", [128, NE], F32)
              t8 = sb("t8", [128, 8], F32)
              eqk = sb("eqk", [128, NE], F32)
              posn = sb("posn", [128, NE], F32)
              tot = sb("tot", [128, NE], F32)
              mskb = sb("mskb", [128, NE], BF16)
              sl4 = sb("sl4", [128, 4], F32)
              ebase = sb("ebase", [128, NE], F32)
              trash = sb("trash", [128, 1], F32)
              ev4 = sb("ev4", [128, 4], F32)
              onesb = sb("onesb", [128, 128], BF16)

              MI = cst[:, 1, :]
              MS_ = cst[:, 2, :]
              MST = cst[:, 3, :]
              MP = cst[:, 4, :]
              for j in range(2):
                  S.pool(lambda e, j=j: e.memset(V1[j][:], 1.0), writes=[("V1", j)])
                  S.pool(lambda e, j=j: e.memset(Tst[j][:], 0.0), writes=[("Tst", j, hh) for hh in range(8)])
              S.pool(lambda e: e.memset(ones_f[:], 1.0), writes=["ones_f"])
              S.pool(lambda e: e.memset(onesb[:], 1.0), writes=["onesb"])
              S.pool(lambda e: e.memset(tot[:], 0.0), writes=["tot"])
              S.pool(lambda e: e.iota(ebase[:], pattern=[[CAP, NE]], base=0, channel_multiplier=0, allow_small_or_imprecise_dtypes=True), writes=["ebase"])
              S.pool(lambda e: e.iota(trash[:], pattern=[[0, 1]], base=NSLOT, channel_multiplier=1, allow_small_or_imprecise_dtypes=True), writes=["trash"])
              S.dve(lambda e: e.tensor_copy(out=msk2[:, 0:128], in_=MS_), reads=["cst"], writes=["msk2"])
              S.dve(lambda e: e.tensor_copy(out=msk2[:, 128:256], in_=MI), reads=["cst"], writes=["msk2"])

              def layer_norm_stats(src, key):
                  for h in range(2):
                      S.dve(lambda e, h=h: e.bn_stats(out=st6[:, h, :], in_=src[:, h * 512:(h + 1) * 512]), reads=[key], writes=["st6"])
                  S.dve(lambda e: e.bn_aggr(out=mv[:], in_=st6[:].rearrange("p a b -> p (a b)")), reads=["st6"], writes=["mv"])
                  S.dve(lambda e: e.tensor_scalar(out=rstd[:], in0=mv[:, 1:2], scalar1=LN_EPS, scalar2=None, op0=ALU.add), reads=["mv"], writes=["rstd"])
                  S.act(lambda e: e.activation(out=rstd[:], in_=rstd[:], func=AF.Sqrt), reads=["rstd"], writes=["rstd"])
                  S.dve(lambda e: e.reciprocal(out=rstd[:], in_=rstd[:]), reads=["rstd"], writes=["rstd"])

              for i in range(ntiles):
                  cur, prv = i % 2, (i + 1) % 2
                  cut_tile[0] = i
                  rows = slice(i * 128, (i + 1) * 128)
                  S.dma("sp", lambda e, rows=rows: e.dma_start(out=xt[:], in_=x[rows, :]), writes=["xt"])
                  layer_norm_stats(xt, "xt")
                  S.dve(lambda e: e.tensor_scalar(out=t1k[:], in0=xt[:], scalar1=mv[:, 0:1], scalar2=rstd[:], op0=ALU.subtract, op1=ALU.mult), reads=["xt", "mv", "rstd"], writes=["t1k"])
                  S.dve(lambda e: e.tensor_tensor(out=t1k[:], in0=t1k[:], in1=modb[:, 1, :], op=ALU.mult), reads=["t1k", "modb"], writes=["t1k"])
                  S.dve(lambda e: e.tensor_tensor(out=hb[:], in0=t1k[:], in1=modb[:, 0, :], op=ALU.add), reads=["t1k", "modb"], writes=["hb"])
                  cut("A_h", t1k[:], "t1k")
                  for k in range(8):
                      S.pe(lambda e, k=k: e.transpose(out=pTb[:, k * 128:(k + 1) * 128], in_=hb[:, k * 128:(k + 1) * 128], identity=ident_b), reads=["hb", "cstb"], writes=["pTb"])
                  S.act(lambda e: e.copy(out=hT[:].rearrange("p a b -> p (a b)"), in_=pTb[:]), reads=["pTb"], writes=["hT"])
                  cut("A_hT", t1k[:], "hT")
                  groups = [(0, 512), (512, 1024), (1024, 1536), (1536, 2048), (2048, 2464)]
                  for g, (c0, c1) in enumerate(groups):
                      pp = pA if g % 2 == 0 else pB
                      n = c1 - c0
                      for k in range(8):
                          S.pe(lambda e, k=k, pp=pp, c0=c0, c1=c1, n=n: e.matmul(pp[:, 0:n], lhsT=hT[:, k, :], rhs=w_in_bf[:, k, c0:c1], start=(k == 0), stop=(k == 7)),
                               reads=["hT", "w_in_bf"], writes=[pp.name])
                      if g == 0:
                          S.act(lambda e, pp=pp: e.copy(out=qk[:, 0:8, :].rearrange("p a b -> p (a b)"), in_=pp[:]), reads=[pp.name], writes=["qk"])
                          cut("A_g0", qk[:, 0:8, :].rearrange("p a b -> p (a b)"), "qk")
                      elif g < 4:
                          S.act(lambda e, pp=pp, g=g: e.copy(out=rwc[:, (g - 1) * 512:g * 512], in_=pp[:]), reads=[pp.name], writes=["rwc"])
                      else:
                          S.act(lambda e, pp=pp: e.copy(out=g4t[:], in_=pp[:, 0:416]), reads=[pp.name], writes=["g4t"])
                          S.act(lambda e: e.copy(out=rwc[:, 1536:1696], in_=g4t[:, 0:160]), reads=["g4t"], writes=["rwc"])
                          S.act(lambda e: e.copy(out=qk[:, 8:10, :].rearrange("p a b -> p (a b)"), in_=g4t[:, 160:288]), reads=["g4t"], writes=["qk"])
                          S.act(lambda e, cur=cur: e.copy(out=V1[cur][:, 0, 0:64], in_=g4t[:, 288:352]), reads=["g4t"], writes=[("V1", cur)])
                          S.act(lambda e, cur=cur: e.copy(out=V1[cur][:, 1, 0:64], in_=g4t[:, 352:416]), reads=["g4t"], writes=[("V1", cur)])
                      if g >= 1:
                          cut("A_g%d" % g, rwc[:, 0:1024], "rwc")
                  cut("A_proj", rwc[:], "rwc")
                  S.dma("sp", lambda e: e.dma_start(out=rwp[1:128, :], in_=rwc[0:127, :]), reads=["rwc"], writes=["rwp"])
                  S.dma("sp", lambda e, cur=cur: e.dma_start(out=rwp[0:1, :], in_=lastrow[cur:cur + 1, :]), reads=["lastrow%d" % cur], writes=["rwp"])
                  S.dma("sp", lambda e, prv=prv: e.dma_start(out=lastrow[prv:prv + 1, :], in_=rwc[127:128, :]), reads=["rwc"], writes=["lastrow%d" % prv])
                  S.dve(lambda e: e.tensor_tensor(out=rwp[:], in0=rwp[:], in1=rwc[:], op=ALU.subtract), reads=["rwp", "rwc"], writes=["rwp"])
                  S.dve(lambda e: e.tensor_tensor(out=rwp[:], in0=rwp[:], in1=mub[:], op=ALU.mult), reads=["rwp", "mub"], writes=["rwp"])
                  S.dve(lambda e: e.tensor_tensor(out=rwp[:], in0=rwp[:], in1=rwc[:], op=ALU.add), reads=["rwp", "rwc"], writes=["rwp"])
                  cut("A_mix", rwp[:], "rwp")
                  cb = cosT[:, i, :].unsqueeze(1).broadcast_to([128, 10, 8])
                  sbn = sinT[:, i, :].unsqueeze(1).broadcast_to([128, 10, 8])
                  a1, a2 = qk[:, :, 0:8], qk[:, :, 8:16]
                  S.dve(lambda e, cb=cb: e.tensor_tensor(out=rt[:, 0], in0=a1, in1=cb, op=ALU.mult), reads=["qk", "cosT"], writes=["rt0"])
                  S.dve(lambda e, sbn=sbn: e.tensor_tensor(out=rt[:, 1], in0=a2, in1=sbn, op=ALU.mult), reads=["qk", "sinT"], writes=["rt1"])
                  S.dve(lambda e, cb=cb: e.tensor_tensor(out=rt[:, 2], in0=a2, in1=cb, op=ALU.mult), reads=["qk", "cosT"], writes=["rt2"])
                  S.dve(lambda e, sbn=sbn: e.tensor_tensor(out=rt[:, 3], in0=a1, in1=sbn, op=ALU.mult), reads=["qk", "sinT"], writes=["rt3"])
                  S.act(lambda e: e.copy(out=qkb[:, :, 16:64], in_=qk[:, :, 16:64]), reads=["qk"], writes=["qkb"])
                  S.dve(lambda e: e.tensor_tensor(out=qkb[:, :, 0:8], in0=rt[:, 0], in1=rt[:, 1], op=ALU.subtract), reads=["rt0", "rt1"], writes=["qkb"])
                  S.dve(lambda e: e.tensor_tensor(out=qkb[:, :, 8:16], in0=rt[:, 2], in1=rt[:, 3], op=ALU.add), reads=["rt2", "rt3"], writes=["qkb"])
                  for h in range(8):
                      S.pe(lambda e, h=h: e.transpose(out=pTb2[0:64, h * 128:(h + 1) * 128], in_=qkb[:, h, :], identity=ident_b), reads=["qkb", "cstb"], writes=["pTb2"])
                  S.act(lambda e: e.activation(out=qT[:].rearrange("p a b -> p (a b)"), in_=pTb2[0:64, :], func=AF.Copy, scale=0.125), reads=["pTb2"], writes=["qT"])
                  for h in range(2):
                      S.pe(lambda e, h=h: e.transpose(out=pTb[0:64, h * 128:(h + 1) * 128], in_=qkb[:, 8 + h, :], identity=ident_b), reads=["qkb", "cstb"], writes=["pTb"])
                  S.act(lambda e, cur=cur: e.copy(out=kT[cur][:].rearrange("p a b -> p (a b)"), in_=pTb[0:64, 0:256]), reads=["pTb"], writes=[("kT", cur)])
                  for g in range(2):
                      rq = qT[:, 4 * g:4 * g + 4, :]
                      if i > 0:
                          S.pe(lambda e, g=g, rq=rq, prv=prv: e.matmul(pC[:], lhsT=kT[prv][:, g, :], rhs=rq, start=True, stop=True), reads=[("kT", prv), "qT"], writes=["pC"])
                          S.act(lambda e: e.activation(out=Ee[:], in_=pC[:], func=AF.Exp), reads=["pC"], writes=["Ee"])
                          S.dve(lambda e: e.tensor_tensor(out=PTp[:], in0=Ee[:].rearrange("p (a b) -> p a b", a=4), in1=MP.unsqueeze(1).broadcast_to([128, 4, 128]), op=ALU.mult), reads=["Ee", "cst"], writes=["PTp"])
                      S.pe(lambda e, g=g, rq=rq, cur=cur: e.matmul(pD[:], lhsT=kT[cur][:, g, :], rhs=rq, start=True, stop=True), reads=[("kT", cur), "qT"], writes=["pD"])
                      S.act(lambda e: e.activation(out=Ee[:], in_=pD[:], func=AF.Exp), reads=["pD"], writes=["Ee"])
                      S.dve(lambda e: e.tensor_tensor(out=PTc[:], in0=Ee[:].rearrange("p (a b) -> p a b", a=4), in1=MI.unsqueeze(1).broadcast_to([128, 4, 128]), op=ALU.mult), reads=["Ee", "cst"], writes=["PTc"])
                      pO = pE[:, 0:264].rearrange("p (a b) -> p a b", a=4)
                      for h in range(4):
                          if i > 0:
                              S.pe(lambda e, h=h, g=g, prv=prv, pO=pO: e.matmul(pO[:, h, :], lhsT=PTp[:, h, :], rhs=V1[prv][:, g, :], start=True, stop=False), reads=["PTp", ("V1", prv)], writes=["pE"])
                          S.pe(lambda e, h=h, g=g, cur=cur, pO=pO, i=i: e.matmul(pO[:, h, :], lhsT=PTc[:, h, :], rhs=V1[cur][:, g, :], start=(i == 0), stop=True), reads=["PTc", ("V1", cur)], writes=["pE"])
                      S.dve(lambda e, g=g, pO=pO: e.tensor_tensor(out=den[:, 4 * g:4 * g + 4], in0=pO[:, :, 64], in1=esink[:, 4 * g:4 * g + 4], op=ALU.add), reads=["pE", "esink"], writes=["den"])
                      S.dve(lambda e, g=g: e.reciprocal(out=den[:, 4 * g:4 * g + 4], in_=den[:, 4 * g:4 * g + 4]), reads=["den"], writes=["den"])
                      S.dve(lambda e, g=g, pO=pO: e.tensor_tensor(out=mixb[:, 256 * g:256 * g + 256].rearrange("p (a b) -> p a b", a=4), in0=pO[:, :, 0:64],
                                                                   in1=den[:, 4 * g:4 * g + 4].unsqueeze(2).broadcast_to([128, 4, 64]), op=ALU.mult), reads=["pE", "den"], writes=["mixb"])
                  cut("A_attn", mixb[:, 0:512], "mixb")
                  r_ = rwp[:, 0:512]
                  k_ = rwp[:, 512:1024]
                  v_ = rwp[:, 1024:1536]
                  W0, A0, KK, KA, RK, LNW, LNB = [rwcb[:, j, :] for j in range(7)]
                  S.act(lambda e: e.activation(out=lo_in[:, 0:32], in_=rwp[:, 1536:1568], func=AF.Tanh), reads=["rwp"], writes=["lo_in"])
                  S.act(lambda e: e.activation(out=lo_in[:, 64:160], in_=rwp[:, 1600:1696], func=AF.Sigmoid), reads=["rwp"], writes=["lo_in"])
                  S.dve(lambda e: e.tensor_copy(out=lo_in[:, 32:64], in_=rwp[:, 1568:1600]), reads=["rwp"], writes=["lo_in"])
                  for j, (c0, n) in enumerate(((0, 32), (32, 32), (64, 96))):
                      S.pe(lambda e, j=j, c0=c0, n=n: e.transpose(out=pC[0:n, j * 128:(j + 1) * 128], in_=lo_in[:, c0:c0 + n], identity=ident_f), reads=["lo_in", "cst"], writes=["pC"])
                      S.act(lambda e, j=j, n=n: e.copy(out=loT[0:n, j, :], in_=pC[0:n, j * 128:(j + 1) * 128]), reads=["pC"], writes=["loT"])
                  for j, (pp, n) in enumerate(((pA, 32), (pB, 32))):
                      S.pe(lambda e, j=j, pp=pp, n=n: e.matmul(pp[:], lhsT=loT[0:n, j, :], rhs=lor[0:n, j, :], start=True, stop=True), reads=["loT", "lor"], writes=[pp.name])
                  S.dve(lambda e: e.tensor_tensor(out=f1[:], in0=pA[:], in1=W0, op=ALU.add), reads=["pA", "rwcb"], writes=["rwc"])
                  S.act(lambda e: e.activation(out=lw[:], in_=f1[:], func=AF.Sigmoid), reads=["rwc"], writes=["lw"])
                  S.dve(lambda e: e.tensor_scalar(out=lw[:], in0=lw[:], scalar1=-float(np.exp(-0.5)), scalar2=None, op0=ALU.mult), reads=["lw"], writes=["lw"])
                  S.dve(lambda e: e.tensor_tensor(out=f2[:], in0=pB[:], in1=A0, op=ALU.add), reads=["pB", "rwcb"], writes=["rwc"])
                  S.act(lambda e: e.activation(out=av[:], in_=f2[:], func=AF.Sigmoid), reads=["rwc"], writes=["av"])
                  S.pe(lambda e: e.matmul(pA[:], lhsT=MI, rhs=lw[:], start=True, stop=True), reads=["cst", "lw"], writes=["pA"])
                  for h in range(8):
                      S.pe(lambda e, h=h: e.matmul(pB[0:64, h:h + 1], lhsT=lw[:, h * 64:(h + 1) * 64], rhs=ones_f[:], start=True, stop=True), reads=["lw", "ones_f"], writes=["pB"])
                  S.act(lambda e: e.activation(out=dC[:], in_=pB[0:64, 0:8], func=AF.Exp), reads=["pB"], writes=["dC"])
                  S.act(lambda e: e.activation(out=eP[:], in_=pA[:], func=AF.Exp), reads=["pA"], writes=["eP"])
                  S.act(lambda e: e.activation(out=eN[:], in_=pA[:], func=AF.Exp, scale=-1.0), reads=["pA"], writes=["eN"])
                  S.dve(lambda e: e.tensor_tensor(out=f1[:], in0=pA[:], in1=lw[:], op=ALU.subtract), reads=["pA", "lw"], writes=["rwc"])
                  S.act(lambda e: e.activation(out=lw[:], in_=f1[:], func=AF.Exp), reads=["rwc"], writes=["lw"])
                  v3 = lambda ap: ap.rearrange("p (a b) -> p a b", a=8)
                  S.dve(lambda e: e.tensor_tensor(out=f2[:], in0=k_, in1=KK, op=ALU.mult), reads=["rwp", "rwcb"], writes=["rwc"])
                  S.dve(lambda e: e.tensor_tensor(out=f3[:], in0=f2[:], in1=f2[:], op=ALU.mult), reads=["rwc"], writes=["rwc"])
                  S.dve(lambda e: e.tensor_reduce(out=s8[:, 0, :], in_=v3(f3[:]), axis=AX.X, op=ALU.add), reads=["rwc"], writes=["s80"])
                  S.act(lambda e: e.activation(out=s8[:, 0, :], in_=s8[:, 0, :], func=AF.Sqrt), reads=["s80"], writes=["s80"])
                  S.dve(lambda e: e.tensor_scalar(out=s8[:, 0, :], in0=s8[:, 0, :], scalar1=1e-12, scalar2=None, op0=ALU.max), reads=["s80"], writes=["s80"])
                  S.dve(lambda e: e.reciprocal(out=s8[:, 0, :], in_=s8[:, 0, :]), reads=["s80"], writes=["s80"])
                  S.dve(lambda e: e.tensor_tensor(out=v3(f2[:]), in0=v3(f2[:]), in1=s8[:, 0, :].unsqueeze(2).broadcast_to([128, 8, 64]), op=ALU.mult), reads=["rwc", "s80"], writes=["rwc"])
                  S.dve(lambda e: e.scalar_tensor_tensor(out=f3[:], in0=av[:], scalar=-1.0, in1=KA, op0=ALU.add, op1=ALU.mult), reads=["av", "rwcb"], writes=["rwc"])
                  S.dve(lambda e: e.scalar_tensor_tensor(out=kmod[:], in0=f3[:], scalar=1.0, in1=k_, op0=ALU.add, op1=ALU.mult), reads=["rwc", "rwp"], writes=["kmod"])
                  S.dve(lambda e: e.scalar_tensor_tensor(out=At[:], in0=f2[:], scalar=-1.0, in1=lw[:], op0=ALU.mult, op1=ALU.mult), reads=["rwc", "lw"], writes=["At"])
                  S.dve(lambda e: e.tensor_tensor(out=f4[:], in0=f2[:], in1=av[:], op=ALU.mult), reads=["rwc", "av"], writes=["f4"])
                  S.dve(lambda e: e.tensor_tensor(out=Bt[:], in0=f4[:], in1=eN[:], op=ALU.mult), reads=["f4", "eN"], writes=["Bt"])
                  S.dve(lambda e: e.tensor_tensor(out=Kt[:], in0=kmod[:], in1=eN[:], op=ALU.mult), reads=["kmod", "eN"], writes=["Kt"])
                  S.dve(lambda e: e.tensor_tensor(out=Rt[:], in0=r_, in1=eP[:], op=ALU.mult), reads=["rwp", "eP"], writes=["Rt"])
                  S.act(lambda e: e.copy(out=Vb[:], in_=v_), reads=["rwp"], writes=["Vb"])
                  cut("A_prep", kmod[:], "Vb")
                  for (src, skey, dst, dkey, j, pp) in ((At, "At", arT, "arT", 0, pTb), (Rt, "Rt", arT, "arT", 1, pTb2), (Bt, "Bt", bkT, "bkT", 0, pTb), (Kt, "Kt", bkT, "bkT", 1, pTb2)):
                      for h in range(8):
                          S.pe(lambda e, h=h, src=src, pp=pp: e.transpose(out=pp[0:64, h * 128:(h + 1) * 128], in_=src[:, h * 64:(h + 1) * 64], identity=ident_b), reads=[skey, "cstb"], writes=[pp.name])
                      S.act(lambda e, dst=dst, j=j, pp=pp: e.copy(out=dst[:, :, j, :], in_=pp[0:64, :].rearrange("p (a b) -> p a b", a=8)), reads=[pp.name], writes=[dkey])
                  cut("A_tr", kmod[:], "bkT")
                  Told, Tnew = Tst[cur], Tst[prv]

                  def head_gen(h, Told=Told, Tnew=Tnew, cur=cur, prv=prv):
                      p = h % 2
                      bX = pC if p == 0 else pA
                      bN = pD if p == 0 else pB
                      XA, XN, Lp, RHSs, Us = XAs[p], XNs[p], Lps[p], RHSss[p], Uss[p]
                      kXA, kRH, kUs = ("XA", p), ("RHSs", p), ("Us", p)
                      kXN = lambda j: ("XN", p, j)
                      kLp = lambda j: ("Lp", p, j)
                      RU = bN[:, 384:448]
                      TT = bN[0:64, 448:512]
                      arh = arT[:, h].rearrange("p a b -> p (a b)")
                      S.pe(lambda e: e.matmul(bX[:, 0:256], lhsT=bkT[:, h, 0, :], rhs=arh, start=True, stop=True), reads=["bkT", "arT"], writes=[bX.name])
                      S.pe(lambda e: e.matmul(bX[:, 256:512], lhsT=bkT[:, h, 1, :], rhs=arh, start=True, stop=True), reads=["bkT", "arT"], writes=[bX.name])
                      S.pe(lambda e: e.matmul(bN[:, 0:128], lhsT=arT[:, h, 0, :], rhs=bkT[:, h, 0, :], start=True, stop=True), reads=["bkT", "arT"], writes=[bN.name])
                      S.dve(lambda e: e.tensor_tensor(out=XA[:], in0=bX[:].rearrange("p (a b) -> p a b", a=2), in1=msk2[:].unsqueeze(1).broadcast_to([128, 2, 256]), op=ALU.mult), reads=[bX.name, "msk2"], writes=[kXA])
                      S.dve(lambda e: e.tensor_tensor(out=Lp[0][:], in0=bN[:, 0:128], in1=MST, op=ALU.mult), reads=[bN.name, "cst"], writes=[kLp(0)])
                      S.dve(lambda e: e.tensor_copy(out=XN[0][:, 0:128], in_=XA[:, 0, 0:128]), reads=[kXA], writes=[kXN(0)])
                      S.dve(lambda e: e.tensor_tensor(out=XN[0][:, 128:256], in0=XA[:, 0, 0:128], in1=ident_b, op=ALU.add), reads=[kXA, "cstb"], writes=[kXN(0)])
                      yield
                      S.pe(lambda e: e.matmul(bN[:, 0:128], lhsT=Lp[0][:], rhs=XN[0][:, 0:128], start=True, stop=True), reads=[kLp(0), kXN(0)], writes=[bN.name])
                      S.pe(lambda e: e.matmul(bN[:, 128:256], lhsT=ident_b, rhs=XN[0][:, 128:256], start=True, stop=True), reads=["cstb", kXN(0)], writes=[bN.name])
                      S.pe(lambda e: e.matmul(bN[:, 256:384], lhsT=XN[0][:, 0:128], rhs=Lp[0][:], start=True, stop=True), reads=[kLp(0), kXN(0)], writes=[bN.name])
                      S.act(lambda e: e.copy(out=XN[1][:], in_=bN[:, 0:256]), reads=[bN.name], writes=[kXN(1)])
                      S.act(lambda e: e.copy(out=Lp[1][:], in_=bN[:, 256:384]), reads=[bN.name], writes=[kLp(1)])
                      yield
                      c = 1
                      for lvl in range(1, 7):
                          n = 1 - c
                          if lvl < 6:
                              S.pe(lambda e, c=c: e.matmul(bN[:, 0:256], lhsT=Lp[c][:], rhs=XN[c][:], start=True, stop=False), reads=[kLp(c), kXN(c)], writes=[bN.name])
                              S.pe(lambda e, c=c: e.matmul(bN[:, 128:256], lhsT=ident_b, rhs=XN[c][:, 128:256], start=False, stop=True), reads=["cstb", kXN(c)], writes=[bN.name])
                              S.pe(lambda e, c=c: e.matmul(bN[:, 256:384], lhsT=XN[c][:, 0:128], rhs=Lp[c][:], start=True, stop=True), reads=[kLp(c), kXN(c)], writes=[bN.name])
                              S.act(lambda e, n=n: e.copy(out=XN[n][:], in_=bN[:, 0:256]), reads=[bN.name], writes=[kXN(n)])
                              S.act(lambda e, n=n: e.copy(out=Lp[n][:], in_=bN[:, 256:384]), reads=[bN.name], writes=[kLp(n)])
                          else:
                              S.pe(lambda e, c=c: e.matmul(bN[:, 128:256], lhsT=Lp[c][:], rhs=XN[c][:, 128:256], start=True, stop=False), reads=[kLp(c), kXN(c)], writes=[bN.name])
                              S.pe(lambda e, c=c: e.matmul(bN[:, 128:256], lhsT=ident_b, rhs=XN[c][:, 128:256], start=False, stop=True), reads=["cstb", kXN(c)], writes=[bN.name])
                              S.act(lambda e, n=n: e.copy(out=XN[n][:, 128:256], in_=bN[:, 128:256]), reads=[bN.name], writes=[kXN(n)])
                          c = n
                          yield
                      Nf = XN[c][:, 128:256]
                      S.pe(lambda e: e.matmul(RU, lhsT=arT[:, h, 0, :], rhs=Told[:, h, :], start=True, stop=False), reads=["arT", ("Tst", cur, h)], writes=[bN.name])
                      S.pe(lambda e: e.matmul(RU, lhsT=XA[:, 1, 0:128], rhs=Vb[:, h * 64:(h + 1) * 64], start=False, stop=True), reads=[kXA, "Vb"], writes=[bN.name])
                      S.act(lambda e: e.copy(out=RHSs[:], in_=RU), reads=[bN.name], writes=[kRH])
                      yield
                      S.pe(lambda e: e.matmul(RU, lhsT=Nf, rhs=RHSs[:], start=True, stop=True), reads=[kXN(c), kRH], writes=[bN.name])
                      S.act(lambda e: e.copy(out=Us[:], in_=RU), reads=[bN.name], writes=[kUs])
                      yield
                      S.pe(lambda e: e.matmul(pF[:, h * 64:(h + 1) * 64], lhsT=arT[:, h, 1, :], rhs=Told[:, h, :], start=True, stop=False), reads=["arT", ("Tst", cur, h)], writes=["pF"])
                      S.pe(lambda e: e.matmul(pF[:, h * 64:(h + 1) * 64], lhsT=XA[:, 0, 128:256], rhs=Us[:], start=False, stop=False), reads=[kXA, kUs], writes=["pF"])
                      S.pe(lambda e: e.matmul(pF[:, h * 64:(h + 1) * 64], lhsT=XA[:, 1, 128:256], rhs=Vb[:, h * 64:(h + 1) * 64], start=False, stop=True), reads=[kXA, "Vb"], writes=["pF"])
                      S.pe(lambda e: e.matmul(TT, lhsT=ident_b[0:64, 0:64], rhs=Told[:, h, :], start=True, stop=False), reads=["cstb", ("Tst", cur, h)], writes=[bN.name])
                      S.pe(lambda e: e.matmul(TT, lhsT=Bt[:, h * 64:(h + 1) * 64], rhs=Us[:], start=False, stop=False), reads=["Bt", kUs], writes=[bN.name])
                      S.pe(lambda e: e.matmul(TT, lhsT=Kt[:, h * 64:(h + 1) * 64], rhs=Vb[:, h * 64:(h + 1) * 64], start=False, stop=True), reads=["Kt", "Vb"], writes=[bN.name])
                      S.act(lambda e: e.activation(out=Tnew[:, h, :], in_=TT, func=AF.Copy, scale=dC[:, h:h + 1]), reads=[bN.name, "dC"], writes=[("Tst", prv, h)])
                      yield

                  for hp in range(0, 8, 2):
                      gens = [head_gen(hp), head_gen(hp + 1)]
                      alive = [True, True]
                      while any(alive):
                          for gi, g in enumerate(gens):
                              if alive[gi]:
                                  try:
                                      next(g)
                                  except StopIteration:
                                      alive[gi] = False
                  cut("A_heads", kmod[:], "pF")
                  S.act(lambda e: e.copy(out=f4[:], in_=pF[:]), reads=["pF"], writes=["f4"])
                  S.dve(lambda e: e.tensor_reduce(out=s8[:, 1, :], in_=v3(f4[:]), axis=AX.X, op=ALU.add), reads=["f4"], writes=["s81"])
                  S.dve(lambda e: e.tensor_tensor(out=f3[:], in0=f4[:], in1=f4[:], op=ALU.mult), reads=["f4"], writes=["rwc"])
                  S.dve(lambda e: e.tensor_reduce(out=s8[:, 2, :], in_=v3(f3[:]), axis=AX.X, op=ALU.add), reads=["rwc"], writes=["s82"])
                  S.dve(lambda e: e.tensor_scalar(out=s8[:, 1, :], in0=s8[:, 1, :], scalar1=1.0 / 64, scalar2=None, op0=ALU.mult), reads=["s81"], writes=["s81"])
                  S.dve(lambda e: e.tensor_tensor(out=s8[:, 3, :], in0=s8[:, 1, :], in1=s8[:, 1, :], op=ALU.mult), reads=["s81"], writes=["s83"])
                  S.dve(lambda e: e.scalar_tensor_tensor(out=s8[:, 2, :], in0=s8[:, 2, :], scalar=1.0 / 64, in1=s8[:, 3, :], op0=ALU.mult, op1=ALU.subtract), reads=["s82", "s83"], writes=["s82"])
                  S.dve(lambda e: e.tensor_scalar(out=s8[:, 2, :], in0=s8[:, 2, :], scalar1=GN_EPS, scalar2=None, op0=ALU.add), reads=["s82"], writes=["s82"])
                  S.act(lambda e: e.activation(out=s8[:, 2, :], in_=s8[:, 2, :], func=AF.Sqrt), reads=["s82"], writes=["s82"])
                  S.dve(lambda e: e.reciprocal(out=s8[:, 2, :], in_=s8[:, 2, :]), reads=["s82"], writes=["s82"])
                  S.dve(lambda e: e.tensor_tensor(out=v3(f4[:]), in0=v3(f4[:]), in1=s8[:, 1, :].unsqueeze(2).broadcast_to([128, 8, 64]), op=ALU.subtract), reads=["f4", "s81"], writes=["f4"])
                  S.dve(lambda e: e.tensor_tensor(out=v3(f4[:]), in0=v3(f4[:]), in1=s8[:, 2, :].unsqueeze(2).broadcast_to([128, 8, 64]), op=ALU.mult), reads=["f4", "s82"], writes=["f4"])
                  S.dve(lambda e: e.tensor_tensor(out=f4[:], in0=f4[:], in1=LNW, op=ALU.mult), reads=["f4", "rwcb"], writes=["f4"])
                  S.dve(lambda e: e.tensor_tensor(out=f4[:], in0=f4[:], in1=LNB, op=ALU.add), reads=["f4", "rwcb"], writes=["f4"])
                  S.dve(lambda e: e.tensor_tensor(out=eP[:], in0=r_, in1=kmod[:], op=ALU.mult), reads=["rwp", "kmod"], writes=["eP"])
                  S.dve(lambda e: e.tensor_tensor(out=eP[:], in0=eP[:], in1=RK, op=ALU.mult), reads=["eP", "rwcb"], writes=["eP"])
                  S.dve(lambda e: e.tensor_reduce(out=s8[:, 3, :], in_=v3(eP[:]), axis=AX.X, op=ALU.add), reads=["eP"], writes=["s83"])
                  S.dve(lambda e: e.tensor_tensor(out=v3(eN[:]), in0=v3(v_), in1=s8[:, 3, :].unsqueeze(2).broadcast_to([128, 8, 64]), op=ALU.mult), reads=["rwp", "s83"], writes=["eN"])
                  S.dve(lambda e: e.tensor_tensor(out=f4[:], in0=f4[:], in1=eN[:], op=ALU.add), reads=["f4", "eN"], writes=["f4"])
                  S.pe(lambda e: e.matmul(pA[:], lhsT=loT[0:96, 2, :], rhs=lor[0:96, 2, :], start=True, stop=True), reads=["loT", "lor"], writes=["pA"])
                  S.dve(lambda e: e.tensor_tensor(out=mixb[:, 512:1024], in0=f4[:], in1=pA[:], op=ALU.mult), reads=["f4", "pA"], writes=["mixb"])
                  cut("A_rwkv", mixb[:, 512:1024], "mixb")
                  for k in range(8):
                      S.pe(lambda e, k=k: e.transpose(out=pTb[:, k * 128:(k + 1) * 128], in_=mixb[:, k * 128:(k + 1) * 128], identity=ident_b), reads=["mixb", "cstb"], writes=["pTb"])
                  S.act(lambda e: e.copy(out=hT[:].rearrange("p a b -> p (a b)"), in_=pTb[:]), reads=["pTb"], writes=["hT"])
                  for hf, pp in ((0, pA), (1, pB)):
                      for k in range(8):
                          S.pe(lambda e, k=k, hf=hf, pp=pp: e.matmul(pp[:], lhsT=hT[:, k, :], rhs=w_out_bf[:, k, hf * 512:(hf + 1) * 512], start=(k == 0), stop=(k == 7)), reads=["hT", "w_out_bf"], writes=[pp.name])
                      S.dve(lambda e, hf=hf, pp=pp: e.tensor_tensor(out=t1k[:, hf * 512:(hf + 1) * 512], in0=pp[:], in1=modb[:, 2, hf * 512:(hf + 1) * 512], op=ALU.mult), reads=[pp.name, "modb"], writes=["t1k"])
                  S.dve(lambda e: e.scalar_tensor_tensor(out=t1k[:], in0=xt[:], scalar=float(ALPHA), in1=t1k[:], op0=ALU.mult, op1=ALU.add), reads=["xt", "t1k"], writes=["t1k"])
                  layer_norm_stats(t1k, "t1k")
                  S.dve(lambda e: e.tensor_scalar(out=t1k[:], in0=t1k[:], scalar1=mv[:, 0:1], scalar2=rstd[:], op0=ALU.subtract, op1=ALU.mult), reads=["t1k", "mv", "rstd"], writes=["t1k"])
                  S.dve(lambda e: e.tensor_tensor(out=t1k[:], in0=t1k[:], in1=lnpb[:, 0, :], op=ALU.mult), reads=["t1k", "lnpb"], writes=["t1k"])
                  S.dve(lambda e: e.tensor_tensor(out=t1k[:], in0=t1k[:], in1=lnpb[:, 1, :], op=ALU.add), reads=["t1k", "lnpb"], writes=["t1k"])
                  S.dma("sp", lambda e, rows=rows: e.dma_start(out=x1s[rows, :], in_=t1k[:]), reads=["t1k"], writes=["x1s"])
                  if stage == "A1":
                      S.dma("sp", lambda e, rows=rows: e.dma_start(out=dbg[rows, :], in_=t1k[:]), reads=["t1k"])
                  if _os.environ.get("KSKIP") == "router":
                      continue
                  layer_norm_stats(t1k, "t1k")
                  S.dve(lambda e: e.tensor_scalar(out=t1k[:], in0=t1k[:], scalar1=mv[:, 0:1], scalar2=rstd[:], op0=ALU.subtract, op1=ALU.mult), reads=["t1k", "mv", "rstd"], writes=["t1k"])
                  S.dve(lambda e: e.tensor_tensor(out=t1k[:], in0=t1k[:], in1=modb[:, 4, :], op=ALU.mult), reads=["t1k", "modb"], writes=["t1k"])
                  S.dve(lambda e: e.tensor_tensor(out=t1k[:], in0=t1k[:], in1=modb[:, 3, :], op=ALU.add), reads=["t1k", "modb"], writes=["t1k"])
                  S.act(lambda e: e.copy(out=hb[:], in_=t1k[:]), reads=["t1k"], writes=["hb"])
                  for half in range(2):
                      for k in range(4):
                          kk_ = half * 4 + k
                          S.pe(lambda e, k=k, kk_=kk_: e.transpose(out=pC[:, k * 128:(k + 1) * 128], in_=t1k[:, kk_ * 128:(kk_ + 1) * 128], identity=ident_f), reads=["t1k", "cst"], writes=["pC"])
                      S.act(lambda e, half=half: e.copy(out=h2T[:, half * 4:half * 4 + 4, :].rearrange("p a b -> p (a b)"), in_=pC[:]), reads=["pC"], writes=["xt"])
                  for k in range(8):
                      S.pe(lambda e, k=k: e.matmul(pD[:, 0:NE], lhsT=h2T[:, k, :], rhs=wr_f[:, k, :], start=(k == 0), stop=(k == 7)), reads=["xt", "wr_f"], writes=["pD"])
                  S.dve(lambda e: e.tensor_tensor(out=lg[:], in0=pD[:, 0:NE], in1=brb[:], op=ALU.add), reads=["pD", "brb"], writes=["lg"])
                  S.dve(lambda e: e.max(out=t8[:], in_=lg[:]), reads=["lg"], writes=["t8"])
                  S.dve(lambda e: e.tensor_scalar(out=mskb[:], in0=lg[:], scalar1=t8[:, 3:4], scalar2=None, op0=ALU.is_ge), reads=["lg", "t8"], writes=["mskb"])
                  S.pe(lambda e: e.matmul(pD[:, 64:64 + NE], lhsT=cstb[:, 2, :], rhs=mskb[:], start=True, stop=True), reads=["cstb", "mskb"], writes=["pD"])
                  S.pe(lambda e: e.matmul(pD[:, 128:128 + NE], lhsT=onesb[:], rhs=mskb[:], start=True, stop=True), reads=["onesb", "mskb"], writes=["pD"])
                  S.dve(lambda e: e.tensor_tensor(out=posn[:], in0=pD[:, 64:64 + NE], in1=tot[:], op=ALU.add), reads=["pD", "tot"], writes=["posn"])
                  S.dve(lambda e: e.tensor_tensor(out=tot[:], in0=pD[:, 128:128 + NE], in1=tot[:], op=ALU.add), reads=["pD", "tot"], writes=["tot"])
                  S.dve(lambda e: e.tensor_scalar(out=eqk[:], in0=posn[:], scalar1=float(CAP), scalar2=None, op0=ALU.is_lt), reads=["posn"], writes=["eqk"])
                  S.dve(lambda e: e.tensor_tensor(out=posn[:], in0=posn[:], in1=ebase[:], op=ALU.add), reads=["posn", "ebase"], writes=["posn"])
                  S.dve(lambda e: e.tensor_scalar(out=posn[:], in0=posn[:], scalar1=trash[:, 0:1], scalar2=None, op0=ALU.subtract), reads=["posn", "trash"], writes=["posn"])
                  S.dve(lambda e: e.tensor_tensor(out=posn[:], in0=posn[:], in1=eqk[:], op=ALU.mult), reads=["posn", "eqk"], writes=["posn"])
                  S.dve(lambda e: e.tensor_scalar(out=posn[:], in0=posn[:], scalar1=trash[:, 0:1], scalar2=None, op0=ALU.add), reads=["posn", "trash"], writes=["posn"])
                  for k4 in range(4):
                      S.dve(lambda e, k4=k4: e.tensor_scalar(out=eqk[:], in0=lg[:], scalar1=t8[:, k4:k4 + 1], scalar2=None, op0=ALU.is_equal), reads=["lg", "t8"], writes=["eqk"])
                      S.dve(lambda e: e.tensor_tensor(out=eqk[:], in0=eqk[:], in1=posn[:], op=ALU.mult), reads=["eqk", "posn"], writes=["eqk"])
                      S.dve(lambda e, k4=k4: e.tensor_reduce(out=sl4[:, k4:k4 + 1], in_=eqk[:], axis=AX.X, op=ALU.add), reads=["eqk"], writes=["sl4"])
                  S.dve(lambda e, i=i: e.tensor_copy(out=slot4[:, i, :], in_=sl4[:]), reads=["sl4"], writes=["slot4"])
                  S.dve(lambda e: e.tensor_scalar(out=ev4[:], in0=t8[:, 0:4], scalar1=t8[:, 0:1], scalar2=None, op0=ALU.subtract), reads=["t8"], writes=["ev4"])
                  S.act(lambda e: e.activation(out=ev4[:], in_=ev4[:], func=AF.Exp), reads=["ev4"], writes=["ev4"])
                  S.dve(lambda e: e.tensor_reduce(out=t8[:, 7:8], in_=ev4[:], axis=AX.X, op=ALU.add), reads=["ev4"], writes=["t8"])
                  S.dve(lambda e: e.reciprocal(out=t8[:, 7:8], in_=t8[:, 7:8]), reads=["t8"], writes=["t8"])
                  S.dve(lambda e: e.tensor_scalar(out=ev4[:], in0=ev4[:], scalar1=t8[:, 7:8], scalar2=None, op0=ALU.mult), reads=["ev4", "t8"], writes=["ev4"])
                  S.dve(lambda e: e.tensor_scalar(out=sl4[:], in0=sl4[:], scalar1=float(NSLOT), scalar2=None, op0=ALU.is_lt), reads=["sl4"], writes=["sl4"])
                  S.dve(lambda e, i=i: e.tensor_tensor(out=gate4[:, i, :], in0=ev4[:], in1=sl4[:], op=ALU.mult), reads=["ev4", "sl4"], writes=["gate4"])
                  for k4 in range(4 if stage in ("full", "A_scat", "BC") else 0):
                      S.dma("pool", lambda e, i=i, k4=k4: e.indirect_dma_start(out=Xs[:, :], out_offset=bass.IndirectOffsetOnAxis(ap=slot4[:, i, k4:k4 + 1], axis=0), in_=hb[:], in_offset=None),
                            reads=["hb", "slot4"], writes=["Xs"])
              S.dve(lambda e: e.memset(junk[:, 0:1], 0.0), reads=["slot4", "gate4", "modb", "cst", "cstb"], writes=["junk"])
          S.add("dve", lambda e: e.memset(junk[:, 1:2], 0.0), reads=[], writes=["junk"], barrier=True)

          if stage in ("A1",):
              S.emit()
              return nc

          with contextlib.ExitStack() as st:
              sb = lambda name, shape, d: st.enter_context(nc.sbuf_tensor(name, shape, d))
              Wgu = [sb("Wgu%d" % j, [128, 8, 2 * D], BF16) for j in range(2)]
              Wd = [sb("Wd%d" % j, [128, 8, D], BF16) for j in range(2)]
              bdn = [sb("bdn%d" % j, [1, D], BF16) for j in range(2)]
              bgu = sb("bgu", [128, NE, 16], F32)
              Xe = sb("Xe", [128, 6, D], BF16)
              XT = sb("XT", [128, 8, CAP], BF16)
              aT = sb("aT", [128, 8, CAP], BF16)
              G = sb("G", [128, 384], F32)
              Sg = sb("Sg", [128, 384], F32)
              Uc = sb("Uc", [128, 384], F32)
              Yo = [sb("Yo%d" % j, [128, D], F32) for j in range(2)]
              ones1 = sb("ones1", [1, 128], BF16)
              zt = sb("zt", [128, D], F32)
              S.dma("sp", lambda e: e.dma_start(out=bgu[:], in_=bguT[:, :, :]), writes=["bgu"])
              S.pool(lambda e: e.memset(ones1[:], 1.0), writes=["ones1"])
              S.pool(lambda e: e.memset(zt[:], 0.0), writes=["zt"])
              S.dma("sp", lambda e: e.dma_start(out=Ys[NSLOT:NSLOT + 128, :], in_=zt[:]), reads=["zt"], writes=["Ys"])

              def load_w(ex):
                  j = ex % 2
                  S.dma("pool", lambda e, j=j, ex=ex: e.dma_start(out=bdn[j][:], in_=b_dn[:, ex, :]), writes=[("bdn", j)])
                  gv_ = w_gu[ex].rearrange("(k p) n -> p k n", p=128)
                  dv_ = w_dn[ex].rearrange("(k p) n -> p k n", p=128)
                  for k in range(0, 8, 2):
                      S.dma("pool", lambda e, k=k, j=j, gv_=gv_: e.dma_start(out=Wgu[j][:, k:k + 2, :], in_=gv_[:, k:k + 2, :]), writes=[("Wgu", j)])
                  for k in range(0, 8, 4):
                      S.dma("pool", lambda e, k=k, j=j, dv_=dv_: e.dma_start(out=Wd[j][:, k:k + 4, :], in_=dv_[:, k:k + 4, :]), writes=[("Wd", j)])

              load_w(0)
              for ex in range(nexp):
                  j = ex % 2
                  if ex + 1 < nexp:
                      load_w(ex + 1)
                  S.dma("sp", lambda e, ex=ex: e.dma_start(out=Xe[:], in_=Xs[ex * CAP:(ex + 1) * CAP, :].rearrange("(s p) d -> p s d", p=128)), reads=["Xs"], writes=["Xe"])
                  for s in range(6):
                      pp = pTb if s % 2 == 0 else pTb2
                      for k in range(8):
                          S.pe(lambda e, s=s, k=k, pp=pp: e.transpose(out=pp[:, k * 128:(k + 1) * 128], in_=Xe[:, s, k * 128:(k + 1) * 128], identity=ident_b), reads=["Xe", "cstb"], writes=[pp.name])
                      S.act(lambda e, s=s, pp=pp: e.copy(out=XT[:, :, s * 128:(s + 1) * 128], in_=pp[:].rearrange("p (a b) -> p a b", a=8)), reads=[pp.name], writes=["XT"])
                  for nh in range(2):
                      n0 = nh * 384
                      for fc in range(8):
                          gb, ub = (pA, pB) if fc % 2 == 0 else (pE, pF)
                          for (pp, col) in ((gb, fc), (ub, 8 + fc)):
                              for k in range(8):
                                  S.pe(lambda e, k=k, pp=pp, col=col, j=j, n0=n0: e.matmul(pp[:, 0:384], lhsT=Wgu[j][:, k, col * 128:(col + 1) * 128], rhs=XT[:, k, n0:n0 + 384], start=(k == 0), stop=(k == 7)),
                                       reads=[("Wgu", j), "XT"], writes=[pp.name])
                          S.dve(lambda e, ex=ex, fc=fc, gb=gb: e.tensor_scalar(out=G[:], in0=gb[:, 0:384], scalar1=bgu[:, ex, fc:fc + 1], scalar2=7.0, op0=ALU.add, op1=ALU.min), reads=[gb.name, "bgu"], writes=["G"])
                          S.act(lambda e: e.activation(out=Sg[:], in_=G[:], func=AF.Sigmoid, scale=1.702), reads=["G"], writes=["Sg"])
                          S.dve(lambda e, ex=ex, fc=fc, ub=ub: e.tensor_scalar(out=Uc[:], in0=ub[:, 0:384], scalar1=bgu[:, ex, 8 + fc:9 + fc], scalar2=7.0, op0=ALU.add, op1=ALU.min), reads=[ub.name, "bgu"], writes=["Uc"])
                          S.dve(lambda e: e.tensor_scalar(out=Uc[:], in0=Uc[:], scalar1=-7.0, scalar2=1.0, op0=ALU.max, op1=ALU.add), reads=["Uc"], writes=["Uc"])
                          S.dve(lambda e: e.tensor_tensor(out=G[:], in0=G[:], in1=Sg[:], op=ALU.mult), reads=["G", "Sg"], writes=["G"])
                          S.dve(lambda e, fc=fc, n0=n0: e.tensor_tensor(out=aT[:, fc, n0:n0 + 384], in0=G[:], in1=Uc[:], op=ALU.mult), reads=["G", "Uc"], writes=["aT"])
                  for s in range(6):
                      yo = Yo[s % 2]
                      for hf, pp in ((0, pC), (1, pD)):
                          S.pe(lambda e, hf=hf, pp=pp, j=j: e.matmul(pp[:], lhsT=ones1[:], rhs=bdn[j][:, hf * 512:(hf + 1) * 512], start=True, stop=False), reads=["ones1", ("bdn", j)], writes=[pp.name])
                          for k in range(8):
                              S.pe(lambda e, k=k, hf=hf, pp=pp, s=s, j=j: e.matmul(pp[:], lhsT=aT[:, k, s * 128:(s + 1) * 128], rhs=Wd[j][:, k, hf * 512:(hf + 1) * 512], start=False, stop=(k == 7)),
                                   reads=["aT", ("Wd", j)], writes=[pp.name])
                          S.act(lambda e, hf=hf, pp=pp, yo=yo: e.copy(out=yo[:, hf * 512:(hf + 1) * 512], in_=pp[:]), reads=[pp.name], writes=[("Yo", s % 2)])
                      S.dma("sp", lambda e, ex=ex, s=s, yo=yo: e.dma_start(out=Ys[ex * CAP + s * 128:ex * CAP + (s + 1) * 128, :], in_=yo[:]), reads=[("Yo", s % 2)], writes=["Ys"])
              S.dve(lambda e: e.memset(junk[:, 2:3], 0.0), reads=["slot4", "gate4", "modb"], writes=["junk"])
          S.add("dve", lambda e: e.memset(junk[:, 3:4], 0.0), reads=[], writes=["junk"], barrier=True)

          with contextlib.ExitStack() as st:
              sb = lambda name, shape, d: st.enter_context(nc.sbuf_tensor(name, shape, d))
              Yg = [sb("Yg%d" % j, [128, 4, D], F32) for j in range(2)]
              x1t = [sb("x1t%d" % j, [128, D], F32) for j in range(2)]
              acc = sb("acc", [128, D], F32)
              lnpc = sb("lnpbC", [128, 2, D], F32)
              S.dma("sp", lambda e: e.dma_start(out=lnpc[:], in_=lnp[:, 2:4, :]), writes=["lnpb"])
              st6c = sb("st6c", [128, 2, 6], F32)
              mvc = sb("mvc", [128, 2], F32)
              rsc = sb("rsc", [128, 1], F32)
              for i in range(ntiles):
                  j = i % 2
                  rows = slice(i * 128, (i + 1) * 128)
                  S.dma("sp", lambda e, rows=rows, j=j: e.dma_start(out=x1t[j][:], in_=x1s[rows, :]), reads=["x1s"], writes=[("x1t", j)])
                  for k4 in range(4):
                      S.dma("pool", lambda e, i=i, k4=k4, j=j: e.indirect_dma_start(out=Yg[j][:, k4, :], out_offset=None, in_=Ys[:, :], in_offset=bass.IndirectOffsetOnAxis(ap=slot4[:, i, k4:k4 + 1], axis=0)),
                            reads=["Ys", "slot4"], writes=[("Yg", j, k4)])
                  S.dve(lambda e, i=i, j=j: e.tensor_scalar(out=acc[:], in0=Yg[j][:, 0, :], scalar1=gate4[:, i, 0:1], scalar2=None, op0=ALU.mult), reads=[("Yg", j, 0), "gate4"], writes=["acc"])
                  for k4 in range(1, 4):
                      S.dve(lambda e, i=i, j=j, k4=k4: e.scalar_tensor_tensor(out=acc[:], in0=Yg[j][:, k4, :], scalar=gate4[:, i, k4:k4 + 1], in1=acc[:], op0=ALU.mult, op1=ALU.add), reads=[("Yg", j, k4), "gate4", "acc"], writes=["acc"])
                  S.dve(lambda e: e.tensor_tensor(out=acc[:], in0=acc[:], in1=modb[:, 5, :], op=ALU.mult), reads=["acc", "modb"], writes=["acc"])
                  S.dve(lambda e, j=j: e.scalar_tensor_tensor(out=acc[:], in0=x1t[j][:], scalar=float(ALPHA), in1=acc[:], op0=ALU.mult, op1=ALU.add), reads=[("x1t", j), "acc"], writes=["acc"])
                  for h in range(2):
                      S.dve(lambda e, h=h: e.bn_stats(out=st6c[:, h, :], in_=acc[:, h * 512:(h + 1) * 512]), reads=["acc"], writes=["st6c"])
                  S.dve(lambda e: e.bn_aggr(out=mvc[:], in_=st6c[:].rearrange("p a b -> p (a b)")), reads=["st6c"], writes=["mvc"])
                  S.dve(lambda e: e.tensor_scalar(out=rsc[:], in0=mvc[:, 1:2], scalar1=LN_EPS, scalar2=None, op0=ALU.add), reads=["mvc"], writes=["rsc"])
                  S.act(lambda e: e.activation(out=rsc[:], in_=rsc[:], func=AF.Sqrt), reads=["rsc"], writes=["rsc"])
                  S.dve(lambda e: e.reciprocal(out=rsc[:], in_=rsc[:]), reads=["rsc"], writes=["rsc"])
                  S.dve(lambda e: e.tensor_scalar(out=acc[:], in0=acc[:], scalar1=mvc[:, 0:1], scalar2=rsc[:], op0=ALU.subtract, op1=ALU.mult), reads=["acc", "mvc", "rsc"], writes=["acc"])
                  S.dve(lambda e: e.tensor_tensor(out=acc[:], in0=acc[:], in1=lnpc[:, 0, :], op=ALU.mult), reads=["acc", "lnpb"], writes=["acc"])
                  S.dve(lambda e, j=j: e.tensor_tensor(out=x1t[j][:], in0=acc[:], in1=lnpc[:, 1, :], op=ALU.add), reads=["acc", "lnpb"], writes=[("x1t", j)])
                  S.dma("sp", lambda e, rows=rows, j=j: e.dma_start(out=out[rows, :], in_=x1t[j][:]), reads=[("x1t", j)], writes=["out"])

    except _Cut:
        pass
    S.emit()
    return nc


def _prep_shared(inp):
    f = lambda a: np.ascontiguousarray(np.asarray(a), dtype=np.float32)
    bc = lambda v, n=128: np.ascontiguousarray(np.broadcast_to(np.asarray(v, np.float32).reshape(1, -1), (n, np.asarray(v).size)))
    w_in = f(inp["w_in"][0])
    perm = np.concatenate([np.arange(0, 512), np.arange(768, 2464), np.arange(512, 640), np.arange(640, 768)])
    sh = {}
    sh["w_ada"] = f(inp["w_ada"][0])
    sh["b_ada_b"] = bc(inp["b_ada"][0])
    sh["w_in"] = np.ascontiguousarray(w_in[:, perm])
    sh["mu_b"] = bc(inp["shift_mu"][0])
    rows = [inp["rwkv_w0"][0], inp["rwkv_a0"][0], inp["rwkv_k_k"][0], inp["rwkv_k_a"][0], np.asarray(inp["rwkv_r_k"][0]).reshape(-1), inp["rwkv_ln_w"][0], inp["rwkv_ln_b"][0]]
    sh["rwc_b"] = np.ascontiguousarray(np.stack([bc(r) for r in rows], axis=1))
    lora = np.zeros((96, 3, 512), np.float32)
    lora[0:32, 0] = inp["rwkv_w2"][0]
    lora[0:32, 1] = inp["rwkv_a2"][0]
    lora[0:96, 2] = inp["rwkv_g2"][0]
    sh["lora"] = lora
    sh["sinks_b"] = bc(inp["attn_sinks"][0])
    invf = (500000.0 ** (-np.arange(0, 16, 2, dtype=np.float32) / 16)).astype(np.float32)
    sh["invf_b"] = bc(invf)
    sh["w_out"] = f(inp["w_out"][0])
    sh["lnp"] = np.ascontiguousarray(np.stack([bc(inp[k][0]) for k in ("ln1_g", "ln1_b", "ln2_g", "ln2_b")], axis=1))
    sh["w_router"] = f(inp["w_router"][0])
    sh["b_router_b"] = bc(inp["b_router"][0])
    sh["w_gu"] = f(inp["w_gate_up"][0])
    sh["bguT"] = np.ascontiguousarray(f(inp["b_gate_up"][0]).reshape(NE, 16, 128).transpose(2, 0, 1))
    sh["w_dn"] = f(inp["w_down"][0])
    sh["b_dn"] = f(inp["b_down"][0]).reshape(1, NE, D)
    jj = np.arange(128)[:, None]
    tt = np.arange(128)[None, :]
    sh["consts"] = np.ascontiguousarray(np.stack([np.eye(128), (jj <= tt), (jj < tt), (jj > tt), (jj > tt)], axis=1).astype(np.float32))
    return sh


def _prep_core(inp, b, sh):
    m = dict(sh)
    m["x"] = np.ascontiguousarray(np.asarray(inp["x"][b], np.float32))
    m["posT"] = np.ascontiguousarray(np.asarray(inp["positions"][b], np.int32).reshape(NT, 128).T)
    c = np.asarray(inp["c"][b], np.float32)
    m["cB"] = np.ascontiguousarray(np.broadcast_to(c.reshape(8, 128).T[:, :, None], (128, 8, 128)))
    return m


_NC_CACHE = {}


def kernel(**inputs):
    sh = _prep_shared(inputs)
    in_maps = [_prep_core(inputs, b, sh) for b in range(8)]
    if "full" not in _NC_CACHE:
        _NC_CACHE["full"] = build("full")
    nc = _NC_CACHE["full"]
    res = run_bass_kernel_spmd(nc, in_maps, core_ids=list(range(8)))
    return np.stack([np.asarray(r["out"], np.float32) for r in res.results], axis=0)
```
